# Optimizing a Trainium2 kernel written in Bass

```python
import math
import jax, jax.numpy as jnp
from jax import lax
import numpy as np

D_MODEL = 1024
BATCH = 8
SEQ = 4096
DEPTH = 4

HEAD_DIM = 64
N_HEADS_DIL = D_MODEL // 128
N_HEADS_FOX = D_MODEL // 128
BRANCH_W = N_HEADS_DIL * HEAD_DIM
D_SSM = BRANCH_W
SSM_GROUP = 16
N_SSM_GROUPS = D_SSM // SSM_GROUP
SSM_STATE = 64
DT_MIN = 1e-3
DT_MAX = 1e-1
DIL_PATTERNS = ((128, 1), (512, 4), (2048, 16))
ROPE_THETA = 500000.0
ROPE_DIM = HEAD_DIM // 4
Q_BLOCK = 128
N_MEM = 256
N_HEADS_X = 4
HEAD_DIM_X = D_MODEL // N_HEADS_X
D_FF = 2816
N_EXPERTS = 8
TOP_K = 2
D_FF_EXPERT = 1408
N_DENSE = (DEPTH + 1) // 2
N_MOE = DEPTH // 2
N_BRANCH = 3
DEEPNORM_ALPHA = (2 * DEPTH) ** 0.25
DEEPNORM_BETA = (8 * DEPTH) ** -0.25
LN_EPS = 1e-5
IN_SPLITS = (D_SSM, 3 * BRANCH_W, 3 * BRANCH_W, N_HEADS_FOX, N_BRANCH * D_MODEL)
N_IN = sum(IN_SPLITS)

kernel_name = "hybrid_s5_dilated_fox_moe_deepnorm"

F32 = jnp.float32


def layer_norm(x, g, b):
    xf = x.astype(F32)
    mu = jnp.mean(xf, axis=-1, keepdims=True)
    var = jnp.mean(jnp.square(xf - mu), axis=-1, keepdims=True)
    y = (xf - mu) * lax.rsqrt(var + LN_EPS) * g.astype(F32) + b.astype(F32)
    return y.astype(x.dtype)


def rope_tables(positions):
    inv_freq = ROPE_THETA ** (-jnp.arange(0, ROPE_DIM, 2, dtype=F32) / ROPE_DIM)
    ang = positions.astype(F32)[..., None] * inv_freq
    return jnp.cos(ang)[:, :, None, :], jnp.sin(ang)[:, :, None, :]


def partial_rotary(t, cos, sin):
    half = ROPE_DIM // 2
    t1 = t[..., :half].astype(F32)
    t2 = t[..., half:ROPE_DIM].astype(F32)
    rot = jnp.concatenate([t1 * cos - t2 * sin, t2 * cos + t1 * sin], axis=-1).astype(t.dtype)
    return jnp.concatenate([rot, t[..., ROPE_DIM:]], axis=-1)


def s5_mixer(u, lam_re, lam_im, log_dt, b_re, b_im, c_re, c_im, d_skip, w_glu):
    B_, S_, _ = u.shape
    uf = u.astype(F32).reshape(B_, S_, N_SSM_GROUPS, SSM_GROUP)
    lam = lax.complex(lam_re.astype(F32), lam_im.astype(F32))
    dt = jnp.exp(log_dt.astype(F32))[:, None]
    lam_bar = jnp.exp(lam * dt)
    b_bar = ((lam_bar - 1.0) / lam)[..., None] * lax.complex(b_re.astype(F32), b_im.astype(F32))
    bu = lax.complex(jnp.einsum('bsgc,gpc->sbgp', uf, jnp.real(b_bar)),
                     jnp.einsum('bsgc,gpc->sbgp', uf, jnp.imag(b_bar)))
    a = jnp.broadcast_to(lam_bar, (S_, 1) + lam_bar.shape)

    def combine(e1, e2):
        a1, b1 = e1
        a2, b2 = e2
        return a1 * a2, a2 * b1 + b2

    _, states = lax.associative_scan(combine, (a, bu), axis=0)
    y = (jnp.einsum('sbgp,gcp->bsgc', jnp.real(states), c_re.astype(F32))
         - jnp.einsum('sbgp,gcp->bsgc', jnp.imag(states), c_im.astype(F32)))
    y = y.reshape(B_, S_, D_SSM) + d_skip.astype(F32) * u.astype(F32)
    y = jax.nn.gelu(y).astype(u.dtype)
    return y * jax.nn.sigmoid(y @ w_glu)


def dilated_band_attention(q, k, v, dil, window):
    B_, S_, H, E = q.shape
    w = window // dil
    L = S_ // dil
    nblk = -(-L // w)
    Lp = nblk * w

    def to_blocks(t):
        t = t.reshape(B_, L, dil, H, E).transpose(0, 2, 3, 1, 4)
        t = jnp.pad(t, ((0, 0), (0, 0), (0, 0), (0, Lp - L), (0, 0)))
        return t.reshape(B_, dil, H, nblk, w, E)

    def with_prev(t):
        prev = jnp.pad(t, ((0, 0), (0, 0), (0, 0), (1, 0), (0, 0), (0, 0)))[:, :, :, :-1]
        return jnp.concatenate([prev, t], axis=-2)

    qb = to_blocks(q)
    kk = with_prev(to_blocks(k))
    vv = with_prev(to_blocks(v))
    s = jnp.einsum('bdhnqe,bdhnke->bdhnqk', qb, kk, preferred_element_type=F32)
    i = jnp.arange(w)[:, None]
    j = jnp.arange(2 * w)[None, :]
    dist = i - j + w
    blk = jnp.arange(nblk)[:, None, None]
    mask = (dist >= 0) & (dist <= w) & ((blk > 0) | (j >= w))
    s = jnp.where(mask, s, -jnp.inf)
    m = jnp.max(s, axis=-1, keepdims=True)
    p = jnp.exp(s - m)
    den = jnp.sum(p, axis=-1, keepdims=True)
    o = jnp.einsum('bdhnqk,bdhnke->bdhnqe', (p / den).astype(v.dtype), vv, preferred_element_type=F32)
    lse = (m + jnp.log(den))[..., 0]

    def from_blocks(t):
        t = t.reshape((B_, dil, H, Lp) + t.shape[5:])[:, :, :, :L]
        t = jnp.moveaxis(t, 3, 1)
        return t.reshape((B_, S_, H) + t.shape[4:])

    return from_blocks(o), from_blocks(lse)


def dilated_mixture(q, k, v):
    res = [dilated_band_attention(q, k, v, dil, win) for (win, dil) in DIL_PATTERNS]
    outs = jnp.stack([r[0] for r in res], axis=0)
    lse = jnp.stack([r[1] for r in res], axis=0)
    wts = jax.nn.softmax(lse, axis=0)
    return jnp.sum(wts[..., None] * outs, axis=0)


def forgetting_attention(q, k, v, log_f):
    B_, S_, H, E = q.shape
    nblk = S_ // Q_BLOCK
    c = lax.cumsum(log_f, axis=1).transpose(0, 2, 1)
    kt = k.transpose(0, 2, 1, 3)
    vt = v.transpose(0, 2, 1, 3)
    qb = q.reshape(B_, nblk, Q_BLOCK, H, E).transpose(1, 0, 3, 2, 4)
    cq = c.reshape(B_, H, nblk, Q_BLOCK).transpose(2, 0, 1, 3)
    starts = jnp.arange(nblk, dtype=jnp.int32) * Q_BLOCK
    kpos = jnp.arange(S_, dtype=jnp.int32)

    def block(args):
        qi, ci, st = args
        s = jnp.einsum('bhqe,bhke->bhqk', qi, kt, preferred_element_type=F32)
        s = s + (ci[..., None] - c[:, :, None, :])
        qpos = st + jnp.arange(Q_BLOCK, dtype=jnp.int32)
        s = jnp.where(kpos[None, :] <= qpos[:, None], s, -jnp.inf)
        p = jax.nn.softmax(s, axis=-1)
        return jnp.einsum('bhqk,bhke->bhqe', p.astype(vt.dtype), vt, preferred_element_type=F32)

    o = lax.map(block, (qb, cq, starts))
    return o.transpose(1, 0, 3, 2, 4).reshape(B_, S_, H, E)


def hybrid_mixer(x, cos, sin, w_in, b_forget, lam_re, lam_im, log_dt, b_re, b_im,
                 c_re, c_im, d_skip, w_glu, w_branch, w_out):
    B_, S_, _ = x.shape
    proj = x @ w_in
    offsets = np.cumsum(IN_SPLITS)[:-1].tolist()
    u, qkv_dil, qkv_fox, f_logit, g_logit = jnp.split(proj, offsets, axis=-1)
    scale = HEAD_DIM ** -0.5

    y_ssm = s5_mixer(u, lam_re, lam_im, log_dt, b_re, b_im, c_re, c_im, d_skip, w_glu)

    qd, kd, vd = [t.reshape(B_, S_, N_HEADS_DIL, HEAD_DIM) for t in jnp.split(qkv_dil, 3, axis=-1)]
    qd = partial_rotary(qd, cos, sin) * scale
    kd = partial_rotary(kd, cos, sin)
    y_dil = dilated_mixture(qd, kd, vd).astype(x.dtype).reshape(B_, S_, BRANCH_W)

    qf, kf, vf = [t.reshape(B_, S_, N_HEADS_FOX, HEAD_DIM) for t in jnp.split(qkv_fox, 3, axis=-1)]
    log_f = jax.nn.log_sigmoid(f_logit.astype(F32) + b_forget.astype(F32))
    y_fox = forgetting_attention(qf * scale, kf, vf, log_f).astype(x.dtype).reshape(B_, S_, BRANCH_W)

    ys = jnp.stack([y_ssm, y_dil, y_fox], axis=2)
    gates = jax.nn.sigmoid(g_logit.reshape(B_, S_, N_BRANCH, D_MODEL))
    merged = jnp.sum(gates * jnp.einsum('bsnc,ncd->bsnd', ys, w_branch), axis=2)
    return merged @ w_out


def memory_cross_attention(x, mem, wq, wk, wv, wo):
    B_, S_, _ = x.shape
    q = (x @ wq).reshape(B_, S_, N_HEADS_X, HEAD_DIM_X) * HEAD_DIM_X ** -0.5
    k = (mem @ wk).reshape(B_, -1, N_HEADS_X, HEAD_DIM_X)
    v = (mem @ wv).reshape(B_, -1, N_HEADS_X, HEAD_DIM_X)
    s = jnp.einsum('bshe,bmhe->bhsm', q, k, preferred_element_type=F32)
    p = jax.nn.softmax(s, axis=-1).astype(v.dtype)
    o = jnp.einsum('bhsm,bmhe->bshe', p, v).reshape(B_, S_, D_MODEL)
    return o @ wo


def swiglu(x, wg, wu, wd):
    return (jax.nn.silu(x @ wg) * (x @ wu)) @ wd


def moe_ffn(x, w_router, b_router, wg, wu, wd):
    logits = (x @ w_router).astype(F32) + b_router.astype(F32)
    top_v, top_i = lax.top_k(logits, TOP_K)
    top_w = jax.nn.softmax(top_v, axis=-1)
    gate = jnp.sum(jax.nn.one_hot(top_i, N_EXPERTS, dtype=F32) * top_w[..., None], axis=-2)
    out = jnp.zeros_like(x)
    for e in range(N_EXPERTS):
        out = out + gate[..., e:e + 1].astype(x.dtype) * swiglu(x, wg[e], wu[e], wd[e])
    return out


def setup_inputs(seed: int = 0) -> dict:
    key = jax.random.key(seed)
    keys = iter(jax.random.split(key, 48))

    def nrm(shape, scale=1.0):
        return jax.random.normal(next(keys), shape, F32) * scale

    def gain(shape):
        return 1.0 + nrm(shape, 0.02)

    G, P, C = N_SSM_GROUPS, SSM_STATE, SSM_GROUP
    n_idx = jnp.arange(P, dtype=F32)
    beta = DEEPNORM_BETA
    return {
        "x": nrm((BATCH, SEQ, D_MODEL)),
        "mem": nrm((BATCH, N_MEM, D_MODEL)),
        "positions": jax.random.randint(next(keys), (BATCH, 1), 0, 1024, dtype=jnp.int32)
                     + jnp.arange(SEQ, dtype=jnp.int32)[None, :],
        "w_in": nrm((DEPTH, D_MODEL, N_IN), D_MODEL ** -0.5),
        "b_forget": jax.random.uniform(next(keys), (DEPTH, N_HEADS_FOX), F32, 1.0, 6.0),
        "ssm_lambda_re": -0.5 + nrm((DEPTH, G, P), 0.02),
        "ssm_lambda_im": math.pi * n_idx + nrm((DEPTH, G, P), 0.02),
        "ssm_log_dt": jax.random.uniform(next(keys), (DEPTH, G), F32, math.log(DT_MIN), math.log(DT_MAX)),
        "ssm_b_re": nrm((DEPTH, G, P, C), (2 * C) ** -0.5),
        "ssm_b_im": nrm((DEPTH, G, P, C), (2 * C) ** -0.5),
        "ssm_c_re": nrm((DEPTH, G, C, P), P ** -0.5),
        "ssm_c_im": nrm((DEPTH, G, C, P), P ** -0.5),
        "ssm_d": nrm((DEPTH, D_SSM), 0.5),
        "w_glu": nrm((DEPTH, D_SSM, D_SSM), D_SSM ** -0.5),
        "w_branch": nrm((DEPTH, N_BRANCH, BRANCH_W, D_MODEL), BRANCH_W ** -0.5),
        "w_mix_out": nrm((DEPTH, D_MODEL, D_MODEL), beta * D_MODEL ** -0.5),
        "ln_mix_g": gain((DEPTH, D_MODEL)),
        "ln_mix_b": nrm((DEPTH, D_MODEL), 0.02),
        "w_xq": nrm((DEPTH, D_MODEL, D_MODEL), D_MODEL ** -0.5),
        "w_xk": nrm((DEPTH, D_MODEL, D_MODEL), D_MODEL ** -0.5),
        "w_xv": nrm((DEPTH, D_MODEL, D_MODEL), D_MODEL ** -0.5),
        "w_xo": nrm((DEPTH, D_MODEL, D_MODEL), beta * D_MODEL ** -0.5),
        "ln_x_g": gain((DEPTH, D_MODEL)),
        "ln_x_b": nrm((DEPTH, D_MODEL), 0.02),
        "ffn_w_gate": nrm((N_DENSE, D_MODEL, D_FF), D_MODEL ** -0.5),
        "ffn_w_up": nrm((N_DENSE, D_MODEL, D_FF), D_MODEL ** -0.5),
        "ffn_w_down": nrm((N_DENSE, D_FF, D_MODEL), beta * D_FF ** -0.5),
        "moe_w_router": nrm((N_MOE, D_MODEL, N_EXPERTS), D_MODEL ** -0.5),
        "moe_b_router": nrm((N_MOE, N_EXPERTS), 0.01),
        "moe_w_gate": nrm((N_MOE, N_EXPERTS, D_MODEL, D_FF_EXPERT), D_MODEL ** -0.5),
        "moe_w_up": nrm((N_MOE, N_EXPERTS, D_MODEL, D_FF_EXPERT), D_MODEL ** -0.5),
        "moe_w_down": nrm((N_MOE, N_EXPERTS, D_FF_EXPERT, D_MODEL), beta * D_FF_EXPERT ** -0.5),
        "ln_ffn_g": gain((DEPTH, D_MODEL)),
        "ln_ffn_b": nrm((DEPTH, D_MODEL), 0.02),
    }


def reference(x, mem, positions, w_in, b_forget, ssm_lambda_re, ssm_lambda_im, ssm_log_dt,
              ssm_b_re, ssm_b_im, ssm_c_re, ssm_c_im, ssm_d, w_glu, w_branch, w_mix_out,
              ln_mix_g, ln_mix_b, w_xq, w_xk, w_xv, w_xo, ln_x_g, ln_x_b,
              ffn_w_gate, ffn_w_up, ffn_w_down, moe_w_router, moe_b_router,
              moe_w_gate, moe_w_up, moe_w_down, ln_ffn_g, ln_ffn_b):
    cos, sin = rope_tables(positions)
    for l in range(DEPTH):
        mix = hybrid_mixer(x, cos, sin, w_in[l], b_forget[l], ssm_lambda_re[l], ssm_lambda_im[l],
                           ssm_log_dt[l], ssm_b_re[l], ssm_b_im[l], ssm_c_re[l], ssm_c_im[l],
                           ssm_d[l], w_glu[l], w_branch[l], w_mix_out[l])
        x = layer_norm(DEEPNORM_ALPHA * x + mix, ln_mix_g[l], ln_mix_b[l])
        xa = memory_cross_attention(x, mem, w_xq[l], w_xk[l], w_xv[l], w_xo[l])
        x = layer_norm(DEEPNORM_ALPHA * x + xa, ln_x_g[l], ln_x_b[l])
        i = l // 2
        if l % 2 == 0:
            ff = swiglu(x, ffn_w_gate[i], ffn_w_up[i], ffn_w_down[i])
        else:
            ff = moe_ffn(x, moe_w_router[i], moe_b_router[i], moe_w_gate[i], moe_w_up[i], moe_w_down[i])
        x = layer_norm(DEEPNORM_ALPHA * x + ff, ln_ffn_g[l], ln_ffn_b[l])
    return x
```

```python
import contextlib
import math

import numpy as np
import concourse.bass as bass
import concourse.mybir as mybir
from concourse.bass_utils import run_bass_kernel_spmd

F32 = mybir.dt.float32
BF16 = mybir.dt.bfloat16
I32 = mybir.dt.int32
AF = mybir.ActivationFunctionType
ALU = mybir.AluOpType
AX = mybir.AxisListType

CENG = ("pe", "act", "dve", "pool", "sp")

D = 1024
S = 4096
DEPTH = 4
NIN = 6664
BW = 512
DFF = 2816
NEXP = 8
DFE = 1408
NMEM = 256
ALPHA = (2 * DEPTH) ** 0.25
LN_EPS = 1e-5
ROPE_THETA = 500000.0
O_U, O_QD, O_KD, O_VD, O_QF, O_KF, O_VF, O_F, O_G = 0, 512, 1024, 1536, 2048, 2560, 3072, 3584, 3592
VA = 520


class Buf:
    __slots__ = ("name", "w", "r", "excl")

    def __init__(self, name="", excl=False):
        self.name = name
        self.w = None
        self.r = {}
        self.excl = excl


class Op:
    __slots__ = ("eng", "fn", "waits", "sig", "count", "dma")

    def __init__(self, eng, fn, dma=None):
        self.eng = eng
        self.fn = fn
        self.waits = []
        self.sig = False
        self.count = 0
        self.dma = dma


class KB:
    SB_LO = 16640
    SB_HI = 229376

    def __init__(self, nc, n_dma_slots=12):
        self.nc = nc
        self.ops = {e: [] for e in CENG}
        self.seen_c = {e: {s: -1 for s in CENG} for e in CENG}
        self.seen_d = {e: {} for e in CENG}
        self.dq = {"sp": [], "pool": [], "act": []}
        self.slot_cnt = {}
        self.slot_rr = {"sp": 0, "pool": 0, "act": 0}
        for q in self.dq:
            for i in range(n_dma_slots if q != "act" else 4):
                sid = (q, i)
                self.dq[q].append(sid)
                self.slot_cnt[sid] = 0
        self.sb_off = self.SB_LO
        self.n_alloc = 0
        self.ps = []
        self.ps_rr = 0
        self.pa_rr = 0

    def sb(self, name, shape, dtype):
        esz = {F32: 4, BF16: 2, I32: 4}[dtype]
        n = esz
        for d in shape[1:]:
            n *= d
        n = (n + 63) // 64 * 64
        off = self.sb_off
        assert off + n <= self.SB_HI, "SBUF overflow at %s: %d + %d" % (name, off, n)
        self.sb_off += n
        self.n_alloc += 1
        t = self.nc.alloc_sbuf_tensor_at("%s_%d" % (name, self.n_alloc), list(shape), dtype, offset=off)
        return t, Buf(name)

    def mark(self):
        return self.sb_off

    def reset(self, m):
        self.sb_off = m

    def psum(self):
        p = self.ps[self.ps_rr % 5]
        self.ps_rr += 1
        return p

    def psum_acc(self):
        p = self.ps[5 + self.pa_rr % 3]
        self.pa_rr += 1
        return p

    def _collect(self, eng, reads, writes, is_dma):
        ev = []
        for b in reads:
            if b.w is not None:
                ev.append(b.w)
            if b.excl:
                for k, e in b.r.items():
                    if e[0] == "d" or e[1] != eng:
                        ev.append(e)
        for b in writes:
            if b.w is not None:
                if is_dma or b.w[0] == "d" or b.w[1] != eng or eng != "pe":
                    ev.append(b.w)
            for k, e in b.r.items():
                if is_dma or e[0] == "d" or e[1] != eng or eng != "pe":
                    ev.append(e)
        return ev

    def _reduce(self, eng, evs):
        best_c = {}
        best_d = {}
        for e in evs:
            if e[0] == "c":
                if e[2] > best_c.get(e[1], -1):
                    best_c[e[1]] = e[2]
            else:
                if e[2] > best_d.get(e[1], -1):
                    best_d[e[1]] = e[2]
        out = []
        for s, i in best_c.items():
            if i > self.seen_c[eng][s]:
                self.seen_c[eng][s] = i
                out.append(("c", s, i))
                self.ops[s][i].sig = True
        for sl, v in best_d.items():
            if v > self.seen_d[eng].get(sl, 0):
                self.seen_d[eng][sl] = v
                out.append(("d", sl, v))
        return out

    def _update(self, ev, reads, writes, rkey):
        for b in reads:
            b.r[rkey] = ev
        for b in writes:
            b.w = ev
            b.r = {}

    def op(self, eng, fn, reads=(), writes=()):
        o = Op(eng, fn)
        evs = self._collect(eng, reads, writes, False)
        o.waits = self._reduce(eng, evs)
        idx = len(self.ops[eng])
        self.ops[eng].append(o)
        self._update(("c", eng, idx), reads, writes, eng)
        return o

    def dma(self, q, out, in_, reads=(), writes=(), **kw):
        slots = self.dq[q]
        sid = slots[self.slot_rr[q] % len(slots)]
        self.slot_rr[q] += 1
        evs = self._collect(q, reads, writes, True)
        if self.slot_cnt[sid] > 0:
            evs.append(("d", sid, self.slot_cnt[sid] * 16))
        self.slot_cnt[sid] += 1
        val = self.slot_cnt[sid] * 16
        o = Op(q, lambda e: e.dma_start(out=out, in_=in_, **kw), dma=(sid, val))
        o.waits = self._reduce(q, evs)
        self.ops[q].append(o)
        self._update(("d", sid, val), reads, writes, ("d", sid))
        return o

    def barrier(self):
        evs = []
        for e in CENG:
            if e != "dve":
                for i in range(len(self.ops[e]) - 1, -1, -1):
                    if self.ops[e][i].dma is None and self.ops[e][i].fn is not None:
                        evs.append(("c", e, i))
                        break
        dev = [("d", sid, c * 16) for sid, c in self.slot_cnt.items() if c > 0]
        tok = self.bar_tile
        o = Op("dve", lambda e: e.memset(tok[:], 0.0))
        evs += self._collect("dve", [], [self.bar_buf], False)
        o.waits = self._reduce("dve", evs + dev)
        idx = len(self.ops["dve"])
        self.ops["dve"].append(o)
        self._update(("c", "dve", idx), [], [self.bar_buf], "dve")
        for e in CENG:
            if e == "dve":
                continue
            o2 = Op(e, None)
            o2.waits = self._reduce(e, [("c", "dve", idx)] + dev)
            self.ops[e].append(o2)

    def finish(self, stack):
        nc = self.nc
        for e in CENG:
            c = 0
            for o in self.ops[e]:
                if o.sig:
                    c += 1
                    o.count = c
        sems = {e: stack.enter_context(nc.semaphore("s_" + e)) for e in CENG}
        dsems = {}
        for q, slots in self.dq.items():
            for sid in slots:
                if self.slot_cnt[sid] > 0:
                    dsems[sid] = stack.enter_context(nc.semaphore("d_%s%d" % sid))
        block = stack.enter_context(nc.Block())
        ops = self.ops

        def replay(ename):
            def run(eng):
                for o in ops[ename]:
                    for w in o.waits:
                        if w[0] == "c":
                            eng.wait_ge(sems[w[1]], ops[w[1]][w[2]].count)
                        else:
                            eng.wait_ge(dsems[w[1]], w[2])
                    if o.fn is None:
                        continue
                    ins = o.fn(eng)
                    if o.dma is not None:
                        ins.then_inc(dsems[o.dma[0]], 16)
                    elif o.sig:
                        ins.then_inc(sems[ename], 1)
            return run

        block.tensor(replay("pe"))
        block.scalar(replay("act"))
        block.vector(replay("dve"))
        block.gpsimd(replay("pool"))
        block.sync(replay("sp"))
        return {e: len(ops[e]) for e in CENG}


class Prog:
    def __init__(self, nc, stack, layers=range(DEPTH), stages="ABC", debug=()):
        self.nc = nc
        self.stack = stack
        self.kb = KB(nc)
        self.layers = list(layers)
        self.stages = stages
        self.debug = set(debug)
        self.inp = {}
        self.scr = {}

    def din(self, name, shape, dtype=F32):
        t = self.nc.dram_tensor(name, list(shape), dtype, kind="ExternalInput").ap()
        self.inp[name] = t
        return t

    def dscr(self, name, shape, dtype):
        kind = "ExternalOutput" if name in self.debug else "Internal"
        t = self.nc.dram_tensor(name, list(shape), dtype, kind=kind).ap()
        self.scr[name] = t
        return t

    SHAPES = {
        "x": ([S, D], F32), "mem": ([NMEM, D], F32), "positions": ([128, 32], I32),
        "w_in": ([DEPTH, D, NIN], F32), "b_forget": ([DEPTH, 8], F32),
        "ssm_lambda_re": ([DEPTH, 32, 64], F32), "ssm_lambda_im": ([DEPTH, 32, 64], F32), "ssm_log_dt": ([DEPTH, 32], F32),
        "ssm_b_re": ([DEPTH, 32, 64, 16], F32), "ssm_b_im": ([DEPTH, 32, 64, 16], F32),
        "ssm_c_re": ([DEPTH, 32, 16, 64], F32), "ssm_c_im": ([DEPTH, 32, 16, 64], F32),
        "ssm_d": ([DEPTH, 512], F32), "w_glu": ([DEPTH, 512, 512], F32), "w_branch": ([DEPTH, 3, 512, D], F32),
        "w_mix_out": ([DEPTH, D, D], F32), "ln_mix_g": ([DEPTH, D], F32), "ln_mix_b": ([DEPTH, D], F32),
        "w_xq": ([DEPTH, D, D], F32), "w_xk": ([DEPTH, D, D], F32), "w_xv": ([DEPTH, D, D], F32), "w_xo": ([DEPTH, D, D], F32),
        "ln_x_g": ([DEPTH, D], F32), "ln_x_b": ([DEPTH, D], F32),
        "ffn_w_gate": ([2, D, DFF], F32), "ffn_w_up": ([2, D, DFF], F32), "ffn_w_down": ([2, DFF, D], F32),
        "moe_w_router": ([2, D, NEXP], F32), "moe_b_router": ([2, NEXP], F32),
        "moe_w_gate": ([2, NEXP, D, DFE], F32), "moe_w_up": ([2, NEXP, D, DFE], F32), "moe_w_down": ([2, NEXP, DFE, D], F32),
        "ln_ffn_g": ([DEPTH, D], F32), "ln_ffn_b": ([DEPTH, D], F32),
    }

    def I(self, name):
        if name not in self.inp:
            shp, dt_ = self.SHAPES[name]
            self.din(name, shp, dt_)
        return self.inp[name]

    def declare(self):
        self.x = self.I("x")
        self.pos = self.I("positions")
        self.out = self.nc.dram_tensor("out", [S, D], F32, kind="ExternalOutput").ap()
        self.xres = self.dscr("xres", [D, S], F32)
        self.xTd = self.dscr("xTd", [D, S], BF16)
        self.u_tm = self.dscr("u_tm", [S, 512], BF16)
        self.qdT = self.dscr("qdT", [512, S], BF16)
        self.kdT = self.dscr("kdT", [512, S], BF16)
        self.vd = self.dscr("vd", [S, VA], BF16)
        self.qfT = self.dscr("qfT", [512, S], BF16)
        self.kfT = self.dscr("kfT", [512, S], BF16)
        self.vf = self.dscr("vf", [S, VA], BF16)
        self.fl = self.dscr("fl", [8, S], F32)
        self.gT = self.dscr("gT", [3 * D, S], BF16)
        self.ysT = self.dscr("ysT", [3 * BW, S], BF16)
        self.dscr("augq", [8, 6, S], BF16)
        self.dscr("augk", [8, 6, S], BF16)

    def setup(self):
        kb, nc = self.kb, self.nc
        st = self.stack
        kb.ps = [(st.enter_context(nc.psum_tensor("ps%d" % i, [128, 512], F32)), Buf("ps%d" % i, True)) for i in range(8)]
        bar, bar_b = kb.sb("bar", [128, 8], F32)
        kb.bar_tile = bar
        kb.bar_buf = bar_b
        self.identf, self.identf_b = kb.sb("identf", [128, 128], F32)
        self.identb, self.identb_b = kb.sb("identb", [128, 128], BF16)
        idf, idb = self.identf, self.identb
        kb.op("pool", lambda e: e.memset(idf[:], 1.0), writes=[self.identf_b])
        kb.op("pool", lambda e: e.affine_select(out=idf[:], in_=idf[:], pattern=[[1, 128]], compare_op=ALU.is_equal,
                                                fill=0.0, base=0, channel_multiplier=-1),
              reads=[self.identf_b], writes=[self.identf_b])
        kb.op("dve", lambda e: e.tensor_copy(out=idb[:], in_=idf[:]), reads=[self.identf_b], writes=[self.identb_b])
        self.rot = {}
        for n_ in ["cos", "sin", "cosq", "sinq"]:
            self.rot[n_] = kb.sb("rot_" + n_, [128, 32, 8], F32)
        self.memT, self.memT_b = kb.sb("memT", [128, 8, NMEM], BF16)
        self._build_rot(kb.mark())
        self._build_mem()

    def _build_rot(self, m2):
        kb = self.kb
        posi, posi_b = kb.sb("posi2", [128, 32], I32)
        kb.dma("sp", posi[:], self.pos[:, :], writes=[posi_b])
        posf, posf_b = kb.sb("posf2", [128, 32], F32)
        kb.op("dve", lambda e: e.tensor_copy(out=posf[:], in_=posi[:]), reads=[posi_b], writes=[posf_b])
        ang, ang_b = kb.sb("ang2", [128, 32, 8], F32)
        for i in range(8):
            f32 = float(np.float32(ROPE_THETA ** (-(2.0 * i) / 16.0)))
            kb.op("dve", lambda e, i=i, f32=f32: e.tensor_scalar(out=ang[:, :, i], in0=posf[:], scalar1=f32, scalar2=None, op0=ALU.mult),
                  reads=[posf_b], writes=[ang_b])
        TWO_PI = 2.0 * math.pi
        C1 = 6.28125
        C2 = TWO_PI - C1

        def reduce_sin(dst, dst_b, shift, scale):
            a2, a2_b = kb.sb("a2", [128, 256], F32)
            kf, kf_b = kb.sb("kf", [128, 256], F32)
            ki, ki_b = kb.sb("ki", [128, 256], I32)
            angf = ang[:].rearrange("p a b -> p (a b)")
            kb.op("dve", lambda e: e.tensor_scalar(out=a2[:], in0=angf, scalar1=float(shift), scalar2=None, op0=ALU.add),
                  reads=[ang_b], writes=[a2_b])
            kb.op("dve", lambda e: e.tensor_scalar(out=kf[:], in0=a2[:], scalar1=float(1.0 / TWO_PI), scalar2=None, op0=ALU.mult),
                  reads=[a2_b], writes=[kf_b])
            kb.op("dve", lambda e: e.tensor_copy(out=ki[:], in_=kf[:]), reads=[kf_b], writes=[ki_b])
            kb.op("dve", lambda e: e.tensor_copy(out=kf[:], in_=ki[:]), reads=[ki_b], writes=[kf_b])
            kb.op("dve", lambda e: e.scalar_tensor_tensor(out=a2[:], in0=kf[:], scalar=-C1, in1=a2[:], op0=ALU.mult, op1=ALU.add),
                  reads=[kf_b, a2_b], writes=[a2_b])
            kb.op("dve", lambda e: e.scalar_tensor_tensor(out=a2[:], in0=kf[:], scalar=-C2, in1=a2[:], op0=ALU.mult, op1=ALU.add),
                  reads=[kf_b, a2_b], writes=[a2_b])
            kb.op("dve", lambda e: e.tensor_scalar(out=a2[:], in0=a2[:], scalar1=float(-math.pi), scalar2=float(math.pi), op0=ALU.max, op1=ALU.min),
                  reads=[a2_b], writes=[a2_b])
            d = dst[:].rearrange("p a b -> p (a b)")
            kb.op("act", lambda e: e.activation(out=d, in_=a2[:], func=AF.Sin), reads=[a2_b], writes=[dst_b])
            if scale != 1.0:
                kb.op("dve", lambda e: e.tensor_scalar(out=d, in0=d, scalar1=float(scale), scalar2=None, op0=ALU.mult),
                      reads=[dst_b], writes=[dst_b])

        reduce_sin(*self.rot["sin"], 0.0, 1.0)
        reduce_sin(*self.rot["cos"], math.pi / 2, 1.0)
        reduce_sin(*self.rot["sinq"], 0.0, 0.125)
        reduce_sin(*self.rot["cosq"], math.pi / 2, 0.125)
        kb.barrier()
        kb.reset(m2)
        self.arena0 = m2

    def _build_mem(self):
        kb = self.kb
        kb.reset(self.arena0)
        mem = self.I("mem")
        mi = [kb.sb("memin%d" % i, [128, D], F32) for i in range(2)]
        for mt in range(2):
            t, t_b = mi[mt]
            kb.dma("sp", t[:], mem[mt * 128:(mt + 1) * 128, :], writes=[t_b])
            for half in range(2):
                ps, ps_b = kb.psum()
                for j in range(4):
                    kt = half * 4 + j
                    kb.op("pe", lambda e, ps=ps, t=t, kt=kt, j=j: e.transpose(ps[:, j * 128:(j + 1) * 128], t[:, kt * 128:(kt + 1) * 128], self.identf[:]),
                          reads=[t_b, self.identf_b], writes=[ps_b])
                kb.op("act", lambda e, ps=ps, half=half, mt=mt: e.activation(out=self.memT[:, half * 4:half * 4 + 4, mt * 128:(mt + 1) * 128],
                                                                             in_=ps[:].rearrange("p (a b) -> p a b", a=4), func=AF.Copy),
                      reads=[ps_b], writes=[self.memT_b])
        kb.barrier()
        kb.reset(self.arena0)

    def stage0(self):
        kb = self.kb
        kb.reset(self.arena0)
        self.xT, self.xT_b = kb.sb("xT", [128, 8, S], BF16)
        xT = self.xT
        xin = [kb.sb("xin%d" % i, [128, D], F32) for i in range(2)]
        stg = [kb.sb("xstg%d" % i, [128, 8, 128], F32) for i in range(2)]
        for tt in range(32):
            xi, xi_b = xin[tt % 2]
            kb.dma("sp", xi[:], self.x[tt * 128:(tt + 1) * 128, :], writes=[xi_b])
            sg, sg_b = stg[tt % 2]
            for half in range(2):
                ps, ps_b = kb.psum()
                for j in range(4):
                    kt = half * 4 + j
                    kb.op("pe", lambda e, ps=ps, xi=xi, kt=kt, j=j: e.transpose(ps[:, j * 128:(j + 1) * 128], xi[:, kt * 128:(kt + 1) * 128],
                                                                               self.identf[:]),
                          reads=[xi_b, self.identf_b], writes=[ps_b])
                pv = ps[:].rearrange("p (a b) -> p a b", a=4)
                kb.op("act", lambda e, pv=pv, half=half, tt=tt: e.activation(out=xT[:, half * 4:half * 4 + 4, tt * 128:(tt + 1) * 128], in_=pv, func=AF.Copy),
                      reads=[ps_b], writes=[self.xT_b])
                kb.op("dve", lambda e, pv=pv, half=half, sg=sg: e.tensor_copy(out=sg[:, half * 4:half * 4 + 4, :], in_=pv),
                      reads=[ps_b], writes=[sg_b])
            kb.dma("sp", self.xres.rearrange("(kt p) t -> p kt t", p=128)[:, :, tt * 128:(tt + 1) * 128], sg[:], reads=[sg_b])
        kb.dma("sp", self.xTd.rearrange("(kt p) t -> p kt t", p=128), xT[:], reads=[self.xT_b])
        kb.barrier()

    def stageA(self, l):
        kb = self.kb
        kb.reset(self.arena0)
        self.xT, self.xT_b = kb.sb("xT", [128, 8, S], BF16)
        xT, xT_b = self.xT, self.xT_b
        kb.dma("sp", xT[:], self.xTd.rearrange("(kt p) t -> p kt t", p=128), writes=[xT_b])
        w = self.I("w_in")[l]
        wv = w.rearrange("(kt p) n -> p kt n", p=128)
        wbs = [kb.sb("wA%d" % i, [128, 8, 512], BF16) for i in range(3)]
        wb8, wb8_b = kb.sb("wA8", [128, 8, 8], BF16)
        self._wi = 0

        def load_w(c0):
            wb, wb_b = wbs[self._wi % 3]
            self._wi += 1
            kb.dma("pool", wb[:], wv[:, :, c0:c0 + 512], writes=[wb_b])
            return wb, wb_b

        stg_u = [kb.sb("stgu%d" % i, [128, 512], BF16) for i in range(3)]
        stg_v = [kb.sb("stgv%d" % i, [128, 8, 65], BF16) for i in range(3)]
        for sv, sv_b in stg_v:
            kb.op("pool", lambda e, sv=sv: e.memset(sv[:], 1.0), writes=[sv_b])
        stg_r = [kb.sb("stgr%d" % i, [128, 512], BF16) for i in range(3)]
        stg_t = [kb.sb("stgt%d" % i, [128, 4, 512], BF16) for i in range(2)]
        rtmp = [kb.sb("rtmp%d" % i, [128, 8, 8], F32) for i in range(4)]

        def tok_block(c0, kind):
            wb, wb_b = load_w(c0)
            for tt in range(32):
                ps, ps_b = kb.psum()
                for kt in range(8):
                    kb.op("pe", lambda e, ps=ps, wb=wb, kt=kt, tt=tt: e.matmul(ps[:], xT[:, kt, tt * 128:(tt + 1) * 128], wb[:, kt, :],
                                                                              start=(kt == 0), stop=(kt == 7)),
                          reads=[xT_b, wb_b], writes=[ps_b])
                if kind == "u":
                    sg, sg_b = stg_u[tt % 3]
                    kb.op("act", lambda e, sg=sg, ps=ps: e.activation(out=sg[:], in_=ps[:], func=AF.Copy), reads=[ps_b], writes=[sg_b])
                    kb.dma("sp", self.u_tm[tt * 128:(tt + 1) * 128, :], sg[:], reads=[sg_b])
                elif kind in ("vd", "vf"):
                    sg, sg_b = stg_v[tt % 3]
                    kb.op("act", lambda e, sg=sg, ps=ps: e.activation(out=sg[:, :, 0:64], in_=ps[:].rearrange("p (h e) -> p h e", h=8), func=AF.Copy),
                          reads=[ps_b], writes=[sg_b])
                    dst = self.vd if kind == "vd" else self.vf
                    kb.dma("sp", dst[tt * 128:(tt + 1) * 128, :], sg[:].rearrange("p h e -> p (h e)"), reads=[sg_b])
                else:
                    isq = kind == "qd"
                    sg, sg_b = stg_r[tt % 3]
                    kb.op("act", lambda e, sg=sg, ps=ps, isq=isq: e.activation(out=sg[:], in_=ps[:], func=AF.Copy, scale=(0.125 if isq else 1.0)),
                          reads=[ps_b], writes=[sg_b])
                    cs, cs_b = self.rot["cosq" if isq else "cos"]
                    sn, sn_b = self.rot["sinq" if isq else "sin"]
                    pv = ps[:].rearrange("p (h e) -> p h e", h=8)
                    sgv = sg[:].rearrange("p (h e) -> p h e", h=8)
                    t1, t2 = pv[:, :, 0:8], pv[:, :, 8:16]
                    cb = cs[:, tt:tt + 1, :].to_broadcast([128, 8, 8])
                    sb_ = sn[:, tt:tt + 1, :].to_broadcast([128, 8, 8])
                    (ra, ra_b), (rb, rb_b), (rc, rc_b), (rd, rd_b) = rtmp
                    kb.op("dve", lambda e, ra=ra, t1=t1, cb=cb: e.tensor_tensor(out=ra[:], in0=t1, in1=cb, op=ALU.mult), reads=[ps_b, cs_b], writes=[ra_b])
                    kb.op("dve", lambda e, rb=rb, t2=t2, sb_=sb_: e.tensor_tensor(out=rb[:], in0=t2, in1=sb_, op=ALU.mult), reads=[ps_b, sn_b], writes=[rb_b])
                    kb.op("dve", lambda e, rc=rc, t2=t2, cb=cb: e.tensor_tensor(out=rc[:], in0=t2, in1=cb, op=ALU.mult), reads=[ps_b, cs_b], writes=[rc_b])
                    kb.op("dve", lambda e, rd=rd, t1=t1, sb_=sb_: e.tensor_tensor(out=rd[:], in0=t1, in1=sb_, op=ALU.mult), reads=[ps_b, sn_b], writes=[rd_b])
                    kb.op("dve", lambda e, sgv=sgv, ra=ra, rb=rb: e.tensor_tensor(out=sgv[:, :, 0:8], in0=ra[:], in1=rb[:], op=ALU.subtract),
                          reads=[ra_b, rb_b, sg_b], writes=[sg_b])
                    kb.op("dve", lambda e, sgv=sgv, rc=rc, rd=rd: e.tensor_tensor(out=sgv[:, :, 8:16], in0=rc[:], in1=rd[:], op=ALU.add),
                          reads=[rc_b, rd_b, sg_b], writes=[sg_b])
                    ps2, ps2_b = kb.psum()
                    for j in range(4):
                        kb.op("pe", lambda e, ps2=ps2, sg=sg, j=j: e.matmul(ps2[:, j * 128:(j + 1) * 128], sg[:, j * 128:(j + 1) * 128], self.identb[:],
                                                                          start=True, stop=True),
                              reads=[sg_b, self.identb_b], writes=[ps2_b])
                    g4 = tt // 4
                    tq = tt % 4
                    tg, tg_b = stg_t[g4 % 2]
                    kb.op("act", lambda e, tg=tg, ps2=ps2, tq=tq: e.activation(out=tg[:, :, tq * 128:(tq + 1) * 128], in_=ps2[:].rearrange("p (a b) -> p a b", a=4), func=AF.Copy),
                          reads=[ps2_b], writes=[tg_b])
                    if tq == 3:
                        dst = self.qdT if isq else self.kdT
                        kb.dma("sp", dst.rearrange("(a p) t -> p a t", p=128)[:, :, g4 * 512:(g4 + 1) * 512], tg[:], reads=[tg_b])

        if "u" in self.blocksA:
            tok_block(O_U, "u")
        if "qd" in self.blocksA:
            tok_block(O_QD, "qd")
            tok_block(O_KD, "kd")
        if "vd" in self.blocksA:
            tok_block(O_VD, "vd")
        if "vf" in self.blocksA:
            tok_block(O_VF, "vf")

        stg_f = [kb.sb("stgf%d" % i, [128, S], BF16) for i in range(3)]
        self._fi = 0

        def feat_block(c0, kind, dst):
            wb, wb_b = load_w(c0)
            for j in range(4):
                sg, sg_b = stg_f[self._fi % 3]
                self._fi += 1
                for tg in range(8):
                    ps, ps_b = kb.psum()
                    for kt in range(8):
                        kb.op("pe", lambda e, ps=ps, wb=wb, kt=kt, tg=tg, j=j: e.matmul(ps[:], wb[:, kt, j * 128:(j + 1) * 128], xT[:, kt, tg * 512:(tg + 1) * 512],
                                                                                      start=(kt == 0), stop=(kt == 7)),
                              reads=[xT_b, wb_b], writes=[ps_b])
                    if kind == "g":
                        kb.op("act", lambda e, sg=sg, ps=ps, tg=tg: e.activation(out=sg[:, tg * 512:(tg + 1) * 512], in_=ps[:], func=AF.Sigmoid),
                              reads=[ps_b], writes=[sg_b])
                    else:
                        sc = 0.125 if kind == "qf" else 1.0
                        if tg % 2 == 0:
                            kb.op("act", lambda e, sg=sg, ps=ps, tg=tg, sc=sc: e.activation(out=sg[:, tg * 512:(tg + 1) * 512], in_=ps[:], func=AF.Copy, scale=sc),
                                  reads=[ps_b], writes=[sg_b])
                        else:
                            kb.op("dve", lambda e, sg=sg, ps=ps, tg=tg, sc=sc: e.tensor_scalar(out=sg[:, tg * 512:(tg + 1) * 512], in0=ps[:], scalar1=sc, scalar2=None, op0=ALU.mult),
                                  reads=[ps_b], writes=[sg_b])
                kb.dma("sp", dst[j * 128:(j + 1) * 128, :], sg[:], reads=[sg_b])

        if "qf" in self.blocksA:
            feat_block(O_QF, "qf", self.qfT)
            feat_block(O_KF, "kf", self.kfT)
        if "g" in self.blocksA:
            for gb in range(6):
                feat_block(O_G + gb * 512, "g", self.gT[gb * 512:(gb + 1) * 512, :])
        if "f" in self.blocksA:
            kb.dma("pool", wb8[:], wv[:, :, O_F:O_F + 8], writes=[wb8_b])
            fs, fs_b = kb.sb("fstg", [8, S], F32)
            for tg in range(8):
                ps, ps_b = kb.psum()
                for kt in range(8):
                    kb.op("pe", lambda e, ps=ps, kt=kt, tg=tg: e.matmul(ps[0:8, :], wb8[:, kt, :], xT[:, kt, tg * 512:(tg + 1) * 512], start=(kt == 0), stop=(kt == 7)),
                          reads=[xT_b, wb8_b], writes=[ps_b])
                kb.op("dve", lambda e, ps=ps, tg=tg: e.tensor_copy(out=fs[:, tg * 512:(tg + 1) * 512], in_=ps[0:8, :]), reads=[ps_b], writes=[fs_b])
            kb.dma("sp", self.fl[:, :], fs[:], reads=[fs_b])
        kb.barrier()

    blocksA = ("u", "qd", "vd", "vf", "qf", "g", "f")

    def build(self):
        self.declare()
        self.setup()
        if "N" not in self.stages:
            self.stage0()
        if "Z" in self.stages:
            kb = self.kb
            kb.reset(self.arena0)
            zt, zt_b = kb.sb("zfill", [128, S], BF16)
            kb.op("pool", lambda e: e.memset(zt[:], 0.0), writes=[zt_b])
            for a in range(12):
                kb.dma("sp", self.ysT[a * 128:(a + 1) * 128, :], zt[:], reads=[zt_b])
            kb.barrier()
        for l in self.layers:
            if "A" in self.stages:
                self.stageA(l)
            if "1" in self.stages:
                self.stageB1(l)
            if "2" in self.stages:
                self.stageB2(l)
            if "3" in self.stages:
                self.stageB3(l)
            if "C" in self.stages:
                self.stageC(l, l == DEPTH - 1 or l == self.layers[-1])
        self.kb.barrier()
        return self.kb.finish(self.stack)


def build_program(layers=range(DEPTH), stages="A123C", debug=()):
    nc = bass.Bass("TRN2", target_bir_lowering=False)
    stack = contextlib.ExitStack()
    with stack:
        p = Prog(nc, stack, layers, stages, debug)
        n = p.build()
    return nc, p, n


INPUT_NAMES = ["x", "mem", "positions", "w_in", "b_forget", "ssm_lambda_re", "ssm_lambda_im", "ssm_log_dt",
               "ssm_b_re", "ssm_b_im", "ssm_c_re", "ssm_c_im", "ssm_d", "w_glu", "w_branch", "w_mix_out",
               "ln_mix_g", "ln_mix_b", "w_xq", "w_xk", "w_xv", "w_xo", "ln_x_g", "ln_x_b",
               "ffn_w_gate", "ffn_w_up", "ffn_w_down", "moe_w_router", "moe_b_router",
               "moe_w_gate", "moe_w_up", "moe_w_down", "ln_ffn_g", "ln_ffn_b"]


def make_in_maps(inputs, n=8, names=None):
    maps = []
    names = names or INPUT_NAMES
    shared = {k: np.ascontiguousarray(np.asarray(inputs[k])) for k in names if k not in ("x", "mem", "positions")}
    x = np.asarray(inputs["x"])
    mem = np.asarray(inputs["mem"])
    pos = np.asarray(inputs["positions"])
    for i in range(n):
        m = dict(shared)
        if "x" in names:
            m["x"] = np.ascontiguousarray(x[i])
        if "mem" in names:
            m["mem"] = np.ascontiguousarray(mem[i])
        if "positions" in names:
            m["positions"] = np.ascontiguousarray(pos[i].astype(np.int32).reshape(32, 128).T)
        maps.append(m)
    return maps


def kernel(**inputs):
    nc, p, n = build_program()
    in_maps = make_in_maps(inputs, names=list(p.inp.keys()))
    res = run_bass_kernel_spmd(nc, in_maps, core_ids=list(range(8)))
    return np.stack([np.asarray(r["out"]) for r in res.results], axis=0).astype(np.float32)


def _stageB3(self, l):
    kb = self.kb
    kb.reset(self.arena0)
    NEG = -30000.0
    mk, mk_b = kb.sb("fmask", [128, 4, 512], BF16)
    mkf, mkf_b = kb.sb("fmaskf", [128, 512], F32)
    for j in range(4):
        kb.op("pool", lambda e: e.memset(mkf[:], 0.0), writes=[mkf_b])
        kb.op("pool", lambda e, j=j: e.affine_select(out=mkf[:], in_=mkf[:], pattern=[[1, 512]], compare_op=ALU.is_ge,
                                                     fill=NEG, base=-j * 128, channel_multiplier=-1),
              reads=[mkf_b], writes=[mkf_b])
        kb.op("dve", lambda e, j=j: e.tensor_copy(out=mk[:, j, :], in_=mkf[:]), reads=[mkf_b], writes=[mk_b])
    ones, ones_b = kb.sb("fones", [128, 64], BF16)
    kb.op("pool", lambda e: e.memset(ones[:], 1.0), writes=[ones_b])
    flt, flt_b = kb.sb("flt", [8, S], F32)
    kb.dma("sp", flt[:], self.fl[:, :], writes=[flt_b])
    bf_, bf_b = kb.sb("bfg", [8, 1], F32)
    kb.dma("sp", bf_[:], self.I("b_forget")[l].rearrange("(h o) -> h o", o=1), writes=[bf_b])
    nb, nb_b = kb.sb("nbfg", [8, 1], F32)
    kb.op("dve", lambda e: e.tensor_scalar(out=nb[:], in0=bf_[:], scalar1=-1.0, scalar2=None, op0=ALU.mult), reads=[bf_b], writes=[nb_b])
    kb.op("act", lambda e: e.activation(out=flt[:], in_=flt[:], func=AF.Exp, scale=-1.0, bias=nb[:]), reads=[flt_b, nb_b], writes=[flt_b])
    kb.op("act", lambda e: e.activation(out=flt[:], in_=flt[:], func=AF.Ln, bias=1.0), reads=[flt_b], writes=[flt_b])
    onesf, onesf_b = kb.sb("onesf", [8, S], F32)
    kb.op("pool", lambda e: e.memset(onesf[:], 1.0), writes=[onesf_b])
    ncum, ncum_b = kb.sb("ncum", [8, S], F32)
    kb.op("dve", lambda e: e.tensor_tensor_scan(out=ncum[:], data0=onesf[:], data1=flt[:], initial=0.0, op0=ALU.mult, op1=ALU.add),
          reads=[onesf_b, flt_b], writes=[ncum_b])
    augk, augk_b = kb.sb("augk", [8, 6, S], BF16)
    augq, augq_b = kb.sb("augq", [8, 6, S], BF16)
    kb.op("pool", lambda e: e.memset(augk[:, 0:3, :], 1.0), writes=[augk_b])
    kb.op("pool", lambda e: e.memset(augq[:, 3:6, :], 1.0), writes=[augq_b])
    rem, rem_b = kb.sb("rem", [8, S], F32)
    kb.op("dve", lambda e: e.tensor_copy(out=augk[:, 3, :], in_=ncum[:]), reads=[ncum_b, augk_b], writes=[augk_b])
    kb.op("dve", lambda e: e.tensor_tensor(out=rem[:], in0=ncum[:], in1=augk[:, 3, :], op=ALU.subtract), reads=[ncum_b, augk_b], writes=[rem_b])
    kb.op("dve", lambda e: e.tensor_copy(out=augk[:, 4, :], in_=rem[:]), reads=[rem_b, augk_b], writes=[augk_b])
    kb.op("dve", lambda e: e.tensor_tensor(out=rem[:], in0=rem[:], in1=augk[:, 4, :], op=ALU.subtract), reads=[rem_b, augk_b], writes=[rem_b])
    kb.op("dve", lambda e: e.tensor_copy(out=augk[:, 5, :], in_=rem[:]), reads=[rem_b, augk_b], writes=[augk_b])
    kb.op("dve", lambda e: e.tensor_scalar(out=augq[:, 0:3, :], in0=augk[:, 3:6, :], scalar1=-1.0, scalar2=None, op0=ALU.mult),
          reads=[augk_b, augq_b], writes=[augq_b])
    aqd = self.scr["augq"]
    akd = self.scr["augk"]
    kb.dma("sp", aqd[:, :, :], augq[:], reads=[augq_b])
    kb.dma("sp", akd[:, :, :], augk[:], reads=[augk_b])
    kb.barrier()
    kb.reset(self.arena0 + 4 * 1024 + 2048 + 256)
    vall, vall_b = kb.sb("fvall", [128, 32, VA], BF16)
    for k0 in range(0, 32, 8):
        kb.dma("sp", vall[:, k0:k0 + 8, :], self.vf.rearrange("(kt p) c -> p kt c", p=128)[:, k0:k0 + 8, :], writes=[vall_b])
    qk = [(kb.sb("fq%d" % i, [70, S], BF16), kb.sb("fk%d" % i, [70, S], BF16)) for i in range(2)]
    pts = [kb.sb("fpt%d" % i, [128, 512], BF16) for i in range(4)]
    osb = [kb.sb("fosb%d" % i, [65, 512], F32) for i in range(2)]
    rds = [kb.sb("frd%d" % i, [65, 512], F32) for i in range(2)]
    rhl = [kb.sb("frhl%d" % i, [65, 2, 512], BF16) for i in range(2)]
    yh = [kb.sb("fyh%d" % i, [64, S], BF16) for i in range(2)]
    self._it = 0
    for h in range(8):
        (qh, qh_b), (kh, kh_b) = qk[h % 2]
        kb.dma("sp", qh[0:64, :], self.qfT[h * 64:(h + 1) * 64, :], writes=[qh_b])
        kb.dma("sp", qh[64:70, :], aqd[h], writes=[qh_b])
        kb.dma("sp", kh[0:64, :], self.kfT[h * 64:(h + 1) * 64, :], writes=[kh_b])
        kb.dma("sp", kh[64:70, :], akd[h], writes=[kh_b])
        yo, yo_b = yh[h % 2]
        for g in range(8):
            po, po_b = kb.psum_acc()
            nkb = 4 * g + 4
            pend = {}

            def emit_qk(kbi, g=g, qh=qh, kh=kh, qh_b=qh_b, kh_b=kh_b):
                ps, ps_b = kb.psum()
                diag = kbi >= 4 * g
                kb.op("pe", lambda e, ps=ps, kh=kh, qh=qh, kbi=kbi, g=g, diag=diag: e.matmul(ps[:], kh[0:70, kbi * 128:(kbi + 1) * 128], qh[0:70, g * 512:(g + 1) * 512],
                                                                                         start=True, stop=not diag),
                      reads=[kh_b, qh_b], writes=[ps_b])
                if diag:
                    j = kbi - 4 * g
                    kb.op("pe", lambda e, ps=ps, j=j: e.matmul(ps[:], self.identb[:], mk[:, j, :], start=False, stop=True),
                          reads=[self.identb_b, mk_b], writes=[ps_b])
                pt, pt_b = pts[self._it % 4]
                self._it += 1
                kb.op("act", lambda e, pt=pt, ps=ps: e.activation(out=pt[:], in_=ps[:], func=AF.Exp), reads=[ps_b], writes=[pt_b])
                pend[kbi] = (pt, pt_b)

            def emit_pv(kbi, po=po, po_b=po_b, h=h, nkb=nkb):
                pt, pt_b = pend.pop(kbi)
                kb.op("pe", lambda e, po=po, pt=pt, kbi=kbi, h=h, nkb=nkb: e.matmul(po[0:65, :], vall[:, kbi, h * 65:(h + 1) * 65], pt[:],
                                                                                    start=(kbi == 0), stop=(kbi == nkb - 1)),
                      reads=[vall_b, pt_b], writes=[po_b])
            LA = 2
            for kbi in range(min(LA, nkb)):
                emit_qk(kbi)
            for kbi in range(nkb):
                if kbi + LA < nkb:
                    emit_qk(kbi + LA)
                emit_pv(kbi)
            self._attn_finalize(po, po_b, osb[g % 2], rds[g % 2], rhl[g % 2], ones, ones_b, yo, yo_b, g)
        kb.dma("sp", self.ysT[2 * BW + h * 64:2 * BW + (h + 1) * 64, :], yo[:], reads=[yo_b])
    kb.barrier()


def _attn_finalize(self, po, po_b, osb_, rds_, rhl_, ones, ones_b, yo, yo_b, g):
    kb = self.kb
    (ob, ob_b), (rd, rd_b), (rh, rh_b) = osb_, rds_, rhl_
    kb.op("act", lambda e: e.activation(out=ob[:], in_=po[0:65, :], func=AF.Copy), reads=[po_b], writes=[ob_b])
    kb.op("dve", lambda e: e.reciprocal(out=rd[64:65, :], in_=ob[64:65, :]), reads=[ob_b], writes=[rd_b])
    kb.op("dve", lambda e: e.tensor_copy(out=rh[64:65, 0, :], in_=rd[64:65, :]), reads=[rd_b], writes=[rh_b])
    kb.op("dve", lambda e: e.tensor_tensor(out=rd[64:65, :], in0=rd[64:65, :], in1=rh[64:65, 0, :], op=ALU.subtract), reads=[rd_b, rh_b], writes=[rd_b])
    kb.op("dve", lambda e: e.tensor_copy(out=rh[64:65, 1, :], in_=rd[64:65, :]), reads=[rd_b, rh_b], writes=[rh_b])
    pb, pb_b = kb.psum()
    kb.op("pe", lambda e: e.matmul(pb[0:64, :], ones[64:65, 0:64], rh[64:65, 0, :], start=True, stop=False), reads=[ones_b, rh_b], writes=[pb_b])
    kb.op("pe", lambda e: e.matmul(pb[0:64, :], ones[64:65, 0:64], rh[64:65, 1, :], start=False, stop=True), reads=[ones_b, rh_b], writes=[pb_b])
    kb.op("dve", lambda e: e.tensor_tensor(out=yo[0:64, g * 512:(g + 1) * 512], in0=ob[0:64, :], in1=pb[0:64, :], op=ALU.mult),
          reads=[ob_b, pb_b], writes=[yo_b])


Prog.stageB3 = _stageB3
Prog._attn_finalize = _attn_finalize


def _stageB2(self, l):
    kb = self.kb
    kb.reset(self.arena0)
    NEG = -30000.0
    mk, mk_b = kb.sb("dmask", [128, 256], BF16)
    mkf, mkf_b = kb.sb("dmaskf", [128, 256], F32)
    kb.op("pool", lambda e: e.memset(mkf[:], 0.0), writes=[mkf_b])
    kb.op("pool", lambda e: e.affine_select(out=mkf[:, 0:128], in_=mkf[:, 0:128], pattern=[[1, 128]], compare_op=ALU.is_ge,
                                            fill=NEG, base=0, channel_multiplier=-1), reads=[mkf_b], writes=[mkf_b])
    kb.op("pool", lambda e: e.affine_select(out=mkf[:, 128:256], in_=mkf[:, 128:256], pattern=[[-1, 128]], compare_op=ALU.is_ge,
                                            fill=NEG, base=0, channel_multiplier=1), reads=[mkf_b], writes=[mkf_b])
    kb.op("dve", lambda e: e.tensor_copy(out=mk[:], in_=mkf[:]), reads=[mkf_b], writes=[mk_b])
    ones, ones_b = kb.sb("dones", [128, 64], BF16)
    kb.op("pool", lambda e: e.memset(ones[:], 1.0), writes=[ones_b])
    DILS = (1, 4, 16)
    vall = {}
    for d in DILS:
        t, t_b = kb.sb("dv%d" % d, [128, 32, VA], BF16)
        nm = 32 // d
        if d == 1:
            for k0 in range(0, 32, 8):
                kb.dma("sp", t[:, k0:k0 + 8, :], self.vd.rearrange("(m p) c -> p m c", p=128)[:, k0:k0 + 8, :], writes=[t_b])
        else:
            src = self.vd.rearrange("(m p r) c -> p r m c", p=128, r=d)
            for r in range(d):
                kb.dma("sp", t[:, r * nm:(r + 1) * nm, :], src[:, r, :, :], writes=[t_b])
        vall[d] = (t, t_b)
    qk = [(kb.sb("dq%d" % i, [128, S], BF16), kb.sb("dk%d" % i, [128, S], BF16)) for i in range(2)]
    pts = [kb.sb("dpt%d" % i, [128, 256], BF16) for i in range(4)]
    acc = [kb.sb("dacc%d" % i, [65, S], F32) for i in range(2)]
    osb = [kb.sb("dosb%d" % i, [65, 512], F32) for i in range(2)]
    rds = [kb.sb("drd%d" % i, [65, 512], F32) for i in range(2)]
    rhl = [kb.sb("drhl%d" % i, [65, 2, 512], BF16) for i in range(2)]
    yh = [kb.sb("dyh%d" % i, [64, S], BF16) for i in range(2)]
    self._it = 0
    for hp in range(4):
        (qt, qt_b), (kt_, kt_b) = qk[hp % 2]
        kb.dma("sp", qt[:], self.qdT[hp * 128:(hp + 1) * 128, :], writes=[qt_b])
        kb.dma("sp", kt_[:], self.kdT[hp * 128:(hp + 1) * 128, :], writes=[kt_b])
        for hh in range(2):
            h = hp * 2 + hh
            pb0 = hh * 64
            ac, ac_b = acc[h % 2]
            items = []
            for d in DILS:
                nm = 32 // d
                for r in range(d):
                    for m in range(nm):
                        items.append((d, r, m, nm))
            pend = {}
            pos = {}

            def emit_qk(i, pb0=pb0, kt_=kt_, qt=qt, kt_b=kt_b, qt_b=qt_b):
                d, r, m, nm = items[i]
                nq = 256 if m < nm - 1 else 128
                t0 = m * 128 * d + r
                ksl = slice(t0, t0 + 127 * d + 1, d)
                qsl = slice(t0, t0 + (nq - 1) * d + 1, d)
                ps, ps_b = kb.psum()
                kb.op("pe", lambda e, ps=ps, ksl=ksl, qsl=qsl, nq=nq, pb0=pb0, kt_=kt_, qt=qt: e.matmul(ps[:, 0:nq], kt_[pb0:pb0 + 64, ksl], qt[pb0:pb0 + 64, qsl], start=True, stop=False),
                      reads=[kt_b, qt_b], writes=[ps_b])
                kb.op("pe", lambda e, ps=ps, nq=nq: e.matmul(ps[:, 0:nq], self.identb[:], mk[:, 0:nq], start=False, stop=True),
                      reads=[self.identb_b, mk_b], writes=[ps_b])
                pt, pt_b = pts[self._it % 4]
                self._it += 1
                kb.op("act", lambda e, pt=pt, ps=ps, nq=nq: e.activation(out=pt[:, 0:nq], in_=ps[:, 0:nq], func=AF.Exp), reads=[ps_b], writes=[pt_b])
                pend[i] = (pt, pt_b, nq, t0)

            def emit_pv(i, h=h, ac=ac, ac_b=ac_b):
                d, r, m, nm = items[i]
                vt, vt_b = vall[d]
                b = r * nm + m
                pt, pt_b, nq, t0 = pend.pop(i)
                if m == 0:
                    pos[(d, r, 0)] = kb.psum_acc()
                po, po_b = pos.pop((d, r, m))
                kb.op("pe", lambda e, po=po, vt=vt, b=b, h=h, pt=pt, m=m: e.matmul(po[0:65, 0:128], vt[:, b, h * 65:(h + 1) * 65], pt[:, 0:128], start=(m == 0), stop=True),
                      reads=[vt_b, pt_b], writes=[po_b])
                if nq == 256:
                    pos[(d, r, m + 1)] = kb.psum_acc()
                    po2, po2_b = pos[(d, r, m + 1)]
                    kb.op("pe", lambda e, po2=po2, vt=vt, b=b, h=h, pt=pt: e.matmul(po2[0:65, 0:128], vt[:, b, h * 65:(h + 1) * 65], pt[:, 128:256], start=True, stop=False),
                          reads=[vt_b, pt_b], writes=[po2_b])
                if d == 1:
                    kb.op("dve", lambda e, ac=ac, po=po, qs=slice(t0, t0 + 128): e.tensor_copy(out=ac[:, qs], in_=po[0:65, 0:128]),
                          reads=[po_b], writes=[ac_b])
                else:
                    qs = slice(t0, t0 + 127 * d + 1, d)
                    kb.op("dve", lambda e, ac=ac, po=po, qs=qs: e.tensor_tensor(out=ac[:, qs], in0=ac[:, qs], in1=po[0:65, 0:128], op=ALU.add),
                          reads=[po_b, ac_b], writes=[ac_b])
            LA = 2
            for i in range(min(LA, len(items))):
                emit_qk(i)
            for i in range(len(items)):
                if i + LA < len(items):
                    emit_qk(i + LA)
                emit_pv(i)
            yo, yo_b = yh[h % 2]
            for g in range(8):
                self._attn_finalize2(ac, ac_b, rds[g % 2], rhl[g % 2], ones, ones_b, yo, yo_b, g)
            kb.dma("sp", self.ysT[BW + h * 64:BW + (h + 1) * 64, :], yo[:], reads=[yo_b])
    kb.barrier()


def _attn_finalize2(self, ac, ac_b, rds_, rhl_, ones, ones_b, yo, yo_b, g):
    kb = self.kb
    (rd, rd_b), (rh, rh_b) = rds_, rhl_
    sl = slice(g * 512, (g + 1) * 512)
    kb.op("dve", lambda e: e.reciprocal(out=rd[64:65, :], in_=ac[64:65, sl]), reads=[ac_b], writes=[rd_b])
    kb.op("dve", lambda e: e.tensor_copy(out=rh[64:65, 0, :], in_=rd[64:65, :]), reads=[rd_b], writes=[rh_b])
    kb.op("dve", lambda e: e.tensor_tensor(out=rd[64:65, :], in0=rd[64:65, :], in1=rh[64:65, 0, :], op=ALU.subtract), reads=[rd_b, rh_b], writes=[rd_b])
    kb.op("dve", lambda e: e.tensor_copy(out=rh[64:65, 1, :], in_=rd[64:65, :]), reads=[rd_b, rh_b], writes=[rh_b])
    pb, pb_b = kb.psum()
    kb.op("pe", lambda e: e.matmul(pb[0:64, :], ones[64:65, 0:64], rh[64:65, 0, :], start=True, stop=False), reads=[ones_b, rh_b], writes=[pb_b])
    kb.op("pe", lambda e: e.matmul(pb[0:64, :], ones[64:65, 0:64], rh[64:65, 1, :], start=False, stop=True), reads=[ones_b, rh_b], writes=[pb_b])
    kb.op("dve", lambda e: e.tensor_tensor(out=yo[0:64, sl], in0=ac[0:64, sl], in1=pb[0:64, :], op=ALU.mult),
          reads=[ac_b, pb_b], writes=[yo_b])


Prog.stageB2 = _stageB2
Prog._attn_finalize2 = _attn_finalize2


def _stageB1(self, l):
    kb = self.kb
    kb.reset(self.arena0)
    PB = Buf("ssm_prep")
    idf, idf_b = self.identf, self.identf_b
    BSt, _ = kb.sb("BSt", [128, 2, 16, 2, 128], BF16)
    Gt, _ = kb.sb("Gt", [128, 2, 16, 2, 128], BF16)
    Tt, _ = kb.sb("Tt", [128, 32, 128], BF16)
    Dr, _ = kb.sb("Dr", [128, 16, 9], F32)
    Di, _ = kb.sb("Di", [128, 16, 9], F32)
    nDi, _ = kb.sb("nDi", [128, 16, 9], F32)
    m_keep = kb.mark()

    def T_(name, shape, dt_=F32):
        t, _ = kb.sb(name, shape, dt_)
        return t

    def dve(fn, extra_r=(), extra_w=()):
        kb.op("dve", fn, reads=[PB] + list(extra_r), writes=[PB] + list(extra_w))

    def tt(out, a, b, op):
        dve(lambda e: e.tensor_tensor(out=out, in0=a, in1=b, op=op))

    def ts(out, a, s1, op0, s2=None, op1=None):
        if op1 is None:
            dve(lambda e: e.tensor_scalar(out=out, in0=a, scalar1=s1, scalar2=None, op0=op0))
        else:
            dve(lambda e: e.tensor_scalar(out=out, in0=a, scalar1=s1, scalar2=s2, op0=op0, op1=op1))

    def cp(out, a):
        dve(lambda e: e.tensor_copy(out=out, in_=a))

    pp = T_("pp", [16, 3, 128])
    ld = T_("ld", [16, 2])
    kb.dma("sp", pp[:, 0, :], self.I("ssm_lambda_re")[l].rearrange("(q a) p -> q (a p)", a=2), writes=[PB])
    kb.dma("sp", pp[:, 1, :], self.I("ssm_lambda_im")[l].rearrange("(q a) p -> q (a p)", a=2), writes=[PB])
    kb.dma("sp", ld[:], self.I("ssm_log_dt")[l].rearrange("(q a) -> q a", a=2), writes=[PB])
    cp(pp[:, 2, :].rearrange("q (a p) -> q a p", a=2), ld[:].unsqueeze(2).to_broadcast([16, 2, 64]))
    par = T_("par", [128, 3, 16])
    ps, ps_b = kb.psum()
    for i in range(3):
        kb.op("pe", lambda e, i=i, ps=ps: e.transpose(ps[:, i * 16:(i + 1) * 16], pp[:, i, :], idf[0:16, 0:16]), reads=[PB, idf_b], writes=[ps_b])
    dve(lambda e, ps=ps: e.tensor_copy(out=par[:].rearrange("p a q -> p (a q)"), in_=ps[:, 0:48]), extra_r=[ps_b])
    lr, li, ldt = par[:, 0, :], par[:, 1, :], par[:, 2, :]
    Bre = T_("Bre", [128, 16, 16])
    Bim = T_("Bim", [128, 16, 16])
    for (dst, nm) in ((Bre, "ssm_b_re"), (Bim, "ssm_b_im")):
        src = self.I(nm)[l].rearrange("(q a) p c -> (a p) q c", a=2)
        for q0 in range(0, 16, 4):
            kb.dma("sp", dst[:, q0:q0 + 4, :], src[:, q0:q0 + 4, :], writes=[PB])
    Cre = T_("Cre", [128, 16, 16])
    Cim = T_("Cim", [128, 16, 16])
    Z = T_("Z", [32, 16, 128])
    zt = T_("zt", [128, 256])
    for (dst, nm) in ((Cre, "ssm_c_re"), (Cim, "ssm_c_im")):
        dve(lambda e: e.memset(Z[:], 0.0))
        src = self.I(nm)[l].rearrange("(q a) c p -> a c q p", a=2)
        kb.dma("sp", Z[0:16, :, 0:64], src[0], reads=[PB], writes=[PB])
        kb.dma("sp", Z[16:32, :, 64:128], src[1], reads=[PB], writes=[PB])
        for q in range(16):
            if q % 8 == 0:
                ps, ps_b = kb.psum()
            kb.op("pe", lambda e, ps=ps, q=q: e.transpose(ps[:, (q % 8) * 32:(q % 8) * 32 + 32], Z[:, q, :], idf[0:32, 0:32]), reads=[PB, idf_b], writes=[ps_b])
            if q % 8 == 7:
                q0 = q - 7
                dve(lambda e, ps=ps: e.tensor_copy(out=zt[:], in_=ps[:, 0:256]), extra_r=[ps_b])
                pv = zt[:].rearrange("p (q a c) -> p q a c", q=8, a=2)
                dve(lambda e, dst=dst, pv=pv, q0=q0: e.tensor_tensor(out=dst[:, q0:q0 + 8, :], in0=pv[:, :, 0, :], in1=pv[:, :, 1, :], op=ALU.add))
    def S_(name):
        return T_(name, [128, 16])
    dt = S_("dt")
    kb.op("act", lambda e: e.activation(out=dt[:], in_=ldt, func=AF.Exp), reads=[PB], writes=[PB])
    x = S_("x")
    tt(x[:], lr, dt[:], ALU.mult)
    er = S_("er")
    ts(er[:], x[:], 1.0 / 7, ALU.mult, 1.0, ALU.add)
    for k in (6, 5, 4, 3, 2, 1):
        tt(er[:], er[:], x[:], ALU.mult)
        ts(er[:], er[:], 1.0 / k, ALU.mult, 1.0, ALU.add)
    phi = S_("phi")
    tt(phi[:], li, dt[:], ALU.mult)
    ts(phi[:], phi[:], 1.0 / 32, ALU.mult)
    z = S_("z")
    tt(z[:], phi[:], phi[:], ALU.mult)
    cr = S_("cr")
    ci_ = S_("ci")
    cc = [1.0, -1.0 / 2, 1.0 / 24, -1.0 / 720, 1.0 / 40320, -1.0 / 3628800, 1.0 / 479001600]
    sc = [1.0, -1.0 / 6, 1.0 / 120, -1.0 / 5040, 1.0 / 362880, -1.0 / 39916800, 1.0 / 6227020800]
    for (dst, co) in ((cr, cc), (ci_, sc)):
        ts(dst[:], z[:], co[6], ALU.mult, co[5], ALU.add)
        for k in (4, 3, 2, 1, 0):
            tt(dst[:], dst[:], z[:], ALU.mult)
            ts(dst[:], dst[:], co[k], ALU.add)
    tt(ci_[:], ci_[:], phi[:], ALU.mult)
    t1, t2, t3, t4 = S_("t1"), S_("t2"), S_("t3"), S_("t4")

    def cmul(or_, oi, ar, ai, br, bi, a1=t1, a2=t2, a3=t3, a4=t4):
        tt(a1, ar, br, ALU.mult)
        tt(a2, ai, bi, ALU.mult)
        tt(a3, ar, bi, ALU.mult)
        tt(a4, ai, br, ALU.mult)
        tt(or_, a1, a2, ALU.subtract)
        tt(oi, a3, a4, ALU.add)

    for _ in range(5):
        cmul(cr[:], ci_[:], cr[:], ci_[:], cr[:], ci_[:], t1[:], t2[:], t3[:], t4[:])
        tt(t1[:], cr[:], cr[:], ALU.mult)
        tt(t2[:], ci_[:], ci_[:], ALU.mult)
        tt(t1[:], t1[:], t2[:], ALU.add)
        ts(t1[:], t1[:], -0.5, ALU.mult, 1.5, ALU.add)
        tt(cr[:], cr[:], t1[:], ALU.mult)
        tt(ci_[:], ci_[:], t1[:], ALU.mult)
    Pl_r = T_("Plr", [128, 9, 16])
    Pl_i = T_("Pli", [128, 9, 16])
    dve(lambda e: e.memset(Pl_r[:, 0, :], 1.0))
    dve(lambda e: e.memset(Pl_i[:, 0, :], 0.0))
    tt(Pl_r[:, 1, :], cr[:], er[:], ALU.mult)
    tt(Pl_i[:, 1, :], ci_[:], er[:], ALU.mult)
    ar, ai = Pl_r[:, 1, :], Pl_i[:, 1, :]
    for k in range(2, 9):
        cmul(Pl_r[:, k, :], Pl_i[:, k, :], Pl_r[:, k - 1, :], Pl_i[:, k - 1, :], ar, ai, t1[:], t2[:], t3[:], t4[:])
    Nl_r = T_("Nlr", [128, 8, 16])
    Nl_i = T_("Nli", [128, 8, 16])
    dve(lambda e: e.memset(Nl_r[:, 0, :], 1.0))
    dve(lambda e: e.memset(Nl_i[:, 0, :], 0.0))
    tt(t1[:], ar, ar, ALU.mult)
    tt(t2[:], ai, ai, ALU.mult)
    tt(t1[:], t1[:], t2[:], ALU.add)
    dve(lambda e: e.reciprocal(out=t1[:], in_=t1[:]))
    tt(Nl_r[:, 1, :], ar, t1[:], ALU.mult)
    tt(Nl_i[:, 1, :], ai, t1[:], ALU.mult)
    ts(Nl_i[:, 1, :], Nl_i[:, 1, :], -1.0, ALU.mult)
    for k in range(2, 8):
        cmul(Nl_r[:, k, :], Nl_i[:, k, :], Nl_r[:, k - 1, :], Nl_i[:, k - 1, :], Nl_r[:, 1, :], Nl_i[:, 1, :], t1[:], t2[:], t3[:], t4[:])
    cp(Dr[:, :, 0], Pl_r[:, 8, :])
    cp(Di[:, :, 0], Pl_i[:, 8, :])
    for k in range(1, 9):
        cmul(Dr[:, :, k], Di[:, :, k], Dr[:, :, k - 1], Di[:, :, k - 1], Dr[:, :, k - 1], Di[:, :, k - 1], t1[:], t2[:], t3[:], t4[:])
    ts(nDi[:], Di[:], -1.0, ALU.mult)
    qr, qi = S_("qr"), S_("qi")
    nr = S_("nr")
    ts(nr[:], ar, -1.0, ALU.add)
    tt(t1[:], lr, lr, ALU.mult)
    tt(t2[:], li, li, ALU.mult)
    tt(t1[:], t1[:], t2[:], ALU.add)
    dve(lambda e: e.reciprocal(out=t1[:], in_=t1[:]))
    tt(t2[:], nr[:], lr, ALU.mult)
    tt(t3[:], ai, li, ALU.mult)
    tt(t2[:], t2[:], t3[:], ALU.add)
    tt(qr[:], t2[:], t1[:], ALU.mult)
    tt(t2[:], ai, lr, ALU.mult)
    tt(t3[:], nr[:], li, ALU.mult)
    tt(t2[:], t2[:], t3[:], ALU.subtract)
    tt(qi[:], t2[:], t1[:], ALU.mult)
    Br2, Bi2 = T_("Br2", [128, 16, 16]), T_("Bi2", [128, 16, 16])
    u1, u2, u3, u4 = (T_("u%d" % i, [128, 16, 16]) for i in range(4))

    def bq(t):
        return t.unsqueeze(2).to_broadcast([128, 16, 16])
    cmul(Br2[:], Bi2[:], Bre[:], Bim[:], bq(qr[:]), bq(qi[:]), u1[:], u2[:], u3[:], u4[:])
    def Bg(name):
        return T_(name, [128, 16, 8, 16])
    g1, g2, g3, g4 = Bg("g1"), Bg("g2"), Bg("g3"), Bg("g4")
    Wm_r, Wm_i = Bg("Wmr"), Bg("Wmi")

    def over_k(t):
        return t.unsqueeze(2).to_broadcast([128, 16, 8, 16])

    def over_c(t):
        return t.rearrange("p k q -> p q k").unsqueeze(3).to_broadcast([128, 16, 8, 16])

    def over_kc(t):
        return t.unsqueeze(2).unsqueeze(3).to_broadcast([128, 16, 8, 16])

    cmul(Wm_r[:], Wm_i[:], over_k(Br2[:]), over_k(Bi2[:]), over_c(Nl_r[:, 0:8, :]), over_c(Nl_i[:, 0:8, :]), g1[:], g2[:], g3[:], g4[:])
    WmM = T_("WmM", [128, 2, 2, 16, 128], BF16)
    dve(lambda e: e.memset(WmM[:].rearrange("p a b q n -> p (a b q n)"), 0.0))
    for a in range(2):
        for part, src in ((0, Wm_r), (1, Wm_i)):
            cp(WmM[a * 64:(a + 1) * 64, a, part, :, :], src[a * 64:(a + 1) * 64].rearrange("p q k c -> p q (k c)"))
    W7_r, W7_i = Bg("W7r"), Bg("W7i")
    cmul(W7_r[:], W7_i[:], Wm_r[:], Wm_i[:], over_kc(Pl_r[:, 7, :]), over_kc(Pl_i[:, 7, :]), g1[:], g2[:], g3[:], g4[:])
    BSt_b = Buf("BSt")
    kb.op("pool", lambda e: e.memset(BSt[:].rearrange("p a q b n -> p (a q b n)"), 0.0), writes=[BSt_b])
    for part, src in ((0, W7_r), (1, W7_i)):
        for q in range(16):
            if q % 4 == 0:
                ps, ps_b = kb.psum()
            kb.op("pe", lambda e, ps=ps, q=q, src=src: e.transpose(ps[:, (q % 4) * 128:(q % 4) * 128 + 128], src[:, q].rearrange("p k c -> p (k c)"), idf[:]),
                  reads=[PB, idf_b], writes=[ps_b])
            if q % 4 == 3:
                for a in range(2):
                    pv = ps[:].rearrange("p (q n) -> p q n", q=4)[:, :, a * 64:(a + 1) * 64]
                    kb.op("act", lambda e, pv=pv, part=part, q=q, a=a: e.activation(out=BSt[:, part, q - 3:q + 1, a, a * 64:(a + 1) * 64], in_=pv, func=AF.Copy),
                          reads=[ps_b, BSt_b], writes=[BSt_b])
    Cp_r, Cp_i = Bg("Cpr"), Bg("Cpi")
    cmul(Cp_r[:], Cp_i[:], over_k(Cre[:]), over_k(Cim[:]), over_c(Pl_r[:, 0:8, :]), over_c(Pl_i[:, 0:8, :]), g1[:], g2[:], g3[:], g4[:])
    CpB = T_("CpB", [128, 2, 16, 128], BF16)
    cp(CpB[:, 0], Cp_r[:].rearrange("p q k c -> p q (k c)"))
    ts(CpB[:, 1], Cp_i[:].rearrange("p q k c -> p q (k c)"), -1.0, ALU.mult)
    G_r, G_i = W7_r, W7_i
    kb.op("dve", lambda e: e.memset(t1[:], 0.0), reads=[PB, BSt_b], writes=[PB])
    cmul(G_r[:], G_i[:], Cp_r[:], Cp_i[:], over_kc(ar), over_kc(ai), g1[:], g2[:], g3[:], g4[:])
    dve(lambda e: e.memset(Gt[:].rearrange("p a q b n -> p (a q b n)"), 0.0))
    for a in range(2):
        cp(Gt[a * 64:(a + 1) * 64, 0, :, a, :], G_r[a * 64:(a + 1) * 64].rearrange("p q k c -> p q (k c)"))
        ts(Gt[a * 64:(a + 1) * 64, 1, :, a, :], G_i[a * 64:(a + 1) * 64].rearrange("p q k c -> p q (k c)"), -1.0, ALU.mult)
    kidx_i = T_("kidx_i", [128, 1], I32)
    kidx = T_("kidx", [128, 1])
    jidx_i = T_("jidx_i", [128, 8, 16], I32)
    jidx = T_("jidx", [128, 128])
    cmask = T_("cmask", [128, 128])
    kb.op("pool", lambda e: e.iota(kidx_i[:], pattern=[[0, 1]], base=0, channel_multiplier=1), reads=[PB], writes=[PB])
    kb.op("pool", lambda e: e.iota(jidx_i[:], pattern=[[1, 8], [0, 16]], base=0, channel_multiplier=0), reads=[PB], writes=[PB])
    dve(lambda e: e.tensor_single_scalar(out=kidx_i[:], in_=kidx_i[:], scalar=4, op=ALU.arith_shift_right))
    cp(kidx[:], kidx_i[:])
    cp(jidx[:], jidx_i[:].rearrange("p a b -> p (a b)"))
    ts(cmask[:], jidx[:], kidx[:, 0:1], ALU.is_ge)
    dB = T_("dB", [128, 512])
    kb.dma("sp", dB[:], self.I("ssm_d")[l].partition_broadcast(128), writes=[PB])
    IDd = T_("IDd", [128, 32, 128], BF16)
    for g in range(32):
        dve(lambda e, g=g: e.tensor_tensor(out=IDd[:, g, :].rearrange("p (j c) -> p j c", j=8), in0=idf[:].rearrange("p (j c) -> p j c", j=8),
                                           in1=dB[:, g * 16:(g + 1) * 16].unsqueeze(1).to_broadcast([128, 8, 16]), op=ALU.mult), extra_r=[idf_b])
    Tt_b = Buf("Tt")
    for g in range(32):
        q, a = g // 2, g % 2
        if g % 4 == 0:
            ps, ps_b = kb.psum()
        sl = slice((g % 4) * 128, (g % 4) * 128 + 128)
        kb.op("pe", lambda e, ps=ps, sl=sl, q=q, a=a: e.matmul(ps[:, sl], WmM[:, a, 0, q, :], CpB[:, 0, q, :], start=True, stop=False), reads=[PB], writes=[ps_b])
        kb.op("pe", lambda e, ps=ps, sl=sl, q=q, a=a: e.matmul(ps[:, sl], WmM[:, a, 1, q, :], CpB[:, 1, q, :], start=False, stop=False), reads=[PB], writes=[ps_b])
        kb.op("pe", lambda e, ps=ps, sl=sl, g=g: e.matmul(ps[:, sl], self.identb[:], IDd[:, g, :], start=False, stop=True), reads=[PB, self.identb_b], writes=[ps_b])
        if g % 4 == 3:
            kb.op("dve", lambda e, ps=ps, g=g: e.tensor_tensor(out=Tt[:, g - 3:g + 1, :], in0=ps[:].rearrange("p (g n) -> p g n", g=4),
                                                               in1=cmask[:].unsqueeze(1).to_broadcast([128, 4, 128]), op=ALU.mult),
                  reads=[ps_b, PB], writes=[Tt_b])
    kb.barrier()
    kb.reset(m_keep)
    self._ssm_main(l, dict(BSt=BSt, Gt=Gt, Tt=Tt, Dr=Dr, Di=Di, nDi=nDi), m_keep)


def _ssm_main(self, l, tb, m0):
    kb = self.kb
    BSt, Gt, Tt, Dr, Di, nDi = tb["BSt"], tb["Gt"], tb["Tt"], tb["Dr"], tb["Di"], tb["nDi"]
    TB = Buf("ssm_tables")
    U, U_b = kb.sb("U", [128, 32, 512], BF16)
    m_x = kb.mark()
    xts = [kb.sb("Xc%d" % i, [128, 8, 512], BF16) for i in range(2)]
    x2s = [kb.sb("X2c%d" % i, [128, 32, 128], BF16) for i in range(2)]
    kb.reset(m_x)
    ygT, ygT_b = kb.sb("ygT", [128, 4, S], BF16)
    for ct in range(4):
        xt0, xt0_b = xts[ct % 2]
        kb.dma("sp", xt0[:].rearrange("p k c -> p (k c)"), self.u_tm[ct * 1024:(ct + 1) * 1024, :].rearrange("(p k) c -> p (k c)", k=8), writes=[xt0_b])
        xt, xt_b = x2s[ct % 2]
        kb.op("pool", lambda e, xt=xt, xt0=xt0: e.tensor_copy(out=xt[:].rearrange("p g (k c) -> p g k c", k=8),
                                                              in_=xt0[:].rearrange("p k (g c) -> p g k c", g=32)),
              reads=[xt0_b], writes=[xt_b])
        for g in range(32):
            if g % 4 == 0:
                ps, ps_b = kb.psum()
            kb.op("pe", lambda e, ps=ps, g=g, xt=xt: e.matmul(ps[:, (g % 4) * 128:(g % 4) * 128 + 128], xt[:, g, :], self.identb[:], start=True, stop=True),
                  reads=[xt_b, self.identb_b], writes=[ps_b])
            if g % 4 == 3:
                eng = "act" if (g // 4) % 2 == 0 else "dve"
                pv = ps[:].rearrange("p (g n) -> p g n", g=4)
                if eng == "act":
                    kb.op("act", lambda e, pv=pv, g=g, ct=ct: e.activation(out=U[:, g - 3:g + 1, ct * 128:(ct + 1) * 128], in_=pv, func=AF.Copy), reads=[ps_b], writes=[U_b])
                else:
                    kb.op("dve", lambda e, pv=pv, g=g, ct=ct: e.tensor_copy(out=U[:, g - 3:g + 1, ct * 128:(ct + 1) * 128], in_=pv), reads=[ps_b], writes=[U_b])
    Yg, Yg_b = kb.sb("Yg", [128, 4, 8, 512], BF16)
    SA = [kb.sb("SA%d" % i, [128, 2, 512], F32) for i in range(2)]
    SB_ = [kb.sb("SB%d" % i, [128, 2, 512], F32) for i in range(2)]
    Ssh = [kb.sb("Ssh%d" % i, [128, 2, 512], BF16) for i in range(2)]
    for q in range(16):
        (sa, sa_b), (sb2, sb2_b), (ssh, ssh_b) = SA[q % 2], SB_[q % 2], Ssh[q % 2]
        for part in range(2):
            ps, ps_b = kb.psum()
            for a in range(2):
                kb.op("pe", lambda e, ps=ps, part=part, a=a, q=q: e.matmul(ps[:], BSt[:, part, q, a, :], U[:, 2 * q + a, :], start=(a == 0), stop=(a == 1)),
                      reads=[U_b, TB], writes=[ps_b])
            kb.op("act", lambda e, ps=ps, part=part, sa=sa: e.activation(out=sa[:, part, :], in_=ps[:], func=AF.Copy), reads=[ps_b], writes=[sa_b])
        cur, cur_b, nxt, nxt_b = sa, sa_b, sb2, sb2_b
        for k in range(9):
            m = 1 << k
            dr, di, ndi = Dr[:, q, k:k + 1], Di[:, q, k:k + 1], nDi[:, q, k:k + 1]
            kb.op("pool", lambda e, cur=cur, nxt=nxt, m=m: e.tensor_copy(out=nxt[:, :, 0:m], in_=cur[:, :, 0:m]), reads=[cur_b], writes=[nxt_b])
            kb.op("dve", lambda e, cur=cur, nxt=nxt, m=m, dr=dr: e.scalar_tensor_tensor(out=nxt[:, 0, m:512], in0=cur[:, 0, 0:512 - m], scalar=dr, in1=cur[:, 0, m:512], op0=ALU.mult, op1=ALU.add),
                  reads=[cur_b, TB], writes=[nxt_b])
            kb.op("dve", lambda e, cur=cur, nxt=nxt, m=m, ndi=ndi: e.scalar_tensor_tensor(out=nxt[:, 0, m:512], in0=cur[:, 1, 0:512 - m], scalar=ndi, in1=nxt[:, 0, m:512], op0=ALU.mult, op1=ALU.add),
                  reads=[cur_b, nxt_b, TB], writes=[nxt_b])
            kb.op("dve", lambda e, cur=cur, nxt=nxt, m=m, di=di: e.scalar_tensor_tensor(out=nxt[:, 1, m:512], in0=cur[:, 0, 0:512 - m], scalar=di, in1=cur[:, 1, m:512], op0=ALU.mult, op1=ALU.add),
                  reads=[cur_b, TB], writes=[nxt_b])
            kb.op("dve", lambda e, cur=cur, nxt=nxt, m=m, dr=dr: e.scalar_tensor_tensor(out=nxt[:, 1, m:512], in0=cur[:, 1, 0:512 - m], scalar=dr, in1=nxt[:, 1, m:512], op0=ALU.mult, op1=ALU.add),
                  reads=[cur_b, nxt_b, TB], writes=[nxt_b])
            cur, cur_b, nxt, nxt_b = nxt, nxt_b, cur, cur_b
        kb.op("pool", lambda e, ssh=ssh: e.memset(ssh[:, :, 0:1], 0.0), writes=[ssh_b])
        kb.op("act", lambda e, ssh=ssh, cur=cur: e.activation(out=ssh[:, :, 1:512], in_=cur[:, :, 0:511], func=AF.Copy), reads=[cur_b, ssh_b], writes=[ssh_b])
        for ct in range(4):
            if ct % 2 == 0:
                ps, ps_b = kb.psum()
            o0 = (ct % 2) * 256
            csl = slice(ct * 128, (ct + 1) * 128)
            kb.op("pe", lambda e, ps=ps, o0=o0, csl=csl, ssh=ssh, q=q: e.matmul(ps[:, o0:o0 + 256], ssh[:, 0, csl], Gt[:, 0, q].rearrange("p a n -> p (a n)"), start=True, stop=False),
                  reads=[ssh_b, TB], writes=[ps_b])
            kb.op("pe", lambda e, ps=ps, o0=o0, csl=csl, ssh=ssh, q=q: e.matmul(ps[:, o0:o0 + 256], ssh[:, 1, csl], Gt[:, 1, q].rearrange("p a n -> p (a n)"), start=False, stop=False),
                  reads=[ssh_b, TB], writes=[ps_b])
            for a in range(2):
                kb.op("pe", lambda e, ps=ps, o0=o0, csl=csl, a=a, q=q: e.matmul(ps[:, o0 + a * 128:o0 + (a + 1) * 128], U[:, 2 * q + a, csl], Tt[:, 2 * q + a, :], start=False, stop=(a == 1)),
                      reads=[U_b, TB], writes=[ps_b])
            kb.op("act", lambda e, ps=ps, o0=o0, ct=ct, q=q: e.activation(out=Yg[:, ct, :, q * 32:(q + 1) * 32].rearrange("p j (a c) -> p a j c", a=2),
                                                                           in_=ps[:, o0:o0 + 256].rearrange("p (a j c) -> p a j c", a=2, j=8), func=AF.Gelu_apprx_tanh),
                  reads=[ps_b], writes=[Yg_b])
    ev = 0
    for ct in range(4):
        for cht in range(4):
            for jh in range(2):
                ps, ps_b = kb.psum()
                for jj in range(4):
                    j = jh * 4 + jj
                    kb.op("pe", lambda e, ps=ps, jj=jj, j=j, ct=ct, cht=cht: e.matmul(ps[:, jj * 128:(jj + 1) * 128], Yg[:, ct, j, cht * 128:(cht + 1) * 128], self.identb[:], start=True, stop=True),
                          reads=[Yg_b, self.identb_b], writes=[ps_b])
                t0 = ct * 1024 + jh * 4
                dst = ygT[:, cht, ct * 1024:(ct + 1) * 1024].rearrange("p (c j) -> p j c", j=8)[:, jh * 4:jh * 4 + 4, :]
                pv = ps[:].rearrange("p (j c) -> p j c", j=4)
                if ev % 2 == 0:
                    kb.op("act", lambda e, dst=dst, pv=pv: e.activation(out=dst, in_=pv, func=AF.Copy), reads=[ps_b], writes=[ygT_b])
                else:
                    kb.op("dve", lambda e, dst=dst, pv=pv: e.tensor_copy(out=dst, in_=pv), reads=[ps_b], writes=[ygT_b])
                ev += 1
    wg, wg_b = kb.sb("wglu", [128, 4, 512], BF16)
    kb.dma("pool", wg[:], self.I("w_glu")[l].rearrange("(kt p) n -> p kt n", p=128), writes=[wg_b])
    sgs = [kb.sb("sg%d" % i, [128, 512], BF16) for i in range(3)]
    yos = [kb.sb("yo%d" % i, [128, S], BF16) for i in range(2)]
    it = 0
    for mo in range(4):
        yo, yo_b = yos[mo % 2]
        for tg in range(8):
            ps, ps_b = kb.psum()
            tsl = slice(tg * 512, (tg + 1) * 512)
            for kt in range(4):
                kb.op("pe", lambda e, ps=ps, kt=kt, mo=mo, tsl=tsl: e.matmul(ps[:], wg[:, kt, mo * 128:(mo + 1) * 128], ygT[:, kt, tsl], start=(kt == 0), stop=(kt == 3)),
                      reads=[wg_b, ygT_b], writes=[ps_b])
            sg, sg_b = sgs[it % 3]
            it += 1
            kb.op("act", lambda e, sg=sg, ps=ps: e.activation(out=sg[:], in_=ps[:], func=AF.Sigmoid), reads=[ps_b], writes=[sg_b])
            kb.op("pool", lambda e, yo=yo, sg=sg, mo=mo, tsl=tsl: e.tensor_tensor(out=yo[:, tsl], in0=ygT[:, mo, tsl], in1=sg[:], op=ALU.mult),
                  reads=[ygT_b, sg_b], writes=[yo_b])
        kb.dma("sp", self.ysT[mo * 128:(mo + 1) * 128, :], yo[:], reads=[yo_b])
    kb.barrier()


Prog.stageB1 = _stageB1
Prog._ssm_main = _ssm_main


def _stageC(self, l, last):
    kb = self.kb
    kb.reset(self.arena0)
    idf, idf_b = self.identf, self.identf_b
    moe = (l % 2 == 1)
    li2 = l // 2
    ones_m, ones_m_b = kb.sb("ones_m", [128, 128], BF16)
    ones_1, ones_1_b = kb.sb("ones_1", [128, 128], BF16)
    kb.op("pool", lambda e: e.memset(ones_m[:], 1.0 / 1024), writes=[ones_m_b])
    kb.op("pool", lambda e: e.memset(ones_1[:], 1.0), writes=[ones_1_b])
    lnp, lnp_b = kb.sb("lnp", [8, 6, 128], F32)
    for i, nm in enumerate(["ln_mix_g", "ln_mix_b", "ln_x_g", "ln_x_b", "ln_ffn_g", "ln_ffn_b"]):
        kb.dma("sp", lnp[:, i, :], self.I(nm)[l].rearrange("(kt p) -> kt p", p=128), writes=[lnp_b])
    lnT, lnT_b = kb.sb("lnT", [128, 6, 8], F32)
    ps, ps_b = kb.psum()
    for i in range(6):
        kb.op("pe", lambda e, i=i, ps=ps: e.transpose(ps[:, i * 8:(i + 1) * 8], lnp[:, i, :], idf[0:8, 0:8]), reads=[lnp_b, idf_b], writes=[ps_b])
    kb.op("dve", lambda e, ps=ps: e.tensor_copy(out=lnT[:].rearrange("p a b -> p (a b)"), in_=ps[:, 0:48]), reads=[ps_b], writes=[lnT_b])
    NW = 4
    wraw = [kb.sb("wC%d" % i, [128, 4096], BF16) for i in range(NW)]
    self._wc = 0

    def wbuf():
        t = wraw[self._wc % NW]
        self._wc += 1
        return t

    def load_w(src, kt_n, ncols):
        t, t_b = wbuf()
        v = t[:, 0:kt_n * ncols].rearrange("p (k n) -> p k n", k=kt_n)
        sv = src.rearrange("(k p) n -> p k n", p=128)
        for k0 in range(0, kt_n, 8):
            k1 = min(kt_n, k0 + 8)
            kb.dma("pool", v[:, k0:k1, :], sv[:, k0:k1, :], writes=[t_b])
        return v, t_b

    def linear(W, kt_n, n_out, rhs_fn, rhs_bufs, consume):
        cpc = 512
        while kt_n * cpc * 2 > 8192:
            cpc //= 2
        c0 = 0
        while c0 < n_out:
            nc_ = min(cpc, n_out - c0)
            wv, w_b = load_w(W[:, c0:c0 + nc_], kt_n, nc_)
            for j in range(nc_ // 128):
                ps, ps_b = kb.psum()
                for kt in range(kt_n):
                    kb.op("pe", lambda e, ps=ps, wv=wv, kt=kt, j=j: e.matmul(ps[:], wv[:, kt, j * 128:(j + 1) * 128], rhs_fn(kt), start=(kt == 0), stop=(kt == kt_n - 1)),
                          reads=[w_b] + rhs_bufs, writes=[ps_b])
                consume((c0 // 128) + j, ps, ps_b)
            c0 += nc_

    memT, memT_b = self.memT, self.memT_b
    KT, KT_b = kb.sb("KT", [128, 8, NMEM], BF16)
    Vm, Vm_b = kb.sb("Vm", [128, 2, D], BF16)

    def cons_k(ot, ps, ps_b):
        kb.op("act", lambda e: e.activation(out=KT[:, ot, :], in_=ps[:, 0:NMEM], func=AF.Copy), reads=[ps_b], writes=[KT_b])
    def lin_k():
        W = self.I("w_xk")[l]
        for c0 in (0, 512):
            wv, w_b = load_w(W[:, c0:c0 + 512], 8, 512)
            for j in range(4):
                ps, ps_b = kb.psum()
                for kt in range(8):
                    kb.op("pe", lambda e, ps=ps, wv=wv, kt=kt, j=j: e.matmul(ps[:, 0:NMEM], wv[:, kt, j * 128:(j + 1) * 128], memT[:, kt, :], start=(kt == 0), stop=(kt == 7)),
                          reads=[w_b, memT_b], writes=[ps_b])
                cons_k(c0 // 128 + j, ps, ps_b)
    lin_k()
    Wv = self.I("w_xv")[l]
    for c0 in (0, 512):
        wv, w_b = load_w(Wv[:, c0:c0 + 512], 8, 512)
        for mt in range(2):
            ps, ps_b = kb.psum()
            for kt in range(8):
                kb.op("pe", lambda e, ps=ps, wv=wv, kt=kt, mt=mt: e.matmul(ps[:], memT[:, kt, mt * 128:(mt + 1) * 128], wv[:, kt, :], start=(kt == 0), stop=(kt == 7)),
                      reads=[w_b, memT_b], writes=[ps_b])
            kb.op("act", lambda e, ps=ps, mt=mt, c0=c0: e.activation(out=Vm[:, mt, c0:c0 + 512], in_=ps[:], func=AF.Copy), reads=[ps_b], writes=[Vm_b])
    if moe:
        wr32, wr32_b = kb.sb("wr32", [128, 8, 8], F32)
        kb.dma("sp", wr32[:], self.I("moe_w_router")[li2].rearrange("(k p) e -> p k e", p=128), writes=[wr32_b])
        wrh, wrh_b = kb.sb("wrh", [128, 2, 8, 8], BF16)
        wrr, wrr_b = kb.sb("wrr", [128, 8, 8], F32)
        kb.op("dve", lambda e: e.tensor_copy(out=wrh[:, 0], in_=wr32[:]), reads=[wr32_b], writes=[wrh_b])
        kb.op("dve", lambda e: e.tensor_tensor(out=wrr[:], in0=wr32[:], in1=wrh[:, 0], op=ALU.subtract), reads=[wr32_b, wrh_b], writes=[wrr_b])
        kb.op("dve", lambda e: e.tensor_copy(out=wrh[:, 1], in_=wrr[:]), reads=[wrr_b, wrh_b], writes=[wrh_b])
        brB, brB_b = kb.sb("brB", [128, 8], F32)
        kb.dma("sp", brB[:], self.I("moe_b_router")[li2].partition_broadcast(128), writes=[brB_b])
        sel, sel_b = kb.sb("sel", [8, 8, 128], BF16)
        self_f, self_f_b = kb.sb("self", [8, 8, 128], F32)
        kb.op("pool", lambda e: e.memset(self_f[:], 1.0), writes=[self_f_b])
        kb.op("pool", lambda e: e.affine_select(out=self_f[:], in_=self_f[:], pattern=[[1, 8], [0, 128]], compare_op=ALU.is_equal, fill=0.0, base=0, channel_multiplier=-1),
              reads=[self_f_b], writes=[self_f_b])
        kb.op("dve", lambda e: e.tensor_copy(out=sel[:], in_=self_f[:]), reads=[self_f_b], writes=[sel_b])
    xr, xr_b = kb.sb("xr", [128, 8, 512], F32)
    vv, vv_b = kb.sb("vv", [128, 8, 512], F32)
    GB = [kb.sb("G%d" % i, [128, 8, 512], BF16) for i in range(4)]
    m_u = kb.mark()
    ys, ys_b = kb.sb("ysg", [128, 12, 512], BF16)
    gt, gt_b = kb.sb("gtg", [128, 24, 512], BF16)
    kb.reset(m_u)
    nft = 11 if moe else 22
    hh, hh_b = kb.sb("hh", [128, nft, 512], BF16)
    if moe:
        macc, macc_b = kb.sb("macc", [128, 8, 512], F32)
    kb.reset(m_u + 36 * 1024)
    PT = [kb.sb("PT%d" % i, [128, 2, 512], BF16) for i in range(2)]
    tmpf = [kb.sb("tmpf%d" % i, [128, 512], F32) for i in range(4)]
    tmpb = [kb.sb("tmpb%d" % i, [128, 512], BF16) for i in range(3)]
    st_mean, st_mean_b = kb.sb("st_mean", [128, 512], F32)
    st_rstd, st_rstd_b = kb.sb("st_rstd", [128, 512], F32)
    if moe:
        lg, lg_b = kb.sb("lg", [128, 4, 8], F32)
        gsm = {n_: kb.sb("g_" + n_, [128, 4, 8], F32) for n_ in ("eq1", "eq2", "lg2", "gate", "gr")}
        gs1 = {n_: kb.sb("s_" + n_, [128, 4], F32) for n_ in ("m1", "m2", "w1", "w2")}
        gTs, gTs_b = kb.sb("gTs", [8, 512], F32)
        gTh, gTh_b = kb.sb("gTh", [8, 2, 512], BF16)
        gTr, gTr_b = kb.sb("gTr", [8, 512], F32)
        xlo, xlo_b = kb.sb("xlo", [128, 8, 512], BF16)
        gbs, gbs_b = kb.sb("gbs", [128, 512], F32)
    if last:
        ostg = [kb.sb("ostg%d" % i, [128, D], F32) for i in range(2)]
    self._ti = 0

    def tf():
        t = tmpf[self._ti % 4]
        self._ti += 1
        return t

    self._tb = 0

    def tbf():
        t = tmpb[self._tb % 3]
        self._tb += 1
        return t

    def layer_norm(gi):
        (vb, vb_b), (vsq, vsq_b) = GB[1], GB[2]
        kb.op("act", lambda e: e.activation(out=vb[:], in_=vv[:], func=AF.Copy), reads=[vv_b], writes=[vb_b])
        kb.op("act", lambda e: e.activation(out=vsq[:], in_=vv[:], func=AF.Square), reads=[vv_b], writes=[vsq_b])
        pm, pm_b = kb.psum()
        for kt in range(8):
            kb.op("pe", lambda e, kt=kt: e.matmul(pm[:], ones_m[:], vb[:, kt, :], start=(kt == 0), stop=(kt == 7)), reads=[ones_m_b, vb_b], writes=[pm_b])
        pq, pq_b = kb.psum()
        for kt in range(8):
            kb.op("pe", lambda e, kt=kt: e.matmul(pq[:], ones_m[:], vsq[:, kt, :], start=(kt == 0), stop=(kt == 7)), reads=[ones_m_b, vsq_b], writes=[pq_b])
        kb.op("act", lambda e: e.activation(out=st_mean[:], in_=pm[:], func=AF.Copy), reads=[pm_b], writes=[st_mean_b])
        (m2, m2_b) = tf()
        kb.op("dve", lambda e: e.tensor_tensor(out=m2[:], in0=st_mean[:], in1=st_mean[:], op=ALU.mult), reads=[st_mean_b], writes=[m2_b])
        kb.op("dve", lambda e: e.tensor_tensor(out=m2[:], in0=pq[:], in1=m2[:], op=ALU.subtract), reads=[pq_b, m2_b], writes=[m2_b])
        kb.op("dve", lambda e: e.tensor_scalar(out=m2[:], in0=m2[:], scalar1=0.0, scalar2=LN_EPS, op0=ALU.max, op1=ALU.add), reads=[m2_b], writes=[m2_b])
        kb.op("act", lambda e: e.activation(out=m2[:], in_=m2[:], func=AF.Sqrt), reads=[m2_b], writes=[m2_b])
        kb.op("dve", lambda e: e.reciprocal(out=st_rstd[:], in_=m2[:]), reads=[m2_b], writes=[st_rstd_b])
        for kt in range(8):
            eng = "dve" if kt % 2 == 0 else "pool"
            kb.op(eng, lambda e, kt=kt: e.tensor_tensor(out=vv[:, kt, :], in0=vv[:, kt, :], in1=st_mean[:], op=ALU.subtract), reads=[vv_b, st_mean_b], writes=[vv_b])
            kb.op(eng, lambda e, kt=kt: e.tensor_tensor(out=vv[:, kt, :], in0=vv[:, kt, :], in1=st_rstd[:], op=ALU.mult), reads=[vv_b, st_rstd_b], writes=[vv_b])
            kb.op(eng, lambda e, kt=kt: e.tensor_scalar(out=xr[:, kt, :], in0=vv[:, kt, :], scalar1=lnT[:, gi, kt:kt + 1], scalar2=lnT[:, gi + 1, kt:kt + 1], op0=ALU.mult, op1=ALU.add),
                  reads=[vv_b, lnT_b], writes=[xr_b])

    def resid_consume(ot, ps, ps_b):
        kb.op("dve", lambda e: e.scalar_tensor_tensor(out=vv[:, ot, :], in0=xr[:, ot, :], scalar=float(ALPHA), in1=ps[:], op0=ALU.mult, op1=ALU.add),
              reads=[xr_b, ps_b], writes=[vv_b])

    xres_v = self.xres.rearrange("(kt p) t -> p kt t", p=128)
    xTd_v = self.xTd.rearrange("(kt p) t -> p kt t", p=128)
    for tg in range(8):
        tsl = slice(tg * 512, (tg + 1) * 512)
        kb.dma("sp", xr[:], xres_v[:, :, tsl], writes=[xr_b])
        ysv = self.ysT.rearrange("(a p) t -> p a t", p=128)
        for a0 in range(0, 12, 6):
            kb.dma("sp", ys[:, a0:a0 + 6, :], ysv[:, a0:a0 + 6, tsl], writes=[ys_b, hh_b])
        gv = self.gT.rearrange("(a p) t -> p a t", p=128)
        for a0 in range(0, 24, 6):
            kb.dma("sp", gt[:, a0:a0 + 6, :], gv[:, a0:a0 + 6, tsl], writes=[gt_b, hh_b] + ([macc_b] if moe else []))
        mg, mg_b = GB[0]
        wbr = []
        for n_ in range(3):
            wbr.append(load_w(self.I("w_branch")[l][n_], 4, D))
        for dt_ in range(8):
            pss = []
            for n_ in range(3):
                wv, w_b = wbr[n_]
                ps, ps_b = kb.psum()
                for ck in range(4):
                    kb.op("pe", lambda e, ps=ps, wv=wv, ck=ck, n_=n_, dt_=dt_: e.matmul(ps[:], wv[:, ck, dt_ * 128:(dt_ + 1) * 128], ys[:, n_ * 4 + ck, :], start=(ck == 0), stop=(ck == 3)),
                          reads=[w_b, ys_b], writes=[ps_b])
                pss.append((ps, ps_b))
            (a0_, a0_b), (a1_, a1_b), (a2_, a2_b) = tf(), tf(), tf()
            kb.op("dve", lambda e, p=pss[0][0], dt_=dt_, a0_=a0_: e.tensor_tensor(out=a0_[:], in0=p[:], in1=gt[:, dt_, :], op=ALU.mult), reads=[pss[0][1], gt_b], writes=[a0_b])
            kb.op("dve", lambda e, p=pss[1][0], dt_=dt_, a1_=a1_: e.tensor_tensor(out=a1_[:], in0=p[:], in1=gt[:, 8 + dt_, :], op=ALU.mult), reads=[pss[1][1], gt_b], writes=[a1_b])
            kb.op("dve", lambda e, p=pss[2][0], dt_=dt_, a2_=a2_: e.tensor_tensor(out=a2_[:], in0=p[:], in1=gt[:, 16 + dt_, :], op=ALU.mult), reads=[pss[2][1], gt_b], writes=[a2_b])
            kb.op("pool", lambda e, a0_=a0_, a1_=a1_: e.tensor_tensor(out=a0_[:], in0=a0_[:], in1=a1_[:], op=ALU.add), reads=[a0_b, a1_b], writes=[a0_b])
            kb.op("pool", lambda e, a0_=a0_, a2_=a2_, dt_=dt_: e.tensor_tensor(out=mg[:, dt_, :], in0=a0_[:], in1=a2_[:], op=ALU.add), reads=[a0_b, a2_b], writes=[mg_b])
        linear(self.I("w_mix_out")[l], 8, D, lambda kt: mg[:, kt, :], [mg_b], resid_consume)
        layer_norm(0)
        xb, xb_b = GB[3]
        kb.op("act", lambda e: e.activation(out=xb[:], in_=xr[:], func=AF.Copy), reads=[xr_b], writes=[xb_b])
        qT, qT_b = GB[0]

        def cons_q(ot, ps, ps_b):
            kb.op("act", lambda e: e.activation(out=qT[:, ot, :], in_=ps[:], func=AF.Copy, scale=1.0 / 16), reads=[ps_b], writes=[qT_b])
        linear(self.I("w_xq")[l], 8, D, lambda kt: xb[:, kt, :], [xb_b], cons_q)
        oT, oT_b = GB[1]
        for h in range(4):
            pt, pt_b = PT[h % 2]
            for mt in range(2):
                ps, ps_b = kb.psum()
                for ee in range(2):
                    et = 2 * h + ee
                    kb.op("pe", lambda e, ps=ps, et=et, mt=mt, ee=ee: e.matmul(ps[:], KT[:, et, mt * 128:(mt + 1) * 128], qT[:, et, :], start=(ee == 0), stop=(ee == 1)),
                          reads=[KT_b, qT_b], writes=[ps_b])
                kb.op("act", lambda e, ps=ps, pt=pt, mt=mt: e.activation(out=pt[:, mt, :], in_=ps[:], func=AF.Exp), reads=[ps_b], writes=[pt_b])
            pd, pd_b = kb.psum()
            for mt in range(2):
                kb.op("pe", lambda e, pt=pt, mt=mt, pd=pd: e.matmul(pd[:], ones_1[:], pt[:, mt, :], start=(mt == 0), stop=(mt == 1)), reads=[ones_1_b, pt_b], writes=[pd_b])
            rd, rd_b = tf()
            kb.op("dve", lambda e, rd=rd, pd=pd: e.reciprocal(out=rd[:], in_=pd[:]), reads=[pd_b], writes=[rd_b])
            for ee in range(2):
                et = 2 * h + ee
                ps, ps_b = kb.psum()
                for mt in range(2):
                    kb.op("pe", lambda e, ps=ps, et=et, mt=mt, pt=pt: e.matmul(ps[:], Vm[:, mt, et * 128:(et + 1) * 128], pt[:, mt, :], start=(mt == 0), stop=(mt == 1)),
                          reads=[Vm_b, pt_b], writes=[ps_b])
                kb.op("dve", lambda e, ps=ps, et=et, rd=rd: e.tensor_tensor(out=oT[:, et, :], in0=ps[:], in1=rd[:], op=ALU.mult), reads=[ps_b, rd_b], writes=[oT_b])
        linear(self.I("w_xo")[l], 8, D, lambda kt: oT[:, kt, :], [oT_b], resid_consume)
        layer_norm(2)
        kb.op("act", lambda e: e.activation(out=xb[:], in_=xr[:], func=AF.Copy), reads=[xr_b], writes=[xb_b])
        def swiglu(Wg, Wu, Wd, nft_, gate_sb=None, down_consume=None):
            gl = {}

            def cons_g(ot, ps, ps_b):
                sg, sg_b = tbf()
                kb.op("act", lambda e: e.activation(out=sg[:], in_=ps[:], func=AF.Silu), reads=[ps_b], writes=[sg_b])
                gl[ot] = (sg, sg_b)

            def cons_u(ot, ps, ps_b):
                sg, sg_b = gl[ot]
                if gate_sb is None:
                    kb.op("dve", lambda e: e.tensor_tensor(out=hh[:, ot, :], in0=ps[:], in1=sg[:], op=ALU.mult), reads=[ps_b, sg_b], writes=[hh_b])
                else:
                    t_, t_b = tf()
                    kb.op("dve", lambda e: e.tensor_tensor(out=t_[:], in0=ps[:], in1=sg[:], op=ALU.mult), reads=[ps_b, sg_b], writes=[t_b])
                    kb.op("pool", lambda e: e.tensor_tensor(out=hh[:, ot, :], in0=t_[:], in1=gate_sb[0][:], op=ALU.mult), reads=[t_b, gate_sb[1]], writes=[hh_b])
            for f0 in range(0, nft_, 3):
                f1 = min(nft_, f0 + 3)
                n_c = (f1 - f0) * 128
                for (W, cons) in ((Wg, cons_g), (Wu, cons_u)):
                    wv, w_b = load_w(W[:, f0 * 128:f0 * 128 + n_c], 8, n_c)
                    for j in range(f1 - f0):
                        ps, ps_b = kb.psum()
                        for kt in range(8):
                            kb.op("pe", lambda e, ps=ps, wv=wv, kt=kt, j=j: e.matmul(ps[:], wv[:, kt, j * 128:(j + 1) * 128], xb[:, kt, :], start=(kt == 0), stop=(kt == 7)),
                                  reads=[w_b, xb_b], writes=[ps_b])
                        cons(f0 + j, ps, ps_b)
            linear(Wd, nft_, D, lambda kt: hh[:, kt, :], [hh_b], down_consume)

        if not moe:
            swiglu(self.I("ffn_w_gate")[li2], self.I("ffn_w_up")[li2], self.I("ffn_w_down")[li2], 22, None, resid_consume)
        else:
            kb.op("dve", lambda e: e.tensor_tensor(out=vv[:], in0=xr[:], in1=xb[:], op=ALU.subtract), reads=[xr_b, xb_b], writes=[vv_b])
            kb.op("act", lambda e: e.activation(out=xlo[:], in_=vv[:], func=AF.Copy), reads=[vv_b], writes=[xlo_b])
            pl, pl_b = kb.psum()
            for tt in range(4):
                tk = slice(tt * 128, (tt + 1) * 128)
                combos = [(xb, xb_b, 0), (xb, xb_b, 1), (xlo, xlo_b, 0)]
                n_mm = 0
                for (xs, xs_b, wpart) in combos:
                    for kt in range(8):
                        kb.op("pe", lambda e, xs=xs, wpart=wpart, kt=kt, tk=tk, tt=tt, n_mm=n_mm, pl=pl: e.matmul(pl[:, tt * 8:(tt + 1) * 8], xs[:, kt, tk], wrh[:, wpart, kt, :], start=(n_mm == 0), stop=(n_mm == 23)),
                              reads=[xs_b, wrh_b], writes=[pl_b])
                        n_mm += 1
            kb.op("dve", lambda e, pl=pl: e.tensor_tensor(out=lg[:], in0=pl[:, 0:32].rearrange("p (t e) -> p t e", t=4), in1=brB[:].unsqueeze(1).to_broadcast([128, 4, 8]), op=ALU.add),
                  reads=[pl_b, brB_b], writes=[lg_b])
            GQ = Buf("gateq")
            eq1, eq2, lg2, gate, gr = (gsm[n_][0] for n_ in ("eq1", "eq2", "lg2", "gate", "gr"))
            m1, m2_, w1, w2 = (gs1[n_][0] for n_ in ("m1", "m2", "w1", "w2"))

            def gd(fn, eng="dve"):
                kb.op(eng, fn, reads=[GQ, lg_b], writes=[GQ])
            gd(lambda e: e.tensor_reduce(out=m1[:], in_=lg[:], axis=AX.X, op=ALU.max))
            gd(lambda e: e.tensor_tensor(out=eq1[:], in0=lg[:], in1=m1[:].unsqueeze(2).to_broadcast([128, 4, 8]), op=ALU.is_equal))
            gd(lambda e: e.scalar_tensor_tensor(out=lg2[:], in0=eq1[:], scalar=-1.0e30, in1=lg[:], op0=ALU.mult, op1=ALU.add))
            gd(lambda e: e.tensor_reduce(out=m2_[:], in_=lg2[:], axis=AX.X, op=ALU.max))
            gd(lambda e: e.tensor_tensor(out=eq2[:], in0=lg2[:], in1=m2_[:].unsqueeze(2).to_broadcast([128, 4, 8]), op=ALU.is_equal))
            gd(lambda e: e.tensor_tensor(out=w2[:], in0=m1[:], in1=m2_[:], op=ALU.subtract))
            gd(lambda e: e.activation(out=w1[:], in_=w2[:], func=AF.Sigmoid), "act")
            gd(lambda e: e.tensor_scalar(out=w2[:], in0=w1[:], scalar1=-1.0, scalar2=1.0, op0=ALU.mult, op1=ALU.add))
            gd(lambda e: e.tensor_tensor(out=gate[:], in0=eq1[:], in1=w1[:].unsqueeze(2).to_broadcast([128, 4, 8]), op=ALU.mult))
            gd(lambda e: e.tensor_tensor(out=gr[:], in0=eq2[:], in1=w2[:].unsqueeze(2).to_broadcast([128, 4, 8]), op=ALU.mult))
            gd(lambda e: e.tensor_tensor(out=gate[:], in0=gate[:], in1=gr[:], op=ALU.add))
            pg, pg_b = kb.psum()
            for tt in range(4):
                kb.op("pe", lambda e, tt=tt, pg=pg: e.transpose(pg[0:8, tt * 128:(tt + 1) * 128], gate[:, tt, :], idf[:]), reads=[GQ, idf_b], writes=[pg_b])
            kb.op("dve", lambda e, pg=pg: e.tensor_copy(out=gTs[:], in_=pg[0:8, :]), reads=[pg_b], writes=[gTs_b])
            kb.op("dve", lambda e: e.tensor_copy(out=gTh[:, 0, :], in_=gTs[:]), reads=[gTs_b], writes=[gTh_b])
            kb.op("dve", lambda e: e.tensor_tensor(out=gTr[:], in0=gTs[:], in1=gTh[:, 0, :], op=ALU.subtract), reads=[gTs_b, gTh_b], writes=[gTr_b])
            kb.op("dve", lambda e: e.tensor_copy(out=gTh[:, 1, :], in_=gTr[:]), reads=[gTr_b, gTh_b], writes=[gTh_b])
            for ex in range(NEXP):
                pgb, pgb_b = kb.psum()
                for part in range(2):
                    kb.op("pe", lambda e, ex=ex, part=part, pgb=pgb: e.matmul(pgb[:], sel[:, ex, :], gTh[:, part, :], start=(part == 0), stop=(part == 1)),
                          reads=[sel_b, gTh_b], writes=[pgb_b])
                kb.op("act", lambda e, pgb=pgb: e.activation(out=gbs[:], in_=pgb[:], func=AF.Copy), reads=[pgb_b], writes=[gbs_b])

                def dcons(ot, ps, ps_b, ex=ex):
                    if ex == 0:
                        kb.op("dve", lambda e: e.tensor_copy(out=macc[:, ot, :], in_=ps[:]), reads=[ps_b], writes=[macc_b])
                    elif ex < NEXP - 1:
                        kb.op("dve", lambda e: e.tensor_tensor(out=macc[:, ot, :], in0=macc[:, ot, :], in1=ps[:], op=ALU.add), reads=[ps_b, macc_b], writes=[macc_b])
                    else:
                        t_, t_b = tf()
                        kb.op("dve", lambda e: e.tensor_tensor(out=t_[:], in0=macc[:, ot, :], in1=ps[:], op=ALU.add), reads=[ps_b, macc_b], writes=[t_b])
                        kb.op("dve", lambda e: e.scalar_tensor_tensor(out=vv[:, ot, :], in0=xr[:, ot, :], scalar=float(ALPHA), in1=t_[:], op0=ALU.mult, op1=ALU.add),
                              reads=[xr_b, t_b], writes=[vv_b])
                swiglu(self.I("moe_w_gate")[li2][ex], self.I("moe_w_up")[li2][ex], self.I("moe_w_down")[li2][ex], 11, (gbs, gbs_b), dcons)
        layer_norm(4)
        kb.dma("sp", xres_v[:, :, tsl], xr[:], reads=[xr_b])
        kb.op("act", lambda e: e.activation(out=xb[:], in_=xr[:], func=AF.Copy), reads=[xr_b], writes=[xb_b])
        kb.dma("sp", xTd_v[:, :, tsl], xb[:], reads=[xb_b])
        if last:
            for tt in range(4):
                og, og_b = ostg[tt % 2]
                for half in range(2):
                    ps, ps_b = kb.psum()
                    for j in range(4):
                        kt = half * 4 + j
                        kb.op("pe", lambda e, ps=ps, kt=kt, j=j, tt=tt: e.transpose(ps[:, j * 128:(j + 1) * 128], xr[:, kt, tt * 128:(tt + 1) * 128], idf[:]),
                              reads=[xr_b, idf_b], writes=[ps_b])
                    kb.op("act", lambda e, ps=ps, og=og, half=half: e.activation(out=og[:, half * 512:(half + 1) * 512], in_=ps[:], func=AF.Copy), reads=[ps_b], writes=[og_b])
                r0 = tg * 512 + tt * 128
                kb.dma("sp", self.out[r0:r0 + 128, :], og[:], reads=[og_b])
    kb.barrier()


Prog.stageC = _stageC
```

```python
import contextlib
import math

import numpy as np
import concourse.bass as bass
import concourse.mybir as mybir
from concourse.bass_utils import run_bass_kernel_spmd

F32 = mybir.dt.float32
BF16 = mybir.dt.bfloat16
I32 = mybir.dt.int32
AF = mybir.ActivationFunctionType
ALU = mybir.AluOpType
AX = mybir.AxisListType

CENG = ("pe", "act", "dve", "pool", "sp")

D = 1024
S = 4096
DEPTH = 4
NIN = 6664
BW = 512
DFF = 2816
NEXP = 8
DFE = 1408
NMEM = 256
ALPHA = (2 * DEPTH) ** 0.25
LN_EPS = 1e-5
ROPE_THETA = 500000.0
O_U, O_QD, O_KD, O_VD, O_QF, O_KF, O_VF, O_F, O_G = 0, 512, 1024, 1536, 2048, 2560, 3072, 3584, 3592
VA = 520


class Buf:
    __slots__ = ("name", "w", "r", "excl")

    def __init__(self, name="", excl=False):
        self.name = name
        self.w = None
        self.r = {}
        self.excl = excl


class Op:
    __slots__ = ("eng", "fn", "waits", "sig", "count", "dma")

    def __init__(self, eng, fn, dma=None):
        self.eng = eng
        self.fn = fn
        self.waits = []
        self.sig = False
        self.count = 0
        self.dma = dma


class KB:
    SB_LO = 16640
    SB_HI = 229376

    def __init__(self, nc, n_dma_slots=12):
        self.nc = nc
        self.ops = {e: [] for e in CENG}
        self.seen_c = {e: {s: -1 for s in CENG} for e in CENG}
        self.seen_d = {e: {} for e in CENG}
        self.dq = {"sp": [], "pool": [], "act": []}
        self.slot_cnt = {}
        self.slot_rr = {"sp": 0, "pool": 0, "act": 0}
        for q in self.dq:
            for i in range(n_dma_slots if q != "act" else 4):
                sid = (q, i)
                self.dq[q].append(sid)
                self.slot_cnt[sid] = 0
        self.sb_off = self.SB_LO
        self.n_alloc = 0
        self.ps = []
        self.ps_rr = 0
        self.pa_rr = 0

    def sb(self, name, shape, dtype):
        esz = {F32: 4, BF16: 2, I32: 4}[dtype]
        n = esz
        for d in shape[1:]:
            n *= d
        n = (n + 63) // 64 * 64
        off = self.sb_off
        assert off + n <= self.SB_HI, "SBUF overflow at %s: %d + %d" % (name, off, n)
        self.sb_off += n
        self.n_alloc += 1
        t = self.nc.alloc_sbuf_tensor_at("%s_%d" % (name, self.n_alloc), list(shape), dtype, offset=off)
        return t, Buf(name)

    def mark(self):
        return self.sb_off

    def reset(self, m):
        self.sb_off = m

    def psum(self):
        p = self.ps[self.ps_rr % 5]
        self.ps_rr += 1
        return p

    def psum_acc(self):
        p = self.ps[5 + self.pa_rr % 3]
        self.pa_rr += 1
        return p

    def _collect(self, eng, reads, writes, is_dma):
        ev = []
        for b in reads:
            if b.w is not None:
                ev.append(b.w)
            if b.excl:
                for k, e in b.r.items():
                    if e[0] == "d" or e[1] != eng:
                        ev.append(e)
        for b in writes:
            if b.w is not None:
                if is_dma or b.w[0] == "d" or b.w[1] != eng or eng != "pe":
                    ev.append(b.w)
            for k, e in b.r.items():
                if is_dma or e[0] == "d" or e[1] != eng or eng != "pe":
                    ev.append(e)
        return ev

    def _reduce(self, eng, evs):
        best_c = {}
        best_d = {}
        for e in evs:
            if e[0] == "c":
                if e[2] > best_c.get(e[1], -1):
                    best_c[e[1]] = e[2]
            else:
                if e[2] > best_d.get(e[1], -1):
                    best_d[e[1]] = e[2]
        out = []
        for s, i in best_c.items():
            if i > self.seen_c[eng][s]:
                self.seen_c[eng][s] = i
                out.append(("c", s, i))
                self.ops[s][i].sig = True
        for sl, v in best_d.items():
            if v > self.seen_d[eng].get(sl, 0):
                self.seen_d[eng][sl] = v
                out.append(("d", sl, v))
        return out

    def _update(self, ev, reads, writes, rkey):
        for b in reads:
            b.r[rkey] = ev
        for b in writes:
            b.w = ev
            b.r = {}

    def op(self, eng, fn, reads=(), writes=()):
        o = Op(eng, fn)
        evs = self._collect(eng, reads, writes, False)
        o.waits = self._reduce(eng, evs)
        idx = len(self.ops[eng])
        self.ops[eng].append(o)
        self._update(("c", eng, idx), reads, writes, eng)
        return o

    def dma(self, q, out, in_, reads=(), writes=(), **kw):
        slots = self.dq[q]
        sid = slots[self.slot_rr[q] % len(slots)]
        self.slot_rr[q] += 1
        evs = self._collect(q, reads, writes, True)
        if self.slot_cnt[sid] > 0:
            evs.append(("d", sid, self.slot_cnt[sid] * 16))
        self.slot_cnt[sid] += 1
        val = self.slot_cnt[sid] * 16
        o = Op(q, lambda e: e.dma_start(out=out, in_=in_, **kw), dma=(sid, val))
        o.waits = self._reduce(q, evs)
        self.ops[q].append(o)
        self._update(("d", sid, val), reads, writes, ("d", sid))
        return o

    def barrier(self):
        evs = []
        for e in CENG:
            if e != "dve":
                for i in range(len(self.ops[e]) - 1, -1, -1):
                    if self.ops[e][i].dma is None and self.ops[e][i].fn is not None:
                        evs.append(("c", e, i))
                        break
        dev = [("d", sid, c * 16) for sid, c in self.slot_cnt.items() if c > 0]
        tok = self.bar_tile
        o = Op("dve", lambda e: e.memset(tok[:], 0.0))
        evs += self._collect("dve", [], [self.bar_buf], False)
        o.waits = self._reduce("dve", evs + dev)
        idx = len(self.ops["dve"])
        self.ops["dve"].append(o)
        self._update(("c", "dve", idx), [], [self.bar_buf], "dve")
        for e in CENG:
            if e == "dve":
                continue
            o2 = Op(e, None)
            o2.waits = self._reduce(e, [("c", "dve", idx)] + dev)
            self.ops[e].append(o2)

    def finish(self, stack):
        nc = self.nc
        for e in CENG:
            c = 0
            for o in self.ops[e]:
                if o.sig:
                    c += 1
                    o.count = c
        sems = {e: stack.enter_context(nc.semaphore("s_" + e)) for e in CENG}
        dsems = {}
        for q, slots in self.dq.items():
            for sid in slots:
                if self.slot_cnt[sid] > 0:
                    dsems[sid] = stack.enter_context(nc.semaphore("d_%s%d" % sid))
        block = stack.enter_context(nc.Block())
        ops = self.ops

        def replay(ename):
            def run(eng):
                for o in ops[ename]:
                    for w in o.waits:
                        if w[0] == "c":
                            eng.wait_ge(sems[w[1]], ops[w[1]][w[2]].count)
                        else:
                            eng.wait_ge(dsems[w[1]], w[2])
                    if o.fn is None:
                        continue
                    ins = o.fn(eng)
                    if o.dma is not None:
                        ins.then_inc(dsems[o.dma[0]], 16)
                    elif o.sig:
                        ins.then_inc(sems[ename], 1)
            return run

        block.tensor(replay("pe"))
        block.scalar(replay("act"))
        block.vector(replay("dve"))
        block.gpsimd(replay("pool"))
        block.sync(replay("sp"))
        return {e: len(ops[e]) for e in CENG}


class Prog:
    def __init__(self, nc, stack, layers=range(DEPTH), stages="ABC", debug=()):
        self.nc = nc
        self.stack = stack
        self.kb = KB(nc)
        self.layers = list(layers)
        self.stages = stages
        self.debug = set(debug)
        self.inp = {}
        self.scr = {}

    def din(self, name, shape, dtype=F32):
        t = self.nc.dram_tensor(name, list(shape), dtype, kind="ExternalInput").ap()
        self.inp[name] = t
        return t

    def dscr(self, name, shape, dtype):
        kind = "ExternalOutput" if name in self.debug else "Internal"
        t = self.nc.dram_tensor(name, list(shape), dtype, kind=kind).ap()
        self.scr[name] = t
        return t

    SHAPES = {
        "x": ([S, D], F32), "mem": ([NMEM, D], F32), "positions": ([128, 32], I32),
        "w_in": ([DEPTH, D, NIN], F32), "b_forget": ([DEPTH, 8], F32),
        "ssm_lambda_re": ([DEPTH, 32, 64], F32), "ssm_lambda_im": ([DEPTH, 32, 64], F32), "ssm_log_dt": ([DEPTH, 32], F32),
        "ssm_b_re": ([DEPTH, 32, 64, 16], F32), "ssm_b_im": ([DEPTH, 32, 64, 16], F32),
        "ssm_c_re": ([DEPTH, 32, 16, 64], F32), "ssm_c_im": ([DEPTH, 32, 16, 64], F32),
        "ssm_d": ([DEPTH, 512], F32), "w_glu": ([DEPTH, 512, 512], F32), "w_branch": ([DEPTH, 3, 512, D], F32),
        "w_mix_out": ([DEPTH, D, D], F32), "ln_mix_g": ([DEPTH, D], F32), "ln_mix_b": ([DEPTH, D], F32),
        "w_xq": ([DEPTH, D, D], F32), "w_xk": ([DEPTH, D, D], F32), "w_xv": ([DEPTH, D, D], F32), "w_xo": ([DEPTH, D, D], F32),
        "ln_x_g": ([DEPTH, D], F32), "ln_x_b": ([DEPTH, D], F32),
        "ffn_w_gate": ([2, D, DFF], F32), "ffn_w_up": ([2, D, DFF], F32), "ffn_w_down": ([2, DFF, D], F32),
        "moe_w_router": ([2, D, NEXP], F32), "moe_b_router": ([2, NEXP], F32),
        "moe_w_gate": ([2, NEXP, D, DFE], F32), "moe_w_up": ([2, NEXP, D, DFE], F32), "moe_w_down": ([2, NEXP, DFE, D], F32),
        "ln_ffn_g": ([DEPTH, D], F32), "ln_ffn_b": ([DEPTH, D], F32),
    }

    def I(self, name):
        if name not in self.inp:
            shp, dt_ = self.SHAPES[name]
            self.din(name, shp, dt_)
        return self.inp[name]

    def declare(self):
        self.x = self.I("x")
        self.pos = self.I("positions")
        self.out = self.nc.dram_tensor("out", [S, D], F32, kind="ExternalOutput").ap()
        self.xres = self.dscr("xres", [D, S], F32)
        self.xTd = self.dscr("xTd", [D, S], BF16)
        self.u_tm = self.dscr("u_tm", [S, 512], BF16)
        self.qdT = self.dscr("qdT", [512, S], BF16)
        self.kdT = self.dscr("kdT", [512, S], BF16)
        self.vd = self.dscr("vd", [S, VA], BF16)
        self.qfT = self.dscr("qfT", [512, S], BF16)
        self.kfT = self.dscr("kfT", [512, S], BF16)
        self.vf = self.dscr("vf", [S, VA], BF16)
        self.fl = self.dscr("fl", [8, S], F32)
        self.gT = self.dscr("gT", [3 * D, S], BF16)
        self.ysT = self.dscr("ysT", [3 * BW, S], BF16)
        self.wcb = Buf("wconv")
        self.wc = {
            "w_branch": self.dscr("c_wbr", [3, 512, D], BF16), "w_mix_out": self.dscr("c_wo", [D, D], BF16),
            "w_xq": self.dscr("c_wq", [D, D], BF16), "w_xk": self.dscr("c_wk", [D, D], BF16),
            "w_xv": self.dscr("c_wv", [D, D], BF16), "w_xo": self.dscr("c_wxo", [D, D], BF16),
            "ffn_w_gate": self.dscr("c_fg", [D, DFF], BF16), "ffn_w_up": self.dscr("c_fu", [D, DFF], BF16), "ffn_w_down": self.dscr("c_fd", [DFF, D], BF16),
            "moe_w_gate": self.dscr("c_mg", [NEXP, D, DFE], BF16), "moe_w_up": self.dscr("c_mu", [NEXP, D, DFE], BF16), "moe_w_down": self.dscr("c_md", [NEXP, DFE, D], BF16),
        }
        self.dscr("augq", [8, 6, S], BF16)
        self.dscr("augk", [8, 6, S], BF16)

    def setup(self):
        kb, nc = self.kb, self.nc
        st = self.stack
        kb.ps = [(st.enter_context(nc.psum_tensor("ps%d" % i, [128, 512], F32)), Buf("ps%d" % i, True)) for i in range(8)]
        bar, bar_b = kb.sb("bar", [128, 8], F32)
        kb.bar_tile = bar
        kb.bar_buf = bar_b
        self.identf, self.identf_b = kb.sb("identf", [128, 128], F32)
        self.identb, self.identb_b = kb.sb("identb", [128, 128], BF16)
        idf, idb = self.identf, self.identb
        kb.op("pool", lambda e: e.memset(idf[:], 1.0), writes=[self.identf_b])
        kb.op("pool", lambda e: e.affine_select(out=idf[:], in_=idf[:], pattern=[[1, 128]], compare_op=ALU.is_equal,
                                                fill=0.0, base=0, channel_multiplier=-1),
              reads=[self.identf_b], writes=[self.identf_b])
        kb.op("dve", lambda e: e.tensor_copy(out=idb[:], in_=idf[:]), reads=[self.identf_b], writes=[self.identb_b])
        self.rot = {}
        for n_ in ["cos", "sin", "cosq", "sinq"]:
            self.rot[n_] = kb.sb("rot_" + n_, [128, 32, 8], F32)
        self.memT, self.memT_b = kb.sb("memT", [128, 8, NMEM], BF16)
        self._build_rot(kb.mark())
        self._build_mem()

    def _build_rot(self, m2):
        kb = self.kb
        posi, posi_b = kb.sb("posi2", [128, 32], I32)
        kb.dma("sp", posi[:], self.pos[:, :], writes=[posi_b])
        posf, posf_b = kb.sb("posf2", [128, 32], F32)
        kb.op("dve", lambda e: e.tensor_copy(out=posf[:], in_=posi[:]), reads=[posi_b], writes=[posf_b])
        ang, ang_b = kb.sb("ang2", [128, 32, 8], F32)
        for i in range(8):
            f32 = float(np.float32(ROPE_THETA ** (-(2.0 * i) / 16.0)))
            kb.op("dve", lambda e, i=i, f32=f32: e.tensor_scalar(out=ang[:, :, i], in0=posf[:], scalar1=f32, scalar2=None, op0=ALU.mult),
                  reads=[posf_b], writes=[ang_b])
        TWO_PI = 2.0 * math.pi
        C1 = 6.28125
        C2 = TWO_PI - C1

        def reduce_sin(dst, dst_b, shift, scale):
            a2, a2_b = kb.sb("a2", [128, 256], F32)
            kf, kf_b = kb.sb("kf", [128, 256], F32)
            ki, ki_b = kb.sb("ki", [128, 256], I32)
            angf = ang[:].rearrange("p a b -> p (a b)")
            kb.op("dve", lambda e: e.tensor_scalar(out=a2[:], in0=angf, scalar1=float(shift), scalar2=None, op0=ALU.add),
                  reads=[ang_b], writes=[a2_b])
            kb.op("dve", lambda e: e.tensor_scalar(out=kf[:], in0=a2[:], scalar1=float(1.0 / TWO_PI), scalar2=None, op0=ALU.mult),
                  reads=[a2_b], writes=[kf_b])
            kb.op("dve", lambda e: e.tensor_copy(out=ki[:], in_=kf[:]), reads=[kf_b], writes=[ki_b])
            kb.op("dve", lambda e: e.tensor_copy(out=kf[:], in_=ki[:]), reads=[ki_b], writes=[kf_b])
            kb.op("dve", lambda e: e.scalar_tensor_tensor(out=a2[:], in0=kf[:], scalar=-C1, in1=a2[:], op0=ALU.mult, op1=ALU.add),
                  reads=[kf_b, a2_b], writes=[a2_b])
            kb.op("dve", lambda e: e.scalar_tensor_tensor(out=a2[:], in0=kf[:], scalar=-C2, in1=a2[:], op0=ALU.mult, op1=ALU.add),
                  reads=[kf_b, a2_b], writes=[a2_b])
            kb.op("dve", lambda e: e.tensor_scalar(out=a2[:], in0=a2[:], scalar1=float(-math.pi), scalar2=float(math.pi), op0=ALU.max, op1=ALU.min),
                  reads=[a2_b], writes=[a2_b])
            d = dst[:].rearrange("p a b -> p (a b)")
            kb.op("act", lambda e: e.activation(out=d, in_=a2[:], func=AF.Sin), reads=[a2_b], writes=[dst_b])
            if scale != 1.0:
                kb.op("dve", lambda e: e.tensor_scalar(out=d, in0=d, scalar1=float(scale), scalar2=None, op0=ALU.mult),
                      reads=[dst_b], writes=[dst_b])

        reduce_sin(*self.rot["sin"], 0.0, 1.0)
        reduce_sin(*self.rot["cos"], math.pi / 2, 1.0)
        reduce_sin(*self.rot["sinq"], 0.0, 0.125)
        reduce_sin(*self.rot["cosq"], math.pi / 2, 0.125)
        kb.barrier()
        kb.reset(m2)
        self.arena0 = m2

    def _build_mem(self):
        kb = self.kb
        kb.reset(self.arena0)
        mem = self.I("mem")
        mi = [kb.sb("memin%d" % i, [128, D], F32) for i in range(2)]
        for mt in range(2):
            t, t_b = mi[mt]
            kb.dma("sp", t[:], mem[mt * 128:(mt + 1) * 128, :], writes=[t_b])
            for half in range(2):
                ps, ps_b = kb.psum()
                for j in range(4):
                    kt = half * 4 + j
                    kb.op("pe", lambda e, ps=ps, t=t, kt=kt, j=j: e.transpose(ps[:, j * 128:(j + 1) * 128], t[:, kt * 128:(kt + 1) * 128], self.identf[:]),
                          reads=[t_b, self.identf_b], writes=[ps_b])
                kb.op("act", lambda e, ps=ps, half=half, mt=mt: e.activation(out=self.memT[:, half * 4:half * 4 + 4, mt * 128:(mt + 1) * 128],
                                                                             in_=ps[:].rearrange("p (a b) -> p a b", a=4), func=AF.Copy),
                      reads=[ps_b], writes=[self.memT_b])
        kb.barrier()
        kb.reset(self.arena0)

    def stage0(self):
        kb = self.kb
        kb.reset(self.arena0)
        self.xT, self.xT_b = kb.sb("xT", [128, 8, S], BF16)
        xT = self.xT
        xin = [kb.sb("xin%d" % i, [128, D], F32) for i in range(2)]
        stg = [kb.sb("xstg%d" % i, [128, 8, 128], F32) for i in range(2)]
        for tt in range(32):
            xi, xi_b = xin[tt % 2]
            kb.dma("sp", xi[:], self.x[tt * 128:(tt + 1) * 128, :], writes=[xi_b])
            sg, sg_b = stg[tt % 2]
            for half in range(2):
                ps, ps_b = kb.psum()
                for j in range(4):
                    kt = half * 4 + j
                    kb.op("pe", lambda e, ps=ps, xi=xi, kt=kt, j=j: e.transpose(ps[:, j * 128:(j + 1) * 128], xi[:, kt * 128:(kt + 1) * 128],
                                                                               self.identf[:]),
                          reads=[xi_b, self.identf_b], writes=[ps_b])
                pv = ps[:].rearrange("p (a b) -> p a b", a=4)
                kb.op("act", lambda e, pv=pv, half=half, tt=tt: e.activation(out=xT[:, half * 4:half * 4 + 4, tt * 128:(tt + 1) * 128], in_=pv, func=AF.Copy),
                      reads=[ps_b], writes=[self.xT_b])
                kb.op("dve", lambda e, pv=pv, half=half, sg=sg: e.tensor_copy(out=sg[:, half * 4:half * 4 + 4, :], in_=pv),
                      reads=[ps_b], writes=[sg_b])
            kb.dma("sp", self.xres.rearrange("(kt p) t -> p kt t", p=128)[:, :, tt * 128:(tt + 1) * 128], sg[:], reads=[sg_b])
        kb.dma("sp", self.xTd.rearrange("(kt p) t -> p kt t", p=128), xT[:], reads=[self.xT_b])
        kb.barrier()

    def stageA(self, l):
        kb = self.kb
        kb.reset(self.arena0)
        self.xT, self.xT_b = kb.sb("xT", [128, 8, S], BF16)
        xT, xT_b = self.xT, self.xT_b
        kb.dma("sp", xT[:], self.xTd.rearrange("(kt p) t -> p kt t", p=128), writes=[xT_b])
        w = self.I("w_in")[l]
        wv = w.rearrange("(kt p) n -> p kt n", p=128)
        wbs = [kb.sb("wA%d" % i, [128, 8, 512], BF16) for i in range(3)]
        wb8, wb8_b = kb.sb("wA8", [128, 8, 8], BF16)
        self._wi = 0

        def load_w(c0):
            wb, wb_b = wbs[self._wi % 3]
            self._wi += 1
            kb.dma("pool", wb[:], wv[:, :, c0:c0 + 512], writes=[wb_b])
            return wb, wb_b

        stg_u = [kb.sb("stgu%d" % i, [128, 512], BF16) for i in range(3)]
        stg_v = [kb.sb("stgv%d" % i, [128, 8, 65], BF16) for i in range(3)]
        for sv, sv_b in stg_v:
            kb.op("pool", lambda e, sv=sv: e.memset(sv[:], 1.0), writes=[sv_b])
        stg_r = [kb.sb("stgr%d" % i, [128, 512], BF16) for i in range(3)]
        stg_t = [kb.sb("stgt%d" % i, [128, 4, 512], BF16) for i in range(2)]
        rtmp = [kb.sb("rtmp%d" % i, [128, 8, 8], F32) for i in range(4)]

        def tok_block(c0, kind):
            wb, wb_b = load_w(c0)
            for tt in range(32):
                ps, ps_b = kb.psum()
                for kt in range(8):
                    kb.op("pe", lambda e, ps=ps, wb=wb, kt=kt, tt=tt: e.matmul(ps[:], xT[:, kt, tt * 128:(tt + 1) * 128], wb[:, kt, :],
                                                                              start=(kt == 0), stop=(kt == 7)),
                          reads=[xT_b, wb_b], writes=[ps_b])
                if kind == "u":
                    sg, sg_b = stg_u[tt % 3]
                    kb.op("act", lambda e, sg=sg, ps=ps: e.activation(out=sg[:], in_=ps[:], func=AF.Copy), reads=[ps_b], writes=[sg_b])
                    kb.dma("sp", self.u_tm[tt * 128:(tt + 1) * 128, :], sg[:], reads=[sg_b])
                elif kind in ("vd", "vf"):
                    sg, sg_b = stg_v[tt % 3]
                    kb.op("act", lambda e, sg=sg, ps=ps: e.activation(out=sg[:, :, 0:64], in_=ps[:].rearrange("p (h e) -> p h e", h=8), func=AF.Copy),
                          reads=[ps_b], writes=[sg_b])
                    dst = self.vd if kind == "vd" else self.vf
                    kb.dma("sp", dst[tt * 128:(tt + 1) * 128, :], sg[:].rearrange("p h e -> p (h e)"), reads=[sg_b])
                else:
                    isq = kind == "qd"
                    sg, sg_b = stg_r[tt % 3]
                    kb.op("act", lambda e, sg=sg, ps=ps, isq=isq: e.activation(out=sg[:], in_=ps[:], func=AF.Copy, scale=(0.125 if isq else 1.0)),
                          reads=[ps_b], writes=[sg_b])
                    cs, cs_b = self.rot["cosq" if isq else "cos"]
                    sn, sn_b = self.rot["sinq" if isq else "sin"]
                    pv = ps[:].rearrange("p (h e) -> p h e", h=8)
                    sgv = sg[:].rearrange("p (h e) -> p h e", h=8)
                    t1, t2 = pv[:, :, 0:8], pv[:, :, 8:16]
                    cb = cs[:, tt:tt + 1, :].to_broadcast([128, 8, 8])
                    sb_ = sn[:, tt:tt + 1, :].to_broadcast([128, 8, 8])
                    (ra, ra_b), (rb, rb_b), (rc, rc_b), (rd, rd_b) = rtmp
                    kb.op("dve", lambda e, ra=ra, t1=t1, cb=cb: e.tensor_tensor(out=ra[:], in0=t1, in1=cb, op=ALU.mult), reads=[ps_b, cs_b], writes=[ra_b])
                    kb.op("dve", lambda e, rb=rb, t2=t2, sb_=sb_: e.tensor_tensor(out=rb[:], in0=t2, in1=sb_, op=ALU.mult), reads=[ps_b, sn_b], writes=[rb_b])
                    kb.op("dve", lambda e, rc=rc, t2=t2, cb=cb: e.tensor_tensor(out=rc[:], in0=t2, in1=cb, op=ALU.mult), reads=[ps_b, cs_b], writes=[rc_b])
                    kb.op("dve", lambda e, rd=rd, t1=t1, sb_=sb_: e.tensor_tensor(out=rd[:], in0=t1, in1=sb_, op=ALU.mult), reads=[ps_b, sn_b], writes=[rd_b])
                    kb.op("dve", lambda e, sgv=sgv, ra=ra, rb=rb: e.tensor_tensor(out=sgv[:, :, 0:8], in0=ra[:], in1=rb[:], op=ALU.subtract),
                          reads=[ra_b, rb_b, sg_b], writes=[sg_b])
                    kb.op("dve", lambda e, sgv=sgv, rc=rc, rd=rd: e.tensor_tensor(out=sgv[:, :, 8:16], in0=rc[:], in1=rd[:], op=ALU.add),
                          reads=[rc_b, rd_b, sg_b], writes=[sg_b])
                    ps2, ps2_b = kb.psum()
                    for j in range(4):
                        kb.op("pe", lambda e, ps2=ps2, sg=sg, j=j: e.matmul(ps2[:, j * 128:(j + 1) * 128], sg[:, j * 128:(j + 1) * 128], self.identb[:],
                                                                          start=True, stop=True),
                              reads=[sg_b, self.identb_b], writes=[ps2_b])
                    g4 = tt // 4
                    tq = tt % 4
                    tg, tg_b = stg_t[g4 % 2]
                    kb.op("act", lambda e, tg=tg, ps2=ps2, tq=tq: e.activation(out=tg[:, :, tq * 128:(tq + 1) * 128], in_=ps2[:].rearrange("p (a b) -> p a b", a=4), func=AF.Copy),
                          reads=[ps2_b], writes=[tg_b])
                    if tq == 3:
                        dst = self.qdT if isq else self.kdT
                        kb.dma("sp", dst.rearrange("(a p) t -> p a t", p=128)[:, :, g4 * 512:(g4 + 1) * 512], tg[:], reads=[tg_b])

        if "u" in self.blocksA:
            tok_block(O_U, "u")
        if "qd" in self.blocksA:
            tok_block(O_QD, "qd")
            tok_block(O_KD, "kd")
        if "vd" in self.blocksA:
            tok_block(O_VD, "vd")
        if "vf" in self.blocksA:
            tok_block(O_VF, "vf")

        stg_f = [kb.sb("stgf%d" % i, [128, S], BF16) for i in range(3)]
        self._fi = 0

        def feat_block(c0, kind, dst):
            wb, wb_b = load_w(c0)
            for j in range(4):
                sg, sg_b = stg_f[self._fi % 3]
                self._fi += 1
                for tg in range(8):
                    ps, ps_b = kb.psum()
                    for kt in range(8):
                        kb.op("pe", lambda e, ps=ps, wb=wb, kt=kt, tg=tg, j=j: e.matmul(ps[:], wb[:, kt, j * 128:(j + 1) * 128], xT[:, kt, tg * 512:(tg + 1) * 512],
                                                                                      start=(kt == 0), stop=(kt == 7)),
                              reads=[xT_b, wb_b], writes=[ps_b])
                    if kind == "g":
                        kb.op("act", lambda e, sg=sg, ps=ps, tg=tg: e.activation(out=sg[:, tg * 512:(tg + 1) * 512], in_=ps[:], func=AF.Sigmoid),
                              reads=[ps_b], writes=[sg_b])
                    else:
                        sc = 0.125 if kind == "qf" else 1.0
                        if tg % 2 == 0:
                            kb.op("act", lambda e, sg=sg, ps=ps, tg=tg, sc=sc: e.activation(out=sg[:, tg * 512:(tg + 1) * 512], in_=ps[:], func=AF.Copy, scale=sc),
                                  reads=[ps_b], writes=[sg_b])
                        else:
                            kb.op("dve", lambda e, sg=sg, ps=ps, tg=tg, sc=sc: e.tensor_scalar(out=sg[:, tg * 512:(tg + 1) * 512], in0=ps[:], scalar1=sc, scalar2=None, op0=ALU.mult),
                                  reads=[ps_b], writes=[sg_b])
                kb.dma("sp", dst[j * 128:(j + 1) * 128, :], sg[:], reads=[sg_b])

        if "qf" in self.blocksA:
            feat_block(O_QF, "qf", self.qfT)
            feat_block(O_KF, "kf", self.kfT)
        if "g" in self.blocksA:
            for gb in range(6):
                feat_block(O_G + gb * 512, "g", self.gT[gb * 512:(gb + 1) * 512, :])
        if "f" in self.blocksA:
            kb.dma("pool", wb8[:], wv[:, :, O_F:O_F + 8], writes=[wb8_b])
            fs, fs_b = kb.sb("fstg", [8, S], F32)
            for tg in range(8):
                ps, ps_b = kb.psum()
                for kt in range(8):
                    kb.op("pe", lambda e, ps=ps, kt=kt, tg=tg: e.matmul(ps[0:8, :], wb8[:, kt, :], xT[:, kt, tg * 512:(tg + 1) * 512], start=(kt == 0), stop=(kt == 7)),
                          reads=[xT_b, wb8_b], writes=[ps_b])
                kb.op("dve", lambda e, ps=ps, tg=tg: e.tensor_copy(out=fs[:, tg * 512:(tg + 1) * 512], in_=ps[0:8, :]), reads=[ps_b], writes=[fs_b])
            kb.dma("sp", self.fl[:, :], fs[:], reads=[fs_b])
        kb.barrier()

    blocksA = ("u", "qd", "vd", "vf", "qf", "g", "f")

    def convert_weights(self, l):
        kb = self.kb
        li2 = l // 2

        def conv(dst, src):
            K_ = src.shape[0]
            a_n = K_ // 128
            dv = dst.rearrange("(a p) n -> p a n", p=128)
            sv = src.rearrange("(a p) n -> p a n", p=128)
            for a0 in range(0, a_n, 8):
                a1 = min(a_n, a0 + 8)
                kb.dma("pool", dv[:, a0:a1, :], sv[:, a0:a1, :], writes=[self.wcb])
        for n_ in range(3):
            conv(self.wc["w_branch"][n_], self.I("w_branch")[l][n_])
        for nm in ("w_mix_out", "w_xq", "w_xk", "w_xv", "w_xo"):
            conv(self.wc[nm], self.I(nm)[l])
        if l % 2 == 0:
            for nm in ("ffn_w_gate", "ffn_w_up", "ffn_w_down"):
                conv(self.wc[nm], self.I(nm)[li2])
        else:
            for nm in ("moe_w_gate", "moe_w_up", "moe_w_down"):
                for ex in range(NEXP):
                    conv(self.wc[nm][ex], self.I(nm)[li2][ex])

    def build(self):
        self.declare()
        self.setup()
        if "N" not in self.stages:
            self.stage0()
        if "Z" in self.stages:
            kb = self.kb
            kb.reset(self.arena0)
            zt, zt_b = kb.sb("zfill", [128, S], BF16)
            kb.op("pool", lambda e: e.memset(zt[:], 0.0), writes=[zt_b])
            for a in range(12):
                kb.dma("sp", self.ysT[a * 128:(a + 1) * 128, :], zt[:], reads=[zt_b])
            kb.barrier()
        for l in self.layers:
            if "C" in self.stages:
                self.convert_weights(l)
            if "A" in self.stages:
                self.stageA(l)
            if "1" in self.stages:
                self.stageB1(l)
            if "2" in self.stages:
                self.stageB2(l)
            if "3" in self.stages:
                self.stageB3(l)
            if "C" in self.stages:
                self.stageC(l, l == DEPTH - 1 or l == self.layers[-1])
        self.kb.barrier()
        return self.kb.finish(self.stack)


def build_program(layers=range(DEPTH), stages="A123C", debug=()):
    nc = bass.Bass("TRN2", target_bir_lowering=False)
    stack = contextlib.ExitStack()
    with stack:
        p = Prog(nc, stack, layers, stages, debug)
        n = p.build()
    return nc, p, n


INPUT_NAMES = ["x", "mem", "positions", "w_in", "b_forget", "ssm_lambda_re", "ssm_lambda_im", "ssm_log_dt",
               "ssm_b_re", "ssm_b_im", "ssm_c_re", "ssm_c_im", "ssm_d", "w_glu", "w_branch", "w_mix_out",
               "ln_mix_g", "ln_mix_b", "w_xq", "w_xk", "w_xv", "w_xo", "ln_x_g", "ln_x_b",
               "ffn_w_gate", "ffn_w_up", "ffn_w_down", "moe_w_router", "moe_b_router",
               "moe_w_gate", "moe_w_up", "moe_w_down", "ln_ffn_g", "ln_ffn_b"]


def make_in_maps(inputs, n=8, names=None):
    maps = []
    names = names or INPUT_NAMES
    shared = {k: np.ascontiguousarray(np.asarray(inputs[k])) for k in names if k not in ("x", "mem", "positions")}
    x = np.asarray(inputs["x"])
    mem = np.asarray(inputs["mem"])
    pos = np.asarray(inputs["positions"])
    for i in range(n):
        m = dict(shared)
        if "x" in names:
            m["x"] = np.ascontiguousarray(x[i])
        if "mem" in names:
            m["mem"] = np.ascontiguousarray(mem[i])
        if "positions" in names:
            m["positions"] = np.ascontiguousarray(pos[i].astype(np.int32).reshape(32, 128).T)
        maps.append(m)
    return maps


def kernel(**inputs):
    nc, p, n = build_program()
    in_maps = make_in_maps(inputs, names=list(p.inp.keys()))
    res = run_bass_kernel_spmd(nc, in_maps, core_ids=list(range(8)))
    return np.stack([np.asarray(r["out"]) for r in res.results], axis=0).astype(np.float32)


def _stageB3(self, l):
    kb = self.kb
    kb.reset(self.arena0)
    NEG = -30000.0
    mk, mk_b = kb.sb("fmask", [128, 4, 512], BF16)
    mkf, mkf_b = kb.sb("fmaskf", [128, 512], F32)
    for j in range(4):
        kb.op("pool", lambda e: e.memset(mkf[:], 0.0), writes=[mkf_b])
        kb.op("pool", lambda e, j=j: e.affine_select(out=mkf[:], in_=mkf[:], pattern=[[1, 512]], compare_op=ALU.is_ge,
                                                     fill=NEG, base=-j * 128, channel_multiplier=-1),
              reads=[mkf_b], writes=[mkf_b])
        kb.op("dve", lambda e, j=j: e.tensor_copy(out=mk[:, j, :], in_=mkf[:]), reads=[mkf_b], writes=[mk_b])
    ones, ones_b = kb.sb("fones", [128, 64], BF16)
    kb.op("pool", lambda e: e.memset(ones[:], 1.0), writes=[ones_b])
    flt, flt_b = kb.sb("flt", [8, S], F32)
    kb.dma("sp", flt[:], self.fl[:, :], writes=[flt_b])
    bf_, bf_b = kb.sb("bfg", [8, 1], F32)
    kb.dma("sp", bf_[:], self.I("b_forget")[l].rearrange("(h o) -> h o", o=1), writes=[bf_b])
    nb, nb_b = kb.sb("nbfg", [8, 1], F32)
    kb.op("dve", lambda e: e.tensor_scalar(out=nb[:], in0=bf_[:], scalar1=-1.0, scalar2=None, op0=ALU.mult), reads=[bf_b], writes=[nb_b])
    kb.op("act", lambda e: e.activation(out=flt[:], in_=flt[:], func=AF.Exp, scale=-1.0, bias=nb[:]), reads=[flt_b, nb_b], writes=[flt_b])
    kb.op("act", lambda e: e.activation(out=flt[:], in_=flt[:], func=AF.Ln, bias=1.0), reads=[flt_b], writes=[flt_b])
    onesf, onesf_b = kb.sb("onesf", [8, S], F32)
    kb.op("pool", lambda e: e.memset(onesf[:], 1.0), writes=[onesf_b])
    ncum, ncum_b = kb.sb("ncum", [8, S], F32)
    kb.op("dve", lambda e: e.tensor_tensor_scan(out=ncum[:], data0=onesf[:], data1=flt[:], initial=0.0, op0=ALU.mult, op1=ALU.add),
          reads=[onesf_b, flt_b], writes=[ncum_b])
    augk, augk_b = kb.sb("augk", [8, 6, S], BF16)
    augq, augq_b = kb.sb("augq", [8, 6, S], BF16)
    kb.op("pool", lambda e: e.memset(augk[:, 0:3, :], 1.0), writes=[augk_b])
    kb.op("pool", lambda e: e.memset(augq[:, 3:6, :], 1.0), writes=[augq_b])
    rem, rem_b = kb.sb("rem", [8, S], F32)
    kb.op("dve", lambda e: e.tensor_copy(out=augk[:, 3, :], in_=ncum[:]), reads=[ncum_b, augk_b], writes=[augk_b])
    kb.op("dve", lambda e: e.tensor_tensor(out=rem[:], in0=ncum[:], in1=augk[:, 3, :], op=ALU.subtract), reads=[ncum_b, augk_b], writes=[rem_b])
    kb.op("dve", lambda e: e.tensor_copy(out=augk[:, 4, :], in_=rem[:]), reads=[rem_b, augk_b], writes=[augk_b])
    kb.op("dve", lambda e: e.tensor_tensor(out=rem[:], in0=rem[:], in1=augk[:, 4, :], op=ALU.subtract), reads=[rem_b, augk_b], writes=[rem_b])
    kb.op("dve", lambda e: e.tensor_copy(out=augk[:, 5, :], in_=rem[:]), reads=[rem_b, augk_b], writes=[augk_b])
    kb.op("dve", lambda e: e.tensor_scalar(out=augq[:, 0:3, :], in0=augk[:, 3:6, :], scalar1=-1.0, scalar2=None, op0=ALU.mult),
          reads=[augk_b, augq_b], writes=[augq_b])
    aqd = self.scr["augq"]
    akd = self.scr["augk"]
    kb.dma("sp", aqd[:, :, :], augq[:], reads=[augq_b])
    kb.dma("sp", akd[:, :, :], augk[:], reads=[augk_b])
    kb.barrier()
    kb.reset(self.arena0 + 4 * 1024 + 2048 + 256)
    vall, vall_b = kb.sb("fvall", [128, 32, VA], BF16)
    for k0 in range(0, 32, 8):
        kb.dma("sp", vall[:, k0:k0 + 8, :], self.vf.rearrange("(kt p) c -> p kt c", p=128)[:, k0:k0 + 8, :], writes=[vall_b])
    qk = [(kb.sb("fq%d" % i, [70, S], BF16), kb.sb("fk%d" % i, [70, S], BF16)) for i in range(2)]
    pts = [kb.sb("fpt%d" % i, [128, 512], BF16) for i in range(4)]
    osb = [kb.sb("fosb%d" % i, [65, 512], F32) for i in range(2)]
    rds = [kb.sb("frd%d" % i, [65, 512], F32) for i in range(2)]
    rhl = [kb.sb("frhl%d" % i, [65, 2, 512], BF16) for i in range(2)]
    yh = [kb.sb("fyh%d" % i, [64, S], BF16) for i in range(2)]
    self._it = 0
    for h in range(8):
        (qh, qh_b), (kh, kh_b) = qk[h % 2]
        kb.dma("sp", qh[0:64, :], self.qfT[h * 64:(h + 1) * 64, :], writes=[qh_b])
        kb.dma("sp", qh[64:70, :], aqd[h], writes=[qh_b])
        kb.dma("sp", kh[0:64, :], self.kfT[h * 64:(h + 1) * 64, :], writes=[kh_b])
        kb.dma("sp", kh[64:70, :], akd[h], writes=[kh_b])
        yo, yo_b = yh[h % 2]
        for g in range(8):
            po, po_b = kb.psum_acc()
            nkb = 4 * g + 4
            pend = {}

            def emit_qk(kbi, g=g, qh=qh, kh=kh, qh_b=qh_b, kh_b=kh_b):
                ps, ps_b = kb.psum()
                diag = kbi >= 4 * g
                kb.op("pe", lambda e, ps=ps, kh=kh, qh=qh, kbi=kbi, g=g, diag=diag: e.matmul(ps[:], kh[0:70, kbi * 128:(kbi + 1) * 128], qh[0:70, g * 512:(g + 1) * 512],
                                                                                         start=True, stop=not diag),
                      reads=[kh_b, qh_b], writes=[ps_b])
                if diag:
                    j = kbi - 4 * g
                    kb.op("pe", lambda e, ps=ps, j=j: e.matmul(ps[:], self.identb[:], mk[:, j, :], start=False, stop=True),
                          reads=[self.identb_b, mk_b], writes=[ps_b])
                pt, pt_b = pts[self._it % 4]
                self._it += 1
                kb.op("act", lambda e, pt=pt, ps=ps: e.activation(out=pt[:], in_=ps[:], func=AF.Exp), reads=[ps_b], writes=[pt_b])
                pend[kbi] = (pt, pt_b)

            def emit_pv(kbi, po=po, po_b=po_b, h=h, nkb=nkb):
                pt, pt_b = pend.pop(kbi)
                kb.op("pe", lambda e, po=po, pt=pt, kbi=kbi, h=h, nkb=nkb: e.matmul(po[0:65, :], vall[:, kbi, h * 65:(h + 1) * 65], pt[:],
                                                                                    start=(kbi == 0), stop=(kbi == nkb - 1)),
                      reads=[vall_b, pt_b], writes=[po_b])
            LA = 2
            for kbi in range(min(LA, nkb)):
                emit_qk(kbi)
            for kbi in range(nkb):
                if kbi + LA < nkb:
                    emit_qk(kbi + LA)
                emit_pv(kbi)
            self._attn_finalize(po, po_b, osb[g % 2], rds[g % 2], rhl[g % 2], ones, ones_b, yo, yo_b, g)
        kb.dma("sp", self.ysT[2 * BW + h * 64:2 * BW + (h + 1) * 64, :], yo[:], reads=[yo_b])
    kb.barrier()


def _attn_finalize(self, po, po_b, osb_, rds_, rhl_, ones, ones_b, yo, yo_b, g):
    kb = self.kb
    (ob, ob_b), (rd, rd_b), (rh, rh_b) = osb_, rds_, rhl_
    kb.op("act", lambda e: e.activation(out=ob[:], in_=po[0:65, :], func=AF.Copy), reads=[po_b], writes=[ob_b])
    kb.op("dve", lambda e: e.reciprocal(out=rd[64:65, :], in_=ob[64:65, :]), reads=[ob_b], writes=[rd_b])
    kb.op("dve", lambda e: e.tensor_copy(out=rh[64:65, 0, :], in_=rd[64:65, :]), reads=[rd_b], writes=[rh_b])
    kb.op("dve", lambda e: e.tensor_tensor(out=rd[64:65, :], in0=rd[64:65, :], in1=rh[64:65, 0, :], op=ALU.subtract), reads=[rd_b, rh_b], writes=[rd_b])
    kb.op("dve", lambda e: e.tensor_copy(out=rh[64:65, 1, :], in_=rd[64:65, :]), reads=[rd_b, rh_b], writes=[rh_b])
    pb, pb_b = kb.psum()
    kb.op("pe", lambda e: e.matmul(pb[0:64, :], ones[64:65, 0:64], rh[64:65, 0, :], start=True, stop=False), reads=[ones_b, rh_b], writes=[pb_b])
    kb.op("pe", lambda e: e.matmul(pb[0:64, :], ones[64:65, 0:64], rh[64:65, 1, :], start=False, stop=True), reads=[ones_b, rh_b], writes=[pb_b])
    kb.op("dve", lambda e: e.tensor_tensor(out=yo[0:64, g * 512:(g + 1) * 512], in0=ob[0:64, :], in1=pb[0:64, :], op=ALU.mult),
          reads=[ob_b, pb_b], writes=[yo_b])


Prog.stageB3 = _stageB3
Prog._attn_finalize = _attn_finalize


def _stageB2(self, l):
    kb = self.kb
    kb.reset(self.arena0)
    NEG = -30000.0
    mk, mk_b = kb.sb("dmask", [128, 256], BF16)
    mkf, mkf_b = kb.sb("dmaskf", [128, 256], F32)
    kb.op("pool", lambda e: e.memset(mkf[:], 0.0), writes=[mkf_b])
    kb.op("pool", lambda e: e.affine_select(out=mkf[:, 0:128], in_=mkf[:, 0:128], pattern=[[1, 128]], compare_op=ALU.is_ge,
                                            fill=NEG, base=0, channel_multiplier=-1), reads=[mkf_b], writes=[mkf_b])
    kb.op("pool", lambda e: e.affine_select(out=mkf[:, 128:256], in_=mkf[:, 128:256], pattern=[[-1, 128]], compare_op=ALU.is_ge,
                                            fill=NEG, base=0, channel_multiplier=1), reads=[mkf_b], writes=[mkf_b])
    kb.op("dve", lambda e: e.tensor_copy(out=mk[:], in_=mkf[:]), reads=[mkf_b], writes=[mk_b])
    ones, ones_b = kb.sb("dones", [128, 64], BF16)
    kb.op("pool", lambda e: e.memset(ones[:], 1.0), writes=[ones_b])
    DILS = (1, 4, 16)
    vall = {}
    for d in DILS:
        t, t_b = kb.sb("dv%d" % d, [128, 32, VA], BF16)
        nm = 32 // d
        if d == 1:
            for k0 in range(0, 32, 8):
                kb.dma("sp", t[:, k0:k0 + 8, :], self.vd.rearrange("(m p) c -> p m c", p=128)[:, k0:k0 + 8, :], writes=[t_b])
        else:
            src = self.vd.rearrange("(m p r) c -> p r m c", p=128, r=d)
            for r in range(d):
                kb.dma("sp", t[:, r * nm:(r + 1) * nm, :], src[:, r, :, :], writes=[t_b])
        vall[d] = (t, t_b)
    qk = [(kb.sb("dq%d" % i, [128, S], BF16), kb.sb("dk%d" % i, [128, S], BF16)) for i in range(2)]
    pts = [kb.sb("dpt%d" % i, [128, 256], BF16) for i in range(4)]
    acc = [kb.sb("dacc%d" % i, [65, S], F32) for i in range(2)]
    osb = [kb.sb("dosb%d" % i, [65, 512], F32) for i in range(2)]
    rds = [kb.sb("drd%d" % i, [65, 512], F32) for i in range(2)]
    rhl = [kb.sb("drhl%d" % i, [65, 2, 512], BF16) for i in range(2)]
    yh = [kb.sb("dyh%d" % i, [64, S], BF16) for i in range(2)]
    self._it = 0
    for hp in range(4):
        (qt, qt_b), (kt_, kt_b) = qk[hp % 2]
        kb.dma("sp", qt[:], self.qdT[hp * 128:(hp + 1) * 128, :], writes=[qt_b])
        kb.dma("sp", kt_[:], self.kdT[hp * 128:(hp + 1) * 128, :], writes=[kt_b])
        for hh in range(2):
            h = hp * 2 + hh
            pb0 = hh * 64
            ac, ac_b = acc[h % 2]
            items = []
            for d in DILS:
                nm = 32 // d
                for r in range(d):
                    for m in range(nm):
                        items.append((d, r, m, nm))
            pend = {}
            pos = {}

            def emit_qk(i, pb0=pb0, kt_=kt_, qt=qt, kt_b=kt_b, qt_b=qt_b):
                d, r, m, nm = items[i]
                nq = 256 if m < nm - 1 else 128
                t0 = m * 128 * d + r
                ksl = slice(t0, t0 + 127 * d + 1, d)
                qsl = slice(t0, t0 + (nq - 1) * d + 1, d)
                ps, ps_b = kb.psum()
                kb.op("pe", lambda e, ps=ps, ksl=ksl, qsl=qsl, nq=nq, pb0=pb0, kt_=kt_, qt=qt: e.matmul(ps[:, 0:nq], kt_[pb0:pb0 + 64, ksl], qt[pb0:pb0 + 64, qsl], start=True, stop=False),
                      reads=[kt_b, qt_b], writes=[ps_b])
                kb.op("pe", lambda e, ps=ps, nq=nq: e.matmul(ps[:, 0:nq], self.identb[:], mk[:, 0:nq], start=False, stop=True),
                      reads=[self.identb_b, mk_b], writes=[ps_b])
                pt, pt_b = pts[self._it % 4]
                self._it += 1
                kb.op("act", lambda e, pt=pt, ps=ps, nq=nq: e.activation(out=pt[:, 0:nq], in_=ps[:, 0:nq], func=AF.Exp), reads=[ps_b], writes=[pt_b])
                pend[i] = (pt, pt_b, nq, t0)

            def emit_pv(i, h=h, ac=ac, ac_b=ac_b):
                d, r, m, nm = items[i]
                vt, vt_b = vall[d]
                b = r * nm + m
                pt, pt_b, nq, t0 = pend.pop(i)
                if m == 0:
                    pos[(d, r, 0)] = kb.psum_acc()
                po, po_b = pos.pop((d, r, m))
                kb.op("pe", lambda e, po=po, vt=vt, b=b, h=h, pt=pt, m=m: e.matmul(po[0:65, 0:128], vt[:, b, h * 65:(h + 1) * 65], pt[:, 0:128], start=(m == 0), stop=True),
                      reads=[vt_b, pt_b], writes=[po_b])
                if nq == 256:
                    pos[(d, r, m + 1)] = kb.psum_acc()
                    po2, po2_b = pos[(d, r, m + 1)]
                    kb.op("pe", lambda e, po2=po2, vt=vt, b=b, h=h, pt=pt: e.matmul(po2[0:65, 0:128], vt[:, b, h * 65:(h + 1) * 65], pt[:, 128:256], start=True, stop=False),
                          reads=[vt_b, pt_b], writes=[po2_b])
                if d == 1:
                    kb.op("dve", lambda e, ac=ac, po=po, qs=slice(t0, t0 + 128): e.tensor_copy(out=ac[:, qs], in_=po[0:65, 0:128]),
                          reads=[po_b], writes=[ac_b])
                else:
                    qs = slice(t0, t0 + 127 * d + 1, d)
                    kb.op("dve", lambda e, ac=ac, po=po, qs=qs: e.tensor_tensor(out=ac[:, qs], in0=ac[:, qs], in1=po[0:65, 0:128], op=ALU.add),
                          reads=[po_b, ac_b], writes=[ac_b])
            LA = 2
            for i in range(min(LA, len(items))):
                emit_qk(i)
            for i in range(len(items)):
                if i + LA < len(items):
                    emit_qk(i + LA)
                emit_pv(i)
            yo, yo_b = yh[h % 2]
            for g in range(8):
                self._attn_finalize2(ac, ac_b, rds[g % 2], rhl[g % 2], ones, ones_b, yo, yo_b, g)
            kb.dma("sp", self.ysT[BW + h * 64:BW + (h + 1) * 64, :], yo[:], reads=[yo_b])
    kb.barrier()


def _attn_finalize2(self, ac, ac_b, rds_, rhl_, ones, ones_b, yo, yo_b, g):
    kb = self.kb
    (rd, rd_b), (rh, rh_b) = rds_, rhl_
    sl = slice(g * 512, (g + 1) * 512)
    kb.op("dve", lambda e: e.reciprocal(out=rd[64:65, :], in_=ac[64:65, sl]), reads=[ac_b], writes=[rd_b])
    kb.op("dve", lambda e: e.tensor_copy(out=rh[64:65, 0, :], in_=rd[64:65, :]), reads=[rd_b], writes=[rh_b])
    kb.op("dve", lambda e: e.tensor_tensor(out=rd[64:65, :], in0=rd[64:65, :], in1=rh[64:65, 0, :], op=ALU.subtract), reads=[rd_b, rh_b], writes=[rd_b])
    kb.op("dve", lambda e: e.tensor_copy(out=rh[64:65, 1, :], in_=rd[64:65, :]), reads=[rd_b, rh_b], writes=[rh_b])
    pb, pb_b = kb.psum()
    kb.op("pe", lambda e: e.matmul(pb[0:64, :], ones[64:65, 0:64], rh[64:65, 0, :], start=True, stop=False), reads=[ones_b, rh_b], writes=[pb_b])
    kb.op("pe", lambda e: e.matmul(pb[0:64, :], ones[64:65, 0:64], rh[64:65, 1, :], start=False, stop=True), reads=[ones_b, rh_b], writes=[pb_b])
    kb.op("dve", lambda e: e.tensor_tensor(out=yo[0:64, sl], in0=ac[0:64, sl], in1=pb[0:64, :], op=ALU.mult),
          reads=[ac_b, pb_b], writes=[yo_b])


Prog.stageB2 = _stageB2
Prog._attn_finalize2 = _attn_finalize2


def _stageB1(self, l):
    kb = self.kb
    kb.reset(self.arena0)
    PB = Buf("ssm_prep")
    idf, idf_b = self.identf, self.identf_b
    BSt, _ = kb.sb("BSt", [128, 2, 16, 2, 128], BF16)
    Gt, _ = kb.sb("Gt", [128, 2, 16, 2, 128], BF16)
    Tt, _ = kb.sb("Tt", [128, 32, 128], BF16)
    Dr, _ = kb.sb("Dr", [128, 16, 9], F32)
    Di, _ = kb.sb("Di", [128, 16, 9], F32)
    nDi, _ = kb.sb("nDi", [128, 16, 9], F32)
    m_keep = kb.mark()

    def T_(name, shape, dt_=F32):
        t, _ = kb.sb(name, shape, dt_)
        return t

    def dve(fn, extra_r=(), extra_w=()):
        kb.op("dve", fn, reads=[PB] + list(extra_r), writes=[PB] + list(extra_w))

    def tt(out, a, b, op):
        dve(lambda e: e.tensor_tensor(out=out, in0=a, in1=b, op=op))

    def ts(out, a, s1, op0, s2=None, op1=None):
        if op1 is None:
            dve(lambda e: e.tensor_scalar(out=out, in0=a, scalar1=s1, scalar2=None, op0=op0))
        else:
            dve(lambda e: e.tensor_scalar(out=out, in0=a, scalar1=s1, scalar2=s2, op0=op0, op1=op1))

    def cp(out, a):
        dve(lambda e: e.tensor_copy(out=out, in_=a))

    pp = T_("pp", [16, 3, 128])
    ld = T_("ld", [16, 2])
    kb.dma("sp", pp[:, 0, :], self.I("ssm_lambda_re")[l].rearrange("(q a) p -> q (a p)", a=2), writes=[PB])
    kb.dma("sp", pp[:, 1, :], self.I("ssm_lambda_im")[l].rearrange("(q a) p -> q (a p)", a=2), writes=[PB])
    kb.dma("sp", ld[:], self.I("ssm_log_dt")[l].rearrange("(q a) -> q a", a=2), writes=[PB])
    cp(pp[:, 2, :].rearrange("q (a p) -> q a p", a=2), ld[:].unsqueeze(2).to_broadcast([16, 2, 64]))
    par = T_("par", [128, 3, 16])
    ps, ps_b = kb.psum()
    for i in range(3):
        kb.op("pe", lambda e, i=i, ps=ps: e.transpose(ps[:, i * 16:(i + 1) * 16], pp[:, i, :], idf[0:16, 0:16]), reads=[PB, idf_b], writes=[ps_b])
    dve(lambda e, ps=ps: e.tensor_copy(out=par[:].rearrange("p a q -> p (a q)"), in_=ps[:, 0:48]), extra_r=[ps_b])
    lr, li, ldt = par[:, 0, :], par[:, 1, :], par[:, 2, :]
    Bre = T_("Bre", [128, 16, 16])
    Bim = T_("Bim", [128, 16, 16])
    for (dst, nm) in ((Bre, "ssm_b_re"), (Bim, "ssm_b_im")):
        src = self.I(nm)[l].rearrange("(q a) p c -> (a p) q c", a=2)
        for q0 in range(0, 16, 4):
            kb.dma("sp", dst[:, q0:q0 + 4, :], src[:, q0:q0 + 4, :], writes=[PB])
    Cre = T_("Cre", [128, 16, 16])
    Cim = T_("Cim", [128, 16, 16])
    Z = T_("Z", [32, 16, 128])
    zt = T_("zt", [128, 256])
    for (dst, nm) in ((Cre, "ssm_c_re"), (Cim, "ssm_c_im")):
        dve(lambda e: e.memset(Z[:], 0.0))
        src = self.I(nm)[l].rearrange("(q a) c p -> a c q p", a=2)
        kb.dma("sp", Z[0:16, :, 0:64], src[0], reads=[PB], writes=[PB])
        kb.dma("sp", Z[16:32, :, 64:128], src[1], reads=[PB], writes=[PB])
        for q in range(16):
            if q % 8 == 0:
                ps, ps_b = kb.psum()
            kb.op("pe", lambda e, ps=ps, q=q: e.transpose(ps[:, (q % 8) * 32:(q % 8) * 32 + 32], Z[:, q, :], idf[0:32, 0:32]), reads=[PB, idf_b], writes=[ps_b])
            if q % 8 == 7:
                q0 = q - 7
                dve(lambda e, ps=ps: e.tensor_copy(out=zt[:], in_=ps[:, 0:256]), extra_r=[ps_b])
                pv = zt[:].rearrange("p (q a c) -> p q a c", q=8, a=2)
                dve(lambda e, dst=dst, pv=pv, q0=q0: e.tensor_tensor(out=dst[:, q0:q0 + 8, :], in0=pv[:, :, 0, :], in1=pv[:, :, 1, :], op=ALU.add))
    def S_(name):
        return T_(name, [128, 16])
    dt = S_("dt")
    kb.op("act", lambda e: e.activation(out=dt[:], in_=ldt, func=AF.Exp), reads=[PB], writes=[PB])
    x = S_("x")
    tt(x[:], lr, dt[:], ALU.mult)
    er = S_("er")
    ts(er[:], x[:], 1.0 / 7, ALU.mult, 1.0, ALU.add)
    for k in (6, 5, 4, 3, 2, 1):
        tt(er[:], er[:], x[:], ALU.mult)
        ts(er[:], er[:], 1.0 / k, ALU.mult, 1.0, ALU.add)
    phi = S_("phi")
    tt(phi[:], li, dt[:], ALU.mult)
    ts(phi[:], phi[:], 1.0 / 32, ALU.mult)
    z = S_("z")
    tt(z[:], phi[:], phi[:], ALU.mult)
    cr = S_("cr")
    ci_ = S_("ci")
    cc = [1.0, -1.0 / 2, 1.0 / 24, -1.0 / 720, 1.0 / 40320, -1.0 / 3628800, 1.0 / 479001600]
    sc = [1.0, -1.0 / 6, 1.0 / 120, -1.0 / 5040, 1.0 / 362880, -1.0 / 39916800, 1.0 / 6227020800]
    for (dst, co) in ((cr, cc), (ci_, sc)):
        ts(dst[:], z[:], co[6], ALU.mult, co[5], ALU.add)
        for k in (4, 3, 2, 1, 0):
            tt(dst[:], dst[:], z[:], ALU.mult)
            ts(dst[:], dst[:], co[k], ALU.add)
    tt(ci_[:], ci_[:], phi[:], ALU.mult)
    t1, t2, t3, t4 = S_("t1"), S_("t2"), S_("t3"), S_("t4")

    def cmul(or_, oi, ar, ai, br, bi, a1=t1, a2=t2, a3=t3, a4=t4):
        tt(a1, ar, br, ALU.mult)
        tt(a2, ai, bi, ALU.mult)
        tt(a3, ar, bi, ALU.mult)
        tt(a4, ai, br, ALU.mult)
        tt(or_, a1, a2, ALU.subtract)
        tt(oi, a3, a4, ALU.add)

    for _ in range(5):
        cmul(cr[:], ci_[:], cr[:], ci_[:], cr[:], ci_[:], t1[:], t2[:], t3[:], t4[:])
        tt(t1[:], cr[:], cr[:], ALU.mult)
        tt(t2[:], ci_[:], ci_[:], ALU.mult)
        tt(t1[:], t1[:], t2[:], ALU.add)
        ts(t1[:], t1[:], -0.5, ALU.mult, 1.5, ALU.add)
        tt(cr[:], cr[:], t1[:], ALU.mult)
        tt(ci_[:], ci_[:], t1[:], ALU.mult)
    Pl_r = T_("Plr", [128, 9, 16])
    Pl_i = T_("Pli", [128, 9, 16])
    dve(lambda e: e.memset(Pl_r[:, 0, :], 1.0))
    dve(lambda e: e.memset(Pl_i[:, 0, :], 0.0))
    tt(Pl_r[:, 1, :], cr[:], er[:], ALU.mult)
    tt(Pl_i[:, 1, :], ci_[:], er[:], ALU.mult)
    ar, ai = Pl_r[:, 1, :], Pl_i[:, 1, :]
    for k in range(2, 9):
        cmul(Pl_r[:, k, :], Pl_i[:, k, :], Pl_r[:, k - 1, :], Pl_i[:, k - 1, :], ar, ai, t1[:], t2[:], t3[:], t4[:])
    Nl_r = T_("Nlr", [128, 8, 16])
    Nl_i = T_("Nli", [128, 8, 16])
    dve(lambda e: e.memset(Nl_r[:, 0, :], 1.0))
    dve(lambda e: e.memset(Nl_i[:, 0, :], 0.0))
    tt(t1[:], ar, ar, ALU.mult)
    tt(t2[:], ai, ai, ALU.mult)
    tt(t1[:], t1[:], t2[:], ALU.add)
    dve(lambda e: e.reciprocal(out=t1[:], in_=t1[:]))
    tt(Nl_r[:, 1, :], ar, t1[:], ALU.mult)
    tt(Nl_i[:, 1, :], ai, t1[:], ALU.mult)
    ts(Nl_i[:, 1, :], Nl_i[:, 1, :], -1.0, ALU.mult)
    for k in range(2, 8):
        cmul(Nl_r[:, k, :], Nl_i[:, k, :], Nl_r[:, k - 1, :], Nl_i[:, k - 1, :], Nl_r[:, 1, :], Nl_i[:, 1, :], t1[:], t2[:], t3[:], t4[:])
    cp(Dr[:, :, 0], Pl_r[:, 8, :])
    cp(Di[:, :, 0], Pl_i[:, 8, :])
    for k in range(1, 9):
        cmul(Dr[:, :, k], Di[:, :, k], Dr[:, :, k - 1], Di[:, :, k - 1], Dr[:, :, k - 1], Di[:, :, k - 1], t1[:], t2[:], t3[:], t4[:])
    ts(nDi[:], Di[:], -1.0, ALU.mult)
    qr, qi = S_("qr"), S_("qi")
    nr = S_("nr")
    ts(nr[:], ar, -1.0, ALU.add)
    tt(t1[:], lr, lr, ALU.mult)
    tt(t2[:], li, li, ALU.mult)
    tt(t1[:], t1[:], t2[:], ALU.add)
    dve(lambda e: e.reciprocal(out=t1[:], in_=t1[:]))
    tt(t2[:], nr[:], lr, ALU.mult)
    tt(t3[:], ai, li, ALU.mult)
    tt(t2[:], t2[:], t3[:], ALU.add)
    tt(qr[:], t2[:], t1[:], ALU.mult)
    tt(t2[:], ai, lr, ALU.mult)
    tt(t3[:], nr[:], li, ALU.mult)
    tt(t2[:], t2[:], t3[:], ALU.subtract)
    tt(qi[:], t2[:], t1[:], ALU.mult)
    Br2, Bi2 = T_("Br2", [128, 16, 16]), T_("Bi2", [128, 16, 16])
    u1, u2, u3, u4 = (T_("u%d" % i, [128, 16, 16]) for i in range(4))

    def bq(t):
        return t.unsqueeze(2).to_broadcast([128, 16, 16])
    cmul(Br2[:], Bi2[:], Bre[:], Bim[:], bq(qr[:]), bq(qi[:]), u1[:], u2[:], u3[:], u4[:])
    def Bg(name):
        return T_(name, [128, 16, 8, 16])
    g1, g2, g3, g4 = Bg("g1"), Bg("g2"), Bg("g3"), Bg("g4")
    Wm_r, Wm_i = Bg("Wmr"), Bg("Wmi")

    def over_k(t):
        return t.unsqueeze(2).to_broadcast([128, 16, 8, 16])

    def over_c(t):
        return t.rearrange("p k q -> p q k").unsqueeze(3).to_broadcast([128, 16, 8, 16])

    def over_kc(t):
        return t.unsqueeze(2).unsqueeze(3).to_broadcast([128, 16, 8, 16])

    cmul(Wm_r[:], Wm_i[:], over_k(Br2[:]), over_k(Bi2[:]), over_c(Nl_r[:, 0:8, :]), over_c(Nl_i[:, 0:8, :]), g1[:], g2[:], g3[:], g4[:])
    WmM = T_("WmM", [128, 2, 2, 16, 128], BF16)
    dve(lambda e: e.memset(WmM[:].rearrange("p a b q n -> p (a b q n)"), 0.0))
    for a in range(2):
        for part, src in ((0, Wm_r), (1, Wm_i)):
            cp(WmM[a * 64:(a + 1) * 64, a, part, :, :], src[a * 64:(a + 1) * 64].rearrange("p q k c -> p q (k c)"))
    W7_r, W7_i = Bg("W7r"), Bg("W7i")
    cmul(W7_r[:], W7_i[:], Wm_r[:], Wm_i[:], over_kc(Pl_r[:, 7, :]), over_kc(Pl_i[:, 7, :]), g1[:], g2[:], g3[:], g4[:])
    BSt_b = Buf("BSt")
    kb.op("pool", lambda e: e.memset(BSt[:].rearrange("p a q b n -> p (a q b n)"), 0.0), writes=[BSt_b])
    for part, src in ((0, W7_r), (1, W7_i)):
        for q in range(16):
            if q % 4 == 0:
                ps, ps_b = kb.psum()
            kb.op("pe", lambda e, ps=ps, q=q, src=src: e.transpose(ps[:, (q % 4) * 128:(q % 4) * 128 + 128], src[:, q].rearrange("p k c -> p (k c)"), idf[:]),
                  reads=[PB, idf_b], writes=[ps_b])
            if q % 4 == 3:
                for a in range(2):
                    pv = ps[:].rearrange("p (q n) -> p q n", q=4)[:, :, a * 64:(a + 1) * 64]
                    kb.op("act", lambda e, pv=pv, part=part, q=q, a=a: e.activation(out=BSt[:, part, q - 3:q + 1, a, a * 64:(a + 1) * 64], in_=pv, func=AF.Copy),
                          reads=[ps_b, BSt_b], writes=[BSt_b])
    Cp_r, Cp_i = Bg("Cpr"), Bg("Cpi")
    cmul(Cp_r[:], Cp_i[:], over_k(Cre[:]), over_k(Cim[:]), over_c(Pl_r[:, 0:8, :]), over_c(Pl_i[:, 0:8, :]), g1[:], g2[:], g3[:], g4[:])
    CpB = T_("CpB", [128, 2, 16, 128], BF16)
    cp(CpB[:, 0], Cp_r[:].rearrange("p q k c -> p q (k c)"))
    ts(CpB[:, 1], Cp_i[:].rearrange("p q k c -> p q (k c)"), -1.0, ALU.mult)
    G_r, G_i = W7_r, W7_i
    kb.op("dve", lambda e: e.memset(t1[:], 0.0), reads=[PB, BSt_b], writes=[PB])
    cmul(G_r[:], G_i[:], Cp_r[:], Cp_i[:], over_kc(ar), over_kc(ai), g1[:], g2[:], g3[:], g4[:])
    dve(lambda e: e.memset(Gt[:].rearrange("p a q b n -> p (a q b n)"), 0.0))
    for a in range(2):
        cp(Gt[a * 64:(a + 1) * 64, 0, :, a, :], G_r[a * 64:(a + 1) * 64].rearrange("p q k c -> p q (k c)"))
        ts(Gt[a * 64:(a + 1) * 64, 1, :, a, :], G_i[a * 64:(a + 1) * 64].rearrange("p q k c -> p q (k c)"), -1.0, ALU.mult)
    kidx_i = T_("kidx_i", [128, 1], I32)
    kidx = T_("kidx", [128, 1])
    jidx_i = T_("jidx_i", [128, 8, 16], I32)
    jidx = T_("jidx", [128, 128])
    cmask = T_("cmask", [128, 128])
    kb.op("pool", lambda e: e.iota(kidx_i[:], pattern=[[0, 1]], base=0, channel_multiplier=1), reads=[PB], writes=[PB])
    kb.op("pool", lambda e: e.iota(jidx_i[:], pattern=[[1, 8], [0, 16]], base=0, channel_multiplier=0), reads=[PB], writes=[PB])
    dve(lambda e: e.tensor_single_scalar(out=kidx_i[:], in_=kidx_i[:], scalar=4, op=ALU.arith_shift_right))
    cp(kidx[:], kidx_i[:])
    cp(jidx[:], jidx_i[:].rearrange("p a b -> p (a b)"))
    ts(cmask[:], jidx[:], kidx[:, 0:1], ALU.is_ge)
    dB = T_("dB", [128, 512])
    kb.dma("sp", dB[:], self.I("ssm_d")[l].partition_broadcast(128), writes=[PB])
    IDd = T_("IDd", [128, 32, 128], BF16)
    for g in range(32):
        dve(lambda e, g=g: e.tensor_tensor(out=IDd[:, g, :].rearrange("p (j c) -> p j c", j=8), in0=idf[:].rearrange("p (j c) -> p j c", j=8),
                                           in1=dB[:, g * 16:(g + 1) * 16].unsqueeze(1).to_broadcast([128, 8, 16]), op=ALU.mult), extra_r=[idf_b])
    Tt_b = Buf("Tt")
    for g in range(32):
        q, a = g // 2, g % 2
        if g % 4 == 0:
            ps, ps_b = kb.psum()
        sl = slice((g % 4) * 128, (g % 4) * 128 + 128)
        kb.op("pe", lambda e, ps=ps, sl=sl, q=q, a=a: e.matmul(ps[:, sl], WmM[:, a, 0, q, :], CpB[:, 0, q, :], start=True, stop=False), reads=[PB], writes=[ps_b])
        kb.op("pe", lambda e, ps=ps, sl=sl, q=q, a=a: e.matmul(ps[:, sl], WmM[:, a, 1, q, :], CpB[:, 1, q, :], start=False, stop=False), reads=[PB], writes=[ps_b])
        kb.op("pe", lambda e, ps=ps, sl=sl, g=g: e.matmul(ps[:, sl], self.identb[:], IDd[:, g, :], start=False, stop=True), reads=[PB, self.identb_b], writes=[ps_b])
        if g % 4 == 3:
            kb.op("dve", lambda e, ps=ps, g=g: e.tensor_tensor(out=Tt[:, g - 3:g + 1, :], in0=ps[:].rearrange("p (g n) -> p g n", g=4),
                                                               in1=cmask[:].unsqueeze(1).to_broadcast([128, 4, 128]), op=ALU.mult),
                  reads=[ps_b, PB], writes=[Tt_b])
    kb.barrier()
    kb.reset(m_keep)
    self._ssm_main(l, dict(BSt=BSt, Gt=Gt, Tt=Tt, Dr=Dr, Di=Di, nDi=nDi), m_keep)


def _ssm_main(self, l, tb, m0):
    kb = self.kb
    BSt, Gt, Tt, Dr, Di, nDi = tb["BSt"], tb["Gt"], tb["Tt"], tb["Dr"], tb["Di"], tb["nDi"]
    TB = Buf("ssm_tables")
    U, U_b = kb.sb("U", [128, 32, 512], BF16)
    m_x = kb.mark()
    xts = [kb.sb("Xc%d" % i, [128, 8, 512], BF16) for i in range(2)]
    x2s = [kb.sb("X2c%d" % i, [128, 32, 128], BF16) for i in range(2)]
    kb.reset(m_x)
    ygT, ygT_b = kb.sb("ygT", [128, 4, S], BF16)
    for ct in range(4):
        xt0, xt0_b = xts[ct % 2]
        kb.dma("sp", xt0[:].rearrange("p k c -> p (k c)"), self.u_tm[ct * 1024:(ct + 1) * 1024, :].rearrange("(p k) c -> p (k c)", k=8), writes=[xt0_b])
        xt, xt_b = x2s[ct % 2]
        kb.op("pool", lambda e, xt=xt, xt0=xt0: e.tensor_copy(out=xt[:].rearrange("p g (k c) -> p g k c", k=8),
                                                              in_=xt0[:].rearrange("p k (g c) -> p g k c", g=32)),
              reads=[xt0_b], writes=[xt_b])
        for g in range(32):
            if g % 4 == 0:
                ps, ps_b = kb.psum()
            kb.op("pe", lambda e, ps=ps, g=g, xt=xt: e.matmul(ps[:, (g % 4) * 128:(g % 4) * 128 + 128], xt[:, g, :], self.identb[:], start=True, stop=True),
                  reads=[xt_b, self.identb_b], writes=[ps_b])
            if g % 4 == 3:
                eng = "act" if (g // 4) % 2 == 0 else "dve"
                pv = ps[:].rearrange("p (g n) -> p g n", g=4)
                if eng == "act":
                    kb.op("act", lambda e, pv=pv, g=g, ct=ct: e.activation(out=U[:, g - 3:g + 1, ct * 128:(ct + 1) * 128], in_=pv, func=AF.Copy), reads=[ps_b], writes=[U_b])
                else:
                    kb.op("dve", lambda e, pv=pv, g=g, ct=ct: e.tensor_copy(out=U[:, g - 3:g + 1, ct * 128:(ct + 1) * 128], in_=pv), reads=[ps_b], writes=[U_b])
    Yg, Yg_b = kb.sb("Yg", [128, 4, 8, 512], BF16)
    SA = [kb.sb("SA%d" % i, [128, 2, 512], F32) for i in range(2)]
    SB_ = [kb.sb("SB%d" % i, [128, 2, 512], F32) for i in range(2)]
    Ssh = [kb.sb("Ssh%d" % i, [128, 2, 512], BF16) for i in range(2)]
    for q in range(16):
        (sa, sa_b), (sb2, sb2_b), (ssh, ssh_b) = SA[q % 2], SB_[q % 2], Ssh[q % 2]
        for part in range(2):
            ps, ps_b = kb.psum()
            for a in range(2):
                kb.op("pe", lambda e, ps=ps, part=part, a=a, q=q: e.matmul(ps[:], BSt[:, part, q, a, :], U[:, 2 * q + a, :], start=(a == 0), stop=(a == 1)),
                      reads=[U_b, TB], writes=[ps_b])
            kb.op("act", lambda e, ps=ps, part=part, sa=sa: e.activation(out=sa[:, part, :], in_=ps[:], func=AF.Copy), reads=[ps_b], writes=[sa_b])
        cur, cur_b, nxt, nxt_b = sa, sa_b, sb2, sb2_b
        for k in range(9):
            m = 1 << k
            dr, di, ndi = Dr[:, q, k:k + 1], Di[:, q, k:k + 1], nDi[:, q, k:k + 1]
            kb.op("pool", lambda e, cur=cur, nxt=nxt, m=m: e.tensor_copy(out=nxt[:, :, 0:m], in_=cur[:, :, 0:m]), reads=[cur_b], writes=[nxt_b])
            kb.op("dve", lambda e, cur=cur, nxt=nxt, m=m, dr=dr: e.scalar_tensor_tensor(out=nxt[:, 0, m:512], in0=cur[:, 0, 0:512 - m], scalar=dr, in1=cur[:, 0, m:512], op0=ALU.mult, op1=ALU.add),
                  reads=[cur_b, TB], writes=[nxt_b])
            kb.op("dve", lambda e, cur=cur, nxt=nxt, m=m, ndi=ndi: e.scalar_tensor_tensor(out=nxt[:, 0, m:512], in0=cur[:, 1, 0:512 - m], scalar=ndi, in1=nxt[:, 0, m:512], op0=ALU.mult, op1=ALU.add),
                  reads=[cur_b, nxt_b, TB], writes=[nxt_b])
            kb.op("dve", lambda e, cur=cur, nxt=nxt, m=m, di=di: e.scalar_tensor_tensor(out=nxt[:, 1, m:512], in0=cur[:, 0, 0:512 - m], scalar=di, in1=cur[:, 1, m:512], op0=ALU.mult, op1=ALU.add),
                  reads=[cur_b, TB], writes=[nxt_b])
            kb.op("dve", lambda e, cur=cur, nxt=nxt, m=m, dr=dr: e.scalar_tensor_tensor(out=nxt[:, 1, m:512], in0=cur[:, 1, 0:512 - m], scalar=dr, in1=nxt[:, 1, m:512], op0=ALU.mult, op1=ALU.add),
                  reads=[cur_b, nxt_b, TB], writes=[nxt_b])
            cur, cur_b, nxt, nxt_b = nxt, nxt_b, cur, cur_b
        kb.op("pool", lambda e, ssh=ssh: e.memset(ssh[:, :, 0:1], 0.0), writes=[ssh_b])
        kb.op("act", lambda e, ssh=ssh, cur=cur: e.activation(out=ssh[:, :, 1:512], in_=cur[:, :, 0:511], func=AF.Copy), reads=[cur_b, ssh_b], writes=[ssh_b])
        for ct in range(4):
            if ct % 2 == 0:
                ps, ps_b = kb.psum()
            o0 = (ct % 2) * 256
            csl = slice(ct * 128, (ct + 1) * 128)
            kb.op("pe", lambda e, ps=ps, o0=o0, csl=csl, ssh=ssh, q=q: e.matmul(ps[:, o0:o0 + 256], ssh[:, 0, csl], Gt[:, 0, q].rearrange("p a n -> p (a n)"), start=True, stop=False),
                  reads=[ssh_b, TB], writes=[ps_b])
            kb.op("pe", lambda e, ps=ps, o0=o0, csl=csl, ssh=ssh, q=q: e.matmul(ps[:, o0:o0 + 256], ssh[:, 1, csl], Gt[:, 1, q].rearrange("p a n -> p (a n)"), start=False, stop=False),
                  reads=[ssh_b, TB], writes=[ps_b])
            for a in range(2):
                kb.op("pe", lambda e, ps=ps, o0=o0, csl=csl, a=a, q=q: e.matmul(ps[:, o0 + a * 128:o0 + (a + 1) * 128], U[:, 2 * q + a, csl], Tt[:, 2 * q + a, :], start=False, stop=(a == 1)),
                      reads=[U_b, TB], writes=[ps_b])
            kb.op("act", lambda e, ps=ps, o0=o0, ct=ct, q=q: e.activation(out=Yg[:, ct, :, q * 32:(q + 1) * 32].rearrange("p j (a c) -> p a j c", a=2),
                                                                           in_=ps[:, o0:o0 + 256].rearrange("p (a j c) -> p a j c", a=2, j=8), func=AF.Gelu_apprx_tanh),
                  reads=[ps_b], writes=[Yg_b])
    ev = 0
    for ct in range(4):
        for cht in range(4):
            for jh in range(2):
                ps, ps_b = kb.psum()
                for jj in range(4):
                    j = jh * 4 + jj
                    kb.op("pe", lambda e, ps=ps, jj=jj, j=j, ct=ct, cht=cht: e.matmul(ps[:, jj * 128:(jj + 1) * 128], Yg[:, ct, j, cht * 128:(cht + 1) * 128], self.identb[:], start=True, stop=True),
                          reads=[Yg_b, self.identb_b], writes=[ps_b])
                t0 = ct * 1024 + jh * 4
                dst = ygT[:, cht, ct * 1024:(ct + 1) * 1024].rearrange("p (c j) -> p j c", j=8)[:, jh * 4:jh * 4 + 4, :]
                pv = ps[:].rearrange("p (j c) -> p j c", j=4)
                if ev % 2 == 0:
                    kb.op("act", lambda e, dst=dst, pv=pv: e.activation(out=dst, in_=pv, func=AF.Copy), reads=[ps_b], writes=[ygT_b])
                else:
                    kb.op("dve", lambda e, dst=dst, pv=pv: e.tensor_copy(out=dst, in_=pv), reads=[ps_b], writes=[ygT_b])
                ev += 1
    wg, wg_b = kb.sb("wglu", [128, 4, 512], BF16)
    kb.dma("pool", wg[:], self.I("w_glu")[l].rearrange("(kt p) n -> p kt n", p=128), writes=[wg_b])
    sgs = [kb.sb("sg%d" % i, [128, 512], BF16) for i in range(3)]
    yos = [kb.sb("yo%d" % i, [128, S], BF16) for i in range(2)]
    it = 0
    for mo in range(4):
        yo, yo_b = yos[mo % 2]
        for tg in range(8):
            ps, ps_b = kb.psum()
            tsl = slice(tg * 512, (tg + 1) * 512)
            for kt in range(4):
                kb.op("pe", lambda e, ps=ps, kt=kt, mo=mo, tsl=tsl: e.matmul(ps[:], wg[:, kt, mo * 128:(mo + 1) * 128], ygT[:, kt, tsl], start=(kt == 0), stop=(kt == 3)),
                      reads=[wg_b, ygT_b], writes=[ps_b])
            sg, sg_b = sgs[it % 3]
            it += 1
            kb.op("act", lambda e, sg=sg, ps=ps: e.activation(out=sg[:], in_=ps[:], func=AF.Sigmoid), reads=[ps_b], writes=[sg_b])
            kb.op("pool", lambda e, yo=yo, sg=sg, mo=mo, tsl=tsl: e.tensor_tensor(out=yo[:, tsl], in0=ygT[:, mo, tsl], in1=sg[:], op=ALU.mult),
                  reads=[ygT_b, sg_b], writes=[yo_b])
        kb.dma("sp", self.ysT[mo * 128:(mo + 1) * 128, :], yo[:], reads=[yo_b])
    kb.barrier()


Prog.stageB1 = _stageB1
Prog._ssm_main = _ssm_main


def _stageC(self, l, last):
    kb = self.kb
    kb.reset(self.arena0)
    idf, idf_b = self.identf, self.identf_b
    moe = (l % 2 == 1)
    li2 = l // 2
    ones_m, ones_m_b = kb.sb("ones_m", [128, 128], BF16)
    ones_1, ones_1_b = kb.sb("ones_1", [128, 128], BF16)
    kb.op("pool", lambda e: e.memset(ones_m[:], 1.0 / 1024), writes=[ones_m_b])
    kb.op("pool", lambda e: e.memset(ones_1[:], 1.0), writes=[ones_1_b])
    lnp, lnp_b = kb.sb("lnp", [8, 6, 128], F32)
    for i, nm in enumerate(["ln_mix_g", "ln_mix_b", "ln_x_g", "ln_x_b", "ln_ffn_g", "ln_ffn_b"]):
        kb.dma("sp", lnp[:, i, :], self.I(nm)[l].rearrange("(kt p) -> kt p", p=128), writes=[lnp_b])
    lnT, lnT_b = kb.sb("lnT", [128, 6, 8], F32)
    ps, ps_b = kb.psum()
    for i in range(6):
        kb.op("pe", lambda e, i=i, ps=ps: e.transpose(ps[:, i * 8:(i + 1) * 8], lnp[:, i, :], idf[0:8, 0:8]), reads=[lnp_b, idf_b], writes=[ps_b])
    kb.op("dve", lambda e, ps=ps: e.tensor_copy(out=lnT[:].rearrange("p a b -> p (a b)"), in_=ps[:, 0:48]), reads=[ps_b], writes=[lnT_b])
    NW = 4
    wraw = [kb.sb("wC%d" % i, [128, 4096], BF16) for i in range(NW)]
    self._wc = 0

    def wbuf():
        t = wraw[self._wc % NW]
        self._wc += 1
        return t

    def load_w(src, kt_n, ncols):
        t, t_b = wbuf()
        v = t[:, 0:kt_n * ncols].rearrange("p (k n) -> p k n", k=kt_n)
        sv = src.rearrange("(k p) n -> p k n", p=128)
        for k0 in range(0, kt_n, 8):
            k1 = min(kt_n, k0 + 8)
            kb.dma("sp", v[:, k0:k1, :], sv[:, k0:k1, :], reads=[self.wcb], writes=[t_b])
        return v, t_b

    def linear(W, kt_n, n_out, rhs_fn, rhs_bufs, consume):
        cpc = 512
        while kt_n * cpc * 2 > 8192:
            cpc //= 2
        c0 = 0
        while c0 < n_out:
            nc_ = min(cpc, n_out - c0)
            wv, w_b = load_w(W[:, c0:c0 + nc_], kt_n, nc_)
            for j in range(nc_ // 128):
                ps, ps_b = kb.psum()
                for kt in range(kt_n):
                    kb.op("pe", lambda e, ps=ps, wv=wv, kt=kt, j=j: e.matmul(ps[:], wv[:, kt, j * 128:(j + 1) * 128], rhs_fn(kt), start=(kt == 0), stop=(kt == kt_n - 1)),
                          reads=[w_b] + rhs_bufs, writes=[ps_b])
                consume((c0 // 128) + j, ps, ps_b)
            c0 += nc_

    memT, memT_b = self.memT, self.memT_b
    KT, KT_b = kb.sb("KT", [128, 8, NMEM], BF16)
    Vm, Vm_b = kb.sb("Vm", [128, 2, D], BF16)

    def cons_k(ot, ps, ps_b):
        kb.op("act", lambda e: e.activation(out=KT[:, ot, :], in_=ps[:, 0:NMEM], func=AF.Copy), reads=[ps_b], writes=[KT_b])
    def lin_k():
        W = self.wc["w_xk"]
        for c0 in (0, 512):
            wv, w_b = load_w(W[:, c0:c0 + 512], 8, 512)
            for j in range(4):
                ps, ps_b = kb.psum()
                for kt in range(8):
                    kb.op("pe", lambda e, ps=ps, wv=wv, kt=kt, j=j: e.matmul(ps[:, 0:NMEM], wv[:, kt, j * 128:(j + 1) * 128], memT[:, kt, :], start=(kt == 0), stop=(kt == 7)),
                          reads=[w_b, memT_b], writes=[ps_b])
                cons_k(c0 // 128 + j, ps, ps_b)
    lin_k()
    Wv = self.wc["w_xv"]
    for c0 in (0, 512):
        wv, w_b = load_w(Wv[:, c0:c0 + 512], 8, 512)
        for mt in range(2):
            ps, ps_b = kb.psum()
            for kt in range(8):
                kb.op("pe", lambda e, ps=ps, wv=wv, kt=kt, mt=mt: e.matmul(ps[:], memT[:, kt, mt * 128:(mt + 1) * 128], wv[:, kt, :], start=(kt == 0), stop=(kt == 7)),
                      reads=[w_b, memT_b], writes=[ps_b])
            kb.op("act", lambda e, ps=ps, mt=mt, c0=c0: e.activation(out=Vm[:, mt, c0:c0 + 512], in_=ps[:], func=AF.Copy), reads=[ps_b], writes=[Vm_b])
    if moe:
        wr32, wr32_b = kb.sb("wr32", [128, 8, 8], F32)
        kb.dma("sp", wr32[:], self.I("moe_w_router")[li2].rearrange("(k p) e -> p k e", p=128), writes=[wr32_b])
        wrh, wrh_b = kb.sb("wrh", [128, 2, 8, 8], BF16)
        wrr, wrr_b = kb.sb("wrr", [128, 8, 8], F32)
        kb.op("dve", lambda e: e.tensor_copy(out=wrh[:, 0], in_=wr32[:]), reads=[wr32_b], writes=[wrh_b])
        kb.op("dve", lambda e: e.tensor_tensor(out=wrr[:], in0=wr32[:], in1=wrh[:, 0], op=ALU.subtract), reads=[wr32_b, wrh_b], writes=[wrr_b])
        kb.op("dve", lambda e: e.tensor_copy(out=wrh[:, 1], in_=wrr[:]), reads=[wrr_b, wrh_b], writes=[wrh_b])
        brB, brB_b = kb.sb("brB", [128, 8], F32)
        kb.dma("sp", brB[:], self.I("moe_b_router")[li2].partition_broadcast(128), writes=[brB_b])
        sel, sel_b = kb.sb("sel", [8, 8, 128], BF16)
        self_f, self_f_b = kb.sb("self", [8, 8, 128], F32)
        kb.op("pool", lambda e: e.memset(self_f[:], 1.0), writes=[self_f_b])
        kb.op("pool", lambda e: e.affine_select(out=self_f[:], in_=self_f[:], pattern=[[1, 8], [0, 128]], compare_op=ALU.is_equal, fill=0.0, base=0, channel_multiplier=-1),
              reads=[self_f_b], writes=[self_f_b])
        kb.op("dve", lambda e: e.tensor_copy(out=sel[:], in_=self_f[:]), reads=[self_f_b], writes=[sel_b])
    xr, xr_b = kb.sb("xr", [128, 8, 512], F32)
    vv, vv_b = kb.sb("vv", [128, 8, 512], F32)
    GB = [kb.sb("G%d" % i, [128, 8, 512], BF16) for i in range(4)]
    m_u = kb.mark()
    ys, ys_b = kb.sb("ysg", [128, 12, 512], BF16)
    gt, gt_b = kb.sb("gtg", [128, 24, 512], BF16)
    kb.reset(m_u)
    nft = 11 if moe else 22
    hh, hh_b = kb.sb("hh", [128, nft, 512], BF16)
    if moe:
        macc, macc_b = kb.sb("macc", [128, 8, 512], F32)
    kb.reset(m_u + 36 * 1024)
    PT = [kb.sb("PT%d" % i, [128, 2, 512], BF16) for i in range(2)]
    tmpf = [kb.sb("tmpf%d" % i, [128, 512], F32) for i in range(4)]
    tmpb = [kb.sb("tmpb%d" % i, [128, 512], BF16) for i in range(3)]
    st_mean, st_mean_b = kb.sb("st_mean", [128, 512], F32)
    st_rstd, st_rstd_b = kb.sb("st_rstd", [128, 512], F32)
    if moe:
        lg, lg_b = kb.sb("lg", [128, 4, 8], F32)
        gsm = {n_: kb.sb("g_" + n_, [128, 4, 8], F32) for n_ in ("eq1", "eq2", "lg2", "gate", "gr")}
        gs1 = {n_: kb.sb("s_" + n_, [128, 4], F32) for n_ in ("m1", "m2", "w1", "w2")}
        gTs, gTs_b = kb.sb("gTs", [8, 512], F32)
        gTh, gTh_b = kb.sb("gTh", [8, 2, 512], BF16)
        gTr, gTr_b = kb.sb("gTr", [8, 512], F32)
        xlo, xlo_b = kb.sb("xlo", [128, 8, 512], BF16)
        gbs, gbs_b = kb.sb("gbs", [128, 512], F32)
    if last:
        ostg = [kb.sb("ostg%d" % i, [128, D], F32) for i in range(2)]
    self._ti = 0

    def tf():
        t = tmpf[self._ti % 4]
        self._ti += 1
        return t

    self._tb = 0

    def tbf():
        t = tmpb[self._tb % 3]
        self._tb += 1
        return t

    def layer_norm(gi):
        (vb, vb_b), (vsq, vsq_b) = GB[1], GB[2]
        kb.op("act", lambda e: e.activation(out=vb[:], in_=vv[:], func=AF.Copy), reads=[vv_b], writes=[vb_b])
        kb.op("act", lambda e: e.activation(out=vsq[:], in_=vv[:], func=AF.Square), reads=[vv_b], writes=[vsq_b])
        pm, pm_b = kb.psum()
        for kt in range(8):
            kb.op("pe", lambda e, kt=kt: e.matmul(pm[:], ones_m[:], vb[:, kt, :], start=(kt == 0), stop=(kt == 7)), reads=[ones_m_b, vb_b], writes=[pm_b])
        pq, pq_b = kb.psum()
        for kt in range(8):
            kb.op("pe", lambda e, kt=kt: e.matmul(pq[:], ones_m[:], vsq[:, kt, :], start=(kt == 0), stop=(kt == 7)), reads=[ones_m_b, vsq_b], writes=[pq_b])
        kb.op("act", lambda e: e.activation(out=st_mean[:], in_=pm[:], func=AF.Copy), reads=[pm_b], writes=[st_mean_b])
        (m2, m2_b) = tf()
        kb.op("dve", lambda e: e.tensor_tensor(out=m2[:], in0=st_mean[:], in1=st_mean[:], op=ALU.mult), reads=[st_mean_b], writes=[m2_b])
        kb.op("dve", lambda e: e.tensor_tensor(out=m2[:], in0=pq[:], in1=m2[:], op=ALU.subtract), reads=[pq_b, m2_b], writes=[m2_b])
        kb.op("dve", lambda e: e.tensor_scalar(out=m2[:], in0=m2[:], scalar1=0.0, scalar2=LN_EPS, op0=ALU.max, op1=ALU.add), reads=[m2_b], writes=[m2_b])
        kb.op("act", lambda e: e.activation(out=m2[:], in_=m2[:], func=AF.Sqrt), reads=[m2_b], writes=[m2_b])
        kb.op("dve", lambda e: e.reciprocal(out=st_rstd[:], in_=m2[:]), reads=[m2_b], writes=[st_rstd_b])
        for kt in range(8):
            eng = "dve" if kt % 2 == 0 else "pool"
            kb.op(eng, lambda e, kt=kt: e.tensor_tensor(out=vv[:, kt, :], in0=vv[:, kt, :], in1=st_mean[:], op=ALU.subtract), reads=[vv_b, st_mean_b], writes=[vv_b])
            kb.op(eng, lambda e, kt=kt: e.tensor_tensor(out=vv[:, kt, :], in0=vv[:, kt, :], in1=st_rstd[:], op=ALU.mult), reads=[vv_b, st_rstd_b], writes=[vv_b])
            kb.op(eng, lambda e, kt=kt: e.tensor_scalar(out=xr[:, kt, :], in0=vv[:, kt, :], scalar1=lnT[:, gi, kt:kt + 1], scalar2=lnT[:, gi + 1, kt:kt + 1], op0=ALU.mult, op1=ALU.add),
                  reads=[vv_b, lnT_b], writes=[xr_b])

    def resid_consume(ot, ps, ps_b):
        kb.op("dve", lambda e: e.scalar_tensor_tensor(out=vv[:, ot, :], in0=xr[:, ot, :], scalar=float(ALPHA), in1=ps[:], op0=ALU.mult, op1=ALU.add),
              reads=[xr_b, ps_b], writes=[vv_b])

    xres_v = self.xres.rearrange("(kt p) t -> p kt t", p=128)
    xTd_v = self.xTd.rearrange("(kt p) t -> p kt t", p=128)
    for tg in range(8):
        tsl = slice(tg * 512, (tg + 1) * 512)
        kb.dma("pool", xr[:], xres_v[:, :, tsl], writes=[xr_b])
        ysv = self.ysT.rearrange("(a p) t -> p a t", p=128)
        for a0 in range(0, 12, 6):
            kb.dma("pool", ys[:, a0:a0 + 6, :], ysv[:, a0:a0 + 6, tsl], writes=[ys_b, hh_b])
        gv = self.gT.rearrange("(a p) t -> p a t", p=128)
        for a0 in range(0, 24, 6):
            kb.dma("pool", gt[:, a0:a0 + 6, :], gv[:, a0:a0 + 6, tsl], writes=[gt_b, hh_b] + ([macc_b] if moe else []))
        mg, mg_b = GB[0]
        wbr = []
        for n_ in range(3):
            wbr.append(load_w(self.wc["w_branch"][n_], 4, D))
        for dt_ in range(8):
            pss = []
            for n_ in range(3):
                wv, w_b = wbr[n_]
                ps, ps_b = kb.psum()
                for ck in range(4):
                    kb.op("pe", lambda e, ps=ps, wv=wv, ck=ck, n_=n_, dt_=dt_: e.matmul(ps[:], wv[:, ck, dt_ * 128:(dt_ + 1) * 128], ys[:, n_ * 4 + ck, :], start=(ck == 0), stop=(ck == 3)),
                          reads=[w_b, ys_b], writes=[ps_b])
                pss.append((ps, ps_b))
            (a0_, a0_b), (a1_, a1_b), (a2_, a2_b) = tf(), tf(), tf()
            kb.op("dve", lambda e, p=pss[0][0], dt_=dt_, a0_=a0_: e.tensor_tensor(out=a0_[:], in0=p[:], in1=gt[:, dt_, :], op=ALU.mult), reads=[pss[0][1], gt_b], writes=[a0_b])
            kb.op("dve", lambda e, p=pss[1][0], dt_=dt_, a1_=a1_: e.tensor_tensor(out=a1_[:], in0=p[:], in1=gt[:, 8 + dt_, :], op=ALU.mult), reads=[pss[1][1], gt_b], writes=[a1_b])
            kb.op("dve", lambda e, p=pss[2][0], dt_=dt_, a2_=a2_: e.tensor_tensor(out=a2_[:], in0=p[:], in1=gt[:, 16 + dt_, :], op=ALU.mult), reads=[pss[2][1], gt_b], writes=[a2_b])
            kb.op("pool", lambda e, a0_=a0_, a1_=a1_: e.tensor_tensor(out=a0_[:], in0=a0_[:], in1=a1_[:], op=ALU.add), reads=[a0_b, a1_b], writes=[a0_b])
            kb.op("pool", lambda e, a0_=a0_, a2_=a2_, dt_=dt_: e.tensor_tensor(out=mg[:, dt_, :], in0=a0_[:], in1=a2_[:], op=ALU.add), reads=[a0_b, a2_b], writes=[mg_b])
        linear(self.wc["w_mix_out"], 8, D, lambda kt: mg[:, kt, :], [mg_b], resid_consume)
        layer_norm(0)
        xb, xb_b = GB[3]
        kb.op("act", lambda e: e.activation(out=xb[:], in_=xr[:], func=AF.Copy), reads=[xr_b], writes=[xb_b])
        qT, qT_b = GB[0]

        def cons_q(ot, ps, ps_b):
            kb.op("act", lambda e: e.activation(out=qT[:, ot, :], in_=ps[:], func=AF.Copy, scale=1.0 / 16), reads=[ps_b], writes=[qT_b])
        linear(self.wc["w_xq"], 8, D, lambda kt: xb[:, kt, :], [xb_b], cons_q)
        oT, oT_b = GB[1]
        for h in range(4):
            pt, pt_b = PT[h % 2]
            for mt in range(2):
                ps, ps_b = kb.psum()
                for ee in range(2):
                    et = 2 * h + ee
                    kb.op("pe", lambda e, ps=ps, et=et, mt=mt, ee=ee: e.matmul(ps[:], KT[:, et, mt * 128:(mt + 1) * 128], qT[:, et, :], start=(ee == 0), stop=(ee == 1)),
                          reads=[KT_b, qT_b], writes=[ps_b])
                kb.op("act", lambda e, ps=ps, pt=pt, mt=mt: e.activation(out=pt[:, mt, :], in_=ps[:], func=AF.Exp), reads=[ps_b], writes=[pt_b])
            pd, pd_b = kb.psum()
            for mt in range(2):
                kb.op("pe", lambda e, pt=pt, mt=mt, pd=pd: e.matmul(pd[:], ones_1[:], pt[:, mt, :], start=(mt == 0), stop=(mt == 1)), reads=[ones_1_b, pt_b], writes=[pd_b])
            rd, rd_b = tf()
            kb.op("dve", lambda e, rd=rd, pd=pd: e.reciprocal(out=rd[:], in_=pd[:]), reads=[pd_b], writes=[rd_b])
            for ee in range(2):
                et = 2 * h + ee
                ps, ps_b = kb.psum()
                for mt in range(2):
                    kb.op("pe", lambda e, ps=ps, et=et, mt=mt, pt=pt: e.matmul(ps[:], Vm[:, mt, et * 128:(et + 1) * 128], pt[:, mt, :], start=(mt == 0), stop=(mt == 1)),
                          reads=[Vm_b, pt_b], writes=[ps_b])
                kb.op("dve", lambda e, ps=ps, et=et, rd=rd: e.tensor_tensor(out=oT[:, et, :], in0=ps[:], in1=rd[:], op=ALU.mult), reads=[ps_b, rd_b], writes=[oT_b])
        linear(self.wc["w_xo"], 8, D, lambda kt: oT[:, kt, :], [oT_b], resid_consume)
        layer_norm(2)
        kb.op("act", lambda e: e.activation(out=xb[:], in_=xr[:], func=AF.Copy), reads=[xr_b], writes=[xb_b])
        def swiglu(Wg, Wu, Wd, nft_, gate_sb=None, down_consume=None):
            gl = {}

            def cons_g(ot, ps, ps_b):
                sg, sg_b = tbf()
                kb.op("act", lambda e: e.activation(out=sg[:], in_=ps[:], func=AF.Silu), reads=[ps_b], writes=[sg_b])
                gl[ot] = (sg, sg_b)

            def cons_u(ot, ps, ps_b):
                sg, sg_b = gl[ot]
                if gate_sb is None:
                    kb.op("dve", lambda e: e.tensor_tensor(out=hh[:, ot, :], in0=ps[:], in1=sg[:], op=ALU.mult), reads=[ps_b, sg_b], writes=[hh_b])
                else:
                    t_, t_b = tf()
                    kb.op("dve", lambda e: e.tensor_tensor(out=t_[:], in0=ps[:], in1=sg[:], op=ALU.mult), reads=[ps_b, sg_b], writes=[t_b])
                    kb.op("pool", lambda e: e.tensor_tensor(out=hh[:, ot, :], in0=t_[:], in1=gate_sb[0][:], op=ALU.mult), reads=[t_b, gate_sb[1]], writes=[hh_b])
            for f0 in range(0, nft_, 3):
                f1 = min(nft_, f0 + 3)
                n_c = (f1 - f0) * 128
                for (W, cons) in ((Wg, cons_g), (Wu, cons_u)):
                    wv, w_b = load_w(W[:, f0 * 128:f0 * 128 + n_c], 8, n_c)
                    for j in range(f1 - f0):
                        ps, ps_b = kb.psum()
                        for kt in range(8):
                            kb.op("pe", lambda e, ps=ps, wv=wv, kt=kt, j=j: e.matmul(ps[:], wv[:, kt, j * 128:(j + 1) * 128], xb[:, kt, :], start=(kt == 0), stop=(kt == 7)),
                                  reads=[w_b, xb_b], writes=[ps_b])
                        cons(f0 + j, ps, ps_b)
            linear(Wd, nft_, D, lambda kt: hh[:, kt, :], [hh_b], down_consume)

        if not moe:
            swiglu(self.wc["ffn_w_gate"], self.wc["ffn_w_up"], self.wc["ffn_w_down"], 22, None, resid_consume)
        else:
            kb.op("dve", lambda e: e.tensor_tensor(out=vv[:], in0=xr[:], in1=xb[:], op=ALU.subtract), reads=[xr_b, xb_b], writes=[vv_b])
            kb.op("act", lambda e: e.activation(out=xlo[:], in_=vv[:], func=AF.Copy), reads=[vv_b], writes=[xlo_b])
            pl, pl_b = kb.psum()
            for tt in range(4):
                tk = slice(tt * 128, (tt + 1) * 128)
                combos = [(xb, xb_b, 0), (xb, xb_b, 1), (xlo, xlo_b, 0)]
                n_mm = 0
                for (xs, xs_b, wpart) in combos:
                    for kt in range(8):
                        kb.op("pe", lambda e, xs=xs, wpart=wpart, kt=kt, tk=tk, tt=tt, n_mm=n_mm, pl=pl: e.matmul(pl[:, tt * 8:(tt + 1) * 8], xs[:, kt, tk], wrh[:, wpart, kt, :], start=(n_mm == 0), stop=(n_mm == 23)),
                              reads=[xs_b, wrh_b], writes=[pl_b])
                        n_mm += 1
            kb.op("dve", lambda e, pl=pl: e.tensor_tensor(out=lg[:], in0=pl[:, 0:32].rearrange("p (t e) -> p t e", t=4), in1=brB[:].unsqueeze(1).to_broadcast([128, 4, 8]), op=ALU.add),
                  reads=[pl_b, brB_b], writes=[lg_b])
            GQ = Buf("gateq")
            eq1, eq2, lg2, gate, gr = (gsm[n_][0] for n_ in ("eq1", "eq2", "lg2", "gate", "gr"))
            m1, m2_, w1, w2 = (gs1[n_][0] for n_ in ("m1", "m2", "w1", "w2"))

            def gd(fn, eng="dve"):
                kb.op(eng, fn, reads=[GQ, lg_b], writes=[GQ])
            gd(lambda e: e.tensor_reduce(out=m1[:], in_=lg[:], axis=AX.X, op=ALU.max))
            gd(lambda e: e.tensor_tensor(out=eq1[:], in0=lg[:], in1=m1[:].unsqueeze(2).to_broadcast([128, 4, 8]), op=ALU.is_equal))
            gd(lambda e: e.scalar_tensor_tensor(out=lg2[:], in0=eq1[:], scalar=-1.0e30, in1=lg[:], op0=ALU.mult, op1=ALU.add))
            gd(lambda e: e.tensor_reduce(out=m2_[:], in_=lg2[:], axis=AX.X, op=ALU.max))
            gd(lambda e: e.tensor_tensor(out=eq2[:], in0=lg2[:], in1=m2_[:].unsqueeze(2).to_broadcast([128, 4, 8]), op=ALU.is_equal))
            gd(lambda e: e.tensor_tensor(out=w2[:], in0=m1[:], in1=m2_[:], op=ALU.subtract))
            gd(lambda e: e.activation(out=w1[:], in_=w2[:], func=AF.Sigmoid), "act")
            gd(lambda e: e.tensor_scalar(out=w2[:], in0=w1[:], scalar1=-1.0, scalar2=1.0, op0=ALU.mult, op1=ALU.add))
            gd(lambda e: e.tensor_tensor(out=gate[:], in0=eq1[:], in1=w1[:].unsqueeze(2).to_broadcast([128, 4, 8]), op=ALU.mult))
            gd(lambda e: e.tensor_tensor(out=gr[:], in0=eq2[:], in1=w2[:].unsqueeze(2).to_broadcast([128, 4, 8]), op=ALU.mult))
            gd(lambda e: e.tensor_tensor(out=gate[:], in0=gate[:], in1=gr[:], op=ALU.add))
            pg, pg_b = kb.psum()
            for tt in range(4):
                kb.op("pe", lambda e, tt=tt, pg=pg: e.transpose(pg[0:8, tt * 128:(tt + 1) * 128], gate[:, tt, :], idf[:]), reads=[GQ, idf_b], writes=[pg_b])
            kb.op("dve", lambda e, pg=pg: e.tensor_copy(out=gTs[:], in_=pg[0:8, :]), reads=[pg_b], writes=[gTs_b])
            kb.op("dve", lambda e: e.tensor_copy(out=gTh[:, 0, :], in_=gTs[:]), reads=[gTs_b], writes=[gTh_b])
            kb.op("dve", lambda e: e.tensor_tensor(out=gTr[:], in0=gTs[:], in1=gTh[:, 0, :], op=ALU.subtract), reads=[gTs_b, gTh_b], writes=[gTr_b])
            kb.op("dve", lambda e: e.tensor_copy(out=gTh[:, 1, :], in_=gTr[:]), reads=[gTr_b, gTh_b], writes=[gTh_b])
            for ex in range(NEXP):
                pgb, pgb_b = kb.psum()
                for part in range(2):
                    kb.op("pe", lambda e, ex=ex, part=part, pgb=pgb: e.matmul(pgb[:], sel[:, ex, :], gTh[:, part, :], start=(part == 0), stop=(part == 1)),
                          reads=[sel_b, gTh_b], writes=[pgb_b])
                kb.op("act", lambda e, pgb=pgb: e.activation(out=gbs[:], in_=pgb[:], func=AF.Copy), reads=[pgb_b], writes=[gbs_b])

                def dcons(ot, ps, ps_b, ex=ex):
                    if ex == 0:
                        kb.op("dve", lambda e: e.tensor_copy(out=macc[:, ot, :], in_=ps[:]), reads=[ps_b], writes=[macc_b])
                    elif ex < NEXP - 1:
                        kb.op("dve", lambda e: e.tensor_tensor(out=macc[:, ot, :], in0=macc[:, ot, :], in1=ps[:], op=ALU.add), reads=[ps_b, macc_b], writes=[macc_b])
                    else:
                        t_, t_b = tf()
                        kb.op("dve", lambda e: e.tensor_tensor(out=t_[:], in0=macc[:, ot, :], in1=ps[:], op=ALU.add), reads=[ps_b, macc_b], writes=[t_b])
                        kb.op("dve", lambda e: e.scalar_tensor_tensor(out=vv[:, ot, :], in0=xr[:, ot, :], scalar=float(ALPHA), in1=t_[:], op0=ALU.mult, op1=ALU.add),
                              reads=[xr_b, t_b], writes=[vv_b])
                swiglu(self.wc["moe_w_gate"][ex], self.wc["moe_w_up"][ex], self.wc["moe_w_down"][ex], 11, (gbs, gbs_b), dcons)
        layer_norm(4)
        kb.dma("pool", xres_v[:, :, tsl], xr[:], reads=[xr_b])
        kb.op("act", lambda e: e.activation(out=xb[:], in_=xr[:], func=AF.Copy), reads=[xr_b], writes=[xb_b])
        kb.dma("pool", xTd_v[:, :, tsl], xb[:], reads=[xb_b])
        if last:
            for tt in range(4):
                og, og_b = ostg[tt % 2]
                for half in range(2):
                    ps, ps_b = kb.psum()
                    for j in range(4):
                        kt = half * 4 + j
                        kb.op("pe", lambda e, ps=ps, kt=kt, j=j, tt=tt: e.transpose(ps[:, j * 128:(j + 1) * 128], xr[:, kt, tt * 128:(tt + 1) * 128], idf[:]),
                              reads=[xr_b, idf_b], writes=[ps_b])
                    kb.op("act", lambda e, ps=ps, og=og, half=half: e.activation(out=og[:, half * 512:(half + 1) * 512], in_=ps[:], func=AF.Copy), reads=[ps_b], writes=[og_b])
                r0 = tg * 512 + tt * 128
                kb.dma("sp", self.out[r0:r0 + 128, :], og[:], reads=[og_b])
    kb.barrier()


Prog.stageC = _stageC
```

```python
import contextlib
import math

import numpy as np
import concourse.bass as bass
import concourse.mybir as mybir
from concourse.bass_utils import run_bass_kernel_spmd

F32 = mybir.dt.float32
BF16 = mybir.dt.bfloat16
I32 = mybir.dt.int32
AF = mybir.ActivationFunctionType
ALU = mybir.AluOpType
AX = mybir.AxisListType

CENG = ("pe", "act", "dve", "pool", "sp")

D = 1024
S = 4096
DEPTH = 4
NIN = 6664
BW = 512
DFF = 2816
NEXP = 8
DFE = 1408
NMEM = 256
ALPHA = (2 * DEPTH) ** 0.25
LN_EPS = 1e-5
ROPE_THETA = 500000.0
O_U, O_QD, O_KD, O_VD, O_QF, O_KF, O_VF, O_F, O_G = 0, 512, 1024, 1536, 2048, 2560, 3072, 3584, 3592
VA = 520


class Buf:
    __slots__ = ("name", "w", "r", "excl")

    def __init__(self, name="", excl=False):
        self.name = name
        self.w = None
        self.r = {}
        self.excl = excl


class Op:
    __slots__ = ("eng", "fn", "waits", "sig", "count", "dma")

    def __init__(self, eng, fn, dma=None):
        self.eng = eng
        self.fn = fn
        self.waits = []
        self.sig = False
        self.count = 0
        self.dma = dma


class KB:
    SB_LO = 16640
    SB_HI = 229376

    def __init__(self, nc, n_dma_slots=12):
        self.nc = nc
        self.ops = {e: [] for e in CENG}
        self.seen_c = {e: {s: -1 for s in CENG} for e in CENG}
        self.seen_d = {e: {} for e in CENG}
        self.dq = {"sp": [], "pool": [], "act": []}
        self.slot_cnt = {}
        self.slot_rr = {"sp": 0, "pool": 0, "act": 0}
        for q in self.dq:
            for i in range(n_dma_slots if q != "act" else 4):
                sid = (q, i)
                self.dq[q].append(sid)
                self.slot_cnt[sid] = 0
        self.sb_off = self.SB_LO
        self.n_alloc = 0
        self.ps = []
        self.ps_rr = 0
        self.pa_rr = 0

    def sb(self, name, shape, dtype):
        esz = {F32: 4, BF16: 2, I32: 4}[dtype]
        n = esz
        for d in shape[1:]:
            n *= d
        n = (n + 63) // 64 * 64
        off = self.sb_off
        assert off + n <= self.SB_HI, "SBUF overflow at %s: %d + %d" % (name, off, n)
        self.sb_off += n
        self.n_alloc += 1
        t = self.nc.alloc_sbuf_tensor_at("%s_%d" % (name, self.n_alloc), list(shape), dtype, offset=off)
        return t, Buf(name)

    def mark(self):
        return self.sb_off

    def reset(self, m):
        self.sb_off = m

    def psum(self):
        p = self.ps[self.ps_rr % 5]
        self.ps_rr += 1
        return p

    def psum_acc(self):
        p = self.ps[5 + self.pa_rr % 3]
        self.pa_rr += 1
        return p

    def _collect(self, eng, reads, writes, is_dma):
        ev = []
        for b in reads:
            if b.w is not None:
                ev.append(b.w)
            if b.excl:
                for k, e in b.r.items():
                    if e[0] == "d" or e[1] != eng:
                        ev.append(e)
        for b in writes:
            if b.w is not None:
                if is_dma or b.w[0] == "d" or b.w[1] != eng or eng != "pe":
                    ev.append(b.w)
            for k, e in b.r.items():
                if is_dma or e[0] == "d" or e[1] != eng or eng != "pe":
                    ev.append(e)
        return ev

    def _reduce(self, eng, evs):
        best_c = {}
        best_d = {}
        for e in evs:
            if e[0] == "c":
                if e[2] > best_c.get(e[1], -1):
                    best_c[e[1]] = e[2]
            else:
                if e[2] > best_d.get(e[1], -1):
                    best_d[e[1]] = e[2]
        out = []
        for s, i in best_c.items():
            if i > self.seen_c[eng][s]:
                self.seen_c[eng][s] = i
                out.append(("c", s, i))
                self.ops[s][i].sig = True
        for sl, v in best_d.items():
            if v > self.seen_d[eng].get(sl, 0):
                self.seen_d[eng][sl] = v
                out.append(("d", sl, v))
        return out

    def _update(self, ev, reads, writes, rkey):
        for b in reads:
            b.r[rkey] = ev
        for b in writes:
            b.w = ev
            b.r = {}

    def op(self, eng, fn, reads=(), writes=()):
        o = Op(eng, fn)
        evs = self._collect(eng, reads, writes, False)
        o.waits = self._reduce(eng, evs)
        idx = len(self.ops[eng])
        self.ops[eng].append(o)
        self._update(("c", eng, idx), reads, writes, eng)
        return o

    def dma(self, q, out, in_, reads=(), writes=(), **kw):
        slots = self.dq[q]
        sid = slots[self.slot_rr[q] % len(slots)]
        self.slot_rr[q] += 1
        evs = self._collect(q, reads, writes, True)
        if self.slot_cnt[sid] > 0:
            evs.append(("d", sid, self.slot_cnt[sid] * 16))
        self.slot_cnt[sid] += 1
        val = self.slot_cnt[sid] * 16
        o = Op(q, lambda e: e.dma_start(out=out, in_=in_, **kw), dma=(sid, val))
        o.waits = self._reduce(q, evs)
        self.ops[q].append(o)
        self._update(("d", sid, val), reads, writes, ("d", sid))
        return o

    def barrier(self):
        evs = []
        for e in CENG:
            if e != "dve":
                for i in range(len(self.ops[e]) - 1, -1, -1):
                    if self.ops[e][i].dma is None and self.ops[e][i].fn is not None:
                        evs.append(("c", e, i))
                        break
        dev = [("d", sid, c * 16) for sid, c in self.slot_cnt.items() if c > 0]
        tok = self.bar_tile
        o = Op("dve", lambda e: e.memset(tok[:], 0.0))
        evs += self._collect("dve", [], [self.bar_buf], False)
        o.waits = self._reduce("dve", evs + dev)
        idx = len(self.ops["dve"])
        self.ops["dve"].append(o)
        self._update(("c", "dve", idx), [], [self.bar_buf], "dve")
        for e in CENG:
            if e == "dve":
                continue
            o2 = Op(e, None)
            o2.waits = self._reduce(e, [("c", "dve", idx)] + dev)
            self.ops[e].append(o2)

    def finish(self, stack):
        nc = self.nc
        for e in CENG:
            c = 0
            for o in self.ops[e]:
                if o.sig:
                    c += 1
                    o.count = c
        sems = {e: stack.enter_context(nc.semaphore("s_" + e)) for e in CENG}
        dsems = {}
        for q, slots in self.dq.items():
            for sid in slots:
                if self.slot_cnt[sid] > 0:
                    dsems[sid] = stack.enter_context(nc.semaphore("d_%s%d" % sid))
        block = stack.enter_context(nc.Block())
        ops = self.ops

        def replay(ename):
            def run(eng):
                for o in ops[ename]:
                    for w in o.waits:
                        if w[0] == "c":
                            eng.wait_ge(sems[w[1]], ops[w[1]][w[2]].count)
                        else:
                            eng.wait_ge(dsems[w[1]], w[2])
                    if o.fn is None:
                        continue
                    ins = o.fn(eng)
                    if o.dma is not None:
                        ins.then_inc(dsems[o.dma[0]], 16)
                    elif o.sig:
                        ins.then_inc(sems[ename], 1)
            return run

        block.tensor(replay("pe"))
        block.scalar(replay("act"))
        block.vector(replay("dve"))
        block.gpsimd(replay("pool"))
        block.sync(replay("sp"))
        return {e: len(ops[e]) for e in CENG}


class Prog:
    def __init__(self, nc, stack, layers=range(DEPTH), stages="ABC", debug=()):
        self.nc = nc
        self.stack = stack
        self.kb = KB(nc)
        self.layers = list(layers)
        self.stages = stages
        self.debug = set(debug)
        self.inp = {}
        self.scr = {}

    def din(self, name, shape, dtype=F32):
        t = self.nc.dram_tensor(name, list(shape), dtype, kind="ExternalInput").ap()
        self.inp[name] = t
        return t

    def dscr(self, name, shape, dtype):
        kind = "ExternalOutput" if name in self.debug else "Internal"
        t = self.nc.dram_tensor(name, list(shape), dtype, kind=kind).ap()
        self.scr[name] = t
        return t

    SHAPES = {
        "x": ([S, D], F32), "mem": ([NMEM, D], F32), "positions": ([128, 32], I32),
        "w_in": ([DEPTH, D, NIN], F32), "b_forget": ([DEPTH, 8], F32),
        "ssm_lambda_re": ([DEPTH, 32, 64], F32), "ssm_lambda_im": ([DEPTH, 32, 64], F32), "ssm_log_dt": ([DEPTH, 32], F32),
        "ssm_b_re": ([DEPTH, 32, 64, 16], F32), "ssm_b_im": ([DEPTH, 32, 64, 16], F32),
        "ssm_c_re": ([DEPTH, 32, 16, 64], F32), "ssm_c_im": ([DEPTH, 32, 16, 64], F32),
        "ssm_d": ([DEPTH, 512], F32), "w_glu": ([DEPTH, 512, 512], F32), "w_branch": ([DEPTH, 3, 512, D], F32),
        "w_mix_out": ([DEPTH, D, D], F32), "ln_mix_g": ([DEPTH, D], F32), "ln_mix_b": ([DEPTH, D], F32),
        "w_xq": ([DEPTH, D, D], F32), "w_xk": ([DEPTH, D, D], F32), "w_xv": ([DEPTH, D, D], F32), "w_xo": ([DEPTH, D, D], F32),
        "ln_x_g": ([DEPTH, D], F32), "ln_x_b": ([DEPTH, D], F32),
        "ffn_w_gate": ([2, D, DFF], F32), "ffn_w_up": ([2, D, DFF], F32), "ffn_w_down": ([2, DFF, D], F32),
        "moe_w_router": ([2, D, NEXP], F32), "moe_b_router": ([2, NEXP], F32),
        "moe_w_gate": ([2, NEXP, D, DFE], F32), "moe_w_up": ([2, NEXP, D, DFE], F32), "moe_w_down": ([2, NEXP, DFE, D], F32),
        "ln_ffn_g": ([DEPTH, D], F32), "ln_ffn_b": ([DEPTH, D], F32),
    }

    def I(self, name):
        if name not in self.inp:
            shp, dt_ = self.SHAPES[name]
            self.din(name, shp, dt_)
        return self.inp[name]

    def declare(self):
        self.x = self.I("x")
        self.pos = self.I("positions")
        self.out = self.nc.dram_tensor("out", [S, D], F32, kind="ExternalOutput").ap()
        self.xres = self.dscr("xres", [D, S], F32)
        self.xTd = self.dscr("xTd", [D, S], BF16)
        self.u_tm = self.dscr("u_tm", [S, 512], BF16)
        self.qdT = self.dscr("qdT", [512, S], BF16)
        self.kdT = self.dscr("kdT", [512, S], BF16)
        self.vd = self.dscr("vd", [S, VA], BF16)
        self.qfT = self.dscr("qfT", [512, S], BF16)
        self.kfT = self.dscr("kfT", [512, S], BF16)
        self.vf = self.dscr("vf", [S, VA], BF16)
        self.fl = self.dscr("fl", [8, S], F32)
        self.gT = self.dscr("gT", [3 * D, S], BF16)
        self.ysT = self.dscr("ysT", [3 * BW, S], BF16)
        self.wcb = Buf("wconv")
        self.wc = {
            "w_branch": self.dscr("c_wbr", [3, 512, D], BF16), "w_mix_out": self.dscr("c_wo", [D, D], BF16),
            "w_xq": self.dscr("c_wq", [D, D], BF16), "w_xk": self.dscr("c_wk", [D, D], BF16),
            "w_xv": self.dscr("c_wv", [D, D], BF16), "w_xo": self.dscr("c_wxo", [D, D], BF16),
            "ffn_w_gate": self.dscr("c_fg", [D, DFF], BF16), "ffn_w_up": self.dscr("c_fu", [D, DFF], BF16), "ffn_w_down": self.dscr("c_fd", [DFF, D], BF16),
            "moe_w_gate": self.dscr("c_mg", [NEXP, D, DFE], BF16), "moe_w_up": self.dscr("c_mu", [NEXP, D, DFE], BF16), "moe_w_down": self.dscr("c_md", [NEXP, DFE, D], BF16),
        }
        self.dscr("augq", [8, 6, S], BF16)
        self.dscr("augk", [8, 6, S], BF16)

    def setup(self):
        kb, nc = self.kb, self.nc
        st = self.stack
        kb.ps = [(st.enter_context(nc.psum_tensor("ps%d" % i, [128, 512], F32)), Buf("ps%d" % i, True)) for i in range(8)]
        bar, bar_b = kb.sb("bar", [128, 8], F32)
        kb.bar_tile = bar
        kb.bar_buf = bar_b
        self.identf, self.identf_b = kb.sb("identf", [128, 128], F32)
        self.identb, self.identb_b = kb.sb("identb", [128, 128], BF16)
        idf, idb = self.identf, self.identb
        kb.op("pool", lambda e: e.memset(idf[:], 1.0), writes=[self.identf_b])
        kb.op("pool", lambda e: e.affine_select(out=idf[:], in_=idf[:], pattern=[[1, 128]], compare_op=ALU.is_equal,
                                                fill=0.0, base=0, channel_multiplier=-1),
              reads=[self.identf_b], writes=[self.identf_b])
        kb.op("dve", lambda e: e.tensor_copy(out=idb[:], in_=idf[:]), reads=[self.identf_b], writes=[self.identb_b])
        self.rot = {}
        for n_ in ["cos", "sin", "cosq", "sinq"]:
            self.rot[n_] = kb.sb("rot_" + n_, [128, 32, 8], F32)
        self.memT, self.memT_b = kb.sb("memT", [128, 8, NMEM], BF16)
        self._build_rot(kb.mark())
        self._build_mem()

    def _build_rot(self, m2):
        kb = self.kb
        posi, posi_b = kb.sb("posi2", [128, 32], I32)
        kb.dma("sp", posi[:], self.pos[:, :], writes=[posi_b])
        posf, posf_b = kb.sb("posf2", [128, 32], F32)
        kb.op("dve", lambda e: e.tensor_copy(out=posf[:], in_=posi[:]), reads=[posi_b], writes=[posf_b])
        ang, ang_b = kb.sb("ang2", [128, 32, 8], F32)
        for i in range(8):
            f32 = float(np.float32(ROPE_THETA ** (-(2.0 * i) / 16.0)))
            kb.op("dve", lambda e, i=i, f32=f32: e.tensor_scalar(out=ang[:, :, i], in0=posf[:], scalar1=f32, scalar2=None, op0=ALU.mult),
                  reads=[posf_b], writes=[ang_b])
        TWO_PI = 2.0 * math.pi
        C1 = 6.28125
        C2 = TWO_PI - C1

        def reduce_sin(dst, dst_b, shift, scale):
            a2, a2_b = kb.sb("a2", [128, 256], F32)
            kf, kf_b = kb.sb("kf", [128, 256], F32)
            ki, ki_b = kb.sb("ki", [128, 256], I32)
            angf = ang[:].rearrange("p a b -> p (a b)")
            kb.op("dve", lambda e: e.tensor_scalar(out=a2[:], in0=angf, scalar1=float(shift), scalar2=None, op0=ALU.add),
                  reads=[ang_b], writes=[a2_b])
            kb.op("dve", lambda e: e.tensor_scalar(out=kf[:], in0=a2[:], scalar1=float(1.0 / TWO_PI), scalar2=None, op0=ALU.mult),
                  reads=[a2_b], writes=[kf_b])
            kb.op("dve", lambda e: e.tensor_copy(out=ki[:], in_=kf[:]), reads=[kf_b], writes=[ki_b])
            kb.op("dve", lambda e: e.tensor_copy(out=kf[:], in_=ki[:]), reads=[ki_b], writes=[kf_b])
            kb.op("dve", lambda e: e.scalar_tensor_tensor(out=a2[:], in0=kf[:], scalar=-C1, in1=a2[:], op0=ALU.mult, op1=ALU.add),
                  reads=[kf_b, a2_b], writes=[a2_b])
            kb.op("dve", lambda e: e.scalar_tensor_tensor(out=a2[:], in0=kf[:], scalar=-C2, in1=a2[:], op0=ALU.mult, op1=ALU.add),
                  reads=[kf_b, a2_b], writes=[a2_b])
            kb.op("dve", lambda e: e.tensor_scalar(out=a2[:], in0=a2[:], scalar1=float(-math.pi), scalar2=float(math.pi), op0=ALU.max, op1=ALU.min),
                  reads=[a2_b], writes=[a2_b])
            d = dst[:].rearrange("p a b -> p (a b)")
            kb.op("act", lambda e: e.activation(out=d, in_=a2[:], func=AF.Sin), reads=[a2_b], writes=[dst_b])
            if scale != 1.0:
                kb.op("dve", lambda e: e.tensor_scalar(out=d, in0=d, scalar1=float(scale), scalar2=None, op0=ALU.mult),
                      reads=[dst_b], writes=[dst_b])

        reduce_sin(*self.rot["sin"], 0.0, 1.0)
        reduce_sin(*self.rot["cos"], math.pi / 2, 1.0)
        reduce_sin(*self.rot["sinq"], 0.0, 0.125)
        reduce_sin(*self.rot["cosq"], math.pi / 2, 0.125)
        kb.barrier()
        kb.reset(m2)
        self.arena0 = m2

    def _build_mem(self):
        kb = self.kb
        kb.reset(self.arena0)
        mem = self.I("mem")
        mi = [kb.sb("memin%d" % i, [128, D], F32) for i in range(2)]
        for mt in range(2):
            t, t_b = mi[mt]
            kb.dma("sp", t[:], mem[mt * 128:(mt + 1) * 128, :], writes=[t_b])
            for half in range(2):
                ps, ps_b = kb.psum()
                for j in range(4):
                    kt = half * 4 + j
                    kb.op("pe", lambda e, ps=ps, t=t, kt=kt, j=j: e.transpose(ps[:, j * 128:(j + 1) * 128], t[:, kt * 128:(kt + 1) * 128], self.identf[:]),
                          reads=[t_b, self.identf_b], writes=[ps_b])
                kb.op("act", lambda e, ps=ps, half=half, mt=mt: e.activation(out=self.memT[:, half * 4:half * 4 + 4, mt * 128:(mt + 1) * 128],
                                                                             in_=ps[:].rearrange("p (a b) -> p a b", a=4), func=AF.Copy),
                      reads=[ps_b], writes=[self.memT_b])
        kb.barrier()
        kb.reset(self.arena0)

    def stage0(self):
        kb = self.kb
        kb.reset(self.arena0)
        self.xT, self.xT_b = kb.sb("xT", [128, 8, S], BF16)
        xT = self.xT
        xin = [kb.sb("xin%d" % i, [128, D], F32) for i in range(2)]
        stg = [kb.sb("xstg%d" % i, [128, 8, 128], F32) for i in range(2)]
        for tt in range(32):
            xi, xi_b = xin[tt % 2]
            kb.dma("sp", xi[:], self.x[tt * 128:(tt + 1) * 128, :], writes=[xi_b])
            sg, sg_b = stg[tt % 2]
            for half in range(2):
                ps, ps_b = kb.psum()
                for j in range(4):
                    kt = half * 4 + j
                    kb.op("pe", lambda e, ps=ps, xi=xi, kt=kt, j=j: e.transpose(ps[:, j * 128:(j + 1) * 128], xi[:, kt * 128:(kt + 1) * 128],
                                                                               self.identf[:]),
                          reads=[xi_b, self.identf_b], writes=[ps_b])
                pv = ps[:].rearrange("p (a b) -> p a b", a=4)
                kb.op("act", lambda e, pv=pv, half=half, tt=tt: e.activation(out=xT[:, half * 4:half * 4 + 4, tt * 128:(tt + 1) * 128], in_=pv, func=AF.Copy),
                      reads=[ps_b], writes=[self.xT_b])
                kb.op("dve", lambda e, pv=pv, half=half, sg=sg: e.tensor_copy(out=sg[:, half * 4:half * 4 + 4, :], in_=pv),
                      reads=[ps_b], writes=[sg_b])
            kb.dma("sp", self.xres.rearrange("(kt p) t -> p kt t", p=128)[:, :, tt * 128:(tt + 1) * 128], sg[:], reads=[sg_b])
        kb.dma("sp", self.xTd.rearrange("(kt p) t -> p kt t", p=128), xT[:], reads=[self.xT_b])
        kb.barrier()

    def stageA(self, l):
        kb = self.kb
        kb.reset(self.arena0)
        self.xT, self.xT_b = kb.sb("xT", [128, 8, S], BF16)
        xT, xT_b = self.xT, self.xT_b
        kb.dma("sp", xT[:], self.xTd.rearrange("(kt p) t -> p kt t", p=128), writes=[xT_b])
        w = self.I("w_in")[l]
        wv = w.rearrange("(kt p) n -> p kt n", p=128)
        wbs = [kb.sb("wA%d" % i, [128, 8, 512], BF16) for i in range(3)]
        wb8, wb8_b = kb.sb("wA8", [128, 8, 8], BF16)
        self._wi = 0

        def load_w(c0):
            wb, wb_b = wbs[self._wi % 3]
            self._wi += 1
            kb.dma("pool", wb[:], wv[:, :, c0:c0 + 512], writes=[wb_b])
            return wb, wb_b

        stg_u = [kb.sb("stgu%d" % i, [128, 512], BF16) for i in range(3)]
        stg_v = [kb.sb("stgv%d" % i, [128, 8, 65], BF16) for i in range(3)]
        for sv, sv_b in stg_v:
            kb.op("pool", lambda e, sv=sv: e.memset(sv[:], 1.0), writes=[sv_b])
        stg_r = [kb.sb("stgr%d" % i, [128, 512], BF16) for i in range(3)]
        stg_t = [kb.sb("stgt%d" % i, [128, 4, 512], BF16) for i in range(2)]
        rtmp = [kb.sb("rtmp%d" % i, [128, 8, 8], F32) for i in range(4)]

        def tok_block(c0, kind):
            wb, wb_b = load_w(c0)
            for tt in range(32):
                ps, ps_b = kb.psum()
                for kt in range(8):
                    kb.op("pe", lambda e, ps=ps, wb=wb, kt=kt, tt=tt: e.matmul(ps[:], xT[:, kt, tt * 128:(tt + 1) * 128], wb[:, kt, :],
                                                                              start=(kt == 0), stop=(kt == 7)),
                          reads=[xT_b, wb_b], writes=[ps_b])
                if kind == "u":
                    sg, sg_b = stg_u[tt % 3]
                    kb.op("act", lambda e, sg=sg, ps=ps: e.activation(out=sg[:], in_=ps[:], func=AF.Copy), reads=[ps_b], writes=[sg_b])
                    kb.dma("sp", self.u_tm[tt * 128:(tt + 1) * 128, :], sg[:], reads=[sg_b])
                elif kind in ("vd", "vf"):
                    sg, sg_b = stg_v[tt % 3]
                    kb.op("act", lambda e, sg=sg, ps=ps: e.activation(out=sg[:, :, 0:64], in_=ps[:].rearrange("p (h e) -> p h e", h=8), func=AF.Copy),
                          reads=[ps_b], writes=[sg_b])
                    dst = self.vd if kind == "vd" else self.vf
                    kb.dma("sp", dst[tt * 128:(tt + 1) * 128, :], sg[:].rearrange("p h e -> p (h e)"), reads=[sg_b])
                else:
                    isq = kind == "qd"
                    sg, sg_b = stg_r[tt % 3]
                    kb.op("act", lambda e, sg=sg, ps=ps, isq=isq: e.activation(out=sg[:], in_=ps[:], func=AF.Copy, scale=(0.125 if isq else 1.0)),
                          reads=[ps_b], writes=[sg_b])
                    cs, cs_b = self.rot["cosq" if isq else "cos"]
                    sn, sn_b = self.rot["sinq" if isq else "sin"]
                    pv = ps[:].rearrange("p (h e) -> p h e", h=8)
                    sgv = sg[:].rearrange("p (h e) -> p h e", h=8)
                    t1, t2 = pv[:, :, 0:8], pv[:, :, 8:16]
                    cb = cs[:, tt:tt + 1, :].to_broadcast([128, 8, 8])
                    sb_ = sn[:, tt:tt + 1, :].to_broadcast([128, 8, 8])
                    (ra, ra_b), (rb, rb_b), (rc, rc_b), (rd, rd_b) = rtmp
                    kb.op("dve", lambda e, ra=ra, t1=t1, cb=cb: e.tensor_tensor(out=ra[:], in0=t1, in1=cb, op=ALU.mult), reads=[ps_b, cs_b], writes=[ra_b])
                    kb.op("dve", lambda e, rb=rb, t2=t2, sb_=sb_: e.tensor_tensor(out=rb[:], in0=t2, in1=sb_, op=ALU.mult), reads=[ps_b, sn_b], writes=[rb_b])
                    kb.op("dve", lambda e, rc=rc, t2=t2, cb=cb: e.tensor_tensor(out=rc[:], in0=t2, in1=cb, op=ALU.mult), reads=[ps_b, cs_b], writes=[rc_b])
                    kb.op("dve", lambda e, rd=rd, t1=t1, sb_=sb_: e.tensor_tensor(out=rd[:], in0=t1, in1=sb_, op=ALU.mult), reads=[ps_b, sn_b], writes=[rd_b])
                    kb.op("dve", lambda e, sgv=sgv, ra=ra, rb=rb: e.tensor_tensor(out=sgv[:, :, 0:8], in0=ra[:], in1=rb[:], op=ALU.subtract),
                          reads=[ra_b, rb_b, sg_b], writes=[sg_b])
                    kb.op("dve", lambda e, sgv=sgv, rc=rc, rd=rd: e.tensor_tensor(out=sgv[:, :, 8:16], in0=rc[:], in1=rd[:], op=ALU.add),
                          reads=[rc_b, rd_b, sg_b], writes=[sg_b])
                    ps2, ps2_b = kb.psum()
                    for j in range(4):
                        kb.op("pe", lambda e, ps2=ps2, sg=sg, j=j: e.matmul(ps2[:, j * 128:(j + 1) * 128], sg[:, j * 128:(j + 1) * 128], self.identb[:],
                                                                          start=True, stop=True),
                              reads=[sg_b, self.identb_b], writes=[ps2_b])
                    g4 = tt // 4
                    tq = tt % 4
                    tg, tg_b = stg_t[g4 % 2]
                    kb.op("act", lambda e, tg=tg, ps2=ps2, tq=tq: e.activation(out=tg[:, :, tq * 128:(tq + 1) * 128], in_=ps2[:].rearrange("p (a b) -> p a b", a=4), func=AF.Copy),
                          reads=[ps2_b], writes=[tg_b])
                    if tq == 3:
                        dst = self.qdT if isq else self.kdT
                        kb.dma("sp", dst.rearrange("(a p) t -> p a t", p=128)[:, :, g4 * 512:(g4 + 1) * 512], tg[:], reads=[tg_b])

        if "u" in self.blocksA:
            tok_block(O_U, "u")
        if "qd" in self.blocksA:
            tok_block(O_QD, "qd")
            tok_block(O_KD, "kd")
        if "vd" in self.blocksA:
            tok_block(O_VD, "vd")
        if "vf" in self.blocksA:
            tok_block(O_VF, "vf")

        stg_f = [kb.sb("stgf%d" % i, [128, S], BF16) for i in range(3)]
        self._fi = 0

        def feat_block(c0, kind, dst):
            wb, wb_b = load_w(c0)
            for j in range(4):
                sg, sg_b = stg_f[self._fi % 3]
                self._fi += 1
                for tg in range(8):
                    ps, ps_b = kb.psum()
                    for kt in range(8):
                        kb.op("pe", lambda e, ps=ps, wb=wb, kt=kt, tg=tg, j=j: e.matmul(ps[:], wb[:, kt, j * 128:(j + 1) * 128], xT[:, kt, tg * 512:(tg + 1) * 512],
                                                                                      start=(kt == 0), stop=(kt == 7)),
                              reads=[xT_b, wb_b], writes=[ps_b])
                    if kind == "g":
                        kb.op("act", lambda e, sg=sg, ps=ps, tg=tg: e.activation(out=sg[:, tg * 512:(tg + 1) * 512], in_=ps[:], func=AF.Sigmoid),
                              reads=[ps_b], writes=[sg_b])
                    else:
                        sc = 0.125 if kind == "qf" else 1.0
                        if tg % 2 == 0:
                            kb.op("act", lambda e, sg=sg, ps=ps, tg=tg, sc=sc: e.activation(out=sg[:, tg * 512:(tg + 1) * 512], in_=ps[:], func=AF.Copy, scale=sc),
                                  reads=[ps_b], writes=[sg_b])
                        else:
                            kb.op("dve", lambda e, sg=sg, ps=ps, tg=tg, sc=sc: e.tensor_scalar(out=sg[:, tg * 512:(tg + 1) * 512], in0=ps[:], scalar1=sc, scalar2=None, op0=ALU.mult),
                                  reads=[ps_b], writes=[sg_b])
                kb.dma("sp", dst[j * 128:(j + 1) * 128, :], sg[:], reads=[sg_b])

        if "qf" in self.blocksA:
            feat_block(O_QF, "qf", self.qfT)
            feat_block(O_KF, "kf", self.kfT)
        if "g" in self.blocksA:
            for gb in range(6):
                feat_block(O_G + gb * 512, "g", self.gT[gb * 512:(gb + 1) * 512, :])
        if "f" in self.blocksA:
            kb.dma("pool", wb8[:], wv[:, :, O_F:O_F + 8], writes=[wb8_b])
            fs, fs_b = kb.sb("fstg", [8, S], F32)
            for tg in range(8):
                ps, ps_b = kb.psum()
                for kt in range(8):
                    kb.op("pe", lambda e, ps=ps, kt=kt, tg=tg: e.matmul(ps[0:8, :], wb8[:, kt, :], xT[:, kt, tg * 512:(tg + 1) * 512], start=(kt == 0), stop=(kt == 7)),
                          reads=[xT_b, wb8_b], writes=[ps_b])
                kb.op("dve", lambda e, ps=ps, tg=tg: e.tensor_copy(out=fs[:, tg * 512:(tg + 1) * 512], in_=ps[0:8, :]), reads=[ps_b], writes=[fs_b])
            kb.dma("sp", self.fl[:, :], fs[:], reads=[fs_b])
        kb.barrier()

    blocksA = ("u", "qd", "vd", "vf", "qf", "g", "f")

    def convert_weights(self, l):
        kb = self.kb
        li2 = l // 2

        def conv(dst, src):
            K_ = src.shape[0]
            a_n = K_ // 128
            dv = dst.rearrange("(a p) n -> p a n", p=128)
            sv = src.rearrange("(a p) n -> p a n", p=128)
            for a0 in range(0, a_n, 8):
                a1 = min(a_n, a0 + 8)
                kb.dma("pool", dv[:, a0:a1, :], sv[:, a0:a1, :], writes=[self.wcb])
        for n_ in range(3):
            conv(self.wc["w_branch"][n_], self.I("w_branch")[l][n_])
        for nm in ("w_mix_out", "w_xq", "w_xk", "w_xv", "w_xo"):
            conv(self.wc[nm], self.I(nm)[l])
        if l % 2 == 0:
            for nm in ("ffn_w_gate", "ffn_w_up", "ffn_w_down"):
                conv(self.wc[nm], self.I(nm)[li2])
        else:
            for nm in ("moe_w_gate", "moe_w_up", "moe_w_down"):
                for ex in range(NEXP):
                    conv(self.wc[nm][ex], self.I(nm)[li2][ex])

    def build(self):
        self.declare()
        self.setup()
        if "N" not in self.stages:
            self.stage0()
        if "Z" in self.stages:
            kb = self.kb
            kb.reset(self.arena0)
            zt, zt_b = kb.sb("zfill", [128, S], BF16)
            kb.op("pool", lambda e: e.memset(zt[:], 0.0), writes=[zt_b])
            for a in range(12):
                kb.dma("sp", self.ysT[a * 128:(a + 1) * 128, :], zt[:], reads=[zt_b])
            kb.barrier()
        for l in self.layers:
            if "C" in self.stages:
                self.convert_weights(l)
            if "A" in self.stages:
                self.stageA(l)
            if "1" in self.stages:
                self.stageB1(l)
            if "2" in self.stages:
                self.stageB2(l)
            if "3" in self.stages:
                self.stageB3(l)
            if "C" in self.stages:
                self.stageC(l, l == DEPTH - 1 or l == self.layers[-1])
        self.kb.barrier()
        return self.kb.finish(self.stack)


def build_program(layers=range(DEPTH), stages="A123C", debug=()):
    nc = bass.Bass("TRN2", target_bir_lowering=False)
    stack = contextlib.ExitStack()
    with stack:
        p = Prog(nc, stack, layers, stages, debug)
        n = p.build()
    return nc, p, n


INPUT_NAMES = ["x", "mem", "positions", "w_in", "b_forget", "ssm_lambda_re", "ssm_lambda_im", "ssm_log_dt",
               "ssm_b_re", "ssm_b_im", "ssm_c_re", "ssm_c_im", "ssm_d", "w_glu", "w_branch", "w_mix_out",
               "ln_mix_g", "ln_mix_b", "w_xq", "w_xk", "w_xv", "w_xo", "ln_x_g", "ln_x_b",
               "ffn_w_gate", "ffn_w_up", "ffn_w_down", "moe_w_router", "moe_b_router",
               "moe_w_gate", "moe_w_up", "moe_w_down", "ln_ffn_g", "ln_ffn_b"]


def make_in_maps(inputs, n=8, names=None):
    maps = []
    names = names or INPUT_NAMES
    shared = {k: np.ascontiguousarray(np.asarray(inputs[k])) for k in names if k not in ("x", "mem", "positions")}
    x = np.asarray(inputs["x"])
    mem = np.asarray(inputs["mem"])
    pos = np.asarray(inputs["positions"])
    for i in range(n):
        m = dict(shared)
        if "x" in names:
            m["x"] = np.ascontiguousarray(x[i])
        if "mem" in names:
            m["mem"] = np.ascontiguousarray(mem[i])
        if "positions" in names:
            m["positions"] = np.ascontiguousarray(pos[i].astype(np.int32).reshape(32, 128).T)
        maps.append(m)
    return maps


def kernel(**inputs):
    nc, p, n = build_program()
    in_maps = make_in_maps(inputs, names=list(p.inp.keys()))
    res = run_bass_kernel_spmd(nc, in_maps, core_ids=list(range(8)))
    return np.stack([np.asarray(r["out"]) for r in res.results], axis=0).astype(np.float32)


def _stageB3(self, l):
    kb = self.kb
    kb.reset(self.arena0)
    NEG = -30000.0
    mk, mk_b = kb.sb("fmask", [128, 4, 512], BF16)
    mkf, mkf_b = kb.sb("fmaskf", [128, 512], F32)
    for j in range(4):
        kb.op("pool", lambda e: e.memset(mkf[:], 0.0), writes=[mkf_b])
        kb.op("pool", lambda e, j=j: e.affine_select(out=mkf[:], in_=mkf[:], pattern=[[1, 512]], compare_op=ALU.is_ge,
                                                     fill=NEG, base=-j * 128, channel_multiplier=-1),
              reads=[mkf_b], writes=[mkf_b])
        kb.op("dve", lambda e, j=j: e.tensor_copy(out=mk[:, j, :], in_=mkf[:]), reads=[mkf_b], writes=[mk_b])
    ones, ones_b = kb.sb("fones", [128, 64], BF16)
    kb.op("pool", lambda e: e.memset(ones[:], 1.0), writes=[ones_b])
    flt, flt_b = kb.sb("flt", [8, S], F32)
    kb.dma("sp", flt[:], self.fl[:, :], writes=[flt_b])
    bf_, bf_b = kb.sb("bfg", [8, 1], F32)
    kb.dma("sp", bf_[:], self.I("b_forget")[l].rearrange("(h o) -> h o", o=1), writes=[bf_b])
    nb, nb_b = kb.sb("nbfg", [8, 1], F32)
    kb.op("dve", lambda e: e.tensor_scalar(out=nb[:], in0=bf_[:], scalar1=-1.0, scalar2=None, op0=ALU.mult), reads=[bf_b], writes=[nb_b])
    kb.op("act", lambda e: e.activation(out=flt[:], in_=flt[:], func=AF.Exp, scale=-1.0, bias=nb[:]), reads=[flt_b, nb_b], writes=[flt_b])
    kb.op("act", lambda e: e.activation(out=flt[:], in_=flt[:], func=AF.Ln, bias=1.0), reads=[flt_b], writes=[flt_b])
    onesf, onesf_b = kb.sb("onesf", [8, S], F32)
    kb.op("pool", lambda e: e.memset(onesf[:], 1.0), writes=[onesf_b])
    ncum, ncum_b = kb.sb("ncum", [8, S], F32)
    kb.op("dve", lambda e: e.tensor_tensor_scan(out=ncum[:], data0=onesf[:], data1=flt[:], initial=0.0, op0=ALU.mult, op1=ALU.add),
          reads=[onesf_b, flt_b], writes=[ncum_b])
    augk, augk_b = kb.sb("augk", [8, 6, S], BF16)
    augq, augq_b = kb.sb("augq", [8, 6, S], BF16)
    kb.op("pool", lambda e: e.memset(augk[:, 0:3, :], 1.0), writes=[augk_b])
    kb.op("pool", lambda e: e.memset(augq[:, 3:6, :], 1.0), writes=[augq_b])
    rem, rem_b = kb.sb("rem", [8, S], F32)
    kb.op("dve", lambda e: e.tensor_copy(out=augk[:, 3, :], in_=ncum[:]), reads=[ncum_b, augk_b], writes=[augk_b])
    kb.op("dve", lambda e: e.tensor_tensor(out=rem[:], in0=ncum[:], in1=augk[:, 3, :], op=ALU.subtract), reads=[ncum_b, augk_b], writes=[rem_b])
    kb.op("dve", lambda e: e.tensor_copy(out=augk[:, 4, :], in_=rem[:]), reads=[rem_b, augk_b], writes=[augk_b])
    kb.op("dve", lambda e: e.tensor_tensor(out=rem[:], in0=rem[:], in1=augk[:, 4, :], op=ALU.subtract), reads=[rem_b, augk_b], writes=[rem_b])
    kb.op("dve", lambda e: e.tensor_copy(out=augk[:, 5, :], in_=rem[:]), reads=[rem_b, augk_b], writes=[augk_b])
    kb.op("dve", lambda e: e.tensor_scalar(out=augq[:, 0:3, :], in0=augk[:, 3:6, :], scalar1=-1.0, scalar2=None, op0=ALU.mult),
          reads=[augk_b, augq_b], writes=[augq_b])
    aqd = self.scr["augq"]
    akd = self.scr["augk"]
    kb.dma("sp", aqd[:, :, :], augq[:], reads=[augq_b])
    kb.dma("sp", akd[:, :, :], augk[:], reads=[augk_b])
    kb.barrier()
    kb.reset(self.arena0 + 4 * 1024 + 2048 + 256)
    vall, vall_b = kb.sb("fvall", [128, 32, VA], BF16)
    for k0 in range(0, 32, 8):
        kb.dma("sp", vall[:, k0:k0 + 8, :], self.vf.rearrange("(kt p) c -> p kt c", p=128)[:, k0:k0 + 8, :], writes=[vall_b])
    qk = [(kb.sb("fq%d" % i, [70, S], BF16), kb.sb("fk%d" % i, [70, S], BF16)) for i in range(2)]
    pts = [kb.sb("fpt%d" % i, [128, 512], BF16) for i in range(6)]
    osb = [kb.sb("fosb%d" % i, [65, 512], F32) for i in range(2)]
    rds = [kb.sb("frd%d" % i, [65, 512], F32) for i in range(2)]
    rhl = [kb.sb("frhl%d" % i, [65, 2, 512], BF16) for i in range(2)]
    yh = [kb.sb("fyh%d" % i, [64, S], BF16) for i in range(2)]
    self._it = 0
    for h in range(8):
        (qh, qh_b), (kh, kh_b) = qk[h % 2]
        kb.dma("sp", qh[0:64, :], self.qfT[h * 64:(h + 1) * 64, :], writes=[qh_b])
        kb.dma("sp", qh[64:70, :], aqd[h], writes=[qh_b])
        kb.dma("sp", kh[0:64, :], self.kfT[h * 64:(h + 1) * 64, :], writes=[kh_b])
        kb.dma("sp", kh[64:70, :], akd[h], writes=[kh_b])
        yo, yo_b = yh[h % 2]
        items = [(g, kbi) for g in range(8) for kbi in range(4 * g + 4)]
        pend = {}
        pos = {}
        defer = []

        def emit_qk(i, qh=qh, kh=kh, qh_b=qh_b, kh_b=kh_b):
            g, kbi = items[i]
            ps, ps_b = kb.psum()
            diag = kbi >= 4 * g
            kb.op("pe", lambda e, ps=ps, kh=kh, qh=qh, kbi=kbi, g=g, diag=diag: e.matmul(ps[:], kh[0:70, kbi * 128:(kbi + 1) * 128], qh[0:70, g * 512:(g + 1) * 512],
                                                                                     start=True, stop=not diag),
                  reads=[kh_b, qh_b], writes=[ps_b])
            if diag:
                j = kbi - 4 * g
                kb.op("pe", lambda e, ps=ps, j=j: e.matmul(ps[:], self.identb[:], mk[:, j, :], start=False, stop=True),
                      reads=[self.identb_b, mk_b], writes=[ps_b])
            pt, pt_b = pts[self._it % len(pts)]
            self._it += 1
            kb.op("act", lambda e, pt=pt, ps=ps: e.activation(out=pt[:], in_=ps[:], func=AF.Exp), reads=[ps_b], writes=[pt_b])
            pend[i] = (pt, pt_b)

        def emit_pv(i, h=h, yo=yo, yo_b=yo_b):
            g, kbi = items[i]
            nkb = 4 * g + 4
            if kbi == 0:
                pos[g] = kb.psum_acc()
            po, po_b = pos[g]
            pt, pt_b = pend.pop(i)
            kb.op("pe", lambda e, po=po, pt=pt, kbi=kbi, h=h, nkb=nkb: e.matmul(po[0:65, :], vall[:, kbi, h * 65:(h + 1) * 65], pt[:],
                                                                                start=(kbi == 0), stop=(kbi == nkb - 1)),
                  reads=[vall_b, pt_b], writes=[po_b])
            if kbi == nkb - 1:
                fin = self._attn_finalize_split(po, po_b, osb[g % 2], rds[g % 2], rhl[g % 2], ones, ones_b, yo, yo_b, g)
                defer.append([3, fin])
        LA = 3
        for i in range(min(LA, len(items))):
            emit_qk(i)
        for i in range(len(items)):
            if i + LA < len(items):
                emit_qk(i + LA)
            emit_pv(i)
            for dref in list(defer):
                dref[0] -= 1
                if dref[0] <= 0:
                    dref[1]()
                    defer.remove(dref)
        for dref in defer:
            dref[1]()
        kb.dma("sp", self.ysT[2 * BW + h * 64:2 * BW + (h + 1) * 64, :], yo[:], reads=[yo_b])
    kb.barrier()


def _attn_finalize(self, po, po_b, osb_, rds_, rhl_, ones, ones_b, yo, yo_b, g):
    kb = self.kb
    (ob, ob_b), (rd, rd_b), (rh, rh_b) = osb_, rds_, rhl_
    kb.op("act", lambda e: e.activation(out=ob[:], in_=po[0:65, :], func=AF.Copy), reads=[po_b], writes=[ob_b])
    kb.op("dve", lambda e: e.reciprocal(out=rd[64:65, :], in_=ob[64:65, :]), reads=[ob_b], writes=[rd_b])
    kb.op("dve", lambda e: e.tensor_copy(out=rh[64:65, 0, :], in_=rd[64:65, :]), reads=[rd_b], writes=[rh_b])
    kb.op("dve", lambda e: e.tensor_tensor(out=rd[64:65, :], in0=rd[64:65, :], in1=rh[64:65, 0, :], op=ALU.subtract), reads=[rd_b, rh_b], writes=[rd_b])
    kb.op("dve", lambda e: e.tensor_copy(out=rh[64:65, 1, :], in_=rd[64:65, :]), reads=[rd_b, rh_b], writes=[rh_b])
    pb, pb_b = kb.psum()
    kb.op("pe", lambda e: e.matmul(pb[0:64, :], ones[64:65, 0:64], rh[64:65, 0, :], start=True, stop=False), reads=[ones_b, rh_b], writes=[pb_b])
    kb.op("pe", lambda e: e.matmul(pb[0:64, :], ones[64:65, 0:64], rh[64:65, 1, :], start=False, stop=True), reads=[ones_b, rh_b], writes=[pb_b])
    kb.op("dve", lambda e: e.tensor_tensor(out=yo[0:64, g * 512:(g + 1) * 512], in0=ob[0:64, :], in1=pb[0:64, :], op=ALU.mult),
          reads=[ob_b, pb_b], writes=[yo_b])


def _attn_finalize_split(self, po, po_b, osb_, rds_, rhl_, ones, ones_b, yo, yo_b, g):
    kb = self.kb
    (ob, ob_b), (rd, rd_b), (rh, rh_b) = osb_, rds_, rhl_
    kb.op("act", lambda e: e.activation(out=ob[:], in_=po[0:65, :], func=AF.Copy), reads=[po_b], writes=[ob_b])
    kb.op("dve", lambda e: e.reciprocal(out=rd[64:65, :], in_=ob[64:65, :]), reads=[ob_b], writes=[rd_b])
    kb.op("dve", lambda e: e.tensor_copy(out=rh[64:65, 0, :], in_=rd[64:65, :]), reads=[rd_b], writes=[rh_b])
    kb.op("dve", lambda e: e.tensor_tensor(out=rd[64:65, :], in0=rd[64:65, :], in1=rh[64:65, 0, :], op=ALU.subtract), reads=[rd_b, rh_b], writes=[rd_b])
    kb.op("dve", lambda e: e.tensor_copy(out=rh[64:65, 1, :], in_=rd[64:65, :]), reads=[rd_b, rh_b], writes=[rh_b])

    def part2():
        pb, pb_b = kb.psum()
        kb.op("pe", lambda e: e.matmul(pb[0:64, :], ones[64:65, 0:64], rh[64:65, 0, :], start=True, stop=False), reads=[ones_b, rh_b], writes=[pb_b])
        kb.op("pe", lambda e: e.matmul(pb[0:64, :], ones[64:65, 0:64], rh[64:65, 1, :], start=False, stop=True), reads=[ones_b, rh_b], writes=[pb_b])
        kb.op("dve", lambda e: e.tensor_tensor(out=yo[0:64, g * 512:(g + 1) * 512], in0=ob[0:64, :], in1=pb[0:64, :], op=ALU.mult),
              reads=[ob_b, pb_b], writes=[yo_b])
    return part2


Prog.stageB3 = _stageB3
Prog._attn_finalize = _attn_finalize
Prog._attn_finalize_split = _attn_finalize_split


def _stageB2(self, l):
    kb = self.kb
    kb.reset(self.arena0)
    NEG = -30000.0
    mk, mk_b = kb.sb("dmask", [128, 256], BF16)
    mkf, mkf_b = kb.sb("dmaskf", [128, 256], F32)
    kb.op("pool", lambda e: e.memset(mkf[:], 0.0), writes=[mkf_b])
    kb.op("pool", lambda e: e.affine_select(out=mkf[:, 0:128], in_=mkf[:, 0:128], pattern=[[1, 128]], compare_op=ALU.is_ge,
                                            fill=NEG, base=0, channel_multiplier=-1), reads=[mkf_b], writes=[mkf_b])
    kb.op("pool", lambda e: e.affine_select(out=mkf[:, 128:256], in_=mkf[:, 128:256], pattern=[[-1, 128]], compare_op=ALU.is_ge,
                                            fill=NEG, base=0, channel_multiplier=1), reads=[mkf_b], writes=[mkf_b])
    kb.op("dve", lambda e: e.tensor_copy(out=mk[:], in_=mkf[:]), reads=[mkf_b], writes=[mk_b])
    ones, ones_b = kb.sb("dones", [128, 64], BF16)
    kb.op("pool", lambda e: e.memset(ones[:], 1.0), writes=[ones_b])
    DILS = (1, 4, 16)
    vall = {}
    for d in DILS:
        t, t_b = kb.sb("dv%d" % d, [128, 32, VA], BF16)
        nm = 32 // d
        if d == 1:
            for k0 in range(0, 32, 8):
                kb.dma("sp", t[:, k0:k0 + 8, :], self.vd.rearrange("(m p) c -> p m c", p=128)[:, k0:k0 + 8, :], writes=[t_b])
        else:
            src = self.vd.rearrange("(m p r) c -> p r m c", p=128, r=d)
            for r in range(d):
                kb.dma("sp", t[:, r * nm:(r + 1) * nm, :], src[:, r, :, :], writes=[t_b])
        vall[d] = (t, t_b)
    qk = [(kb.sb("dq%d" % i, [128, S], BF16), kb.sb("dk%d" % i, [128, S], BF16)) for i in range(2)]
    pts = [kb.sb("dpt%d" % i, [128, 256], BF16) for i in range(6)]
    acc = [kb.sb("dacc%d" % i, [65, S], F32) for i in range(2)]
    osb = [kb.sb("dosb%d" % i, [65, 512], F32) for i in range(2)]
    rds = [kb.sb("drd%d" % i, [65, 512], F32) for i in range(2)]
    rhl = [kb.sb("drhl%d" % i, [65, 2, 512], BF16) for i in range(2)]
    yh = [kb.sb("dyh%d" % i, [64, S], BF16) for i in range(2)]
    self._it = 0
    for hp in range(4):
        (qt, qt_b), (kt_, kt_b) = qk[hp % 2]
        kb.dma("sp", qt[:], self.qdT[hp * 128:(hp + 1) * 128, :], writes=[qt_b])
        kb.dma("sp", kt_[:], self.kdT[hp * 128:(hp + 1) * 128, :], writes=[kt_b])
        for hh in range(2):
            h = hp * 2 + hh
            pb0 = hh * 64
            ac, ac_b = acc[h % 2]
            items = []
            for d in DILS:
                nm = 32 // d
                for r in range(d):
                    for m in range(nm):
                        items.append((d, r, m, nm))
            pend = {}
            pos = {}

            def emit_qk(i, pb0=pb0, kt_=kt_, qt=qt, kt_b=kt_b, qt_b=qt_b):
                d, r, m, nm = items[i]
                nq = 256 if m < nm - 1 else 128
                t0 = m * 128 * d + r
                ksl = slice(t0, t0 + 127 * d + 1, d)
                qsl = slice(t0, t0 + (nq - 1) * d + 1, d)
                ps, ps_b = kb.psum()
                kb.op("pe", lambda e, ps=ps, ksl=ksl, qsl=qsl, nq=nq, pb0=pb0, kt_=kt_, qt=qt: e.matmul(ps[:, 0:nq], kt_[pb0:pb0 + 64, ksl], qt[pb0:pb0 + 64, qsl], start=True, stop=False),
                      reads=[kt_b, qt_b], writes=[ps_b])
                kb.op("pe", lambda e, ps=ps, nq=nq: e.matmul(ps[:, 0:nq], self.identb[:], mk[:, 0:nq], start=False, stop=True),
                      reads=[self.identb_b, mk_b], writes=[ps_b])
                pt, pt_b = pts[self._it % len(pts)]
                self._it += 1
                kb.op("act", lambda e, pt=pt, ps=ps, nq=nq: e.activation(out=pt[:, 0:nq], in_=ps[:, 0:nq], func=AF.Exp), reads=[ps_b], writes=[pt_b])
                pend[i] = (pt, pt_b, nq, t0)

            def emit_pv(i, h=h, ac=ac, ac_b=ac_b):
                d, r, m, nm = items[i]
                vt, vt_b = vall[d]
                b = r * nm + m
                pt, pt_b, nq, t0 = pend.pop(i)
                if m == 0:
                    pos[(d, r, 0)] = kb.psum_acc()
                po, po_b = pos.pop((d, r, m))
                kb.op("pe", lambda e, po=po, vt=vt, b=b, h=h, pt=pt, m=m: e.matmul(po[0:65, 0:128], vt[:, b, h * 65:(h + 1) * 65], pt[:, 0:128], start=(m == 0), stop=True),
                      reads=[vt_b, pt_b], writes=[po_b])
                if nq == 256:
                    pos[(d, r, m + 1)] = kb.psum_acc()
                    po2, po2_b = pos[(d, r, m + 1)]
                    kb.op("pe", lambda e, po2=po2, vt=vt, b=b, h=h, pt=pt: e.matmul(po2[0:65, 0:128], vt[:, b, h * 65:(h + 1) * 65], pt[:, 128:256], start=True, stop=False),
                          reads=[vt_b, pt_b], writes=[po2_b])
                if d == 1:
                    kb.op("dve", lambda e, ac=ac, po=po, qs=slice(t0, t0 + 128): e.tensor_copy(out=ac[:, qs], in_=po[0:65, 0:128]),
                          reads=[po_b], writes=[ac_b])
                else:
                    qs = slice(t0, t0 + 127 * d + 1, d)
                    kb.op("dve", lambda e, ac=ac, po=po, qs=qs: e.tensor_tensor(out=ac[:, qs], in0=ac[:, qs], in1=po[0:65, 0:128], op=ALU.add),
                          reads=[po_b, ac_b], writes=[ac_b])
            LA = 3
            for i in range(min(LA, len(items))):
                emit_qk(i)
            for i in range(len(items)):
                if i + LA < len(items):
                    emit_qk(i + LA)
                emit_pv(i)
            yo, yo_b = yh[h % 2]
            for g in range(8):
                self._attn_finalize2(ac, ac_b, rds[g % 2], rhl[g % 2], ones, ones_b, yo, yo_b, g)
            kb.dma("sp", self.ysT[BW + h * 64:BW + (h + 1) * 64, :], yo[:], reads=[yo_b])
    kb.barrier()


def _attn_finalize2(self, ac, ac_b, rds_, rhl_, ones, ones_b, yo, yo_b, g):
    kb = self.kb
    (rd, rd_b), (rh, rh_b) = rds_, rhl_
    sl = slice(g * 512, (g + 1) * 512)
    kb.op("dve", lambda e: e.reciprocal(out=rd[64:65, :], in_=ac[64:65, sl]), reads=[ac_b], writes=[rd_b])
    kb.op("dve", lambda e: e.tensor_copy(out=rh[64:65, 0, :], in_=rd[64:65, :]), reads=[rd_b], writes=[rh_b])
    kb.op("dve", lambda e: e.tensor_tensor(out=rd[64:65, :], in0=rd[64:65, :], in1=rh[64:65, 0, :], op=ALU.subtract), reads=[rd_b, rh_b], writes=[rd_b])
    kb.op("dve", lambda e: e.tensor_copy(out=rh[64:65, 1, :], in_=rd[64:65, :]), reads=[rd_b, rh_b], writes=[rh_b])
    pb, pb_b = kb.psum()
    kb.op("pe", lambda e: e.matmul(pb[0:64, :], ones[64:65, 0:64], rh[64:65, 0, :], start=True, stop=False), reads=[ones_b, rh_b], writes=[pb_b])
    kb.op("pe", lambda e: e.matmul(pb[0:64, :], ones[64:65, 0:64], rh[64:65, 1, :], start=False, stop=True), reads=[ones_b, rh_b], writes=[pb_b])
    kb.op("dve", lambda e: e.tensor_tensor(out=yo[0:64, sl], in0=ac[0:64, sl], in1=pb[0:64, :], op=ALU.mult),
          reads=[ac_b, pb_b], writes=[yo_b])


Prog.stageB2 = _stageB2
Prog._attn_finalize2 = _attn_finalize2


def _stageB1(self, l):
    kb = self.kb
    kb.reset(self.arena0)
    PB = Buf("ssm_prep")
    idf, idf_b = self.identf, self.identf_b
    BSt, _ = kb.sb("BSt", [128, 2, 16, 2, 128], BF16)
    Gt, _ = kb.sb("Gt", [128, 2, 16, 2, 128], BF16)
    Tt, _ = kb.sb("Tt", [128, 32, 128], BF16)
    Dr, _ = kb.sb("Dr", [128, 16, 9], F32)
    Di, _ = kb.sb("Di", [128, 16, 9], F32)
    nDi, _ = kb.sb("nDi", [128, 16, 9], F32)
    m_keep = kb.mark()

    def T_(name, shape, dt_=F32):
        t, _ = kb.sb(name, shape, dt_)
        return t

    def dve(fn, extra_r=(), extra_w=()):
        kb.op("dve", fn, reads=[PB] + list(extra_r), writes=[PB] + list(extra_w))

    def tt(out, a, b, op):
        dve(lambda e: e.tensor_tensor(out=out, in0=a, in1=b, op=op))

    def ts(out, a, s1, op0, s2=None, op1=None):
        if op1 is None:
            dve(lambda e: e.tensor_scalar(out=out, in0=a, scalar1=s1, scalar2=None, op0=op0))
        else:
            dve(lambda e: e.tensor_scalar(out=out, in0=a, scalar1=s1, scalar2=s2, op0=op0, op1=op1))

    def cp(out, a):
        dve(lambda e: e.tensor_copy(out=out, in_=a))

    pp = T_("pp", [16, 3, 128])
    ld = T_("ld", [16, 2])
    kb.dma("sp", pp[:, 0, :], self.I("ssm_lambda_re")[l].rearrange("(q a) p -> q (a p)", a=2), writes=[PB])
    kb.dma("sp", pp[:, 1, :], self.I("ssm_lambda_im")[l].rearrange("(q a) p -> q (a p)", a=2), writes=[PB])
    kb.dma("sp", ld[:], self.I("ssm_log_dt")[l].rearrange("(q a) -> q a", a=2), writes=[PB])
    cp(pp[:, 2, :].rearrange("q (a p) -> q a p", a=2), ld[:].unsqueeze(2).to_broadcast([16, 2, 64]))
    par = T_("par", [128, 3, 16])
    ps, ps_b = kb.psum()
    for i in range(3):
        kb.op("pe", lambda e, i=i, ps=ps: e.transpose(ps[:, i * 16:(i + 1) * 16], pp[:, i, :], idf[0:16, 0:16]), reads=[PB, idf_b], writes=[ps_b])
    dve(lambda e, ps=ps: e.tensor_copy(out=par[:].rearrange("p a q -> p (a q)"), in_=ps[:, 0:48]), extra_r=[ps_b])
    lr, li, ldt = par[:, 0, :], par[:, 1, :], par[:, 2, :]
    Bre = T_("Bre", [128, 16, 16])
    Bim = T_("Bim", [128, 16, 16])
    for (dst, nm) in ((Bre, "ssm_b_re"), (Bim, "ssm_b_im")):
        src = self.I(nm)[l].rearrange("(q a) p c -> (a p) q c", a=2)
        for q0 in range(0, 16, 4):
            kb.dma("sp", dst[:, q0:q0 + 4, :], src[:, q0:q0 + 4, :], writes=[PB])
    Cre = T_("Cre", [128, 16, 16])
    Cim = T_("Cim", [128, 16, 16])
    Z = T_("Z", [32, 16, 128])
    zt = T_("zt", [128, 256])
    for (dst, nm) in ((Cre, "ssm_c_re"), (Cim, "ssm_c_im")):
        dve(lambda e: e.memset(Z[:], 0.0))
        src = self.I(nm)[l].rearrange("(q a) c p -> a c q p", a=2)
        kb.dma("sp", Z[0:16, :, 0:64], src[0], reads=[PB], writes=[PB])
        kb.dma("sp", Z[16:32, :, 64:128], src[1], reads=[PB], writes=[PB])
        for q in range(16):
            if q % 8 == 0:
                ps, ps_b = kb.psum()
            kb.op("pe", lambda e, ps=ps, q=q: e.transpose(ps[:, (q % 8) * 32:(q % 8) * 32 + 32], Z[:, q, :], idf[0:32, 0:32]), reads=[PB, idf_b], writes=[ps_b])
            if q % 8 == 7:
                q0 = q - 7
                dve(lambda e, ps=ps: e.tensor_copy(out=zt[:], in_=ps[:, 0:256]), extra_r=[ps_b])
                pv = zt[:].rearrange("p (q a c) -> p q a c", q=8, a=2)
                dve(lambda e, dst=dst, pv=pv, q0=q0: e.tensor_tensor(out=dst[:, q0:q0 + 8, :], in0=pv[:, :, 0, :], in1=pv[:, :, 1, :], op=ALU.add))
    def S_(name):
        return T_(name, [128, 16])
    dt = S_("dt")
    kb.op("act", lambda e: e.activation(out=dt[:], in_=ldt, func=AF.Exp), reads=[PB], writes=[PB])
    x = S_("x")
    tt(x[:], lr, dt[:], ALU.mult)
    er = S_("er")
    ts(er[:], x[:], 1.0 / 7, ALU.mult, 1.0, ALU.add)
    for k in (6, 5, 4, 3, 2, 1):
        tt(er[:], er[:], x[:], ALU.mult)
        ts(er[:], er[:], 1.0 / k, ALU.mult, 1.0, ALU.add)
    phi = S_("phi")
    tt(phi[:], li, dt[:], ALU.mult)
    ts(phi[:], phi[:], 1.0 / 32, ALU.mult)
    z = S_("z")
    tt(z[:], phi[:], phi[:], ALU.mult)
    cr = S_("cr")
    ci_ = S_("ci")
    cc = [1.0, -1.0 / 2, 1.0 / 24, -1.0 / 720, 1.0 / 40320, -1.0 / 3628800, 1.0 / 479001600]
    sc = [1.0, -1.0 / 6, 1.0 / 120, -1.0 / 5040, 1.0 / 362880, -1.0 / 39916800, 1.0 / 6227020800]
    for (dst, co) in ((cr, cc), (ci_, sc)):
        ts(dst[:], z[:], co[6], ALU.mult, co[5], ALU.add)
        for k in (4, 3, 2, 1, 0):
            tt(dst[:], dst[:], z[:], ALU.mult)
            ts(dst[:], dst[:], co[k], ALU.add)
    tt(ci_[:], ci_[:], phi[:], ALU.mult)
    t1, t2, t3, t4 = S_("t1"), S_("t2"), S_("t3"), S_("t4")

    def cmul(or_, oi, ar, ai, br, bi, a1=t1, a2=t2, a3=t3, a4=t4):
        tt(a1, ar, br, ALU.mult)
        tt(a2, ai, bi, ALU.mult)
        tt(a3, ar, bi, ALU.mult)
        tt(a4, ai, br, ALU.mult)
        tt(or_, a1, a2, ALU.subtract)
        tt(oi, a3, a4, ALU.add)

    for _ in range(5):
        cmul(cr[:], ci_[:], cr[:], ci_[:], cr[:], ci_[:], t1[:], t2[:], t3[:], t4[:])
        tt(t1[:], cr[:], cr[:], ALU.mult)
        tt(t2[:], ci_[:], ci_[:], ALU.mult)
        tt(t1[:], t1[:], t2[:], ALU.add)
        ts(t1[:], t1[:], -0.5, ALU.mult, 1.5, ALU.add)
        tt(cr[:], cr[:], t1[:], ALU.mult)
        tt(ci_[:], ci_[:], t1[:], ALU.mult)
    Pl_r = T_("Plr", [128, 9, 16])
    Pl_i = T_("Pli", [128, 9, 16])
    dve(lambda e: e.memset(Pl_r[:, 0, :], 1.0))
    dve(lambda e: e.memset(Pl_i[:, 0, :], 0.0))
    tt(Pl_r[:, 1, :], cr[:], er[:], ALU.mult)
    tt(Pl_i[:, 1, :], ci_[:], er[:], ALU.mult)
    ar, ai = Pl_r[:, 1, :], Pl_i[:, 1, :]
    for k in range(2, 9):
        cmul(Pl_r[:, k, :], Pl_i[:, k, :], Pl_r[:, k - 1, :], Pl_i[:, k - 1, :], ar, ai, t1[:], t2[:], t3[:], t4[:])
    Nl_r = T_("Nlr", [128, 8, 16])
    Nl_i = T_("Nli", [128, 8, 16])
    dve(lambda e: e.memset(Nl_r[:, 0, :], 1.0))
    dve(lambda e: e.memset(Nl_i[:, 0, :], 0.0))
    tt(t1[:], ar, ar, ALU.mult)
    tt(t2[:], ai, ai, ALU.mult)
    tt(t1[:], t1[:], t2[:], ALU.add)
    dve(lambda e: e.reciprocal(out=t1[:], in_=t1[:]))
    tt(Nl_r[:, 1, :], ar, t1[:], ALU.mult)
    tt(Nl_i[:, 1, :], ai, t1[:], ALU.mult)
    ts(Nl_i[:, 1, :], Nl_i[:, 1, :], -1.0, ALU.mult)
    for k in range(2, 8):
        cmul(Nl_r[:, k, :], Nl_i[:, k, :], Nl_r[:, k - 1, :], Nl_i[:, k - 1, :], Nl_r[:, 1, :], Nl_i[:, 1, :], t1[:], t2[:], t3[:], t4[:])
    cp(Dr[:, :, 0], Pl_r[:, 8, :])
    cp(Di[:, :, 0], Pl_i[:, 8, :])
    for k in range(1, 9):
        cmul(Dr[:, :, k], Di[:, :, k], Dr[:, :, k - 1], Di[:, :, k - 1], Dr[:, :, k - 1], Di[:, :, k - 1], t1[:], t2[:], t3[:], t4[:])
    ts(nDi[:], Di[:], -1.0, ALU.mult)
    qr, qi = S_("qr"), S_("qi")
    nr = S_("nr")
    ts(nr[:], ar, -1.0, ALU.add)
    tt(t1[:], lr, lr, ALU.mult)
    tt(t2[:], li, li, ALU.mult)
    tt(t1[:], t1[:], t2[:], ALU.add)
    dve(lambda e: e.reciprocal(out=t1[:], in_=t1[:]))
    tt(t2[:], nr[:], lr, ALU.mult)
    tt(t3[:], ai, li, ALU.mult)
    tt(t2[:], t2[:], t3[:], ALU.add)
    tt(qr[:], t2[:], t1[:], ALU.mult)
    tt(t2[:], ai, lr, ALU.mult)
    tt(t3[:], nr[:], li, ALU.mult)
    tt(t2[:], t2[:], t3[:], ALU.subtract)
    tt(qi[:], t2[:], t1[:], ALU.mult)
    Br2, Bi2 = T_("Br2", [128, 16, 16]), T_("Bi2", [128, 16, 16])
    u1, u2, u3, u4 = (T_("u%d" % i, [128, 16, 16]) for i in range(4))

    def bq(t):
        return t.unsqueeze(2).to_broadcast([128, 16, 16])
    cmul(Br2[:], Bi2[:], Bre[:], Bim[:], bq(qr[:]), bq(qi[:]), u1[:], u2[:], u3[:], u4[:])
    def Bg(name):
        return T_(name, [128, 16, 8, 16])
    g1, g2, g3, g4 = Bg("g1"), Bg("g2"), Bg("g3"), Bg("g4")
    Wm_r, Wm_i = Bg("Wmr"), Bg("Wmi")

    def over_k(t):
        return t.unsqueeze(2).to_broadcast([128, 16, 8, 16])

    def over_c(t):
        return t.rearrange("p k q -> p q k").unsqueeze(3).to_broadcast([128, 16, 8, 16])

    def over_kc(t):
        return t.unsqueeze(2).unsqueeze(3).to_broadcast([128, 16, 8, 16])

    cmul(Wm_r[:], Wm_i[:], over_k(Br2[:]), over_k(Bi2[:]), over_c(Nl_r[:, 0:8, :]), over_c(Nl_i[:, 0:8, :]), g1[:], g2[:], g3[:], g4[:])
    WmM = T_("WmM", [128, 2, 2, 16, 128], BF16)
    dve(lambda e: e.memset(WmM[:].rearrange("p a b q n -> p (a b q n)"), 0.0))
    for a in range(2):
        for part, src in ((0, Wm_r), (1, Wm_i)):
            cp(WmM[a * 64:(a + 1) * 64, a, part, :, :], src[a * 64:(a + 1) * 64].rearrange("p q k c -> p q (k c)"))
    W7_r, W7_i = Bg("W7r"), Bg("W7i")
    cmul(W7_r[:], W7_i[:], Wm_r[:], Wm_i[:], over_kc(Pl_r[:, 7, :]), over_kc(Pl_i[:, 7, :]), g1[:], g2[:], g3[:], g4[:])
    BSt_b = Buf("BSt")
    kb.op("pool", lambda e: e.memset(BSt[:].rearrange("p a q b n -> p (a q b n)"), 0.0), writes=[BSt_b])
    for part, src in ((0, W7_r), (1, W7_i)):
        for q in range(16):
            if q % 4 == 0:
                ps, ps_b = kb.psum()
            kb.op("pe", lambda e, ps=ps, q=q, src=src: e.transpose(ps[:, (q % 4) * 128:(q % 4) * 128 + 128], src[:, q].rearrange("p k c -> p (k c)"), idf[:]),
                  reads=[PB, idf_b], writes=[ps_b])
            if q % 4 == 3:
                for a in range(2):
                    pv = ps[:].rearrange("p (q n) -> p q n", q=4)[:, :, a * 64:(a + 1) * 64]
                    kb.op("act", lambda e, pv=pv, part=part, q=q, a=a: e.activation(out=BSt[:, part, q - 3:q + 1, a, a * 64:(a + 1) * 64], in_=pv, func=AF.Copy),
                          reads=[ps_b, BSt_b], writes=[BSt_b])
    Cp_r, Cp_i = Bg("Cpr"), Bg("Cpi")
    cmul(Cp_r[:], Cp_i[:], over_k(Cre[:]), over_k(Cim[:]), over_c(Pl_r[:, 0:8, :]), over_c(Pl_i[:, 0:8, :]), g1[:], g2[:], g3[:], g4[:])
    CpB = T_("CpB", [128, 2, 16, 128], BF16)
    cp(CpB[:, 0], Cp_r[:].rearrange("p q k c -> p q (k c)"))
    ts(CpB[:, 1], Cp_i[:].rearrange("p q k c -> p q (k c)"), -1.0, ALU.mult)
    G_r, G_i = W7_r, W7_i
    kb.op("dve", lambda e: e.memset(t1[:], 0.0), reads=[PB, BSt_b], writes=[PB])
    cmul(G_r[:], G_i[:], Cp_r[:], Cp_i[:], over_kc(ar), over_kc(ai), g1[:], g2[:], g3[:], g4[:])
    dve(lambda e: e.memset(Gt[:].rearrange("p a q b n -> p (a q b n)"), 0.0))
    for a in range(2):
        cp(Gt[a * 64:(a + 1) * 64, 0, :, a, :], G_r[a * 64:(a + 1) * 64].rearrange("p q k c -> p q (k c)"))
        ts(Gt[a * 64:(a + 1) * 64, 1, :, a, :], G_i[a * 64:(a + 1) * 64].rearrange("p q k c -> p q (k c)"), -1.0, ALU.mult)
    kidx_i = T_("kidx_i", [128, 1], I32)
    kidx = T_("kidx", [128, 1])
    jidx_i = T_("jidx_i", [128, 8, 16], I32)
    jidx = T_("jidx", [128, 128])
    cmask = T_("cmask", [128, 128])
    kb.op("pool", lambda e: e.iota(kidx_i[:], pattern=[[0, 1]], base=0, channel_multiplier=1), reads=[PB], writes=[PB])
    kb.op("pool", lambda e: e.iota(jidx_i[:], pattern=[[1, 8], [0, 16]], base=0, channel_multiplier=0), reads=[PB], writes=[PB])
    dve(lambda e: e.tensor_single_scalar(out=kidx_i[:], in_=kidx_i[:], scalar=4, op=ALU.arith_shift_right))
    cp(kidx[:], kidx_i[:])
    cp(jidx[:], jidx_i[:].rearrange("p a b -> p (a b)"))
    ts(cmask[:], jidx[:], kidx[:, 0:1], ALU.is_ge)
    dB = T_("dB", [128, 512])
    kb.dma("sp", dB[:], self.I("ssm_d")[l].partition_broadcast(128), writes=[PB])
    IDd = T_("IDd", [128, 32, 128], BF16)
    for g in range(32):
        dve(lambda e, g=g: e.tensor_tensor(out=IDd[:, g, :].rearrange("p (j c) -> p j c", j=8), in0=idf[:].rearrange("p (j c) -> p j c", j=8),
                                           in1=dB[:, g * 16:(g + 1) * 16].unsqueeze(1).to_broadcast([128, 8, 16]), op=ALU.mult), extra_r=[idf_b])
    Tt_b = Buf("Tt")
    for g in range(32):
        q, a = g // 2, g % 2
        if g % 4 == 0:
            ps, ps_b = kb.psum()
        sl = slice((g % 4) * 128, (g % 4) * 128 + 128)
        kb.op("pe", lambda e, ps=ps, sl=sl, q=q, a=a: e.matmul(ps[:, sl], WmM[:, a, 0, q, :], CpB[:, 0, q, :], start=True, stop=False), reads=[PB], writes=[ps_b])
        kb.op("pe", lambda e, ps=ps, sl=sl, q=q, a=a: e.matmul(ps[:, sl], WmM[:, a, 1, q, :], CpB[:, 1, q, :], start=False, stop=False), reads=[PB], writes=[ps_b])
        kb.op("pe", lambda e, ps=ps, sl=sl, g=g: e.matmul(ps[:, sl], self.identb[:], IDd[:, g, :], start=False, stop=True), reads=[PB, self.identb_b], writes=[ps_b])
        if g % 4 == 3:
            kb.op("dve", lambda e, ps=ps, g=g: e.tensor_tensor(out=Tt[:, g - 3:g + 1, :], in0=ps[:].rearrange("p (g n) -> p g n", g=4),
                                                               in1=cmask[:].unsqueeze(1).to_broadcast([128, 4, 128]), op=ALU.mult),
                  reads=[ps_b, PB], writes=[Tt_b])
    kb.barrier()
    kb.reset(m_keep)
    self._ssm_main(l, dict(BSt=BSt, Gt=Gt, Tt=Tt, Dr=Dr, Di=Di, nDi=nDi), m_keep)


def _ssm_main(self, l, tb, m0):
    kb = self.kb
    BSt, Gt, Tt, Dr, Di, nDi = tb["BSt"], tb["Gt"], tb["Tt"], tb["Dr"], tb["Di"], tb["nDi"]
    TB = Buf("ssm_tables")
    U, U_b = kb.sb("U", [128, 32, 512], BF16)
    m_x = kb.mark()
    xts = [kb.sb("Xc%d" % i, [128, 8, 512], BF16) for i in range(2)]
    x2s = [kb.sb("X2c%d" % i, [128, 32, 128], BF16) for i in range(2)]
    kb.reset(m_x)
    ygT, ygT_b = kb.sb("ygT", [128, 4, S], BF16)
    for ct in range(4):
        xt0, xt0_b = xts[ct % 2]
        kb.dma("sp", xt0[:].rearrange("p k c -> p (k c)"), self.u_tm[ct * 1024:(ct + 1) * 1024, :].rearrange("(p k) c -> p (k c)", k=8), writes=[xt0_b])
        xt, xt_b = x2s[ct % 2]
        kb.op("pool", lambda e, xt=xt, xt0=xt0: e.tensor_copy(out=xt[:].rearrange("p g (k c) -> p g k c", k=8),
                                                              in_=xt0[:].rearrange("p k (g c) -> p g k c", g=32)),
              reads=[xt0_b], writes=[xt_b])
        for g in range(32):
            if g % 4 == 0:
                ps, ps_b = kb.psum()
            kb.op("pe", lambda e, ps=ps, g=g, xt=xt: e.matmul(ps[:, (g % 4) * 128:(g % 4) * 128 + 128], xt[:, g, :], self.identb[:], start=True, stop=True),
                  reads=[xt_b, self.identb_b], writes=[ps_b])
            if g % 4 == 3:
                eng = "act" if (g // 4) % 2 == 0 else "dve"
                pv = ps[:].rearrange("p (g n) -> p g n", g=4)
                if eng == "act":
                    kb.op("act", lambda e, pv=pv, g=g, ct=ct: e.activation(out=U[:, g - 3:g + 1, ct * 128:(ct + 1) * 128], in_=pv, func=AF.Copy), reads=[ps_b], writes=[U_b])
                else:
                    kb.op("dve", lambda e, pv=pv, g=g, ct=ct: e.tensor_copy(out=U[:, g - 3:g + 1, ct * 128:(ct + 1) * 128], in_=pv), reads=[ps_b], writes=[U_b])
    Yg, Yg_b = kb.sb("Yg", [128, 4, 8, 512], BF16)
    SA = [kb.sb("SA%d" % i, [128, 2, 512], F32) for i in range(2)]
    SB_ = [kb.sb("SB%d" % i, [128, 2, 512], F32) for i in range(2)]
    Ssh = [kb.sb("Ssh%d" % i, [128, 2, 512], BF16) for i in range(2)]
    for q in range(16):
        (sa, sa_b), (sb2, sb2_b), (ssh, ssh_b) = SA[q % 2], SB_[q % 2], Ssh[q % 2]
        for part in range(2):
            ps, ps_b = kb.psum()
            for a in range(2):
                kb.op("pe", lambda e, ps=ps, part=part, a=a, q=q: e.matmul(ps[:], BSt[:, part, q, a, :], U[:, 2 * q + a, :], start=(a == 0), stop=(a == 1)),
                      reads=[U_b, TB], writes=[ps_b])
            kb.op("act", lambda e, ps=ps, part=part, sa=sa: e.activation(out=sa[:, part, :], in_=ps[:], func=AF.Copy), reads=[ps_b], writes=[sa_b])
        cur, cur_b, nxt, nxt_b = sa, sa_b, sb2, sb2_b
        for k in range(9):
            m = 1 << k
            dr, di, ndi = Dr[:, q, k:k + 1], Di[:, q, k:k + 1], nDi[:, q, k:k + 1]
            kb.op("pool", lambda e, cur=cur, nxt=nxt, m=m: e.tensor_copy(out=nxt[:, :, 0:m], in_=cur[:, :, 0:m]), reads=[cur_b], writes=[nxt_b])
            kb.op("dve", lambda e, cur=cur, nxt=nxt, m=m, dr=dr: e.scalar_tensor_tensor(out=nxt[:, 0, m:512], in0=cur[:, 0, 0:512 - m], scalar=dr, in1=cur[:, 0, m:512], op0=ALU.mult, op1=ALU.add),
                  reads=[cur_b, TB], writes=[nxt_b])
            kb.op("dve", lambda e, cur=cur, nxt=nxt, m=m, ndi=ndi: e.scalar_tensor_tensor(out=nxt[:, 0, m:512], in0=cur[:, 1, 0:512 - m], scalar=ndi, in1=nxt[:, 0, m:512], op0=ALU.mult, op1=ALU.add),
                  reads=[cur_b, nxt_b, TB], writes=[nxt_b])
            kb.op("dve", lambda e, cur=cur, nxt=nxt, m=m, di=di: e.scalar_tensor_tensor(out=nxt[:, 1, m:512], in0=cur[:, 0, 0:512 - m], scalar=di, in1=cur[:, 1, m:512], op0=ALU.mult, op1=ALU.add),
                  reads=[cur_b, TB], writes=[nxt_b])
            kb.op("dve", lambda e, cur=cur, nxt=nxt, m=m, dr=dr: e.scalar_tensor_tensor(out=nxt[:, 1, m:512], in0=cur[:, 1, 0:512 - m], scalar=dr, in1=nxt[:, 1, m:512], op0=ALU.mult, op1=ALU.add),
                  reads=[cur_b, nxt_b, TB], writes=[nxt_b])
            cur, cur_b, nxt, nxt_b = nxt, nxt_b, cur, cur_b
        kb.op("pool", lambda e, ssh=ssh: e.memset(ssh[:, :, 0:1], 0.0), writes=[ssh_b])
        kb.op("act", lambda e, ssh=ssh, cur=cur: e.activation(out=ssh[:, :, 1:512], in_=cur[:, :, 0:511], func=AF.Copy), reads=[cur_b, ssh_b], writes=[ssh_b])
        for ct in range(4):
            if ct % 2 == 0:
                ps, ps_b = kb.psum()
            o0 = (ct % 2) * 256
            csl = slice(ct * 128, (ct + 1) * 128)
            kb.op("pe", lambda e, ps=ps, o0=o0, csl=csl, ssh=ssh, q=q: e.matmul(ps[:, o0:o0 + 256], ssh[:, 0, csl], Gt[:, 0, q].rearrange("p a n -> p (a n)"), start=True, stop=False),
                  reads=[ssh_b, TB], writes=[ps_b])
            kb.op("pe", lambda e, ps=ps, o0=o0, csl=csl, ssh=ssh, q=q: e.matmul(ps[:, o0:o0 + 256], ssh[:, 1, csl], Gt[:, 1, q].rearrange("p a n -> p (a n)"), start=False, stop=False),
                  reads=[ssh_b, TB], writes=[ps_b])
            for a in range(2):
                kb.op("pe", lambda e, ps=ps, o0=o0, csl=csl, a=a, q=q: e.matmul(ps[:, o0 + a * 128:o0 + (a + 1) * 128], U[:, 2 * q + a, csl], Tt[:, 2 * q + a, :], start=False, stop=(a == 1)),
                      reads=[U_b, TB], writes=[ps_b])
            kb.op("act", lambda e, ps=ps, o0=o0, ct=ct, q=q: e.activation(out=Yg[:, ct, :, q * 32:(q + 1) * 32].rearrange("p j (a c) -> p a j c", a=2),
                                                                           in_=ps[:, o0:o0 + 256].rearrange("p (a j c) -> p a j c", a=2, j=8), func=AF.Gelu_apprx_tanh),
                  reads=[ps_b], writes=[Yg_b])
    ev = 0
    for ct in range(4):
        for cht in range(4):
            for jh in range(2):
                ps, ps_b = kb.psum()
                for jj in range(4):
                    j = jh * 4 + jj
                    kb.op("pe", lambda e, ps=ps, jj=jj, j=j, ct=ct, cht=cht: e.matmul(ps[:, jj * 128:(jj + 1) * 128], Yg[:, ct, j, cht * 128:(cht + 1) * 128], self.identb[:], start=True, stop=True),
                          reads=[Yg_b, self.identb_b], writes=[ps_b])
                t0 = ct * 1024 + jh * 4
                dst = ygT[:, cht, ct * 1024:(ct + 1) * 1024].rearrange("p (c j) -> p j c", j=8)[:, jh * 4:jh * 4 + 4, :]
                pv = ps[:].rearrange("p (j c) -> p j c", j=4)
                if ev % 2 == 0:
                    kb.op("act", lambda e, dst=dst, pv=pv: e.activation(out=dst, in_=pv, func=AF.Copy), reads=[ps_b], writes=[ygT_b])
                else:
                    kb.op("dve", lambda e, dst=dst, pv=pv: e.tensor_copy(out=dst, in_=pv), reads=[ps_b], writes=[ygT_b])
                ev += 1
    wg, wg_b = kb.sb("wglu", [128, 4, 512], BF16)
    kb.dma("pool", wg[:], self.I("w_glu")[l].rearrange("(kt p) n -> p kt n", p=128), writes=[wg_b])
    sgs = [kb.sb("sg%d" % i, [128, 512], BF16) for i in range(3)]
    yos = [kb.sb("yo%d" % i, [128, S], BF16) for i in range(2)]
    it = 0
    for mo in range(4):
        yo, yo_b = yos[mo % 2]
        for tg in range(8):
            ps, ps_b = kb.psum()
            tsl = slice(tg * 512, (tg + 1) * 512)
            for kt in range(4):
                kb.op("pe", lambda e, ps=ps, kt=kt, mo=mo, tsl=tsl: e.matmul(ps[:], wg[:, kt, mo * 128:(mo + 1) * 128], ygT[:, kt, tsl], start=(kt == 0), stop=(kt == 3)),
                      reads=[wg_b, ygT_b], writes=[ps_b])
            sg, sg_b = sgs[it % 3]
            it += 1
            kb.op("act", lambda e, sg=sg, ps=ps: e.activation(out=sg[:], in_=ps[:], func=AF.Sigmoid), reads=[ps_b], writes=[sg_b])
            kb.op("pool", lambda e, yo=yo, sg=sg, mo=mo, tsl=tsl: e.tensor_tensor(out=yo[:, tsl], in0=ygT[:, mo, tsl], in1=sg[:], op=ALU.mult),
                  reads=[ygT_b, sg_b], writes=[yo_b])
        kb.dma("sp", self.ysT[mo * 128:(mo + 1) * 128, :], yo[:], reads=[yo_b])
    kb.barrier()


Prog.stageB1 = _stageB1
Prog._ssm_main = _ssm_main


def _stageC(self, l, last):
    kb = self.kb
    kb.reset(self.arena0)
    idf, idf_b = self.identf, self.identf_b
    moe = (l % 2 == 1)
    li2 = l // 2
    ones_m, ones_m_b = kb.sb("ones_m", [128, 128], BF16)
    ones_1, ones_1_b = kb.sb("ones_1", [128, 128], BF16)
    kb.op("pool", lambda e: e.memset(ones_m[:], 1.0 / 1024), writes=[ones_m_b])
    kb.op("pool", lambda e: e.memset(ones_1[:], 1.0), writes=[ones_1_b])
    lnp, lnp_b = kb.sb("lnp", [8, 6, 128], F32)
    for i, nm in enumerate(["ln_mix_g", "ln_mix_b", "ln_x_g", "ln_x_b", "ln_ffn_g", "ln_ffn_b"]):
        kb.dma("sp", lnp[:, i, :], self.I(nm)[l].rearrange("(kt p) -> kt p", p=128), writes=[lnp_b])
    lnT, lnT_b = kb.sb("lnT", [128, 6, 8], F32)
    ps, ps_b = kb.psum()
    for i in range(6):
        kb.op("pe", lambda e, i=i, ps=ps: e.transpose(ps[:, i * 8:(i + 1) * 8], lnp[:, i, :], idf[0:8, 0:8]), reads=[lnp_b, idf_b], writes=[ps_b])
    kb.op("dve", lambda e, ps=ps: e.tensor_copy(out=lnT[:].rearrange("p a b -> p (a b)"), in_=ps[:, 0:48]), reads=[ps_b], writes=[lnT_b])
    NW = 4
    wraw = [kb.sb("wC%d" % i, [128, 4096], BF16) for i in range(NW)]
    self._wc = 0

    def wbuf():
        t = wraw[self._wc % NW]
        self._wc += 1
        return t

    def load_w(src, kt_n, ncols):
        t, t_b = wbuf()
        v = t[:, 0:kt_n * ncols].rearrange("p (k n) -> p k n", k=kt_n)
        sv = src.rearrange("(k p) n -> p k n", p=128)
        for k0 in range(0, kt_n, 8):
            k1 = min(kt_n, k0 + 8)
            kb.dma("sp", v[:, k0:k1, :], sv[:, k0:k1, :], reads=[self.wcb], writes=[t_b])
        return v, t_b

    def linear(W, kt_n, n_out, rhs_fn, rhs_bufs, consume):
        cpc = 512
        while kt_n * cpc * 2 > 8192:
            cpc //= 2
        c0 = 0
        while c0 < n_out:
            nc_ = min(cpc, n_out - c0)
            wv, w_b = load_w(W[:, c0:c0 + nc_], kt_n, nc_)
            for j in range(nc_ // 128):
                ps, ps_b = kb.psum()
                for kt in range(kt_n):
                    kb.op("pe", lambda e, ps=ps, wv=wv, kt=kt, j=j: e.matmul(ps[:], wv[:, kt, j * 128:(j + 1) * 128], rhs_fn(kt), start=(kt == 0), stop=(kt == kt_n - 1)),
                          reads=[w_b] + rhs_bufs, writes=[ps_b])
                consume((c0 // 128) + j, ps, ps_b)
            c0 += nc_

    memT, memT_b = self.memT, self.memT_b
    KT, KT_b = kb.sb("KT", [128, 8, NMEM], BF16)
    Vm, Vm_b = kb.sb("Vm", [128, 2, D], BF16)

    def cons_k(ot, ps, ps_b):
        kb.op("act", lambda e: e.activation(out=KT[:, ot, :], in_=ps[:, 0:NMEM], func=AF.Copy), reads=[ps_b], writes=[KT_b])
    def lin_k():
        W = self.wc["w_xk"]
        for c0 in (0, 512):
            wv, w_b = load_w(W[:, c0:c0 + 512], 8, 512)
            for j in range(4):
                ps, ps_b = kb.psum()
                for kt in range(8):
                    kb.op("pe", lambda e, ps=ps, wv=wv, kt=kt, j=j: e.matmul(ps[:, 0:NMEM], wv[:, kt, j * 128:(j + 1) * 128], memT[:, kt, :], start=(kt == 0), stop=(kt == 7)),
                          reads=[w_b, memT_b], writes=[ps_b])
                cons_k(c0 // 128 + j, ps, ps_b)
    lin_k()
    Wv = self.wc["w_xv"]
    for c0 in (0, 512):
        wv, w_b = load_w(Wv[:, c0:c0 + 512], 8, 512)
        for mt in range(2):
            ps, ps_b = kb.psum()
            for kt in range(8):
                kb.op("pe", lambda e, ps=ps, wv=wv, kt=kt, mt=mt: e.matmul(ps[:], memT[:, kt, mt * 128:(mt + 1) * 128], wv[:, kt, :], start=(kt == 0), stop=(kt == 7)),
                      reads=[w_b, memT_b], writes=[ps_b])
            kb.op("act", lambda e, ps=ps, mt=mt, c0=c0: e.activation(out=Vm[:, mt, c0:c0 + 512], in_=ps[:], func=AF.Copy), reads=[ps_b], writes=[Vm_b])
    if moe:
        wr32, wr32_b = kb.sb("wr32", [128, 8, 8], F32)
        kb.dma("sp", wr32[:], self.I("moe_w_router")[li2].rearrange("(k p) e -> p k e", p=128), writes=[wr32_b])
        wrh, wrh_b = kb.sb("wrh", [128, 2, 8, 8], BF16)
        wrr, wrr_b = kb.sb("wrr", [128, 8, 8], F32)
        kb.op("dve", lambda e: e.tensor_copy(out=wrh[:, 0], in_=wr32[:]), reads=[wr32_b], writes=[wrh_b])
        kb.op("dve", lambda e: e.tensor_tensor(out=wrr[:], in0=wr32[:], in1=wrh[:, 0], op=ALU.subtract), reads=[wr32_b, wrh_b], writes=[wrr_b])
        kb.op("dve", lambda e: e.tensor_copy(out=wrh[:, 1], in_=wrr[:]), reads=[wrr_b, wrh_b], writes=[wrh_b])
        brB, brB_b = kb.sb("brB", [128, 8], F32)
        kb.dma("sp", brB[:], self.I("moe_b_router")[li2].partition_broadcast(128), writes=[brB_b])
        sel, sel_b = kb.sb("sel", [8, 8, 128], BF16)
        self_f, self_f_b = kb.sb("self", [8, 8, 128], F32)
        kb.op("pool", lambda e: e.memset(self_f[:], 1.0), writes=[self_f_b])
        kb.op("pool", lambda e: e.affine_select(out=self_f[:], in_=self_f[:], pattern=[[1, 8], [0, 128]], compare_op=ALU.is_equal, fill=0.0, base=0, channel_multiplier=-1),
              reads=[self_f_b], writes=[self_f_b])
        kb.op("dve", lambda e: e.tensor_copy(out=sel[:], in_=self_f[:]), reads=[self_f_b], writes=[sel_b])
    xr, xr_b = kb.sb("xr", [128, 8, 512], F32)
    vv, vv_b = kb.sb("vv", [128, 8, 512], F32)
    GB = [kb.sb("G%d" % i, [128, 8, 512], BF16) for i in range(4)]
    m_u = kb.mark()
    ys, ys_b = kb.sb("ysg", [128, 12, 512], BF16)
    gt, gt_b = kb.sb("gtg", [128, 24, 512], BF16)
    kb.reset(m_u)
    nft = 11 if moe else 22
    hh, hh_b = kb.sb("hh", [128, nft, 512], BF16)
    if moe:
        macc, macc_b = kb.sb("macc", [128, 8, 512], F32)
    kb.reset(m_u + 36 * 1024)
    PT = [kb.sb("PT%d" % i, [128, 2, 512], BF16) for i in range(2)]
    tmpf = [kb.sb("tmpf%d" % i, [128, 512], F32) for i in range(4)]
    tmpb = [kb.sb("tmpb%d" % i, [128, 512], BF16) for i in range(3)]
    st_mean, st_mean_b = kb.sb("st_mean", [128, 512], F32)
    st_rstd, st_rstd_b = kb.sb("st_rstd", [128, 512], F32)
    if moe:
        lg, lg_b = kb.sb("lg", [128, 4, 8], F32)
        gsm = {n_: kb.sb("g_" + n_, [128, 4, 8], F32) for n_ in ("eq1", "eq2", "lg2", "gate", "gr")}
        gs1 = {n_: kb.sb("s_" + n_, [128, 4], F32) for n_ in ("m1", "m2", "w1", "w2")}
        gTs, gTs_b = kb.sb("gTs", [8, 512], F32)
        gTh, gTh_b = kb.sb("gTh", [8, 2, 512], BF16)
        gTr, gTr_b = kb.sb("gTr", [8, 512], F32)
        xlo, xlo_b = kb.sb("xlo", [128, 8, 512], BF16)
        gbs, gbs_b = kb.sb("gbs", [128, 512], F32)
    if last:
        ostg = [kb.sb("ostg%d" % i, [128, D], F32) for i in range(2)]
    self._ti = 0

    def tf():
        t = tmpf[self._ti % 4]
        self._ti += 1
        return t

    self._tb = 0

    def tbf():
        t = tmpb[self._tb % 3]
        self._tb += 1
        return t

    def layer_norm(gi):
        (vb, vb_b), (vsq, vsq_b) = GB[1], GB[2]
        kb.op("act", lambda e: e.activation(out=vb[:], in_=vv[:], func=AF.Copy), reads=[vv_b], writes=[vb_b])
        kb.op("act", lambda e: e.activation(out=vsq[:], in_=vv[:], func=AF.Square), reads=[vv_b], writes=[vsq_b])
        pm, pm_b = kb.psum()
        for kt in range(8):
            kb.op("pe", lambda e, kt=kt: e.matmul(pm[:], ones_m[:], vb[:, kt, :], start=(kt == 0), stop=(kt == 7)), reads=[ones_m_b, vb_b], writes=[pm_b])
        pq, pq_b = kb.psum()
        for kt in range(8):
            kb.op("pe", lambda e, kt=kt: e.matmul(pq[:], ones_m[:], vsq[:, kt, :], start=(kt == 0), stop=(kt == 7)), reads=[ones_m_b, vsq_b], writes=[pq_b])
        kb.op("act", lambda e: e.activation(out=st_mean[:], in_=pm[:], func=AF.Copy), reads=[pm_b], writes=[st_mean_b])
        (m2, m2_b) = tf()
        kb.op("dve", lambda e: e.tensor_tensor(out=m2[:], in0=st_mean[:], in1=st_mean[:], op=ALU.mult), reads=[st_mean_b], writes=[m2_b])
        kb.op("dve", lambda e: e.tensor_tensor(out=m2[:], in0=pq[:], in1=m2[:], op=ALU.subtract), reads=[pq_b, m2_b], writes=[m2_b])
        kb.op("dve", lambda e: e.tensor_scalar(out=m2[:], in0=m2[:], scalar1=0.0, scalar2=LN_EPS, op0=ALU.max, op1=ALU.add), reads=[m2_b], writes=[m2_b])
        kb.op("act", lambda e: e.activation(out=m2[:], in_=m2[:], func=AF.Sqrt), reads=[m2_b], writes=[m2_b])
        kb.op("dve", lambda e: e.reciprocal(out=st_rstd[:], in_=m2[:]), reads=[m2_b], writes=[st_rstd_b])
        for kt in range(8):
            eng = "dve" if kt % 2 == 0 else "pool"
            kb.op(eng, lambda e, kt=kt: e.tensor_tensor(out=vv[:, kt, :], in0=vv[:, kt, :], in1=st_mean[:], op=ALU.subtract), reads=[vv_b, st_mean_b], writes=[vv_b])
            kb.op(eng, lambda e, kt=kt: e.tensor_tensor(out=vv[:, kt, :], in0=vv[:, kt, :], in1=st_rstd[:], op=ALU.mult), reads=[vv_b, st_rstd_b], writes=[vv_b])
            kb.op(eng, lambda e, kt=kt: e.tensor_scalar(out=xr[:, kt, :], in0=vv[:, kt, :], scalar1=lnT[:, gi, kt:kt + 1], scalar2=lnT[:, gi + 1, kt:kt + 1], op0=ALU.mult, op1=ALU.add),
                  reads=[vv_b, lnT_b], writes=[xr_b])

    def resid_consume(ot, ps, ps_b):
        kb.op("dve", lambda e: e.scalar_tensor_tensor(out=vv[:, ot, :], in0=xr[:, ot, :], scalar=float(ALPHA), in1=ps[:], op0=ALU.mult, op1=ALU.add),
              reads=[xr_b, ps_b], writes=[vv_b])

    xres_v = self.xres.rearrange("(kt p) t -> p kt t", p=128)
    xTd_v = self.xTd.rearrange("(kt p) t -> p kt t", p=128)
    for tg in range(8):
        tsl = slice(tg * 512, (tg + 1) * 512)
        kb.dma("pool", xr[:], xres_v[:, :, tsl], writes=[xr_b])
        ysv = self.ysT.rearrange("(a p) t -> p a t", p=128)
        for a0 in range(0, 12, 6):
            kb.dma("pool", ys[:, a0:a0 + 6, :], ysv[:, a0:a0 + 6, tsl], writes=[ys_b, hh_b])
        gv = self.gT.rearrange("(a p) t -> p a t", p=128)
        for a0 in range(0, 24, 6):
            kb.dma("pool", gt[:, a0:a0 + 6, :], gv[:, a0:a0 + 6, tsl], writes=[gt_b, hh_b] + ([macc_b] if moe else []))
        mg, mg_b = GB[0]
        wbr = []
        for n_ in range(3):
            wbr.append(load_w(self.wc["w_branch"][n_], 4, D))
        for dt_ in range(8):
            pss = []
            for n_ in range(3):
                wv, w_b = wbr[n_]
                ps, ps_b = kb.psum()
                for ck in range(4):
                    kb.op("pe", lambda e, ps=ps, wv=wv, ck=ck, n_=n_, dt_=dt_: e.matmul(ps[:], wv[:, ck, dt_ * 128:(dt_ + 1) * 128], ys[:, n_ * 4 + ck, :], start=(ck == 0), stop=(ck == 3)),
                          reads=[w_b, ys_b], writes=[ps_b])
                pss.append((ps, ps_b))
            (a0_, a0_b), (a1_, a1_b), (a2_, a2_b) = tf(), tf(), tf()
            kb.op("dve", lambda e, p=pss[0][0], dt_=dt_, a0_=a0_: e.tensor_tensor(out=a0_[:], in0=p[:], in1=gt[:, dt_, :], op=ALU.mult), reads=[pss[0][1], gt_b], writes=[a0_b])
            kb.op("dve", lambda e, p=pss[1][0], dt_=dt_, a1_=a1_: e.tensor_tensor(out=a1_[:], in0=p[:], in1=gt[:, 8 + dt_, :], op=ALU.mult), reads=[pss[1][1], gt_b], writes=[a1_b])
            kb.op("dve", lambda e, p=pss[2][0], dt_=dt_, a2_=a2_: e.tensor_tensor(out=a2_[:], in0=p[:], in1=gt[:, 16 + dt_, :], op=ALU.mult), reads=[pss[2][1], gt_b], writes=[a2_b])
            kb.op("pool", lambda e, a0_=a0_, a1_=a1_: e.tensor_tensor(out=a0_[:], in0=a0_[:], in1=a1_[:], op=ALU.add), reads=[a0_b, a1_b], writes=[a0_b])
            kb.op("pool", lambda e, a0_=a0_, a2_=a2_, dt_=dt_: e.tensor_tensor(out=mg[:, dt_, :], in0=a0_[:], in1=a2_[:], op=ALU.add), reads=[a0_b, a2_b], writes=[mg_b])
        linear(self.wc["w_mix_out"], 8, D, lambda kt: mg[:, kt, :], [mg_b], resid_consume)
        layer_norm(0)
        xb, xb_b = GB[3]
        kb.op("act", lambda e: e.activation(out=xb[:], in_=xr[:], func=AF.Copy), reads=[xr_b], writes=[xb_b])
        qT, qT_b = GB[0]

        def cons_q(ot, ps, ps_b):
            kb.op("act", lambda e: e.activation(out=qT[:, ot, :], in_=ps[:], func=AF.Copy, scale=1.0 / 16), reads=[ps_b], writes=[qT_b])
        linear(self.wc["w_xq"], 8, D, lambda kt: xb[:, kt, :], [xb_b], cons_q)
        oT, oT_b = GB[1]
        for h in range(4):
            pt, pt_b = PT[h % 2]
            for mt in range(2):
                ps, ps_b = kb.psum()
                for ee in range(2):
                    et = 2 * h + ee
                    kb.op("pe", lambda e, ps=ps, et=et, mt=mt, ee=ee: e.matmul(ps[:], KT[:, et, mt * 128:(mt + 1) * 128], qT[:, et, :], start=(ee == 0), stop=(ee == 1)),
                          reads=[KT_b, qT_b], writes=[ps_b])
                kb.op("act", lambda e, ps=ps, pt=pt, mt=mt: e.activation(out=pt[:, mt, :], in_=ps[:], func=AF.Exp), reads=[ps_b], writes=[pt_b])
            pd, pd_b = kb.psum()
            for mt in range(2):
                kb.op("pe", lambda e, pt=pt, mt=mt, pd=pd: e.matmul(pd[:], ones_1[:], pt[:, mt, :], start=(mt == 0), stop=(mt == 1)), reads=[ones_1_b, pt_b], writes=[pd_b])
            rd, rd_b = tf()
            kb.op("dve", lambda e, rd=rd, pd=pd: e.reciprocal(out=rd[:], in_=pd[:]), reads=[pd_b], writes=[rd_b])
            for ee in range(2):
                et = 2 * h + ee
                ps, ps_b = kb.psum()
                for mt in range(2):
                    kb.op("pe", lambda e, ps=ps, et=et, mt=mt, pt=pt: e.matmul(ps[:], Vm[:, mt, et * 128:(et + 1) * 128], pt[:, mt, :], start=(mt == 0), stop=(mt == 1)),
                          reads=[Vm_b, pt_b], writes=[ps_b])
                kb.op("dve", lambda e, ps=ps, et=et, rd=rd: e.tensor_tensor(out=oT[:, et, :], in0=ps[:], in1=rd[:], op=ALU.mult), reads=[ps_b, rd_b], writes=[oT_b])
        linear(self.wc["w_xo"], 8, D, lambda kt: oT[:, kt, :], [oT_b], resid_consume)
        layer_norm(2)
        kb.op("act", lambda e: e.activation(out=xb[:], in_=xr[:], func=AF.Copy), reads=[xr_b], writes=[xb_b])
        def swiglu(Wg, Wu, Wd, nft_, gate_sb=None, down_consume=None):
            gl = {}

            def cons_g(ot, ps, ps_b):
                sg, sg_b = tbf()
                kb.op("act", lambda e: e.activation(out=sg[:], in_=ps[:], func=AF.Silu), reads=[ps_b], writes=[sg_b])
                gl[ot] = (sg, sg_b)

            def cons_u(ot, ps, ps_b):
                sg, sg_b = gl[ot]
                if gate_sb is None:
                    kb.op("dve", lambda e: e.tensor_tensor(out=hh[:, ot, :], in0=ps[:], in1=sg[:], op=ALU.mult), reads=[ps_b, sg_b], writes=[hh_b])
                else:
                    t_, t_b = tf()
                    kb.op("dve", lambda e: e.tensor_tensor(out=t_[:], in0=ps[:], in1=sg[:], op=ALU.mult), reads=[ps_b, sg_b], writes=[t_b])
                    kb.op("pool", lambda e: e.tensor_tensor(out=hh[:, ot, :], in0=t_[:], in1=gate_sb[0][:], op=ALU.mult), reads=[t_b, gate_sb[1]], writes=[hh_b])
            for f0 in range(0, nft_, 3):
                f1 = min(nft_, f0 + 3)
                n_c = (f1 - f0) * 128
                for (W, cons) in ((Wg, cons_g), (Wu, cons_u)):
                    wv, w_b = load_w(W[:, f0 * 128:f0 * 128 + n_c], 8, n_c)
                    for j in range(f1 - f0):
                        ps, ps_b = kb.psum()
                        for kt in range(8):
                            kb.op("pe", lambda e, ps=ps, wv=wv, kt=kt, j=j: e.matmul(ps[:], wv[:, kt, j * 128:(j + 1) * 128], xb[:, kt, :], start=(kt == 0), stop=(kt == 7)),
                                  reads=[w_b, xb_b], writes=[ps_b])
                        cons(f0 + j, ps, ps_b)
            linear(Wd, nft_, D, lambda kt: hh[:, kt, :], [hh_b], down_consume)

        if not moe:
            swiglu(self.wc["ffn_w_gate"], self.wc["ffn_w_up"], self.wc["ffn_w_down"], 22, None, resid_consume)
        else:
            kb.op("dve", lambda e: e.tensor_tensor(out=vv[:], in0=xr[:], in1=xb[:], op=ALU.subtract), reads=[xr_b, xb_b], writes=[vv_b])
            kb.op("act", lambda e: e.activation(out=xlo[:], in_=vv[:], func=AF.Copy), reads=[vv_b], writes=[xlo_b])
            pl, pl_b = kb.psum()
            for tt in range(4):
                tk = slice(tt * 128, (tt + 1) * 128)
                combos = [(xb, xb_b, 0), (xb, xb_b, 1), (xlo, xlo_b, 0)]
                n_mm = 0
                for (xs, xs_b, wpart) in combos:
                    for kt in range(8):
                        kb.op("pe", lambda e, xs=xs, wpart=wpart, kt=kt, tk=tk, tt=tt, n_mm=n_mm, pl=pl: e.matmul(pl[:, tt * 8:(tt + 1) * 8], xs[:, kt, tk], wrh[:, wpart, kt, :], start=(n_mm == 0), stop=(n_mm == 23)),
                              reads=[xs_b, wrh_b], writes=[pl_b])
                        n_mm += 1
            kb.op("dve", lambda e, pl=pl: e.tensor_tensor(out=lg[:], in0=pl[:, 0:32].rearrange("p (t e) -> p t e", t=4), in1=brB[:].unsqueeze(1).to_broadcast([128, 4, 8]), op=ALU.add),
                  reads=[pl_b, brB_b], writes=[lg_b])
            GQ = Buf("gateq")
            eq1, eq2, lg2, gate, gr = (gsm[n_][0] for n_ in ("eq1", "eq2", "lg2", "gate", "gr"))
            m1, m2_, w1, w2 = (gs1[n_][0] for n_ in ("m1", "m2", "w1", "w2"))

            def gd(fn, eng="dve"):
                kb.op(eng, fn, reads=[GQ, lg_b], writes=[GQ])
            gd(lambda e: e.tensor_reduce(out=m1[:], in_=lg[:], axis=AX.X, op=ALU.max))
            gd(lambda e: e.tensor_tensor(out=eq1[:], in0=lg[:], in1=m1[:].unsqueeze(2).to_broadcast([128, 4, 8]), op=ALU.is_equal))
            gd(lambda e: e.scalar_tensor_tensor(out=lg2[:], in0=eq1[:], scalar=-1.0e30, in1=lg[:], op0=ALU.mult, op1=ALU.add))
            gd(lambda e: e.tensor_reduce(out=m2_[:], in_=lg2[:], axis=AX.X, op=ALU.max))
            gd(lambda e: e.tensor_tensor(out=eq2[:], in0=lg2[:], in1=m2_[:].unsqueeze(2).to_broadcast([128, 4, 8]), op=ALU.is_equal))
            gd(lambda e: e.tensor_tensor(out=w2[:], in0=m1[:], in1=m2_[:], op=ALU.subtract))
            gd(lambda e: e.activation(out=w1[:], in_=w2[:], func=AF.Sigmoid), "act")
            gd(lambda e: e.tensor_scalar(out=w2[:], in0=w1[:], scalar1=-1.0, scalar2=1.0, op0=ALU.mult, op1=ALU.add))
            gd(lambda e: e.tensor_tensor(out=gate[:], in0=eq1[:], in1=w1[:].unsqueeze(2).to_broadcast([128, 4, 8]), op=ALU.mult))
            gd(lambda e: e.tensor_tensor(out=gr[:], in0=eq2[:], in1=w2[:].unsqueeze(2).to_broadcast([128, 4, 8]), op=ALU.mult))
            gd(lambda e: e.tensor_tensor(out=gate[:], in0=gate[:], in1=gr[:], op=ALU.add))
            pg, pg_b = kb.psum()
            for tt in range(4):
                kb.op("pe", lambda e, tt=tt, pg=pg: e.transpose(pg[0:8, tt * 128:(tt + 1) * 128], gate[:, tt, :], idf[:]), reads=[GQ, idf_b], writes=[pg_b])
            kb.op("dve", lambda e, pg=pg: e.tensor_copy(out=gTs[:], in_=pg[0:8, :]), reads=[pg_b], writes=[gTs_b])
            kb.op("dve", lambda e: e.tensor_copy(out=gTh[:, 0, :], in_=gTs[:]), reads=[gTs_b], writes=[gTh_b])
            kb.op("dve", lambda e: e.tensor_tensor(out=gTr[:], in0=gTs[:], in1=gTh[:, 0, :], op=ALU.subtract), reads=[gTs_b, gTh_b], writes=[gTr_b])
            kb.op("dve", lambda e: e.tensor_copy(out=gTh[:, 1, :], in_=gTr[:]), reads=[gTr_b, gTh_b], writes=[gTh_b])
            for ex in range(NEXP):
                pgb, pgb_b = kb.psum()
                for part in range(2):
                    kb.op("pe", lambda e, ex=ex, part=part, pgb=pgb: e.matmul(pgb[:], sel[:, ex, :], gTh[:, part, :], start=(part == 0), stop=(part == 1)),
                          reads=[sel_b, gTh_b], writes=[pgb_b])
                kb.op("act", lambda e, pgb=pgb: e.activation(out=gbs[:], in_=pgb[:], func=AF.Copy), reads=[pgb_b], writes=[gbs_b])

                def dcons(ot, ps, ps_b, ex=ex):
                    if ex == 0:
                        kb.op("dve", lambda e: e.tensor_copy(out=macc[:, ot, :], in_=ps[:]), reads=[ps_b], writes=[macc_b])
                    elif ex < NEXP - 1:
                        kb.op("dve", lambda e: e.tensor_tensor(out=macc[:, ot, :], in0=macc[:, ot, :], in1=ps[:], op=ALU.add), reads=[ps_b, macc_b], writes=[macc_b])
                    else:
                        t_, t_b = tf()
                        kb.op("dve", lambda e: e.tensor_tensor(out=t_[:], in0=macc[:, ot, :], in1=ps[:], op=ALU.add), reads=[ps_b, macc_b], writes=[t_b])
                        kb.op("dve", lambda e: e.scalar_tensor_tensor(out=vv[:, ot, :], in0=xr[:, ot, :], scalar=float(ALPHA), in1=t_[:], op0=ALU.mult, op1=ALU.add),
                              reads=[xr_b, t_b], writes=[vv_b])
                swiglu(self.wc["moe_w_gate"][ex], self.wc["moe_w_up"][ex], self.wc["moe_w_down"][ex], 11, (gbs, gbs_b), dcons)
        layer_norm(4)
        kb.dma("pool", xres_v[:, :, tsl], xr[:], reads=[xr_b])
        kb.op("act", lambda e: e.activation(out=xb[:], in_=xr[:], func=AF.Copy), reads=[xr_b], writes=[xb_b])
        kb.dma("pool", xTd_v[:, :, tsl], xb[:], reads=[xb_b])
        if last:
            for tt in range(4):
                og, og_b = ostg[tt % 2]
                for half in range(2):
                    ps, ps_b = kb.psum()
                    for j in range(4):
                        kt = half * 4 + j
                        kb.op("pe", lambda e, ps=ps, kt=kt, j=j, tt=tt: e.transpose(ps[:, j * 128:(j + 1) * 128], xr[:, kt, tt * 128:(tt + 1) * 128], idf[:]),
                              reads=[xr_b, idf_b], writes=[ps_b])
                    kb.op("act", lambda e, ps=ps, og=og, half=half: e.activation(out=og[:, half * 512:(half + 1) * 512], in_=ps[:], func=AF.Copy), reads=[ps_b], writes=[og_b])
                r0 = tg * 512 + tt * 128
                kb.dma("sp", self.out[r0:r0 + 128, :], og[:], reads=[og_b])
    kb.barrier()


Prog.stageC = _stageC
```

```python
import contextlib
import math

import numpy as np
import concourse.bass as bass
import concourse.mybir as mybir
from concourse.bass_utils import run_bass_kernel_spmd

F32 = mybir.dt.float32
BF16 = mybir.dt.bfloat16
I32 = mybir.dt.int32
AF = mybir.ActivationFunctionType
ALU = mybir.AluOpType
AX = mybir.AxisListType

CENG = ("pe", "act", "dve", "pool", "sp")

D = 1024
S = 4096
DEPTH = 4
NIN = 6664
BW = 512
DFF = 2816
NEXP = 8
DFE = 1408
NMEM = 256
ALPHA = (2 * DEPTH) ** 0.25
LN_EPS = 1e-5
ROPE_THETA = 500000.0
O_U, O_QD, O_KD, O_VD, O_QF, O_KF, O_VF, O_F, O_G = 0, 512, 1024, 1536, 2048, 2560, 3072, 3584, 3592
VA = 520


class Buf:
    __slots__ = ("name", "w", "r", "excl")

    def __init__(self, name="", excl=False):
        self.name = name
        self.w = None
        self.r = {}
        self.excl = excl


class Op:
    __slots__ = ("eng", "fn", "waits", "sig", "count", "dma")

    def __init__(self, eng, fn, dma=None):
        self.eng = eng
        self.fn = fn
        self.waits = []
        self.sig = False
        self.count = 0
        self.dma = dma


class KB:
    SB_LO = 16640
    SB_HI = 229376

    def __init__(self, nc, n_dma_slots=12):
        self.nc = nc
        self.ops = {e: [] for e in CENG}
        self.seen_c = {e: {s: -1 for s in CENG} for e in CENG}
        self.seen_d = {e: {} for e in CENG}
        self.dq = {"sp": [], "pool": [], "act": []}
        self.slot_cnt = {}
        self.slot_rr = {"sp": 0, "pool": 0, "act": 0}
        for q in self.dq:
            for i in range(n_dma_slots if q != "act" else 4):
                sid = (q, i)
                self.dq[q].append(sid)
                self.slot_cnt[sid] = 0
        self.sb_off = self.SB_LO
        self.n_alloc = 0
        self.ps = []
        self.ps_rr = 0
        self.pa_rr = 0

    def sb(self, name, shape, dtype):
        esz = {F32: 4, BF16: 2, I32: 4}[dtype]
        n = esz
        for d in shape[1:]:
            n *= d
        n = (n + 63) // 64 * 64
        off = self.sb_off
        assert off + n <= self.SB_HI, "SBUF overflow at %s: %d + %d" % (name, off, n)
        self.sb_off += n
        self.n_alloc += 1
        t = self.nc.alloc_sbuf_tensor_at("%s_%d" % (name, self.n_alloc), list(shape), dtype, offset=off)
        return t, Buf(name)

    def mark(self):
        return self.sb_off

    def reset(self, m):
        self.sb_off = m

    def psum(self):
        p = self.ps[self.ps_rr % 5]
        self.ps_rr += 1
        return p

    def psum_acc(self):
        p = self.ps[5 + self.pa_rr % 3]
        self.pa_rr += 1
        return p

    def _collect(self, eng, reads, writes, is_dma):
        ev = []
        for b in reads:
            if b.w is not None:
                ev.append(b.w)
            if b.excl:
                for k, e in b.r.items():
                    if e[0] == "d" or e[1] != eng:
                        ev.append(e)
        for b in writes:
            if b.w is not None:
                if is_dma or b.w[0] == "d" or b.w[1] != eng or eng != "pe":
                    ev.append(b.w)
            for k, e in b.r.items():
                if is_dma or e[0] == "d" or e[1] != eng or eng != "pe":
                    ev.append(e)
        return ev

    def _reduce(self, eng, evs):
        best_c = {}
        best_d = {}
        for e in evs:
            if e[0] == "c":
                if e[2] > best_c.get(e[1], -1):
                    best_c[e[1]] = e[2]
            else:
                if e[2] > best_d.get(e[1], -1):
                    best_d[e[1]] = e[2]
        out = []
        for s, i in best_c.items():
            if i > self.seen_c[eng][s]:
                self.seen_c[eng][s] = i
                out.append(("c", s, i))
                self.ops[s][i].sig = True
        for sl, v in best_d.items():
            if v > self.seen_d[eng].get(sl, 0):
                self.seen_d[eng][sl] = v
                out.append(("d", sl, v))
        return out

    def _update(self, ev, reads, writes, rkey):
        for b in reads:
            b.r[rkey] = ev
        for b in writes:
            b.w = ev
            b.r = {}

    def op(self, eng, fn, reads=(), writes=()):
        o = Op(eng, fn)
        evs = self._collect(eng, reads, writes, False)
        o.waits = self._reduce(eng, evs)
        idx = len(self.ops[eng])
        self.ops[eng].append(o)
        self._update(("c", eng, idx), reads, writes, eng)
        return o

    def dma(self, q, out, in_, reads=(), writes=(), **kw):
        slots = self.dq[q]
        sid = slots[self.slot_rr[q] % len(slots)]
        self.slot_rr[q] += 1
        evs = self._collect(q, reads, writes, True)
        if self.slot_cnt[sid] > 0:
            evs.append(("d", sid, self.slot_cnt[sid] * 16))
        self.slot_cnt[sid] += 1
        val = self.slot_cnt[sid] * 16
        o = Op(q, lambda e: e.dma_start(out=out, in_=in_, **kw), dma=(sid, val))
        o.waits = self._reduce(q, evs)
        self.ops[q].append(o)
        self._update(("d", sid, val), reads, writes, ("d", sid))
        return o

    def barrier(self):
        evs = []
        for e in CENG:
            if e != "dve":
                for i in range(len(self.ops[e]) - 1, -1, -1):
                    if self.ops[e][i].dma is None and self.ops[e][i].fn is not None:
                        evs.append(("c", e, i))
                        break
        dev = [("d", sid, c * 16) for sid, c in self.slot_cnt.items() if c > 0]
        tok = self.bar_tile
        o = Op("dve", lambda e: e.memset(tok[:], 0.0))
        evs += self._collect("dve", [], [self.bar_buf], False)
        o.waits = self._reduce("dve", evs + dev)
        idx = len(self.ops["dve"])
        self.ops["dve"].append(o)
        self._update(("c", "dve", idx), [], [self.bar_buf], "dve")
        for e in CENG:
            if e == "dve":
                continue
            o2 = Op(e, None)
            o2.waits = self._reduce(e, [("c", "dve", idx)] + dev)
            self.ops[e].append(o2)

    def finish(self, stack):
        nc = self.nc
        for e in CENG:
            c = 0
            for o in self.ops[e]:
                if o.sig:
                    c += 1
                    o.count = c
        sems = {e: stack.enter_context(nc.semaphore("s_" + e)) for e in CENG}
        dsems = {}
        for q, slots in self.dq.items():
            for sid in slots:
                if self.slot_cnt[sid] > 0:
                    dsems[sid] = stack.enter_context(nc.semaphore("d_%s%d" % sid))
        block = stack.enter_context(nc.Block())
        ops = self.ops

        def replay(ename):
            def run(eng):
                for o in ops[ename]:
                    for w in o.waits:
                        if w[0] == "c":
                            eng.wait_ge(sems[w[1]], ops[w[1]][w[2]].count)
                        else:
                            eng.wait_ge(dsems[w[1]], w[2])
                    if o.fn is None:
                        continue
                    ins = o.fn(eng)
                    if o.dma is not None:
                        ins.then_inc(dsems[o.dma[0]], 16)
                    elif o.sig:
                        ins.then_inc(sems[ename], 1)
            return run

        block.tensor(replay("pe"))
        block.scalar(replay("act"))
        block.vector(replay("dve"))
        block.gpsimd(replay("pool"))
        block.sync(replay("sp"))
        return {e: len(ops[e]) for e in CENG}


class Prog:
    def __init__(self, nc, stack, layers=range(DEPTH), stages="ABC", debug=()):
        self.nc = nc
        self.stack = stack
        self.kb = KB(nc)
        self.layers = list(layers)
        self.stages = stages
        self.debug = set(debug)
        self.inp = {}
        self.scr = {}

    def din(self, name, shape, dtype=F32):
        t = self.nc.dram_tensor(name, list(shape), dtype, kind="ExternalInput").ap()
        self.inp[name] = t
        return t

    def dscr(self, name, shape, dtype):
        kind = "ExternalOutput" if name in self.debug else "Internal"
        t = self.nc.dram_tensor(name, list(shape), dtype, kind=kind).ap()
        self.scr[name] = t
        return t

    SHAPES = {
        "x": ([S, D], F32), "mem": ([NMEM, D], F32), "positions": ([128, 32], I32),
        "w_in": ([DEPTH, D, NIN], F32), "b_forget": ([DEPTH, 8], F32),
        "ssm_lambda_re": ([DEPTH, 32, 64], F32), "ssm_lambda_im": ([DEPTH, 32, 64], F32), "ssm_log_dt": ([DEPTH, 32], F32),
        "ssm_b_re": ([DEPTH, 32, 64, 16], F32), "ssm_b_im": ([DEPTH, 32, 64, 16], F32),
        "ssm_c_re": ([DEPTH, 32, 16, 64], F32), "ssm_c_im": ([DEPTH, 32, 16, 64], F32),
        "ssm_d": ([DEPTH, 512], F32), "w_glu": ([DEPTH, 512, 512], F32), "w_branch": ([DEPTH, 3, 512, D], F32),
        "w_mix_out": ([DEPTH, D, D], F32), "ln_mix_g": ([DEPTH, D], F32), "ln_mix_b": ([DEPTH, D], F32),
        "w_xq": ([DEPTH, D, D], F32), "w_xk": ([DEPTH, D, D], F32), "w_xv": ([DEPTH, D, D], F32), "w_xo": ([DEPTH, D, D], F32),
        "ln_x_g": ([DEPTH, D], F32), "ln_x_b": ([DEPTH, D], F32),
        "ffn_w_gate": ([2, D, DFF], F32), "ffn_w_up": ([2, D, DFF], F32), "ffn_w_down": ([2, DFF, D], F32),
        "moe_w_router": ([2, D, NEXP], F32), "moe_b_router": ([2, NEXP], F32),
        "moe_w_gate": ([2, NEXP, D, DFE], F32), "moe_w_up": ([2, NEXP, D, DFE], F32), "moe_w_down": ([2, NEXP, DFE, D], F32),
        "ln_ffn_g": ([DEPTH, D], F32), "ln_ffn_b": ([DEPTH, D], F32),
    }

    def I(self, name):
        if name not in self.inp:
            shp, dt_ = self.SHAPES[name]
            self.din(name, shp, dt_)
        return self.inp[name]

    def declare(self):
        self.x = self.I("x")
        self.pos = self.I("positions")
        self.out = self.nc.dram_tensor("out", [S, D], F32, kind="ExternalOutput").ap()
        self.xres = self.dscr("xres", [D, S], F32)
        self.xTd = self.dscr("xTd", [D, S], BF16)
        self.u_tm = self.dscr("u_tm", [S, 512], BF16)
        self.qdT = self.dscr("qdT", [512, S], BF16)
        self.kdT = self.dscr("kdT", [512, S], BF16)
        self.vd = self.dscr("vd", [S, VA], BF16)
        self.qfT = self.dscr("qfT", [512, S], BF16)
        self.kfT = self.dscr("kfT", [512, S], BF16)
        self.vf = self.dscr("vf", [S, VA], BF16)
        self.fl = self.dscr("fl", [8, S], F32)
        self.gT = self.dscr("gT", [3 * D, S], BF16)
        self.ysT = self.dscr("ysT", [3 * BW, S], BF16)
        self.wcb = Buf("wconv")
        self.wc = {
            "w_branch": self.dscr("c_wbr", [3, 512, D], BF16), "w_mix_out": self.dscr("c_wo", [D, D], BF16),
            "w_xq": self.dscr("c_wq", [D, D], BF16), "w_xk": self.dscr("c_wk", [D, D], BF16),
            "w_xv": self.dscr("c_wv", [D, D], BF16), "w_xo": self.dscr("c_wxo", [D, D], BF16),
            "ffn_w_gate": self.dscr("c_fg", [D, DFF], BF16), "ffn_w_up": self.dscr("c_fu", [D, DFF], BF16), "ffn_w_down": self.dscr("c_fd", [DFF, D], BF16),
            "moe_w_gate": self.dscr("c_mg", [NEXP, D, DFE], BF16), "moe_w_up": self.dscr("c_mu", [NEXP, D, DFE], BF16), "moe_w_down": self.dscr("c_md", [NEXP, DFE, D], BF16),
        }
        self.dscr("augq", [8, 6, S], BF16)
        self.dscr("augk", [8, 6, S], BF16)

    def setup(self):
        kb, nc = self.kb, self.nc
        st = self.stack
        kb.ps = [(st.enter_context(nc.psum_tensor("ps%d" % i, [128, 512], F32)), Buf("ps%d" % i, True)) for i in range(8)]
        bar, bar_b = kb.sb("bar", [128, 8], F32)
        kb.bar_tile = bar
        kb.bar_buf = bar_b
        self.identf, self.identf_b = kb.sb("identf", [128, 128], F32)
        self.identb, self.identb_b = kb.sb("identb", [128, 128], BF16)
        idf, idb = self.identf, self.identb
        kb.op("pool", lambda e: e.memset(idf[:], 1.0), writes=[self.identf_b])
        kb.op("pool", lambda e: e.affine_select(out=idf[:], in_=idf[:], pattern=[[1, 128]], compare_op=ALU.is_equal,
                                                fill=0.0, base=0, channel_multiplier=-1),
              reads=[self.identf_b], writes=[self.identf_b])
        kb.op("dve", lambda e: e.tensor_copy(out=idb[:], in_=idf[:]), reads=[self.identf_b], writes=[self.identb_b])
        self.rot = {}
        for n_ in ["cos", "sin", "cosq", "sinq"]:
            self.rot[n_] = kb.sb("rot_" + n_, [128, 32, 8], F32)
        self.memT, self.memT_b = kb.sb("memT", [128, 8, NMEM], BF16)
        self._build_rot(kb.mark())
        self._build_mem()

    def _build_rot(self, m2):
        kb = self.kb
        posi, posi_b = kb.sb("posi2", [128, 32], I32)
        kb.dma("sp", posi[:], self.pos[:, :], writes=[posi_b])
        posf, posf_b = kb.sb("posf2", [128, 32], F32)
        kb.op("dve", lambda e: e.tensor_copy(out=posf[:], in_=posi[:]), reads=[posi_b], writes=[posf_b])
        ang, ang_b = kb.sb("ang2", [128, 32, 8], F32)
        for i in range(8):
            f32 = float(np.float32(ROPE_THETA ** (-(2.0 * i) / 16.0)))
            kb.op("dve", lambda e, i=i, f32=f32: e.tensor_scalar(out=ang[:, :, i], in0=posf[:], scalar1=f32, scalar2=None, op0=ALU.mult),
                  reads=[posf_b], writes=[ang_b])
        TWO_PI = 2.0 * math.pi
        C1 = 6.28125
        C2 = TWO_PI - C1

        def reduce_sin(dst, dst_b, shift, scale):
            a2, a2_b = kb.sb("a2", [128, 256], F32)
            kf, kf_b = kb.sb("kf", [128, 256], F32)
            ki, ki_b = kb.sb("ki", [128, 256], I32)
            angf = ang[:].rearrange("p a b -> p (a b)")
            kb.op("dve", lambda e: e.tensor_scalar(out=a2[:], in0=angf, scalar1=float(shift), scalar2=None, op0=ALU.add),
                  reads=[ang_b], writes=[a2_b])
            kb.op("dve", lambda e: e.tensor_scalar(out=kf[:], in0=a2[:], scalar1=float(1.0 / TWO_PI), scalar2=None, op0=ALU.mult),
                  reads=[a2_b], writes=[kf_b])
            kb.op("dve", lambda e: e.tensor_copy(out=ki[:], in_=kf[:]), reads=[kf_b], writes=[ki_b])
            kb.op("dve", lambda e: e.tensor_copy(out=kf[:], in_=ki[:]), reads=[ki_b], writes=[kf_b])
            kb.op("dve", lambda e: e.scalar_tensor_tensor(out=a2[:], in0=kf[:], scalar=-C1, in1=a2[:], op0=ALU.mult, op1=ALU.add),
                  reads=[kf_b, a2_b], writes=[a2_b])
            kb.op("dve", lambda e: e.scalar_tensor_tensor(out=a2[:], in0=kf[:], scalar=-C2, in1=a2[:], op0=ALU.mult, op1=ALU.add),
                  reads=[kf_b, a2_b], writes=[a2_b])
            kb.op("dve", lambda e: e.tensor_scalar(out=a2[:], in0=a2[:], scalar1=float(-math.pi), scalar2=float(math.pi), op0=ALU.max, op1=ALU.min),
                  reads=[a2_b], writes=[a2_b])
            d = dst[:].rearrange("p a b -> p (a b)")
            kb.op("act", lambda e: e.activation(out=d, in_=a2[:], func=AF.Sin), reads=[a2_b], writes=[dst_b])
            if scale != 1.0:
                kb.op("dve", lambda e: e.tensor_scalar(out=d, in0=d, scalar1=float(scale), scalar2=None, op0=ALU.mult),
                      reads=[dst_b], writes=[dst_b])

        reduce_sin(*self.rot["sin"], 0.0, 1.0)
        reduce_sin(*self.rot["cos"], math.pi / 2, 1.0)
        reduce_sin(*self.rot["sinq"], 0.0, 0.125)
        reduce_sin(*self.rot["cosq"], math.pi / 2, 0.125)
        kb.barrier()
        kb.reset(m2)
        self.arena0 = m2

    def _build_mem(self):
        kb = self.kb
        kb.reset(self.arena0)
        mem = self.I("mem")
        mi = [kb.sb("memin%d" % i, [128, D], F32) for i in range(2)]
        for mt in range(2):
            t, t_b = mi[mt]
            kb.dma("sp", t[:], mem[mt * 128:(mt + 1) * 128, :], writes=[t_b])
            for half in range(2):
                ps, ps_b = kb.psum()
                for j in range(4):
                    kt = half * 4 + j
                    kb.op("pe", lambda e, ps=ps, t=t, kt=kt, j=j: e.transpose(ps[:, j * 128:(j + 1) * 128], t[:, kt * 128:(kt + 1) * 128], self.identf[:]),
                          reads=[t_b, self.identf_b], writes=[ps_b])
                kb.op("act", lambda e, ps=ps, half=half, mt=mt: e.activation(out=self.memT[:, half * 4:half * 4 + 4, mt * 128:(mt + 1) * 128],
                                                                             in_=ps[:].rearrange("p (a b) -> p a b", a=4), func=AF.Copy),
                      reads=[ps_b], writes=[self.memT_b])
        kb.barrier()
        kb.reset(self.arena0)

    def stage0(self):
        kb = self.kb
        kb.reset(self.arena0)
        self.xT, self.xT_b = kb.sb("xT", [128, 8, S], BF16)
        xT = self.xT
        xin = [kb.sb("xin%d" % i, [128, D], F32) for i in range(2)]
        stg = [kb.sb("xstg%d" % i, [128, 8, 128], F32) for i in range(2)]
        for tt in range(32):
            xi, xi_b = xin[tt % 2]
            kb.dma("sp", xi[:], self.x[tt * 128:(tt + 1) * 128, :], writes=[xi_b])
            sg, sg_b = stg[tt % 2]
            for half in range(2):
                ps, ps_b = kb.psum()
                for j in range(4):
                    kt = half * 4 + j
                    kb.op("pe", lambda e, ps=ps, xi=xi, kt=kt, j=j: e.transpose(ps[:, j * 128:(j + 1) * 128], xi[:, kt * 128:(kt + 1) * 128],
                                                                               self.identf[:]),
                          reads=[xi_b, self.identf_b], writes=[ps_b])
                pv = ps[:].rearrange("p (a b) -> p a b", a=4)
                kb.op("act", lambda e, pv=pv, half=half, tt=tt: e.activation(out=xT[:, half * 4:half * 4 + 4, tt * 128:(tt + 1) * 128], in_=pv, func=AF.Copy),
                      reads=[ps_b], writes=[self.xT_b])
                kb.op("dve", lambda e, pv=pv, half=half, sg=sg: e.tensor_copy(out=sg[:, half * 4:half * 4 + 4, :], in_=pv),
                      reads=[ps_b], writes=[sg_b])
            kb.dma("sp", self.xres.rearrange("(kt p) t -> p kt t", p=128)[:, :, tt * 128:(tt + 1) * 128], sg[:], reads=[sg_b])
        kb.dma("sp", self.xTd.rearrange("(kt p) t -> p kt t", p=128), xT[:], reads=[self.xT_b])
        kb.barrier()

    def stageA(self, l):
        kb = self.kb
        kb.reset(self.arena0)
        self.xT, self.xT_b = kb.sb("xT", [128, 8, S], BF16)
        xT, xT_b = self.xT, self.xT_b
        kb.dma("sp", xT[:], self.xTd.rearrange("(kt p) t -> p kt t", p=128), writes=[xT_b])
        w = self.I("w_in")[l]
        wv = w.rearrange("(kt p) n -> p kt n", p=128)
        wbs = [kb.sb("wA%d" % i, [128, 8, 512], BF16) for i in range(3)]
        wb8, wb8_b = kb.sb("wA8", [128, 8, 8], BF16)
        self._wi = 0

        def load_w(c0):
            wb, wb_b = wbs[self._wi % 3]
            self._wi += 1
            kb.dma("pool", wb[:], wv[:, :, c0:c0 + 512], writes=[wb_b])
            return wb, wb_b

        stg_u = [kb.sb("stgu%d" % i, [128, 512], BF16) for i in range(3)]
        stg_v = [kb.sb("stgv%d" % i, [128, 8, 65], BF16) for i in range(3)]
        for sv, sv_b in stg_v:
            kb.op("pool", lambda e, sv=sv: e.memset(sv[:], 1.0), writes=[sv_b])
        stg_r = [kb.sb("stgr%d" % i, [128, 512], BF16) for i in range(3)]
        stg_t = [kb.sb("stgt%d" % i, [128, 4, 512], BF16) for i in range(2)]
        rtmp = [kb.sb("rtmp%d" % i, [128, 8, 8], F32) for i in range(4)]

        def tok_block(c0, kind):
            wb, wb_b = load_w(c0)
            for tt in range(32):
                ps, ps_b = kb.psum()
                for kt in range(8):
                    kb.op("pe", lambda e, ps=ps, wb=wb, kt=kt, tt=tt: e.matmul(ps[:], xT[:, kt, tt * 128:(tt + 1) * 128], wb[:, kt, :],
                                                                              start=(kt == 0), stop=(kt == 7)),
                          reads=[xT_b, wb_b], writes=[ps_b])
                if kind == "u":
                    sg, sg_b = stg_u[tt % 3]
                    kb.op("act", lambda e, sg=sg, ps=ps: e.activation(out=sg[:], in_=ps[:], func=AF.Copy), reads=[ps_b], writes=[sg_b])
                    kb.dma("sp", self.u_tm[tt * 128:(tt + 1) * 128, :], sg[:], reads=[sg_b])
                elif kind in ("vd", "vf"):
                    sg, sg_b = stg_v[tt % 3]
                    kb.op("act", lambda e, sg=sg, ps=ps: e.activation(out=sg[:, :, 0:64], in_=ps[:].rearrange("p (h e) -> p h e", h=8), func=AF.Copy),
                          reads=[ps_b], writes=[sg_b])
                    dst = self.vd if kind == "vd" else self.vf
                    kb.dma("sp", dst[tt * 128:(tt + 1) * 128, :], sg[:].rearrange("p h e -> p (h e)"), reads=[sg_b])
                else:
                    isq = kind == "qd"
                    sg, sg_b = stg_r[tt % 3]
                    kb.op("act", lambda e, sg=sg, ps=ps, isq=isq: e.activation(out=sg[:], in_=ps[:], func=AF.Copy, scale=(0.125 if isq else 1.0)),
                          reads=[ps_b], writes=[sg_b])
                    cs, cs_b = self.rot["cosq" if isq else "cos"]
                    sn, sn_b = self.rot["sinq" if isq else "sin"]
                    pv = ps[:].rearrange("p (h e) -> p h e", h=8)
                    sgv = sg[:].rearrange("p (h e) -> p h e", h=8)
                    t1, t2 = pv[:, :, 0:8], pv[:, :, 8:16]
                    cb = cs[:, tt:tt + 1, :].to_broadcast([128, 8, 8])
                    sb_ = sn[:, tt:tt + 1, :].to_broadcast([128, 8, 8])
                    (ra, ra_b), (rb, rb_b), (rc, rc_b), (rd, rd_b) = rtmp
                    kb.op("dve", lambda e, ra=ra, t1=t1, cb=cb: e.tensor_tensor(out=ra[:], in0=t1, in1=cb, op=ALU.mult), reads=[ps_b, cs_b], writes=[ra_b])
                    kb.op("dve", lambda e, rb=rb, t2=t2, sb_=sb_: e.tensor_tensor(out=rb[:], in0=t2, in1=sb_, op=ALU.mult), reads=[ps_b, sn_b], writes=[rb_b])
                    kb.op("dve", lambda e, rc=rc, t2=t2, cb=cb: e.tensor_tensor(out=rc[:], in0=t2, in1=cb, op=ALU.mult), reads=[ps_b, cs_b], writes=[rc_b])
                    kb.op("dve", lambda e, rd=rd, t1=t1, sb_=sb_: e.tensor_tensor(out=rd[:], in0=t1, in1=sb_, op=ALU.mult), reads=[ps_b, sn_b], writes=[rd_b])
                    kb.op("dve", lambda e, sgv=sgv, ra=ra, rb=rb: e.tensor_tensor(out=sgv[:, :, 0:8], in0=ra[:], in1=rb[:], op=ALU.subtract),
                          reads=[ra_b, rb_b, sg_b], writes=[sg_b])
                    kb.op("dve", lambda e, sgv=sgv, rc=rc, rd=rd: e.tensor_tensor(out=sgv[:, :, 8:16], in0=rc[:], in1=rd[:], op=ALU.add),
                          reads=[rc_b, rd_b, sg_b], writes=[sg_b])
                    ps2, ps2_b = kb.psum()
                    for j in range(4):
                        kb.op("pe", lambda e, ps2=ps2, sg=sg, j=j: e.matmul(ps2[:, j * 128:(j + 1) * 128], sg[:, j * 128:(j + 1) * 128], self.identb[:],
                                                                          start=True, stop=True),
                              reads=[sg_b, self.identb_b], writes=[ps2_b])
                    g4 = tt // 4
                    tq = tt % 4
                    tg, tg_b = stg_t[g4 % 2]
                    kb.op("act", lambda e, tg=tg, ps2=ps2, tq=tq: e.activation(out=tg[:, :, tq * 128:(tq + 1) * 128], in_=ps2[:].rearrange("p (a b) -> p a b", a=4), func=AF.Copy),
                          reads=[ps2_b], writes=[tg_b])
                    if tq == 3:
                        dst = self.qdT if isq else self.kdT
                        kb.dma("sp", dst.rearrange("(a p) t -> p a t", p=128)[:, :, g4 * 512:(g4 + 1) * 512], tg[:], reads=[tg_b])

        if "u" in self.blocksA:
            tok_block(O_U, "u")
        if "qd" in self.blocksA:
            tok_block(O_QD, "qd")
            tok_block(O_KD, "kd")
        if "vd" in self.blocksA:
            tok_block(O_VD, "vd")
        if "vf" in self.blocksA:
            tok_block(O_VF, "vf")

        stg_f = [kb.sb("stgf%d" % i, [128, S], BF16) for i in range(3)]
        self._fi = 0

        def feat_block(c0, kind, dst):
            wb, wb_b = load_w(c0)
            for j in range(4):
                sg, sg_b = stg_f[self._fi % 3]
                self._fi += 1
                for tg in range(8):
                    ps, ps_b = kb.psum()
                    for kt in range(8):
                        kb.op("pe", lambda e, ps=ps, wb=wb, kt=kt, tg=tg, j=j: e.matmul(ps[:], wb[:, kt, j * 128:(j + 1) * 128], xT[:, kt, tg * 512:(tg + 1) * 512],
                                                                                      start=(kt == 0), stop=(kt == 7)),
                              reads=[xT_b, wb_b], writes=[ps_b])
                    if kind == "g":
                        kb.op("act", lambda e, sg=sg, ps=ps, tg=tg: e.activation(out=sg[:, tg * 512:(tg + 1) * 512], in_=ps[:], func=AF.Sigmoid),
                              reads=[ps_b], writes=[sg_b])
                    else:
                        sc = 0.125 if kind == "qf" else 1.0
                        if tg % 2 == 0:
                            kb.op("act", lambda e, sg=sg, ps=ps, tg=tg, sc=sc: e.activation(out=sg[:, tg * 512:(tg + 1) * 512], in_=ps[:], func=AF.Copy, scale=sc),
                                  reads=[ps_b], writes=[sg_b])
                        else:
                            kb.op("dve", lambda e, sg=sg, ps=ps, tg=tg, sc=sc: e.tensor_scalar(out=sg[:, tg * 512:(tg + 1) * 512], in0=ps[:], scalar1=sc, scalar2=None, op0=ALU.mult),
                                  reads=[ps_b], writes=[sg_b])
                kb.dma("sp", dst[j * 128:(j + 1) * 128, :], sg[:], reads=[sg_b])

        if "qf" in self.blocksA:
            feat_block(O_QF, "qf", self.qfT)
            feat_block(O_KF, "kf", self.kfT)
        if "g" in self.blocksA:
            for gb in range(6):
                feat_block(O_G + gb * 512, "g", self.gT[gb * 512:(gb + 1) * 512, :])
        if "f" in self.blocksA:
            kb.dma("pool", wb8[:], wv[:, :, O_F:O_F + 8], writes=[wb8_b])
            fs, fs_b = kb.sb("fstg", [8, S], F32)
            for tg in range(8):
                ps, ps_b = kb.psum()
                for kt in range(8):
                    kb.op("pe", lambda e, ps=ps, kt=kt, tg=tg: e.matmul(ps[0:8, :], wb8[:, kt, :], xT[:, kt, tg * 512:(tg + 1) * 512], start=(kt == 0), stop=(kt == 7)),
                          reads=[xT_b, wb8_b], writes=[ps_b])
                kb.op("dve", lambda e, ps=ps, tg=tg: e.tensor_copy(out=fs[:, tg * 512:(tg + 1) * 512], in_=ps[0:8, :]), reads=[ps_b], writes=[fs_b])
            kb.dma("sp", self.fl[:, :], fs[:], reads=[fs_b])
        kb.barrier()

    blocksA = ("u", "qd", "vd", "vf", "qf", "g", "f")

    def convert_weights(self, l):
        kb = self.kb
        li2 = l // 2

        def conv(dst, src):
            K_ = src.shape[0]
            a_n = K_ // 128
            dv = dst.rearrange("(a p) n -> p a n", p=128)
            sv = src.rearrange("(a p) n -> p a n", p=128)
            for a0 in range(0, a_n, 8):
                a1 = min(a_n, a0 + 8)
                kb.dma("pool", dv[:, a0:a1, :], sv[:, a0:a1, :], writes=[self.wcb])
        for n_ in range(3):
            conv(self.wc["w_branch"][n_], self.I("w_branch")[l][n_])
        for nm in ("w_mix_out", "w_xq", "w_xk", "w_xv", "w_xo"):
            conv(self.wc[nm], self.I(nm)[l])
        if l % 2 == 0:
            for nm in ("ffn_w_gate", "ffn_w_up", "ffn_w_down"):
                conv(self.wc[nm], self.I(nm)[li2])
        else:
            for nm in ("moe_w_gate", "moe_w_up", "moe_w_down"):
                for ex in range(NEXP):
                    conv(self.wc[nm][ex], self.I(nm)[li2][ex])

    def build(self):
        self.declare()
        self.setup()
        if "N" not in self.stages:
            self.stage0()
        if "Z" in self.stages:
            kb = self.kb
            kb.reset(self.arena0)
            zt, zt_b = kb.sb("zfill", [128, S], BF16)
            kb.op("pool", lambda e: e.memset(zt[:], 0.0), writes=[zt_b])
            for a in range(12):
                kb.dma("sp", self.ysT[a * 128:(a + 1) * 128, :], zt[:], reads=[zt_b])
            kb.barrier()
        for l in self.layers:
            if "C" in self.stages:
                self.convert_weights(l)
            if "A" in self.stages:
                self.stageA(l)
            if "1" in self.stages:
                self.stageB1(l)
            if "2" in self.stages:
                self.stageB2(l)
            if "3" in self.stages:
                self.stageB3(l)
            if "C" in self.stages:
                self.stageC(l, l == DEPTH - 1 or l == self.layers[-1])
        self.kb.barrier()
        return self.kb.finish(self.stack)


def build_program(layers=range(DEPTH), stages="A123C", debug=()):
    nc = bass.Bass("TRN2", target_bir_lowering=False)
    stack = contextlib.ExitStack()
    with stack:
        p = Prog(nc, stack, layers, stages, debug)
        n = p.build()
    return nc, p, n


INPUT_NAMES = ["x", "mem", "positions", "w_in", "b_forget", "ssm_lambda_re", "ssm_lambda_im", "ssm_log_dt",
               "ssm_b_re", "ssm_b_im", "ssm_c_re", "ssm_c_im", "ssm_d", "w_glu", "w_branch", "w_mix_out",
               "ln_mix_g", "ln_mix_b", "w_xq", "w_xk", "w_xv", "w_xo", "ln_x_g", "ln_x_b",
               "ffn_w_gate", "ffn_w_up", "ffn_w_down", "moe_w_router", "moe_b_router",
               "moe_w_gate", "moe_w_up", "moe_w_down", "ln_ffn_g", "ln_ffn_b"]


def make_in_maps(inputs, n=8, names=None):
    maps = []
    names = names or INPUT_NAMES
    shared = {k: np.ascontiguousarray(np.asarray(inputs[k])) for k in names if k not in ("x", "mem", "positions")}
    x = np.asarray(inputs["x"])
    mem = np.asarray(inputs["mem"])
    pos = np.asarray(inputs["positions"])
    for i in range(n):
        m = dict(shared)
        if "x" in names:
            m["x"] = np.ascontiguousarray(x[i])
        if "mem" in names:
            m["mem"] = np.ascontiguousarray(mem[i])
        if "positions" in names:
            m["positions"] = np.ascontiguousarray(pos[i].astype(np.int32).reshape(32, 128).T)
        maps.append(m)
    return maps


def kernel(**inputs):
    nc, p, n = build_program()
    in_maps = make_in_maps(inputs, names=list(p.inp.keys()))
    res = run_bass_kernel_spmd(nc, in_maps, core_ids=list(range(8)))
    return np.stack([np.asarray(r["out"]) for r in res.results], axis=0).astype(np.float32)


def _stageB3(self, l):
    kb = self.kb
    kb.reset(self.arena0)
    NEG = -30000.0
    mk, mk_b = kb.sb("fmask", [128, 4, 512], BF16)
    mkf, mkf_b = kb.sb("fmaskf", [128, 512], F32)
    for j in range(4):
        kb.op("pool", lambda e: e.memset(mkf[:], 0.0), writes=[mkf_b])
        kb.op("pool", lambda e, j=j: e.affine_select(out=mkf[:], in_=mkf[:], pattern=[[1, 512]], compare_op=ALU.is_ge,
                                                     fill=NEG, base=-j * 128, channel_multiplier=-1),
              reads=[mkf_b], writes=[mkf_b])
        kb.op("dve", lambda e, j=j: e.tensor_copy(out=mk[:, j, :], in_=mkf[:]), reads=[mkf_b], writes=[mk_b])
    ones, ones_b = kb.sb("fones", [128, 64], BF16)
    kb.op("pool", lambda e: e.memset(ones[:], 1.0), writes=[ones_b])
    flt, flt_b = kb.sb("flt", [8, S], F32)
    kb.dma("sp", flt[:], self.fl[:, :], writes=[flt_b])
    bf_, bf_b = kb.sb("bfg", [8, 1], F32)
    kb.dma("sp", bf_[:], self.I("b_forget")[l].rearrange("(h o) -> h o", o=1), writes=[bf_b])
    nb, nb_b = kb.sb("nbfg", [8, 1], F32)
    kb.op("dve", lambda e: e.tensor_scalar(out=nb[:], in0=bf_[:], scalar1=-1.0, scalar2=None, op0=ALU.mult), reads=[bf_b], writes=[nb_b])
    kb.op("act", lambda e: e.activation(out=flt[:], in_=flt[:], func=AF.Exp, scale=-1.0, bias=nb[:]), reads=[flt_b, nb_b], writes=[flt_b])
    kb.op("act", lambda e: e.activation(out=flt[:], in_=flt[:], func=AF.Ln, bias=1.0), reads=[flt_b], writes=[flt_b])
    onesf, onesf_b = kb.sb("onesf", [8, S], F32)
    kb.op("pool", lambda e: e.memset(onesf[:], 1.0), writes=[onesf_b])
    ncum, ncum_b = kb.sb("ncum", [8, S], F32)
    kb.op("dve", lambda e: e.tensor_tensor_scan(out=ncum[:], data0=onesf[:], data1=flt[:], initial=0.0, op0=ALU.mult, op1=ALU.add),
          reads=[onesf_b, flt_b], writes=[ncum_b])
    augk, augk_b = kb.sb("augk", [8, 6, S], BF16)
    augq, augq_b = kb.sb("augq", [8, 6, S], BF16)
    kb.op("pool", lambda e: e.memset(augk[:, 0:3, :], 1.0), writes=[augk_b])
    kb.op("pool", lambda e: e.memset(augq[:, 3:6, :], 1.0), writes=[augq_b])
    rem, rem_b = kb.sb("rem", [8, S], F32)
    kb.op("dve", lambda e: e.tensor_copy(out=augk[:, 3, :], in_=ncum[:]), reads=[ncum_b, augk_b], writes=[augk_b])
    kb.op("dve", lambda e: e.tensor_tensor(out=rem[:], in0=ncum[:], in1=augk[:, 3, :], op=ALU.subtract), reads=[ncum_b, augk_b], writes=[rem_b])
    kb.op("dve", lambda e: e.tensor_copy(out=augk[:, 4, :], in_=rem[:]), reads=[rem_b, augk_b], writes=[augk_b])
    kb.op("dve", lambda e: e.tensor_tensor(out=rem[:], in0=rem[:], in1=augk[:, 4, :], op=ALU.subtract), reads=[rem_b, augk_b], writes=[rem_b])
    kb.op("dve", lambda e: e.tensor_copy(out=augk[:, 5, :], in_=rem[:]), reads=[rem_b, augk_b], writes=[augk_b])
    kb.op("dve", lambda e: e.tensor_scalar(out=augq[:, 0:3, :], in0=augk[:, 3:6, :], scalar1=-1.0, scalar2=None, op0=ALU.mult),
          reads=[augk_b, augq_b], writes=[augq_b])
    aqd = self.scr["augq"]
    akd = self.scr["augk"]
    kb.dma("sp", aqd[:, :, :], augq[:], reads=[augq_b])
    kb.dma("sp", akd[:, :, :], augk[:], reads=[augk_b])
    kb.barrier()
    kb.reset(self.arena0 + 4 * 1024 + 2048 + 256)
    vall, vall_b = kb.sb("fvall", [128, 32, VA], BF16)
    for k0 in range(0, 32, 8):
        kb.dma("sp", vall[:, k0:k0 + 8, :], self.vf.rearrange("(kt p) c -> p kt c", p=128)[:, k0:k0 + 8, :], writes=[vall_b])
    qk = [(kb.sb("fq%d" % i, [70, S], BF16), kb.sb("fk%d" % i, [70, S], BF16)) for i in range(2)]
    pts = [kb.sb("fpt%d" % i, [128, 512], BF16) for i in range(6)]
    osb = [kb.sb("fosb%d" % i, [65, 512], F32) for i in range(2)]
    rds = [kb.sb("frd%d" % i, [65, 512], F32) for i in range(2)]
    rhl = [kb.sb("frhl%d" % i, [65, 2, 512], BF16) for i in range(2)]
    yh = [kb.sb("fyh%d" % i, [64, S], BF16) for i in range(2)]
    self._it = 0
    for h in range(8):
        (qh, qh_b), (kh, kh_b) = qk[h % 2]
        kb.dma("sp", qh[0:64, :], self.qfT[h * 64:(h + 1) * 64, :], writes=[qh_b])
        kb.dma("sp", qh[64:70, :], aqd[h], writes=[qh_b])
        kb.dma("sp", kh[0:64, :], self.kfT[h * 64:(h + 1) * 64, :], writes=[kh_b])
        kb.dma("sp", kh[64:70, :], akd[h], writes=[kh_b])
        yo, yo_b = yh[h % 2]
        items = [(g, kbi) for g in range(8) for kbi in range(4 * g + 4)]
        pend = {}
        pos = {}
        defer = []

        def emit_qk(i, qh=qh, kh=kh, qh_b=qh_b, kh_b=kh_b):
            g, kbi = items[i]
            ps, ps_b = kb.psum()
            diag = kbi >= 4 * g
            kb.op("pe", lambda e, ps=ps, kh=kh, qh=qh, kbi=kbi, g=g, diag=diag: e.matmul(ps[:], kh[0:70, kbi * 128:(kbi + 1) * 128], qh[0:70, g * 512:(g + 1) * 512],
                                                                                     start=True, stop=not diag),
                  reads=[kh_b, qh_b], writes=[ps_b])
            if diag:
                j = kbi - 4 * g
                kb.op("pe", lambda e, ps=ps, j=j: e.matmul(ps[:], self.identb[:], mk[:, j, :], start=False, stop=True),
                      reads=[self.identb_b, mk_b], writes=[ps_b])
            pt, pt_b = pts[self._it % len(pts)]
            self._it += 1
            kb.op("act", lambda e, pt=pt, ps=ps: e.activation(out=pt[:], in_=ps[:], func=AF.Exp), reads=[ps_b], writes=[pt_b])
            pend[i] = (pt, pt_b)

        def emit_pv(i, h=h, yo=yo, yo_b=yo_b):
            g, kbi = items[i]
            nkb = 4 * g + 4
            if kbi == 0:
                pos[g] = kb.psum_acc()
            po, po_b = pos[g]
            pt, pt_b = pend.pop(i)
            kb.op("pe", lambda e, po=po, pt=pt, kbi=kbi, h=h, nkb=nkb: e.matmul(po[0:65, :], vall[:, kbi, h * 65:(h + 1) * 65], pt[:],
                                                                                start=(kbi == 0), stop=(kbi == nkb - 1)),
                  reads=[vall_b, pt_b], writes=[po_b])
            if kbi == nkb - 1:
                fin = self._attn_finalize_split(po, po_b, osb[g % 2], rds[g % 2], rhl[g % 2], ones, ones_b, yo, yo_b, g)
                defer.append([3, fin])
        LA = 3
        for i in range(min(LA, len(items))):
            emit_qk(i)
        for i in range(len(items)):
            if i + LA < len(items):
                emit_qk(i + LA)
            emit_pv(i)
            for dref in list(defer):
                dref[0] -= 1
                if dref[0] <= 0:
                    dref[1]()
                    defer.remove(dref)
        for dref in defer:
            dref[1]()
        kb.dma("sp", self.ysT[2 * BW + h * 64:2 * BW + (h + 1) * 64, :], yo[:], reads=[yo_b])
    kb.barrier()


def _attn_finalize(self, po, po_b, osb_, rds_, rhl_, ones, ones_b, yo, yo_b, g):
    kb = self.kb
    (ob, ob_b), (rd, rd_b), (rh, rh_b) = osb_, rds_, rhl_
    kb.op("act", lambda e: e.activation(out=ob[:], in_=po[0:65, :], func=AF.Copy), reads=[po_b], writes=[ob_b])
    kb.op("dve", lambda e: e.reciprocal(out=rd[64:65, :], in_=ob[64:65, :]), reads=[ob_b], writes=[rd_b])
    kb.op("dve", lambda e: e.tensor_copy(out=rh[64:65, 0, :], in_=rd[64:65, :]), reads=[rd_b], writes=[rh_b])
    kb.op("dve", lambda e: e.tensor_tensor(out=rd[64:65, :], in0=rd[64:65, :], in1=rh[64:65, 0, :], op=ALU.subtract), reads=[rd_b, rh_b], writes=[rd_b])
    kb.op("dve", lambda e: e.tensor_copy(out=rh[64:65, 1, :], in_=rd[64:65, :]), reads=[rd_b, rh_b], writes=[rh_b])
    pb, pb_b = kb.psum()
    kb.op("pe", lambda e: e.matmul(pb[0:64, :], ones[64:65, 0:64], rh[64:65, 0, :], start=True, stop=False), reads=[ones_b, rh_b], writes=[pb_b])
    kb.op("pe", lambda e: e.matmul(pb[0:64, :], ones[64:65, 0:64], rh[64:65, 1, :], start=False, stop=True), reads=[ones_b, rh_b], writes=[pb_b])
    kb.op("dve", lambda e: e.tensor_tensor(out=yo[0:64, g * 512:(g + 1) * 512], in0=ob[0:64, :], in1=pb[0:64, :], op=ALU.mult),
          reads=[ob_b, pb_b], writes=[yo_b])


def _attn_finalize_split(self, po, po_b, osb_, rds_, rhl_, ones, ones_b, yo, yo_b, g):
    kb = self.kb
    (ob, ob_b), (rd, rd_b), (rh, rh_b) = osb_, rds_, rhl_
    kb.op("act", lambda e: e.activation(out=ob[:], in_=po[0:65, :], func=AF.Copy), reads=[po_b], writes=[ob_b])
    kb.op("dve", lambda e: e.reciprocal(out=rd[64:65, :], in_=ob[64:65, :]), reads=[ob_b], writes=[rd_b])
    kb.op("dve", lambda e: e.tensor_copy(out=rh[64:65, 0, :], in_=rd[64:65, :]), reads=[rd_b], writes=[rh_b])
    kb.op("dve", lambda e: e.tensor_tensor(out=rd[64:65, :], in0=rd[64:65, :], in1=rh[64:65, 0, :], op=ALU.subtract), reads=[rd_b, rh_b], writes=[rd_b])
    kb.op("dve", lambda e: e.tensor_copy(out=rh[64:65, 1, :], in_=rd[64:65, :]), reads=[rd_b, rh_b], writes=[rh_b])

    def part2():
        pb, pb_b = kb.psum()
        kb.op("pe", lambda e: e.matmul(pb[0:64, :], ones[64:65, 0:64], rh[64:65, 0, :], start=True, stop=False), reads=[ones_b, rh_b], writes=[pb_b])
        kb.op("pe", lambda e: e.matmul(pb[0:64, :], ones[64:65, 0:64], rh[64:65, 1, :], start=False, stop=True), reads=[ones_b, rh_b], writes=[pb_b])
        kb.op("dve", lambda e: e.tensor_tensor(out=yo[0:64, g * 512:(g + 1) * 512], in0=ob[0:64, :], in1=pb[0:64, :], op=ALU.mult),
              reads=[ob_b, pb_b], writes=[yo_b])
    return part2


Prog.stageB3 = _stageB3
Prog._attn_finalize = _attn_finalize
Prog._attn_finalize_split = _attn_finalize_split


def _stageB2(self, l):
    kb = self.kb
    kb.reset(self.arena0)
    NEG = -30000.0
    mk, mk_b = kb.sb("dmask", [128, 256], BF16)
    mkf, mkf_b = kb.sb("dmaskf", [128, 256], F32)
    kb.op("pool", lambda e: e.memset(mkf[:], 0.0), writes=[mkf_b])
    kb.op("pool", lambda e: e.affine_select(out=mkf[:, 0:128], in_=mkf[:, 0:128], pattern=[[1, 128]], compare_op=ALU.is_ge,
                                            fill=NEG, base=0, channel_multiplier=-1), reads=[mkf_b], writes=[mkf_b])
    kb.op("pool", lambda e: e.affine_select(out=mkf[:, 128:256], in_=mkf[:, 128:256], pattern=[[-1, 128]], compare_op=ALU.is_ge,
                                            fill=NEG, base=0, channel_multiplier=1), reads=[mkf_b], writes=[mkf_b])
    kb.op("dve", lambda e: e.tensor_copy(out=mk[:], in_=mkf[:]), reads=[mkf_b], writes=[mk_b])
    ones, ones_b = kb.sb("dones", [128, 64], BF16)
    kb.op("pool", lambda e: e.memset(ones[:], 1.0), writes=[ones_b])
    DILS = (1, 4, 16)
    vall = {}
    for d in DILS:
        t, t_b = kb.sb("dv%d" % d, [128, 32, VA], BF16)
        nm = 32 // d
        if d == 1:
            for k0 in range(0, 32, 8):
                kb.dma("sp", t[:, k0:k0 + 8, :], self.vd.rearrange("(m p) c -> p m c", p=128)[:, k0:k0 + 8, :], writes=[t_b])
        else:
            src = self.vd.rearrange("(m p r) c -> p r m c", p=128, r=d)
            for r in range(d):
                kb.dma("sp", t[:, r * nm:(r + 1) * nm, :], src[:, r, :, :], writes=[t_b])
        vall[d] = (t, t_b)
    qk = [(kb.sb("dq%d" % i, [128, S], BF16), kb.sb("dk%d" % i, [128, S], BF16)) for i in range(2)]
    pts = [kb.sb("dpt%d" % i, [128, 256], BF16) for i in range(6)]
    acc = [kb.sb("dacc%d" % i, [65, S], F32) for i in range(2)]
    osb = [kb.sb("dosb%d" % i, [65, 512], F32) for i in range(2)]
    rds = [kb.sb("drd%d" % i, [65, 512], F32) for i in range(2)]
    rhl = [kb.sb("drhl%d" % i, [65, 2, 512], BF16) for i in range(2)]
    yh = [kb.sb("dyh%d" % i, [64, S], BF16) for i in range(2)]
    self._it = 0
    for hp in range(4):
        (qt, qt_b), (kt_, kt_b) = qk[hp % 2]
        kb.dma("sp", qt[:], self.qdT[hp * 128:(hp + 1) * 128, :], writes=[qt_b])
        kb.dma("sp", kt_[:], self.kdT[hp * 128:(hp + 1) * 128, :], writes=[kt_b])
        for hh in range(2):
            h = hp * 2 + hh
            pb0 = hh * 64
            ac, ac_b = acc[h % 2]
            items = []
            for d in DILS:
                nm = 32 // d
                for r in range(d):
                    for m in range(nm):
                        items.append((d, r, m, nm))
            pend = {}
            pos = {}

            def emit_qk(i, pb0=pb0, kt_=kt_, qt=qt, kt_b=kt_b, qt_b=qt_b):
                d, r, m, nm = items[i]
                nq = 256 if m < nm - 1 else 128
                t0 = m * 128 * d + r
                ksl = slice(t0, t0 + 127 * d + 1, d)
                qsl = slice(t0, t0 + (nq - 1) * d + 1, d)
                ps, ps_b = kb.psum()
                kb.op("pe", lambda e, ps=ps, ksl=ksl, qsl=qsl, nq=nq, pb0=pb0, kt_=kt_, qt=qt: e.matmul(ps[:, 0:nq], kt_[pb0:pb0 + 64, ksl], qt[pb0:pb0 + 64, qsl], start=True, stop=False),
                      reads=[kt_b, qt_b], writes=[ps_b])
                kb.op("pe", lambda e, ps=ps, nq=nq: e.matmul(ps[:, 0:nq], self.identb[:], mk[:, 0:nq], start=False, stop=True),
                      reads=[self.identb_b, mk_b], writes=[ps_b])
                pt, pt_b = pts[self._it % len(pts)]
                self._it += 1
                kb.op("act", lambda e, pt=pt, ps=ps, nq=nq: e.activation(out=pt[:, 0:nq], in_=ps[:, 0:nq], func=AF.Exp), reads=[ps_b], writes=[pt_b])
                pend[i] = (pt, pt_b, nq, t0)

            def emit_pv(i, h=h, ac=ac, ac_b=ac_b):
                d, r, m, nm = items[i]
                vt, vt_b = vall[d]
                b = r * nm + m
                pt, pt_b, nq, t0 = pend.pop(i)
                if m == 0:
                    pos[(d, r, 0)] = kb.psum_acc()
                po, po_b = pos.pop((d, r, m))
                kb.op("pe", lambda e, po=po, vt=vt, b=b, h=h, pt=pt, m=m: e.matmul(po[0:65, 0:128], vt[:, b, h * 65:(h + 1) * 65], pt[:, 0:128], start=(m == 0), stop=True),
                      reads=[vt_b, pt_b], writes=[po_b])
                if nq == 256:
                    pos[(d, r, m + 1)] = kb.psum_acc()
                    po2, po2_b = pos[(d, r, m + 1)]
                    kb.op("pe", lambda e, po2=po2, vt=vt, b=b, h=h, pt=pt: e.matmul(po2[0:65, 0:128], vt[:, b, h * 65:(h + 1) * 65], pt[:, 128:256], start=True, stop=False),
                          reads=[vt_b, pt_b], writes=[po2_b])
                if d == 1:
                    kb.op("dve", lambda e, ac=ac, po=po, qs=slice(t0, t0 + 128): e.tensor_copy(out=ac[:, qs], in_=po[0:65, 0:128]),
                          reads=[po_b], writes=[ac_b])
                else:
                    qs = slice(t0, t0 + 127 * d + 1, d)
                    kb.op("dve", lambda e, ac=ac, po=po, qs=qs: e.tensor_tensor(out=ac[:, qs], in0=ac[:, qs], in1=po[0:65, 0:128], op=ALU.add),
                          reads=[po_b, ac_b], writes=[ac_b])
            LA = 3
            for i in range(min(LA, len(items))):
                emit_qk(i)
            for i in range(len(items)):
                if i + LA < len(items):
                    emit_qk(i + LA)
                emit_pv(i)
            yo, yo_b = yh[h % 2]
            for g in range(8):
                self._attn_finalize2(ac, ac_b, rds[g % 2], rhl[g % 2], ones, ones_b, yo, yo_b, g)
            kb.dma("sp", self.ysT[BW + h * 64:BW + (h + 1) * 64, :], yo[:], reads=[yo_b])
    kb.barrier()


def _attn_finalize2(self, ac, ac_b, rds_, rhl_, ones, ones_b, yo, yo_b, g):
    kb = self.kb
    (rd, rd_b), (rh, rh_b) = rds_, rhl_
    sl = slice(g * 512, (g + 1) * 512)
    kb.op("dve", lambda e: e.reciprocal(out=rd[64:65, :], in_=ac[64:65, sl]), reads=[ac_b], writes=[rd_b])
    kb.op("dve", lambda e: e.tensor_copy(out=rh[64:65, 0, :], in_=rd[64:65, :]), reads=[rd_b], writes=[rh_b])
    kb.op("dve", lambda e: e.tensor_tensor(out=rd[64:65, :], in0=rd[64:65, :], in1=rh[64:65, 0, :], op=ALU.subtract), reads=[rd_b, rh_b], writes=[rd_b])
    kb.op("dve", lambda e: e.tensor_copy(out=rh[64:65, 1, :], in_=rd[64:65, :]), reads=[rd_b, rh_b], writes=[rh_b])
    pb, pb_b = kb.psum()
    kb.op("pe", lambda e: e.matmul(pb[0:64, :], ones[64:65, 0:64], rh[64:65, 0, :], start=True, stop=False), reads=[ones_b, rh_b], writes=[pb_b])
    kb.op("pe", lambda e: e.matmul(pb[0:64, :], ones[64:65, 0:64], rh[64:65, 1, :], start=False, stop=True), reads=[ones_b, rh_b], writes=[pb_b])
    kb.op("dve", lambda e: e.tensor_tensor(out=yo[0:64, sl], in0=ac[0:64, sl], in1=pb[0:64, :], op=ALU.mult),
          reads=[ac_b, pb_b], writes=[yo_b])


Prog.stageB2 = _stageB2
Prog._attn_finalize2 = _attn_finalize2


def _stageB1(self, l):
    kb = self.kb
    kb.reset(self.arena0)
    PB = Buf("ssm_prep")
    idf, idf_b = self.identf, self.identf_b
    BSt, _ = kb.sb("BSt", [128, 2, 16, 2, 128], BF16)
    Gt, _ = kb.sb("Gt", [128, 2, 16, 2, 128], BF16)
    Tt, _ = kb.sb("Tt", [128, 32, 128], BF16)
    Dr, _ = kb.sb("Dr", [128, 16, 9], F32)
    Di, _ = kb.sb("Di", [128, 16, 9], F32)
    nDi, _ = kb.sb("nDi", [128, 16, 9], F32)
    m_keep = kb.mark()

    def T_(name, shape, dt_=F32):
        t, _ = kb.sb(name, shape, dt_)
        return t

    def dve(fn, extra_r=(), extra_w=()):
        kb.op("dve", fn, reads=[PB] + list(extra_r), writes=[PB] + list(extra_w))

    def tt(out, a, b, op):
        dve(lambda e: e.tensor_tensor(out=out, in0=a, in1=b, op=op))

    def ts(out, a, s1, op0, s2=None, op1=None):
        if op1 is None:
            dve(lambda e: e.tensor_scalar(out=out, in0=a, scalar1=s1, scalar2=None, op0=op0))
        else:
            dve(lambda e: e.tensor_scalar(out=out, in0=a, scalar1=s1, scalar2=s2, op0=op0, op1=op1))

    def cp(out, a):
        dve(lambda e: e.tensor_copy(out=out, in_=a))

    pp = T_("pp", [16, 3, 128])
    ld = T_("ld", [16, 2])
    kb.dma("sp", pp[:, 0, :], self.I("ssm_lambda_re")[l].rearrange("(q a) p -> q (a p)", a=2), writes=[PB])
    kb.dma("sp", pp[:, 1, :], self.I("ssm_lambda_im")[l].rearrange("(q a) p -> q (a p)", a=2), writes=[PB])
    kb.dma("sp", ld[:], self.I("ssm_log_dt")[l].rearrange("(q a) -> q a", a=2), writes=[PB])
    cp(pp[:, 2, :].rearrange("q (a p) -> q a p", a=2), ld[:].unsqueeze(2).to_broadcast([16, 2, 64]))
    par = T_("par", [128, 3, 16])
    ps, ps_b = kb.psum()
    for i in range(3):
        kb.op("pe", lambda e, i=i, ps=ps: e.transpose(ps[:, i * 16:(i + 1) * 16], pp[:, i, :], idf[0:16, 0:16]), reads=[PB, idf_b], writes=[ps_b])
    dve(lambda e, ps=ps: e.tensor_copy(out=par[:].rearrange("p a q -> p (a q)"), in_=ps[:, 0:48]), extra_r=[ps_b])
    lr, li, ldt = par[:, 0, :], par[:, 1, :], par[:, 2, :]
    Bre = T_("Bre", [128, 16, 16])
    Bim = T_("Bim", [128, 16, 16])
    for (dst, nm) in ((Bre, "ssm_b_re"), (Bim, "ssm_b_im")):
        src = self.I(nm)[l].rearrange("(q a) p c -> (a p) q c", a=2)
        for q0 in range(0, 16, 4):
            kb.dma("sp", dst[:, q0:q0 + 4, :], src[:, q0:q0 + 4, :], writes=[PB])
    Cre = T_("Cre", [128, 16, 16])
    Cim = T_("Cim", [128, 16, 16])
    Z = T_("Z", [32, 16, 128])
    zt = T_("zt", [128, 256])
    for (dst, nm) in ((Cre, "ssm_c_re"), (Cim, "ssm_c_im")):
        dve(lambda e: e.memset(Z[:], 0.0))
        src = self.I(nm)[l].rearrange("(q a) c p -> a c q p", a=2)
        kb.dma("sp", Z[0:16, :, 0:64], src[0], reads=[PB], writes=[PB])
        kb.dma("sp", Z[16:32, :, 64:128], src[1], reads=[PB], writes=[PB])
        for q in range(16):
            if q % 8 == 0:
                ps, ps_b = kb.psum()
            kb.op("pe", lambda e, ps=ps, q=q: e.transpose(ps[:, (q % 8) * 32:(q % 8) * 32 + 32], Z[:, q, :], idf[0:32, 0:32]), reads=[PB, idf_b], writes=[ps_b])
            if q % 8 == 7:
                q0 = q - 7
                dve(lambda e, ps=ps: e.tensor_copy(out=zt[:], in_=ps[:, 0:256]), extra_r=[ps_b])
                pv = zt[:].rearrange("p (q a c) -> p q a c", q=8, a=2)
                dve(lambda e, dst=dst, pv=pv, q0=q0: e.tensor_tensor(out=dst[:, q0:q0 + 8, :], in0=pv[:, :, 0, :], in1=pv[:, :, 1, :], op=ALU.add))
    def S_(name):
        return T_(name, [128, 16])
    dt = S_("dt")
    kb.op("act", lambda e: e.activation(out=dt[:], in_=ldt, func=AF.Exp), reads=[PB], writes=[PB])
    x = S_("x")
    tt(x[:], lr, dt[:], ALU.mult)
    er = S_("er")
    ts(er[:], x[:], 1.0 / 7, ALU.mult, 1.0, ALU.add)
    for k in (6, 5, 4, 3, 2, 1):
        tt(er[:], er[:], x[:], ALU.mult)
        ts(er[:], er[:], 1.0 / k, ALU.mult, 1.0, ALU.add)
    phi = S_("phi")
    tt(phi[:], li, dt[:], ALU.mult)
    ts(phi[:], phi[:], 1.0 / 32, ALU.mult)
    z = S_("z")
    tt(z[:], phi[:], phi[:], ALU.mult)
    cr = S_("cr")
    ci_ = S_("ci")
    cc = [1.0, -1.0 / 2, 1.0 / 24, -1.0 / 720, 1.0 / 40320, -1.0 / 3628800, 1.0 / 479001600]
    sc = [1.0, -1.0 / 6, 1.0 / 120, -1.0 / 5040, 1.0 / 362880, -1.0 / 39916800, 1.0 / 6227020800]
    for (dst, co) in ((cr, cc), (ci_, sc)):
        ts(dst[:], z[:], co[6], ALU.mult, co[5], ALU.add)
        for k in (4, 3, 2, 1, 0):
            tt(dst[:], dst[:], z[:], ALU.mult)
            ts(dst[:], dst[:], co[k], ALU.add)
    tt(ci_[:], ci_[:], phi[:], ALU.mult)
    t1, t2, t3, t4 = S_("t1"), S_("t2"), S_("t3"), S_("t4")

    def cmul(or_, oi, ar, ai, br, bi, a1=t1, a2=t2, a3=t3, a4=t4):
        tt(a1, ar, br, ALU.mult)
        tt(a2, ai, bi, ALU.mult)
        tt(a3, ar, bi, ALU.mult)
        tt(a4, ai, br, ALU.mult)
        tt(or_, a1, a2, ALU.subtract)
        tt(oi, a3, a4, ALU.add)

    for _ in range(5):
        cmul(cr[:], ci_[:], cr[:], ci_[:], cr[:], ci_[:], t1[:], t2[:], t3[:], t4[:])
        tt(t1[:], cr[:], cr[:], ALU.mult)
        tt(t2[:], ci_[:], ci_[:], ALU.mult)
        tt(t1[:], t1[:], t2[:], ALU.add)
        ts(t1[:], t1[:], -0.5, ALU.mult, 1.5, ALU.add)
        tt(cr[:], cr[:], t1[:], ALU.mult)
        tt(ci_[:], ci_[:], t1[:], ALU.mult)
    Pl_r = T_("Plr", [128, 9, 16])
    Pl_i = T_("Pli", [128, 9, 16])
    dve(lambda e: e.memset(Pl_r[:, 0, :], 1.0))
    dve(lambda e: e.memset(Pl_i[:, 0, :], 0.0))
    tt(Pl_r[:, 1, :], cr[:], er[:], ALU.mult)
    tt(Pl_i[:, 1, :], ci_[:], er[:], ALU.mult)
    ar, ai = Pl_r[:, 1, :], Pl_i[:, 1, :]
    for k in range(2, 9):
        cmul(Pl_r[:, k, :], Pl_i[:, k, :], Pl_r[:, k - 1, :], Pl_i[:, k - 1, :], ar, ai, t1[:], t2[:], t3[:], t4[:])
    Nl_r = T_("Nlr", [128, 8, 16])
    Nl_i = T_("Nli", [128, 8, 16])
    dve(lambda e: e.memset(Nl_r[:, 0, :], 1.0))
    dve(lambda e: e.memset(Nl_i[:, 0, :], 0.0))
    tt(t1[:], ar, ar, ALU.mult)
    tt(t2[:], ai, ai, ALU.mult)
    tt(t1[:], t1[:], t2[:], ALU.add)
    dve(lambda e: e.reciprocal(out=t1[:], in_=t1[:]))
    tt(Nl_r[:, 1, :], ar, t1[:], ALU.mult)
    tt(Nl_i[:, 1, :], ai, t1[:], ALU.mult)
    ts(Nl_i[:, 1, :], Nl_i[:, 1, :], -1.0, ALU.mult)
    for k in range(2, 8):
        cmul(Nl_r[:, k, :], Nl_i[:, k, :], Nl_r[:, k - 1, :], Nl_i[:, k - 1, :], Nl_r[:, 1, :], Nl_i[:, 1, :], t1[:], t2[:], t3[:], t4[:])
    cp(Dr[:, :, 0], Pl_r[:, 8, :])
    cp(Di[:, :, 0], Pl_i[:, 8, :])
    for k in range(1, 9):
        cmul(Dr[:, :, k], Di[:, :, k], Dr[:, :, k - 1], Di[:, :, k - 1], Dr[:, :, k - 1], Di[:, :, k - 1], t1[:], t2[:], t3[:], t4[:])
    ts(nDi[:], Di[:], -1.0, ALU.mult)
    qr, qi = S_("qr"), S_("qi")
    nr = S_("nr")
    ts(nr[:], ar, -1.0, ALU.add)
    tt(t1[:], lr, lr, ALU.mult)
    tt(t2[:], li, li, ALU.mult)
    tt(t1[:], t1[:], t2[:], ALU.add)
    dve(lambda e: e.reciprocal(out=t1[:], in_=t1[:]))
    tt(t2[:], nr[:], lr, ALU.mult)
    tt(t3[:], ai, li, ALU.mult)
    tt(t2[:], t2[:], t3[:], ALU.add)
    tt(qr[:], t2[:], t1[:], ALU.mult)
    tt(t2[:], ai, lr, ALU.mult)
    tt(t3[:], nr[:], li, ALU.mult)
    tt(t2[:], t2[:], t3[:], ALU.subtract)
    tt(qi[:], t2[:], t1[:], ALU.mult)
    Br2, Bi2 = T_("Br2", [128, 16, 16]), T_("Bi2", [128, 16, 16])
    u1, u2, u3, u4 = (T_("u%d" % i, [128, 16, 16]) for i in range(4))

    def bq(t):
        return t.unsqueeze(2).to_broadcast([128, 16, 16])
    cmul(Br2[:], Bi2[:], Bre[:], Bim[:], bq(qr[:]), bq(qi[:]), u1[:], u2[:], u3[:], u4[:])
    def Bg(name):
        return T_(name, [128, 16, 8, 16])
    g1, g2, g3, g4 = Bg("g1"), Bg("g2"), Bg("g3"), Bg("g4")
    Wm_r, Wm_i = Bg("Wmr"), Bg("Wmi")

    def over_k(t):
        return t.unsqueeze(2).to_broadcast([128, 16, 8, 16])

    def over_c(t):
        return t.rearrange("p k q -> p q k").unsqueeze(3).to_broadcast([128, 16, 8, 16])

    def over_kc(t):
        return t.unsqueeze(2).unsqueeze(3).to_broadcast([128, 16, 8, 16])

    cmul(Wm_r[:], Wm_i[:], over_k(Br2[:]), over_k(Bi2[:]), over_c(Nl_r[:, 0:8, :]), over_c(Nl_i[:, 0:8, :]), g1[:], g2[:], g3[:], g4[:])
    WmM = T_("WmM", [128, 2, 2, 16, 128], BF16)
    dve(lambda e: e.memset(WmM[:].rearrange("p a b q n -> p (a b q n)"), 0.0))
    for a in range(2):
        for part, src in ((0, Wm_r), (1, Wm_i)):
            cp(WmM[a * 64:(a + 1) * 64, a, part, :, :], src[a * 64:(a + 1) * 64].rearrange("p q k c -> p q (k c)"))
    W7_r, W7_i = Bg("W7r"), Bg("W7i")
    cmul(W7_r[:], W7_i[:], Wm_r[:], Wm_i[:], over_kc(Pl_r[:, 7, :]), over_kc(Pl_i[:, 7, :]), g1[:], g2[:], g3[:], g4[:])
    BSt_b = Buf("BSt")
    kb.op("pool", lambda e: e.memset(BSt[:].rearrange("p a q b n -> p (a q b n)"), 0.0), writes=[BSt_b])
    for part, src in ((0, W7_r), (1, W7_i)):
        for q in range(16):
            if q % 4 == 0:
                ps, ps_b = kb.psum()
            kb.op("pe", lambda e, ps=ps, q=q, src=src: e.transpose(ps[:, (q % 4) * 128:(q % 4) * 128 + 128], src[:, q].rearrange("p k c -> p (k c)"), idf[:]),
                  reads=[PB, idf_b], writes=[ps_b])
            if q % 4 == 3:
                for a in range(2):
                    pv = ps[:].rearrange("p (q n) -> p q n", q=4)[:, :, a * 64:(a + 1) * 64]
                    kb.op("act", lambda e, pv=pv, part=part, q=q, a=a: e.activation(out=BSt[:, part, q - 3:q + 1, a, a * 64:(a + 1) * 64], in_=pv, func=AF.Copy),
                          reads=[ps_b, BSt_b], writes=[BSt_b])
    Cp_r, Cp_i = Bg("Cpr"), Bg("Cpi")
    cmul(Cp_r[:], Cp_i[:], over_k(Cre[:]), over_k(Cim[:]), over_c(Pl_r[:, 0:8, :]), over_c(Pl_i[:, 0:8, :]), g1[:], g2[:], g3[:], g4[:])
    CpB = T_("CpB", [128, 2, 16, 128], BF16)
    cp(CpB[:, 0], Cp_r[:].rearrange("p q k c -> p q (k c)"))
    ts(CpB[:, 1], Cp_i[:].rearrange("p q k c -> p q (k c)"), -1.0, ALU.mult)
    G_r, G_i = W7_r, W7_i
    kb.op("dve", lambda e: e.memset(t1[:], 0.0), reads=[PB, BSt_b], writes=[PB])
    cmul(G_r[:], G_i[:], Cp_r[:], Cp_i[:], over_kc(ar), over_kc(ai), g1[:], g2[:], g3[:], g4[:])
    dve(lambda e: e.memset(Gt[:].rearrange("p a q b n -> p (a q b n)"), 0.0))
    for a in range(2):
        cp(Gt[a * 64:(a + 1) * 64, 0, :, a, :], G_r[a * 64:(a + 1) * 64].rearrange("p q k c -> p q (k c)"))
        ts(Gt[a * 64:(a + 1) * 64, 1, :, a, :], G_i[a * 64:(a + 1) * 64].rearrange("p q k c -> p q (k c)"), -1.0, ALU.mult)
    kidx_i = T_("kidx_i", [128, 1], I32)
    kidx = T_("kidx", [128, 1])
    jidx_i = T_("jidx_i", [128, 8, 16], I32)
    jidx = T_("jidx", [128, 128])
    cmask = T_("cmask", [128, 128])
    kb.op("pool", lambda e: e.iota(kidx_i[:], pattern=[[0, 1]], base=0, channel_multiplier=1), reads=[PB], writes=[PB])
    kb.op("pool", lambda e: e.iota(jidx_i[:], pattern=[[1, 8], [0, 16]], base=0, channel_multiplier=0), reads=[PB], writes=[PB])
    dve(lambda e: e.tensor_single_scalar(out=kidx_i[:], in_=kidx_i[:], scalar=4, op=ALU.arith_shift_right))
    cp(kidx[:], kidx_i[:])
    cp(jidx[:], jidx_i[:].rearrange("p a b -> p (a b)"))
    ts(cmask[:], jidx[:], kidx[:, 0:1], ALU.is_ge)
    dB = T_("dB", [128, 512])
    kb.dma("sp", dB[:], self.I("ssm_d")[l].partition_broadcast(128), writes=[PB])
    IDd = T_("IDd", [128, 32, 128], BF16)
    for g in range(32):
        dve(lambda e, g=g: e.tensor_tensor(out=IDd[:, g, :].rearrange("p (j c) -> p j c", j=8), in0=idf[:].rearrange("p (j c) -> p j c", j=8),
                                           in1=dB[:, g * 16:(g + 1) * 16].unsqueeze(1).to_broadcast([128, 8, 16]), op=ALU.mult), extra_r=[idf_b])
    Tt_b = Buf("Tt")
    for g in range(32):
        q, a = g // 2, g % 2
        if g % 4 == 0:
            ps, ps_b = kb.psum()
        sl = slice((g % 4) * 128, (g % 4) * 128 + 128)
        kb.op("pe", lambda e, ps=ps, sl=sl, q=q, a=a: e.matmul(ps[:, sl], WmM[:, a, 0, q, :], CpB[:, 0, q, :], start=True, stop=False), reads=[PB], writes=[ps_b])
        kb.op("pe", lambda e, ps=ps, sl=sl, q=q, a=a: e.matmul(ps[:, sl], WmM[:, a, 1, q, :], CpB[:, 1, q, :], start=False, stop=False), reads=[PB], writes=[ps_b])
        kb.op("pe", lambda e, ps=ps, sl=sl, g=g: e.matmul(ps[:, sl], self.identb[:], IDd[:, g, :], start=False, stop=True), reads=[PB, self.identb_b], writes=[ps_b])
        if g % 4 == 3:
            kb.op("dve", lambda e, ps=ps, g=g: e.tensor_tensor(out=Tt[:, g - 3:g + 1, :], in0=ps[:].rearrange("p (g n) -> p g n", g=4),
                                                               in1=cmask[:].unsqueeze(1).to_broadcast([128, 4, 128]), op=ALU.mult),
                  reads=[ps_b, PB], writes=[Tt_b])
    kb.barrier()
    kb.reset(m_keep)
    self._ssm_main(l, dict(BSt=BSt, Gt=Gt, Tt=Tt, Dr=Dr, Di=Di, nDi=nDi), m_keep)


def _ssm_main(self, l, tb, m0):
    kb = self.kb
    BSt, Gt, Tt, Dr, Di, nDi = tb["BSt"], tb["Gt"], tb["Tt"], tb["Dr"], tb["Di"], tb["nDi"]
    TB = Buf("ssm_tables")
    U, U_b = kb.sb("U", [128, 32, 512], BF16)
    m_x = kb.mark()
    xts = [kb.sb("Xc%d" % i, [128, 8, 512], BF16) for i in range(2)]
    x2s = [kb.sb("X2c%d" % i, [128, 32, 128], BF16) for i in range(2)]
    kb.reset(m_x)
    ygT, ygT_b = kb.sb("ygT", [128, 4, S], BF16)
    for ct in range(4):
        xt0, xt0_b = xts[ct % 2]
        kb.dma("sp", xt0[:].rearrange("p k c -> p (k c)"), self.u_tm[ct * 1024:(ct + 1) * 1024, :].rearrange("(p k) c -> p (k c)", k=8), writes=[xt0_b])
        xt, xt_b = x2s[ct % 2]
        kb.op("pool", lambda e, xt=xt, xt0=xt0: e.tensor_copy(out=xt[:].rearrange("p g (k c) -> p g k c", k=8),
                                                              in_=xt0[:].rearrange("p k (g c) -> p g k c", g=32)),
              reads=[xt0_b], writes=[xt_b])
        for g in range(32):
            if g % 4 == 0:
                ps, ps_b = kb.psum()
            kb.op("pe", lambda e, ps=ps, g=g, xt=xt: e.matmul(ps[:, (g % 4) * 128:(g % 4) * 128 + 128], xt[:, g, :], self.identb[:], start=True, stop=True),
                  reads=[xt_b, self.identb_b], writes=[ps_b])
            if g % 4 == 3:
                eng = "act" if (g // 4) % 2 == 0 else "dve"
                pv = ps[:].rearrange("p (g n) -> p g n", g=4)
                if eng == "act":
                    kb.op("act", lambda e, pv=pv, g=g, ct=ct: e.activation(out=U[:, g - 3:g + 1, ct * 128:(ct + 1) * 128], in_=pv, func=AF.Copy), reads=[ps_b], writes=[U_b])
                else:
                    kb.op("dve", lambda e, pv=pv, g=g, ct=ct: e.tensor_copy(out=U[:, g - 3:g + 1, ct * 128:(ct + 1) * 128], in_=pv), reads=[ps_b], writes=[U_b])
    Yg, Yg_b = kb.sb("Yg", [128, 4, 8, 512], BF16)
    SA = [kb.sb("SA%d" % i, [128, 2, 512], F32) for i in range(2)]
    SB_ = [kb.sb("SB%d" % i, [128, 2, 512], F32) for i in range(2)]
    Ssh = [kb.sb("Ssh%d" % i, [128, 2, 512], BF16) for i in range(2)]
    for q in range(16):
        (sa, sa_b), (sb2, sb2_b), (ssh, ssh_b) = SA[q % 2], SB_[q % 2], Ssh[q % 2]
        for part in range(2):
            ps, ps_b = kb.psum()
            for a in range(2):
                kb.op("pe", lambda e, ps=ps, part=part, a=a, q=q: e.matmul(ps[:], BSt[:, part, q, a, :], U[:, 2 * q + a, :], start=(a == 0), stop=(a == 1)),
                      reads=[U_b, TB], writes=[ps_b])
            kb.op("act", lambda e, ps=ps, part=part, sa=sa: e.activation(out=sa[:, part, :], in_=ps[:], func=AF.Copy), reads=[ps_b], writes=[sa_b])
        cur, cur_b, nxt, nxt_b = sa, sa_b, sb2, sb2_b
        for k in range(9):
            m = 1 << k
            dr, di, ndi = Dr[:, q, k:k + 1], Di[:, q, k:k + 1], nDi[:, q, k:k + 1]
            kb.op("pool", lambda e, cur=cur, nxt=nxt, m=m: e.tensor_copy(out=nxt[:, :, 0:m], in_=cur[:, :, 0:m]), reads=[cur_b], writes=[nxt_b])
            kb.op("dve", lambda e, cur=cur, nxt=nxt, m=m, dr=dr: e.scalar_tensor_tensor(out=nxt[:, 0, m:512], in0=cur[:, 0, 0:512 - m], scalar=dr, in1=cur[:, 0, m:512], op0=ALU.mult, op1=ALU.add),
                  reads=[cur_b, TB], writes=[nxt_b])
            kb.op("dve", lambda e, cur=cur, nxt=nxt, m=m, ndi=ndi: e.scalar_tensor_tensor(out=nxt[:, 0, m:512], in0=cur[:, 1, 0:512 - m], scalar=ndi, in1=nxt[:, 0, m:512], op0=ALU.mult, op1=ALU.add),
                  reads=[cur_b, nxt_b, TB], writes=[nxt_b])
            kb.op("dve", lambda e, cur=cur, nxt=nxt, m=m, di=di: e.scalar_tensor_tensor(out=nxt[:, 1, m:512], in0=cur[:, 0, 0:512 - m], scalar=di, in1=cur[:, 1, m:512], op0=ALU.mult, op1=ALU.add),
                  reads=[cur_b, TB], writes=[nxt_b])
            kb.op("dve", lambda e, cur=cur, nxt=nxt, m=m, dr=dr: e.scalar_tensor_tensor(out=nxt[:, 1, m:512], in0=cur[:, 1, 0:512 - m], scalar=dr, in1=nxt[:, 1, m:512], op0=ALU.mult, op1=ALU.add),
                  reads=[cur_b, nxt_b, TB], writes=[nxt_b])
            cur, cur_b, nxt, nxt_b = nxt, nxt_b, cur, cur_b
        kb.op("pool", lambda e, ssh=ssh: e.memset(ssh[:, :, 0:1], 0.0), writes=[ssh_b])
        kb.op("act", lambda e, ssh=ssh, cur=cur: e.activation(out=ssh[:, :, 1:512], in_=cur[:, :, 0:511], func=AF.Copy), reads=[cur_b, ssh_b], writes=[ssh_b])
        for ct in range(4):
            if ct % 2 == 0:
                ps, ps_b = kb.psum()
            o0 = (ct % 2) * 256
            csl = slice(ct * 128, (ct + 1) * 128)
            kb.op("pe", lambda e, ps=ps, o0=o0, csl=csl, ssh=ssh, q=q: e.matmul(ps[:, o0:o0 + 256], ssh[:, 0, csl], Gt[:, 0, q].rearrange("p a n -> p (a n)"), start=True, stop=False),
                  reads=[ssh_b, TB], writes=[ps_b])
            kb.op("pe", lambda e, ps=ps, o0=o0, csl=csl, ssh=ssh, q=q: e.matmul(ps[:, o0:o0 + 256], ssh[:, 1, csl], Gt[:, 1, q].rearrange("p a n -> p (a n)"), start=False, stop=False),
                  reads=[ssh_b, TB], writes=[ps_b])
            for a in range(2):
                kb.op("pe", lambda e, ps=ps, o0=o0, csl=csl, a=a, q=q: e.matmul(ps[:, o0 + a * 128:o0 + (a + 1) * 128], U[:, 2 * q + a, csl], Tt[:, 2 * q + a, :], start=False, stop=(a == 1)),
                      reads=[U_b, TB], writes=[ps_b])
            kb.op("act", lambda e, ps=ps, o0=o0, ct=ct, q=q: e.activation(out=Yg[:, ct, :, q * 32:(q + 1) * 32].rearrange("p j (a c) -> p a j c", a=2),
                                                                           in_=ps[:, o0:o0 + 256].rearrange("p (a j c) -> p a j c", a=2, j=8), func=AF.Gelu_apprx_tanh),
                  reads=[ps_b], writes=[Yg_b])
    ev = 0
    for ct in range(4):
        for cht in range(4):
            for jh in range(2):
                ps, ps_b = kb.psum()
                for jj in range(4):
                    j = jh * 4 + jj
                    kb.op("pe", lambda e, ps=ps, jj=jj, j=j, ct=ct, cht=cht: e.matmul(ps[:, jj * 128:(jj + 1) * 128], Yg[:, ct, j, cht * 128:(cht + 1) * 128], self.identb[:], start=True, stop=True),
                          reads=[Yg_b, self.identb_b], writes=[ps_b])
                t0 = ct * 1024 + jh * 4
                dst = ygT[:, cht, ct * 1024:(ct + 1) * 1024].rearrange("p (c j) -> p j c", j=8)[:, jh * 4:jh * 4 + 4, :]
                pv = ps[:].rearrange("p (j c) -> p j c", j=4)
                if ev % 2 == 0:
                    kb.op("act", lambda e, dst=dst, pv=pv: e.activation(out=dst, in_=pv, func=AF.Copy), reads=[ps_b], writes=[ygT_b])
                else:
                    kb.op("dve", lambda e, dst=dst, pv=pv: e.tensor_copy(out=dst, in_=pv), reads=[ps_b], writes=[ygT_b])
                ev += 1
    wg, wg_b = kb.sb("wglu", [128, 4, 512], BF16)
    kb.dma("pool", wg[:], self.I("w_glu")[l].rearrange("(kt p) n -> p kt n", p=128), writes=[wg_b])
    sgs = [kb.sb("sg%d" % i, [128, 512], BF16) for i in range(3)]
    yos = [kb.sb("yo%d" % i, [128, S], BF16) for i in range(2)]
    it = 0
    for mo in range(4):
        yo, yo_b = yos[mo % 2]
        for tg in range(8):
            ps, ps_b = kb.psum()
            tsl = slice(tg * 512, (tg + 1) * 512)
            for kt in range(4):
                kb.op("pe", lambda e, ps=ps, kt=kt, mo=mo, tsl=tsl: e.matmul(ps[:], wg[:, kt, mo * 128:(mo + 1) * 128], ygT[:, kt, tsl], start=(kt == 0), stop=(kt == 3)),
                      reads=[wg_b, ygT_b], writes=[ps_b])
            sg, sg_b = sgs[it % 3]
            it += 1
            kb.op("act", lambda e, sg=sg, ps=ps: e.activation(out=sg[:], in_=ps[:], func=AF.Sigmoid), reads=[ps_b], writes=[sg_b])
            kb.op("pool", lambda e, yo=yo, sg=sg, mo=mo, tsl=tsl: e.tensor_tensor(out=yo[:, tsl], in0=ygT[:, mo, tsl], in1=sg[:], op=ALU.mult),
                  reads=[ygT_b, sg_b], writes=[yo_b])
        kb.dma("sp", self.ysT[mo * 128:(mo + 1) * 128, :], yo[:], reads=[yo_b])
    kb.barrier()


Prog.stageB1 = _stageB1
Prog._ssm_main = _ssm_main


def _stageC(self, l, last):
    kb = self.kb
    kb.reset(self.arena0)
    idf, idf_b = self.identf, self.identf_b
    moe = (l % 2 == 1)
    li2 = l // 2
    ones_m, ones_m_b = kb.sb("ones_m", [128, 128], BF16)
    ones_1, ones_1_b = kb.sb("ones_1", [128, 128], BF16)
    kb.op("pool", lambda e: e.memset(ones_m[:], 1.0 / 1024), writes=[ones_m_b])
    kb.op("pool", lambda e: e.memset(ones_1[:], 1.0), writes=[ones_1_b])
    lnp, lnp_b = kb.sb("lnp", [8, 6, 128], F32)
    for i, nm in enumerate(["ln_mix_g", "ln_mix_b", "ln_x_g", "ln_x_b", "ln_ffn_g", "ln_ffn_b"]):
        kb.dma("sp", lnp[:, i, :], self.I(nm)[l].rearrange("(kt p) -> kt p", p=128), writes=[lnp_b])
    lnT, lnT_b = kb.sb("lnT", [128, 6, 8], F32)
    ps, ps_b = kb.psum()
    for i in range(6):
        kb.op("pe", lambda e, i=i, ps=ps: e.transpose(ps[:, i * 8:(i + 1) * 8], lnp[:, i, :], idf[0:8, 0:8]), reads=[lnp_b, idf_b], writes=[ps_b])
    kb.op("dve", lambda e, ps=ps: e.tensor_copy(out=lnT[:].rearrange("p a b -> p (a b)"), in_=ps[:, 0:48]), reads=[ps_b], writes=[lnT_b])
    NW = 4
    wraw = [kb.sb("wC%d" % i, [128, 4096], BF16) for i in range(NW)]
    self._wc = 0

    def wbuf():
        t = wraw[self._wc % NW]
        self._wc += 1
        return t

    def load_w(src, kt_n, ncols):
        t, t_b = wbuf()
        v = t[:, 0:kt_n * ncols].rearrange("p (k n) -> p k n", k=kt_n)
        sv = src.rearrange("(k p) n -> p k n", p=128)
        for k0 in range(0, kt_n, 8):
            k1 = min(kt_n, k0 + 8)
            kb.dma("sp", v[:, k0:k1, :], sv[:, k0:k1, :], reads=[self.wcb], writes=[t_b])
        return v, t_b

    def linear(W, kt_n, n_out, rhs_fn, rhs_bufs, consume):
        cpc = 512
        while kt_n * cpc * 2 > 8192:
            cpc //= 2
        c0 = 0
        while c0 < n_out:
            nc_ = min(cpc, n_out - c0)
            wv, w_b = load_w(W[:, c0:c0 + nc_], kt_n, nc_)
            for j in range(nc_ // 128):
                ps, ps_b = kb.psum()
                for kt in range(kt_n):
                    kb.op("pe", lambda e, ps=ps, wv=wv, kt=kt, j=j: e.matmul(ps[:], wv[:, kt, j * 128:(j + 1) * 128], rhs_fn(kt), start=(kt == 0), stop=(kt == kt_n - 1)),
                          reads=[w_b] + rhs_bufs, writes=[ps_b])
                consume((c0 // 128) + j, ps, ps_b)
            c0 += nc_

    memT, memT_b = self.memT, self.memT_b
    KT, KT_b = kb.sb("KT", [128, 8, NMEM], BF16)
    Vm, Vm_b = kb.sb("Vm", [128, 2, D], BF16)

    def cons_k(ot, ps, ps_b):
        kb.op("act", lambda e: e.activation(out=KT[:, ot, :], in_=ps[:, 0:NMEM], func=AF.Copy), reads=[ps_b], writes=[KT_b])
    def lin_k():
        W = self.wc["w_xk"]
        for c0 in (0, 512):
            wv, w_b = load_w(W[:, c0:c0 + 512], 8, 512)
            for j in range(4):
                ps, ps_b = kb.psum()
                for kt in range(8):
                    kb.op("pe", lambda e, ps=ps, wv=wv, kt=kt, j=j: e.matmul(ps[:, 0:NMEM], wv[:, kt, j * 128:(j + 1) * 128], memT[:, kt, :], start=(kt == 0), stop=(kt == 7)),
                          reads=[w_b, memT_b], writes=[ps_b])
                cons_k(c0 // 128 + j, ps, ps_b)
    lin_k()
    Wv = self.wc["w_xv"]
    for c0 in (0, 512):
        wv, w_b = load_w(Wv[:, c0:c0 + 512], 8, 512)
        for mt in range(2):
            ps, ps_b = kb.psum()
            for kt in range(8):
                kb.op("pe", lambda e, ps=ps, wv=wv, kt=kt, mt=mt: e.matmul(ps[:], memT[:, kt, mt * 128:(mt + 1) * 128], wv[:, kt, :], start=(kt == 0), stop=(kt == 7)),
                      reads=[w_b, memT_b], writes=[ps_b])
            kb.op("act", lambda e, ps=ps, mt=mt, c0=c0: e.activation(out=Vm[:, mt, c0:c0 + 512], in_=ps[:], func=AF.Copy), reads=[ps_b], writes=[Vm_b])
    if moe:
        wr32, wr32_b = kb.sb("wr32", [128, 8, 8], F32)
        kb.dma("sp", wr32[:], self.I("moe_w_router")[li2].rearrange("(k p) e -> p k e", p=128), writes=[wr32_b])
        wrh, wrh_b = kb.sb("wrh", [128, 2, 8, 8], BF16)
        wrr, wrr_b = kb.sb("wrr", [128, 8, 8], F32)
        kb.op("dve", lambda e: e.tensor_copy(out=wrh[:, 0], in_=wr32[:]), reads=[wr32_b], writes=[wrh_b])
        kb.op("dve", lambda e: e.tensor_tensor(out=wrr[:], in0=wr32[:], in1=wrh[:, 0], op=ALU.subtract), reads=[wr32_b, wrh_b], writes=[wrr_b])
        kb.op("dve", lambda e: e.tensor_copy(out=wrh[:, 1], in_=wrr[:]), reads=[wrr_b, wrh_b], writes=[wrh_b])
        brB, brB_b = kb.sb("brB", [128, 8], F32)
        kb.dma("sp", brB[:], self.I("moe_b_router")[li2].partition_broadcast(128), writes=[brB_b])
        sel, sel_b = kb.sb("sel", [8, 8, 128], BF16)
        self_f, self_f_b = kb.sb("self", [8, 8, 128], F32)
        kb.op("pool", lambda e: e.memset(self_f[:], 1.0), writes=[self_f_b])
        kb.op("pool", lambda e: e.affine_select(out=self_f[:], in_=self_f[:], pattern=[[1, 8], [0, 128]], compare_op=ALU.is_equal, fill=0.0, base=0, channel_multiplier=-1),
              reads=[self_f_b], writes=[self_f_b])
        kb.op("dve", lambda e: e.tensor_copy(out=sel[:], in_=self_f[:]), reads=[self_f_b], writes=[sel_b])
    xr, xr_b = kb.sb("xr", [128, 8, 512], F32)
    vv, vv_b = kb.sb("vv", [128, 8, 512], F32)
    GB = [kb.sb("G%d" % i, [128, 8, 512], BF16) for i in range(4)]
    m_u = kb.mark()
    ys, ys_b = kb.sb("ysg", [128, 12, 512], BF16)
    gt, gt_b = kb.sb("gtg", [128, 24, 512], BF16)
    kb.reset(m_u)
    nft = 11 if moe else 22
    hh, hh_b = kb.sb("hh", [128, nft, 512], BF16)
    if moe:
        macc, macc_b = kb.sb("macc", [128, 8, 512], F32)
    kb.reset(m_u + 36 * 1024)
    PT = [kb.sb("PT%d" % i, [128, 2, 512], BF16) for i in range(2)]
    tmpf = [kb.sb("tmpf%d" % i, [128, 512], F32) for i in range(4)]
    tmpb = [kb.sb("tmpb%d" % i, [128, 512], BF16) for i in range(3)]
    lnfence, lnfence_b = kb.sb("lnfence", [128, 8], F32)
    vv_hb = [Buf("vv_h0"), Buf("vv_h1")]
    st_mean, st_mean_b = kb.sb("st_mean", [128, 512], F32)
    st_rstd, st_rstd_b = kb.sb("st_rstd", [128, 512], F32)
    if moe:
        lg, lg_b = kb.sb("lg", [128, 4, 8], F32)
        gsm = {n_: kb.sb("g_" + n_, [128, 4, 8], F32) for n_ in ("eq1", "eq2", "lg2", "gate", "gr")}
        gs1 = {n_: kb.sb("s_" + n_, [128, 4], F32) for n_ in ("m1", "m2", "w1", "w2")}
        gTs, gTs_b = kb.sb("gTs", [8, 512], F32)
        gTh, gTh_b = kb.sb("gTh", [8, 2, 512], BF16)
        gTr, gTr_b = kb.sb("gTr", [8, 512], F32)
        xlo, xlo_b = kb.sb("xlo", [128, 8, 512], BF16)
        gbs, gbs_b = kb.sb("gbs", [128, 512], F32)
    if last:
        ostg = [kb.sb("ostg%d" % i, [128, D], F32) for i in range(2)]
    self._ti = 0

    def tf():
        t = tmpf[self._ti % 4]
        self._ti += 1
        return t

    self._tb = 0

    def tbf():
        t = tmpb[self._tb % 3]
        self._tb += 1
        return t

    def layer_norm(gi):
        (vb, vb_b), (vsq, vsq_b) = GB[1], GB[2]
        kb.op("pool", lambda e: e.tensor_copy(out=vb[:], in_=vv[:]), reads=[vv_b], writes=[vb_b])
        kb.op("act", lambda e: e.activation(out=vsq[:], in_=vv[:], func=AF.Square), reads=[vv_b], writes=[vsq_b])
        pm, pm_b = kb.psum()
        for kt in range(8):
            kb.op("pe", lambda e, kt=kt: e.matmul(pm[:], ones_m[:], vb[:, kt, :], start=(kt == 0), stop=(kt == 7)), reads=[ones_m_b, vb_b], writes=[pm_b])
        pq, pq_b = kb.psum()
        for kt in range(8):
            kb.op("pe", lambda e, kt=kt: e.matmul(pq[:], ones_m[:], vsq[:, kt, :], start=(kt == 0), stop=(kt == 7)), reads=[ones_m_b, vsq_b], writes=[pq_b])
        kb.op("act", lambda e: e.activation(out=st_mean[:], in_=pm[:], func=AF.Copy), reads=[pm_b], writes=[st_mean_b])
        (m2, m2_b) = tf()
        kb.op("dve", lambda e: e.tensor_tensor(out=m2[:], in0=st_mean[:], in1=st_mean[:], op=ALU.mult), reads=[st_mean_b], writes=[m2_b])
        kb.op("dve", lambda e: e.tensor_tensor(out=m2[:], in0=pq[:], in1=m2[:], op=ALU.subtract), reads=[pq_b, m2_b], writes=[m2_b])
        kb.op("dve", lambda e: e.tensor_scalar(out=m2[:], in0=m2[:], scalar1=0.0, scalar2=LN_EPS, op0=ALU.max, op1=ALU.add), reads=[m2_b], writes=[m2_b])
        kb.op("act", lambda e: e.activation(out=m2[:], in_=m2[:], func=AF.Sqrt), reads=[m2_b], writes=[m2_b])
        kb.op("dve", lambda e: e.reciprocal(out=st_rstd[:], in_=m2[:]), reads=[m2_b], writes=[st_rstd_b])
        xb_, xb_b_ = GB[3]
        for half, eng in ((0, "dve"), (1, "pool")):
            hs = slice(half * 4, half * 4 + 4)
            vb_half = vv_hb[half]
            kb.op(eng, lambda e, hs=hs: e.tensor_tensor(out=vv[:, hs, :], in0=vv[:, hs, :], in1=st_mean[:].unsqueeze(1).to_broadcast([128, 4, 512]), op=ALU.subtract),
                  reads=[vv_b, st_mean_b], writes=[vb_half])
            kb.op(eng, lambda e, hs=hs: e.tensor_tensor(out=vv[:, hs, :], in0=vv[:, hs, :], in1=st_rstd[:].unsqueeze(1).to_broadcast([128, 4, 512]), op=ALU.mult),
                  reads=[vb_half, st_rstd_b], writes=[vb_half])
        for kt in range(8):
            vb_half = vv_hb[kt // 4]
            kb.op("dve", lambda e, kt=kt: e.tensor_scalar(out=xr[:, kt, :], in0=vv[:, kt, :], scalar1=lnT[:, gi, kt:kt + 1], scalar2=lnT[:, gi + 1, kt:kt + 1], op0=ALU.mult, op1=ALU.add),
                  reads=[vb_half, lnT_b], writes=[xr_b])
            kb.op("pool", lambda e, kt=kt: e.tensor_scalar(out=xb_[:, kt, :], in0=vv[:, kt, :], scalar1=lnT[:, gi, kt:kt + 1], scalar2=lnT[:, gi + 1, kt:kt + 1], op0=ALU.mult, op1=ALU.add),
                  reads=[vb_half, lnT_b], writes=[xb_b_])
        kb.op("pool", lambda e: e.memset(lnfence[:], 0.0), reads=[vv_hb[0], vv_hb[1], xr_b, xb_b_], writes=[vv_b, lnfence_b])

    def resid_consume(ot, ps, ps_b):
        kb.op("dve", lambda e: e.scalar_tensor_tensor(out=vv[:, ot, :], in0=xr[:, ot, :], scalar=float(ALPHA), in1=ps[:], op0=ALU.mult, op1=ALU.add),
              reads=[xr_b, ps_b], writes=[vv_b])

    xres_v = self.xres.rearrange("(kt p) t -> p kt t", p=128)
    xTd_v = self.xTd.rearrange("(kt p) t -> p kt t", p=128)
    for tg in range(8):
        tsl = slice(tg * 512, (tg + 1) * 512)
        kb.dma("pool", xr[:], xres_v[:, :, tsl], writes=[xr_b])
        ysv = self.ysT.rearrange("(a p) t -> p a t", p=128)
        for a0 in range(0, 12, 6):
            kb.dma("pool", ys[:, a0:a0 + 6, :], ysv[:, a0:a0 + 6, tsl], writes=[ys_b, hh_b])
        gv = self.gT.rearrange("(a p) t -> p a t", p=128)
        for a0 in range(0, 24, 6):
            kb.dma("pool", gt[:, a0:a0 + 6, :], gv[:, a0:a0 + 6, tsl], writes=[gt_b, hh_b] + ([macc_b] if moe else []))
        mg, mg_b = GB[0]
        wbr = []
        for n_ in range(3):
            wbr.append(load_w(self.wc["w_branch"][n_], 4, D))
        for dt_ in range(8):
            pss = []
            for n_ in range(3):
                wv, w_b = wbr[n_]
                ps, ps_b = kb.psum()
                for ck in range(4):
                    kb.op("pe", lambda e, ps=ps, wv=wv, ck=ck, n_=n_, dt_=dt_: e.matmul(ps[:], wv[:, ck, dt_ * 128:(dt_ + 1) * 128], ys[:, n_ * 4 + ck, :], start=(ck == 0), stop=(ck == 3)),
                          reads=[w_b, ys_b], writes=[ps_b])
                pss.append((ps, ps_b))
            (a0_, a0_b), (a1_, a1_b), (a2_, a2_b) = tf(), tf(), tf()
            kb.op("dve", lambda e, p=pss[0][0], dt_=dt_, a0_=a0_: e.tensor_tensor(out=a0_[:], in0=p[:], in1=gt[:, dt_, :], op=ALU.mult), reads=[pss[0][1], gt_b], writes=[a0_b])
            kb.op("dve", lambda e, p=pss[1][0], dt_=dt_, a1_=a1_: e.tensor_tensor(out=a1_[:], in0=p[:], in1=gt[:, 8 + dt_, :], op=ALU.mult), reads=[pss[1][1], gt_b], writes=[a1_b])
            kb.op("dve", lambda e, p=pss[2][0], dt_=dt_, a2_=a2_: e.tensor_tensor(out=a2_[:], in0=p[:], in1=gt[:, 16 + dt_, :], op=ALU.mult), reads=[pss[2][1], gt_b], writes=[a2_b])
            kb.op("pool", lambda e, a0_=a0_, a1_=a1_: e.tensor_tensor(out=a0_[:], in0=a0_[:], in1=a1_[:], op=ALU.add), reads=[a0_b, a1_b], writes=[a0_b])
            kb.op("pool", lambda e, a0_=a0_, a2_=a2_, dt_=dt_: e.tensor_tensor(out=mg[:, dt_, :], in0=a0_[:], in1=a2_[:], op=ALU.add), reads=[a0_b, a2_b], writes=[mg_b])
        linear(self.wc["w_mix_out"], 8, D, lambda kt: mg[:, kt, :], [mg_b], resid_consume)
        layer_norm(0)
        xb, xb_b = GB[3]
        qT, qT_b = GB[0]

        def cons_q(ot, ps, ps_b):
            kb.op("act", lambda e: e.activation(out=qT[:, ot, :], in_=ps[:], func=AF.Copy, scale=1.0 / 16), reads=[ps_b], writes=[qT_b])
        linear(self.wc["w_xq"], 8, D, lambda kt: xb[:, kt, :], [xb_b], cons_q)
        oT, oT_b = GB[1]
        for h in range(4):
            pt, pt_b = PT[h % 2]
            for mt in range(2):
                ps, ps_b = kb.psum()
                for ee in range(2):
                    et = 2 * h + ee
                    kb.op("pe", lambda e, ps=ps, et=et, mt=mt, ee=ee: e.matmul(ps[:], KT[:, et, mt * 128:(mt + 1) * 128], qT[:, et, :], start=(ee == 0), stop=(ee == 1)),
                          reads=[KT_b, qT_b], writes=[ps_b])
                kb.op("act", lambda e, ps=ps, pt=pt, mt=mt: e.activation(out=pt[:, mt, :], in_=ps[:], func=AF.Exp), reads=[ps_b], writes=[pt_b])
            pd, pd_b = kb.psum()
            for mt in range(2):
                kb.op("pe", lambda e, pt=pt, mt=mt, pd=pd: e.matmul(pd[:], ones_1[:], pt[:, mt, :], start=(mt == 0), stop=(mt == 1)), reads=[ones_1_b, pt_b], writes=[pd_b])
            rd, rd_b = tf()
            kb.op("dve", lambda e, rd=rd, pd=pd: e.reciprocal(out=rd[:], in_=pd[:]), reads=[pd_b], writes=[rd_b])
            for ee in range(2):
                et = 2 * h + ee
                ps, ps_b = kb.psum()
                for mt in range(2):
                    kb.op("pe", lambda e, ps=ps, et=et, mt=mt, pt=pt: e.matmul(ps[:], Vm[:, mt, et * 128:(et + 1) * 128], pt[:, mt, :], start=(mt == 0), stop=(mt == 1)),
                          reads=[Vm_b, pt_b], writes=[ps_b])
                kb.op("dve", lambda e, ps=ps, et=et, rd=rd: e.tensor_tensor(out=oT[:, et, :], in0=ps[:], in1=rd[:], op=ALU.mult), reads=[ps_b, rd_b], writes=[oT_b])
        linear(self.wc["w_xo"], 8, D, lambda kt: oT[:, kt, :], [oT_b], resid_consume)
        layer_norm(2)
        def swiglu(Wg, Wu, Wd, nft_, gate_sb=None, down_consume=None):
            gl = {}

            def cons_g(ot, ps, ps_b):
                sg, sg_b = tbf()
                kb.op("act", lambda e: e.activation(out=sg[:], in_=ps[:], func=AF.Silu), reads=[ps_b], writes=[sg_b])
                gl[ot] = (sg, sg_b)

            def cons_u(ot, ps, ps_b):
                sg, sg_b = gl[ot]
                if gate_sb is None:
                    kb.op("dve", lambda e: e.tensor_tensor(out=hh[:, ot, :], in0=ps[:], in1=sg[:], op=ALU.mult), reads=[ps_b, sg_b], writes=[hh_b])
                else:
                    t_, t_b = tf()
                    kb.op("dve", lambda e: e.tensor_tensor(out=t_[:], in0=ps[:], in1=sg[:], op=ALU.mult), reads=[ps_b, sg_b], writes=[t_b])
                    kb.op("pool", lambda e: e.tensor_tensor(out=hh[:, ot, :], in0=t_[:], in1=gate_sb[0][:], op=ALU.mult), reads=[t_b, gate_sb[1]], writes=[hh_b])
            for f0 in range(0, nft_, 3):
                f1 = min(nft_, f0 + 3)
                n_c = (f1 - f0) * 128
                for (W, cons) in ((Wg, cons_g), (Wu, cons_u)):
                    wv, w_b = load_w(W[:, f0 * 128:f0 * 128 + n_c], 8, n_c)
                    for j in range(f1 - f0):
                        ps, ps_b = kb.psum()
                        for kt in range(8):
                            kb.op("pe", lambda e, ps=ps, wv=wv, kt=kt, j=j: e.matmul(ps[:], wv[:, kt, j * 128:(j + 1) * 128], xb[:, kt, :], start=(kt == 0), stop=(kt == 7)),
                                  reads=[w_b, xb_b], writes=[ps_b])
                        cons(f0 + j, ps, ps_b)
            linear(Wd, nft_, D, lambda kt: hh[:, kt, :], [hh_b], down_consume)

        if not moe:
            swiglu(self.wc["ffn_w_gate"], self.wc["ffn_w_up"], self.wc["ffn_w_down"], 22, None, resid_consume)
        else:
            kb.op("dve", lambda e: e.tensor_tensor(out=vv[:], in0=xr[:], in1=xb[:], op=ALU.subtract), reads=[xr_b, xb_b], writes=[vv_b])
            kb.op("act", lambda e: e.activation(out=xlo[:], in_=vv[:], func=AF.Copy), reads=[vv_b], writes=[xlo_b])
            pl, pl_b = kb.psum()
            for tt in range(4):
                tk = slice(tt * 128, (tt + 1) * 128)
                combos = [(xb, xb_b, 0), (xb, xb_b, 1), (xlo, xlo_b, 0)]
                n_mm = 0
                for (xs, xs_b, wpart) in combos:
                    for kt in range(8):
                        kb.op("pe", lambda e, xs=xs, wpart=wpart, kt=kt, tk=tk, tt=tt, n_mm=n_mm, pl=pl: e.matmul(pl[:, tt * 8:(tt + 1) * 8], xs[:, kt, tk], wrh[:, wpart, kt, :], start=(n_mm == 0), stop=(n_mm == 23)),
                              reads=[xs_b, wrh_b], writes=[pl_b])
                        n_mm += 1
            kb.op("dve", lambda e, pl=pl: e.tensor_tensor(out=lg[:], in0=pl[:, 0:32].rearrange("p (t e) -> p t e", t=4), in1=brB[:].unsqueeze(1).to_broadcast([128, 4, 8]), op=ALU.add),
                  reads=[pl_b, brB_b], writes=[lg_b])
            GQ = Buf("gateq")
            eq1, eq2, lg2, gate, gr = (gsm[n_][0] for n_ in ("eq1", "eq2", "lg2", "gate", "gr"))
            m1, m2_, w1, w2 = (gs1[n_][0] for n_ in ("m1", "m2", "w1", "w2"))

            def gd(fn, eng="dve"):
                kb.op(eng, fn, reads=[GQ, lg_b], writes=[GQ])
            gd(lambda e: e.tensor_reduce(out=m1[:], in_=lg[:], axis=AX.X, op=ALU.max))
            gd(lambda e: e.tensor_tensor(out=eq1[:], in0=lg[:], in1=m1[:].unsqueeze(2).to_broadcast([128, 4, 8]), op=ALU.is_equal))
            gd(lambda e: e.scalar_tensor_tensor(out=lg2[:], in0=eq1[:], scalar=-1.0e30, in1=lg[:], op0=ALU.mult, op1=ALU.add))
            gd(lambda e: e.tensor_reduce(out=m2_[:], in_=lg2[:], axis=AX.X, op=ALU.max))
            gd(lambda e: e.tensor_tensor(out=eq2[:], in0=lg2[:], in1=m2_[:].unsqueeze(2).to_broadcast([128, 4, 8]), op=ALU.is_equal))
            gd(lambda e: e.tensor_tensor(out=w2[:], in0=m1[:], in1=m2_[:], op=ALU.subtract))
            gd(lambda e: e.activation(out=w1[:], in_=w2[:], func=AF.Sigmoid), "act")
            gd(lambda e: e.tensor_scalar(out=w2[:], in0=w1[:], scalar1=-1.0, scalar2=1.0, op0=ALU.mult, op1=ALU.add))
            gd(lambda e: e.tensor_tensor(out=gate[:], in0=eq1[:], in1=w1[:].unsqueeze(2).to_broadcast([128, 4, 8]), op=ALU.mult))
            gd(lambda e: e.tensor_tensor(out=gr[:], in0=eq2[:], in1=w2[:].unsqueeze(2).to_broadcast([128, 4, 8]), op=ALU.mult))
            gd(lambda e: e.tensor_tensor(out=gate[:], in0=gate[:], in1=gr[:], op=ALU.add))
            pg, pg_b = kb.psum()
            for tt in range(4):
                kb.op("pe", lambda e, tt=tt, pg=pg: e.transpose(pg[0:8, tt * 128:(tt + 1) * 128], gate[:, tt, :], idf[:]), reads=[GQ, idf_b], writes=[pg_b])
            kb.op("dve", lambda e, pg=pg: e.tensor_copy(out=gTs[:], in_=pg[0:8, :]), reads=[pg_b], writes=[gTs_b])
            kb.op("dve", lambda e: e.tensor_copy(out=gTh[:, 0, :], in_=gTs[:]), reads=[gTs_b], writes=[gTh_b])
            kb.op("dve", lambda e: e.tensor_tensor(out=gTr[:], in0=gTs[:], in1=gTh[:, 0, :], op=ALU.subtract), reads=[gTs_b, gTh_b], writes=[gTr_b])
            kb.op("dve", lambda e: e.tensor_copy(out=gTh[:, 1, :], in_=gTr[:]), reads=[gTr_b, gTh_b], writes=[gTh_b])
            for ex in range(NEXP):
                pgb, pgb_b = kb.psum()
                for part in range(2):
                    kb.op("pe", lambda e, ex=ex, part=part, pgb=pgb: e.matmul(pgb[:], sel[:, ex, :], gTh[:, part, :], start=(part == 0), stop=(part == 1)),
                          reads=[sel_b, gTh_b], writes=[pgb_b])
                kb.op("act", lambda e, pgb=pgb: e.activation(out=gbs[:], in_=pgb[:], func=AF.Copy), reads=[pgb_b], writes=[gbs_b])

                def dcons(ot, ps, ps_b, ex=ex):
                    if ex == 0:
                        kb.op("dve", lambda e: e.tensor_copy(out=macc[:, ot, :], in_=ps[:]), reads=[ps_b], writes=[macc_b])
                    elif ex < NEXP - 1:
                        kb.op("dve", lambda e: e.tensor_tensor(out=macc[:, ot, :], in0=macc[:, ot, :], in1=ps[:], op=ALU.add), reads=[ps_b, macc_b], writes=[macc_b])
                    else:
                        t_, t_b = tf()
                        kb.op("dve", lambda e: e.tensor_tensor(out=t_[:], in0=macc[:, ot, :], in1=ps[:], op=ALU.add), reads=[ps_b, macc_b], writes=[t_b])
                        kb.op("dve", lambda e: e.scalar_tensor_tensor(out=vv[:, ot, :], in0=xr[:, ot, :], scalar=float(ALPHA), in1=t_[:], op0=ALU.mult, op1=ALU.add),
                              reads=[xr_b, t_b], writes=[vv_b])
                swiglu(self.wc["moe_w_gate"][ex], self.wc["moe_w_up"][ex], self.wc["moe_w_down"][ex], 11, (gbs, gbs_b), dcons)
        layer_norm(4)
        kb.dma("pool", xres_v[:, :, tsl], xr[:], reads=[xr_b])
        kb.dma("pool", xTd_v[:, :, tsl], xb[:], reads=[xb_b])
        if last:
            for tt in range(4):
                og, og_b = ostg[tt % 2]
                for half in range(2):
                    ps, ps_b = kb.psum()
                    for j in range(4):
                        kt = half * 4 + j
                        kb.op("pe", lambda e, ps=ps, kt=kt, j=j, tt=tt: e.transpose(ps[:, j * 128:(j + 1) * 128], xr[:, kt, tt * 128:(tt + 1) * 128], idf[:]),
                              reads=[xr_b, idf_b], writes=[ps_b])
                    kb.op("act", lambda e, ps=ps, og=og, half=half: e.activation(out=og[:, half * 512:(half + 1) * 512], in_=ps[:], func=AF.Copy), reads=[ps_b], writes=[og_b])
                r0 = tg * 512 + tt * 128
                kb.dma("sp", self.out[r0:r0 + 128, :], og[:], reads=[og_b])
    kb.barrier()


Prog.stageC = _stageC
```

```python
import contextlib
import math

import numpy as np
import concourse.bass as bass
import concourse.mybir as mybir
from concourse.bass_utils import run_bass_kernel_spmd

F32 = mybir.dt.float32
BF16 = mybir.dt.bfloat16
I32 = mybir.dt.int32
AF = mybir.ActivationFunctionType
ALU = mybir.AluOpType
AX = mybir.AxisListType

CENG = ("pe", "act", "dve", "pool", "sp")

D = 1024
S = 4096
DEPTH = 4
NIN = 6664
BW = 512
DFF = 2816
NEXP = 8
DFE = 1408
NMEM = 256
ALPHA = (2 * DEPTH) ** 0.25
LN_EPS = 1e-5
ROPE_THETA = 500000.0
O_U, O_QD, O_KD, O_VD, O_QF, O_KF, O_VF, O_F, O_G = 0, 512, 1024, 1536, 2048, 2560, 3072, 3584, 3592
VA = 520


class Buf:
    __slots__ = ("name", "w", "r", "excl")

    def __init__(self, name="", excl=False):
        self.name = name
        self.w = None
        self.r = {}
        self.excl = excl


class Op:
    __slots__ = ("eng", "fn", "waits", "sig", "count", "dma")

    def __init__(self, eng, fn, dma=None):
        self.eng = eng
        self.fn = fn
        self.waits = []
        self.sig = False
        self.count = 0
        self.dma = dma


class KB:
    SB_LO = 16640
    SB_HI = 229376

    def __init__(self, nc, n_dma_slots=12):
        self.nc = nc
        self.ops = {e: [] for e in CENG}
        self.seen_c = {e: {s: -1 for s in CENG} for e in CENG}
        self.seen_d = {e: {} for e in CENG}
        self.dq = {"sp": [], "pool": [], "act": []}
        self.slot_cnt = {}
        self.slot_rr = {"sp": 0, "pool": 0, "act": 0}
        for q in self.dq:
            for i in range(n_dma_slots if q != "act" else 4):
                sid = (q, i)
                self.dq[q].append(sid)
                self.slot_cnt[sid] = 0
        self.sb_off = self.SB_LO
        self.n_alloc = 0
        self.ps = []
        self.ps_rr = 0
        self.pa_rr = 0

    def sb(self, name, shape, dtype):
        esz = {F32: 4, BF16: 2, I32: 4}[dtype]
        n = esz
        for d in shape[1:]:
            n *= d
        n = (n + 63) // 64 * 64
        off = self.sb_off
        assert off + n <= self.SB_HI, "SBUF overflow at %s: %d + %d" % (name, off, n)
        self.sb_off += n
        self.n_alloc += 1
        t = self.nc.alloc_sbuf_tensor_at("%s_%d" % (name, self.n_alloc), list(shape), dtype, offset=off)
        return t, Buf(name)

    def mark(self):
        return self.sb_off

    def reset(self, m):
        self.sb_off = m

    def psum(self):
        p = self.ps[self.ps_rr % 5]
        self.ps_rr += 1
        return p

    def psum_acc(self):
        p = self.ps[5 + self.pa_rr % 3]
        self.pa_rr += 1
        return p

    def _collect(self, eng, reads, writes, is_dma):
        ev = []
        for b in reads:
            if b.w is not None:
                ev.append(b.w)
            if b.excl:
                for k, e in b.r.items():
                    if e[0] == "d" or e[1] != eng:
                        ev.append(e)
        for b in writes:
            if b.w is not None:
                if is_dma or b.w[0] == "d" or b.w[1] != eng or eng != "pe":
                    ev.append(b.w)
            for k, e in b.r.items():
                if is_dma or e[0] == "d" or e[1] != eng or eng != "pe":
                    ev.append(e)
        return ev

    def _reduce(self, eng, evs):
        best_c = {}
        best_d = {}
        for e in evs:
            if e[0] == "c":
                if e[2] > best_c.get(e[1], -1):
                    best_c[e[1]] = e[2]
            else:
                if e[2] > best_d.get(e[1], -1):
                    best_d[e[1]] = e[2]
        out = []
        for s, i in best_c.items():
            if i > self.seen_c[eng][s]:
                self.seen_c[eng][s] = i
                out.append(("c", s, i))
                self.ops[s][i].sig = True
        for sl, v in best_d.items():
            if v > self.seen_d[eng].get(sl, 0):
                self.seen_d[eng][sl] = v
                out.append(("d", sl, v))
        return out

    def _update(self, ev, reads, writes, rkey):
        for b in reads:
            b.r[rkey] = ev
        for b in writes:
            b.w = ev
            b.r = {}

    def op(self, eng, fn, reads=(), writes=()):
        o = Op(eng, fn)
        evs = self._collect(eng, reads, writes, False)
        o.waits = self._reduce(eng, evs)
        idx = len(self.ops[eng])
        self.ops[eng].append(o)
        self._update(("c", eng, idx), reads, writes, eng)
        return o

    def dma(self, q, out, in_, reads=(), writes=(), **kw):
        slots = self.dq[q]
        sid = slots[self.slot_rr[q] % len(slots)]
        self.slot_rr[q] += 1
        evs = self._collect(q, reads, writes, True)
        if self.slot_cnt[sid] > 0:
            evs.append(("d", sid, self.slot_cnt[sid] * 16))
        self.slot_cnt[sid] += 1
        val = self.slot_cnt[sid] * 16
        o = Op(q, lambda e: e.dma_start(out=out, in_=in_, **kw), dma=(sid, val))
        o.waits = self._reduce(q, evs)
        self.ops[q].append(o)
        self._update(("d", sid, val), reads, writes, ("d", sid))
        return o

    def barrier(self):
        evs = []
        for e in CENG:
            if e != "dve":
                for i in range(len(self.ops[e]) - 1, -1, -1):
                    if self.ops[e][i].dma is None and self.ops[e][i].fn is not None:
                        evs.append(("c", e, i))
                        break
        dev = [("d", sid, c * 16) for sid, c in self.slot_cnt.items() if c > 0]
        tok = self.bar_tile
        o = Op("dve", lambda e: e.memset(tok[:], 0.0))
        evs += self._collect("dve", [], [self.bar_buf], False)
        o.waits = self._reduce("dve", evs + dev)
        idx = len(self.ops["dve"])
        self.ops["dve"].append(o)
        self._update(("c", "dve", idx), [], [self.bar_buf], "dve")
        for e in CENG:
            if e == "dve":
                continue
            o2 = Op(e, None)
            o2.waits = self._reduce(e, [("c", "dve", idx)] + dev)
            self.ops[e].append(o2)

    def finish(self, stack):
        nc = self.nc
        for e in CENG:
            c = 0
            for o in self.ops[e]:
                if o.sig:
                    c += 1
                    o.count = c
        sems = {e: stack.enter_context(nc.semaphore("s_" + e)) for e in CENG}
        dsems = {}
        for q, slots in self.dq.items():
            for sid in slots:
                if self.slot_cnt[sid] > 0:
                    dsems[sid] = stack.enter_context(nc.semaphore("d_%s%d" % sid))
        block = stack.enter_context(nc.Block())
        ops = self.ops

        def replay(ename):
            def run(eng):
                for o in ops[ename]:
                    for w in o.waits:
                        if w[0] == "c":
                            eng.wait_ge(sems[w[1]], ops[w[1]][w[2]].count)
                        else:
                            eng.wait_ge(dsems[w[1]], w[2])
                    if o.fn is None:
                        continue
                    ins = o.fn(eng)
                    if o.dma is not None:
                        ins.then_inc(dsems[o.dma[0]], 16)
                    elif o.sig:
                        ins.then_inc(sems[ename], 1)
            return run

        block.tensor(replay("pe"))
        block.scalar(replay("act"))
        block.vector(replay("dve"))
        block.gpsimd(replay("pool"))
        block.sync(replay("sp"))
        return {e: len(ops[e]) for e in CENG}


class Prog:
    def __init__(self, nc, stack, layers=range(DEPTH), stages="ABC", debug=()):
        self.nc = nc
        self.stack = stack
        self.kb = KB(nc)
        self.layers = list(layers)
        self.stages = stages
        self.debug = set(debug)
        self.inp = {}
        self.scr = {}

    def din(self, name, shape, dtype=F32):
        t = self.nc.dram_tensor(name, list(shape), dtype, kind="ExternalInput").ap()
        self.inp[name] = t
        return t

    def dscr(self, name, shape, dtype):
        kind = "ExternalOutput" if name in self.debug else "Internal"
        t = self.nc.dram_tensor(name, list(shape), dtype, kind=kind).ap()
        self.scr[name] = t
        return t

    SHAPES = {
        "x": ([S, D], F32), "mem": ([NMEM, D], F32), "positions": ([128, 32], I32),
        "w_in": ([DEPTH, D, NIN], F32), "b_forget": ([DEPTH, 8], F32),
        "ssm_lambda_re": ([DEPTH, 32, 64], F32), "ssm_lambda_im": ([DEPTH, 32, 64], F32), "ssm_log_dt": ([DEPTH, 32], F32),
        "ssm_b_re": ([DEPTH, 32, 64, 16], F32), "ssm_b_im": ([DEPTH, 32, 64, 16], F32),
        "ssm_c_re": ([DEPTH, 32, 16, 64], F32), "ssm_c_im": ([DEPTH, 32, 16, 64], F32),
        "ssm_d": ([DEPTH, 512], F32), "w_glu": ([DEPTH, 512, 512], F32), "w_branch": ([DEPTH, 3, 512, D], F32),
        "w_mix_out": ([DEPTH, D, D], F32), "ln_mix_g": ([DEPTH, D], F32), "ln_mix_b": ([DEPTH, D], F32),
        "w_xq": ([DEPTH, D, D], F32), "w_xk": ([DEPTH, D, D], F32), "w_xv": ([DEPTH, D, D], F32), "w_xo": ([DEPTH, D, D], F32),
        "ln_x_g": ([DEPTH, D], F32), "ln_x_b": ([DEPTH, D], F32),
        "ffn_w_gate": ([2, D, DFF], F32), "ffn_w_up": ([2, D, DFF], F32), "ffn_w_down": ([2, DFF, D], F32),
        "moe_w_router": ([2, D, NEXP], F32), "moe_b_router": ([2, NEXP], F32),
        "moe_w_gate": ([2, NEXP, D, DFE], F32), "moe_w_up": ([2, NEXP, D, DFE], F32), "moe_w_down": ([2, NEXP, DFE, D], F32),
        "ln_ffn_g": ([DEPTH, D], F32), "ln_ffn_b": ([DEPTH, D], F32),
    }

    def I(self, name):
        if name not in self.inp:
            shp, dt_ = self.SHAPES[name]
            self.din(name, shp, dt_)
        return self.inp[name]

    def declare(self):
        self.x = self.I("x")
        self.pos = self.I("positions")
        self.out = self.nc.dram_tensor("out", [S, D], F32, kind="ExternalOutput").ap()
        self.xres = self.dscr("xres", [D, S], F32)
        self.xTd = self.dscr("xTd", [D, S], BF16)
        self.u_tm = self.dscr("u_tm", [S, 512], BF16)
        self.qdT = self.dscr("qdT", [512, S], BF16)
        self.kdT = self.dscr("kdT", [512, S], BF16)
        self.vd = self.dscr("vd", [S, VA], BF16)
        self.qfT = self.dscr("qfT", [512, S], BF16)
        self.kfT = self.dscr("kfT", [512, S], BF16)
        self.vf = self.dscr("vf", [S, VA], BF16)
        self.fl = self.dscr("fl", [8, S], F32)
        self.gT = self.dscr("gT", [3 * D, S], BF16)
        self.ysT = self.dscr("ysT", [3 * BW, S], BF16)
        self.wcb = Buf("wconv")
        self.wc = {
            "w_branch": self.dscr("c_wbr", [3, 512, D], BF16), "w_mix_out": self.dscr("c_wo", [D, D], BF16),
            "w_xq": self.dscr("c_wq", [D, D], BF16), "w_xk": self.dscr("c_wk", [D, D], BF16),
            "w_xv": self.dscr("c_wv", [D, D], BF16), "w_xo": self.dscr("c_wxo", [D, D], BF16),
            "ffn_w_gate": self.dscr("c_fg", [D, DFF], BF16), "ffn_w_up": self.dscr("c_fu", [D, DFF], BF16), "ffn_w_down": self.dscr("c_fd", [DFF, D], BF16),
            "moe_w_gate": self.dscr("c_mg", [NEXP, D, DFE], BF16), "moe_w_up": self.dscr("c_mu", [NEXP, D, DFE], BF16), "moe_w_down": self.dscr("c_md", [NEXP, DFE, D], BF16),
        }
        self.dscr("augq", [8, 6, S], BF16)
        self.dscr("augk", [8, 6, S], BF16)

    def setup(self):
        kb, nc = self.kb, self.nc
        st = self.stack
        kb.ps = [(st.enter_context(nc.psum_tensor("ps%d" % i, [128, 512], F32)), Buf("ps%d" % i, True)) for i in range(8)]
        bar, bar_b = kb.sb("bar", [128, 8], F32)
        kb.bar_tile = bar
        kb.bar_buf = bar_b
        self.identf, self.identf_b = kb.sb("identf", [128, 128], F32)
        self.identb, self.identb_b = kb.sb("identb", [128, 128], BF16)
        idf, idb = self.identf, self.identb
        kb.op("pool", lambda e: e.memset(idf[:], 1.0), writes=[self.identf_b])
        kb.op("pool", lambda e: e.affine_select(out=idf[:], in_=idf[:], pattern=[[1, 128]], compare_op=ALU.is_equal,
                                                fill=0.0, base=0, channel_multiplier=-1),
              reads=[self.identf_b], writes=[self.identf_b])
        kb.op("dve", lambda e: e.tensor_copy(out=idb[:], in_=idf[:]), reads=[self.identf_b], writes=[self.identb_b])
        self.rot = {}
        for n_ in ["cos", "sin", "cosq", "sinq"]:
            self.rot[n_] = kb.sb("rot_" + n_, [128, 32, 8], F32)
        self.memT, self.memT_b = kb.sb("memT", [128, 8, NMEM], BF16)
        self._build_rot(kb.mark())
        self._build_mem()

    def _build_rot(self, m2):
        kb = self.kb
        posi, posi_b = kb.sb("posi2", [128, 32], I32)
        kb.dma("sp", posi[:], self.pos[:, :], writes=[posi_b])
        posf, posf_b = kb.sb("posf2", [128, 32], F32)
        kb.op("dve", lambda e: e.tensor_copy(out=posf[:], in_=posi[:]), reads=[posi_b], writes=[posf_b])
        ang, ang_b = kb.sb("ang2", [128, 32, 8], F32)
        for i in range(8):
            f32 = float(np.float32(ROPE_THETA ** (-(2.0 * i) / 16.0)))
            kb.op("dve", lambda e, i=i, f32=f32: e.tensor_scalar(out=ang[:, :, i], in0=posf[:], scalar1=f32, scalar2=None, op0=ALU.mult),
                  reads=[posf_b], writes=[ang_b])
        TWO_PI = 2.0 * math.pi
        C1 = 6.28125
        C2 = TWO_PI - C1

        def reduce_sin(dst, dst_b, shift, scale):
            a2, a2_b = kb.sb("a2", [128, 256], F32)
            kf, kf_b = kb.sb("kf", [128, 256], F32)
            ki, ki_b = kb.sb("ki", [128, 256], I32)
            angf = ang[:].rearrange("p a b -> p (a b)")
            kb.op("dve", lambda e: e.tensor_scalar(out=a2[:], in0=angf, scalar1=float(shift), scalar2=None, op0=ALU.add),
                  reads=[ang_b], writes=[a2_b])
            kb.op("dve", lambda e: e.tensor_scalar(out=kf[:], in0=a2[:], scalar1=float(1.0 / TWO_PI), scalar2=None, op0=ALU.mult),
                  reads=[a2_b], writes=[kf_b])
            kb.op("dve", lambda e: e.tensor_copy(out=ki[:], in_=kf[:]), reads=[kf_b], writes=[ki_b])
            kb.op("dve", lambda e: e.tensor_copy(out=kf[:], in_=ki[:]), reads=[ki_b], writes=[kf_b])
            kb.op("dve", lambda e: e.scalar_tensor_tensor(out=a2[:], in0=kf[:], scalar=-C1, in1=a2[:], op0=ALU.mult, op1=ALU.add),
                  reads=[kf_b, a2_b], writes=[a2_b])
            kb.op("dve", lambda e: e.scalar_tensor_tensor(out=a2[:], in0=kf[:], scalar=-C2, in1=a2[:], op0=ALU.mult, op1=ALU.add),
                  reads=[kf_b, a2_b], writes=[a2_b])
            kb.op("dve", lambda e: e.tensor_scalar(out=a2[:], in0=a2[:], scalar1=float(-math.pi), scalar2=float(math.pi), op0=ALU.max, op1=ALU.min),
                  reads=[a2_b], writes=[a2_b])
            d = dst[:].rearrange("p a b -> p (a b)")
            kb.op("act", lambda e: e.activation(out=d, in_=a2[:], func=AF.Sin), reads=[a2_b], writes=[dst_b])
            if scale != 1.0:
                kb.op("dve", lambda e: e.tensor_scalar(out=d, in0=d, scalar1=float(scale), scalar2=None, op0=ALU.mult),
                      reads=[dst_b], writes=[dst_b])

        reduce_sin(*self.rot["sin"], 0.0, 1.0)
        reduce_sin(*self.rot["cos"], math.pi / 2, 1.0)
        reduce_sin(*self.rot["sinq"], 0.0, 0.125)
        reduce_sin(*self.rot["cosq"], math.pi / 2, 0.125)
        kb.barrier()
        kb.reset(m2)
        self.arena0 = m2

    def _build_mem(self):
        kb = self.kb
        kb.reset(self.arena0)
        mem = self.I("mem")
        mi = [kb.sb("memin%d" % i, [128, D], F32) for i in range(2)]
        for mt in range(2):
            t, t_b = mi[mt]
            kb.dma("sp", t[:], mem[mt * 128:(mt + 1) * 128, :], writes=[t_b])
            for half in range(2):
                ps, ps_b = kb.psum()
                for j in range(4):
                    kt = half * 4 + j
                    kb.op("pe", lambda e, ps=ps, t=t, kt=kt, j=j: e.transpose(ps[:, j * 128:(j + 1) * 128], t[:, kt * 128:(kt + 1) * 128], self.identf[:]),
                          reads=[t_b, self.identf_b], writes=[ps_b])
                kb.op("act", lambda e, ps=ps, half=half, mt=mt: e.activation(out=self.memT[:, half * 4:half * 4 + 4, mt * 128:(mt + 1) * 128],
                                                                             in_=ps[:].rearrange("p (a b) -> p a b", a=4), func=AF.Copy),
                      reads=[ps_b], writes=[self.memT_b])
        kb.barrier()
        kb.reset(self.arena0)

    def stage0(self):
        kb = self.kb
        kb.reset(self.arena0)
        self.xT, self.xT_b = kb.sb("xT", [128, 8, S], BF16)
        xT = self.xT
        xin = [kb.sb("xin%d" % i, [128, D], F32) for i in range(2)]
        stg = [kb.sb("xstg%d" % i, [128, 8, 128], F32) for i in range(2)]
        for tt in range(32):
            xi, xi_b = xin[tt % 2]
            kb.dma("sp", xi[:], self.x[tt * 128:(tt + 1) * 128, :], writes=[xi_b])
            sg, sg_b = stg[tt % 2]
            for half in range(2):
                ps, ps_b = kb.psum()
                for j in range(4):
                    kt = half * 4 + j
                    kb.op("pe", lambda e, ps=ps, xi=xi, kt=kt, j=j: e.transpose(ps[:, j * 128:(j + 1) * 128], xi[:, kt * 128:(kt + 1) * 128],
                                                                               self.identf[:]),
                          reads=[xi_b, self.identf_b], writes=[ps_b])
                pv = ps[:].rearrange("p (a b) -> p a b", a=4)
                kb.op("act", lambda e, pv=pv, half=half, tt=tt: e.activation(out=xT[:, half * 4:half * 4 + 4, tt * 128:(tt + 1) * 128], in_=pv, func=AF.Copy),
                      reads=[ps_b], writes=[self.xT_b])
                kb.op("dve", lambda e, pv=pv, half=half, sg=sg: e.tensor_copy(out=sg[:, half * 4:half * 4 + 4, :], in_=pv),
                      reads=[ps_b], writes=[sg_b])
            kb.dma("sp", self.xres.rearrange("(kt p) t -> p kt t", p=128)[:, :, tt * 128:(tt + 1) * 128], sg[:], reads=[sg_b])
        kb.dma("sp", self.xTd.rearrange("(kt p) t -> p kt t", p=128), xT[:], reads=[self.xT_b])
        kb.barrier()

    def stageA(self, l):
        kb = self.kb
        kb.reset(self.arena0)
        self.xT, self.xT_b = kb.sb("xT", [128, 8, S], BF16)
        xT, xT_b = self.xT, self.xT_b
        kb.dma("sp", xT[:], self.xTd.rearrange("(kt p) t -> p kt t", p=128), writes=[xT_b])
        w = self.I("w_in")[l]
        wv = w.rearrange("(kt p) n -> p kt n", p=128)
        wbs = [kb.sb("wA%d" % i, [128, 8, 512], BF16) for i in range(3)]
        wb8, wb8_b = kb.sb("wA8", [128, 8, 8], BF16)
        self._wi = 0

        def load_w(c0):
            wb, wb_b = wbs[self._wi % 3]
            self._wi += 1
            kb.dma("pool", wb[:], wv[:, :, c0:c0 + 512], writes=[wb_b])
            return wb, wb_b

        stg_u = [kb.sb("stgu%d" % i, [128, 512], BF16) for i in range(3)]
        stg_v = [kb.sb("stgv%d" % i, [128, 8, 65], BF16) for i in range(3)]
        for sv, sv_b in stg_v:
            kb.op("pool", lambda e, sv=sv: e.memset(sv[:], 1.0), writes=[sv_b])
        stg_r = [kb.sb("stgr%d" % i, [128, 512], BF16) for i in range(3)]
        stg_t = [kb.sb("stgt%d" % i, [128, 4, 512], BF16) for i in range(2)]
        rtmp = [kb.sb("rtmp%d" % i, [128, 8, 8], F32) for i in range(4)]

        def tok_block(c0, kind):
            wb, wb_b = load_w(c0)
            for tt in range(32):
                ps, ps_b = kb.psum()
                for kt in range(8):
                    kb.op("pe", lambda e, ps=ps, wb=wb, kt=kt, tt=tt: e.matmul(ps[:], xT[:, kt, tt * 128:(tt + 1) * 128], wb[:, kt, :],
                                                                              start=(kt == 0), stop=(kt == 7)),
                          reads=[xT_b, wb_b], writes=[ps_b])
                if kind == "u":
                    sg, sg_b = stg_u[tt % 3]
                    kb.op("act", lambda e, sg=sg, ps=ps: e.activation(out=sg[:], in_=ps[:], func=AF.Copy), reads=[ps_b], writes=[sg_b])
                    kb.dma("sp", self.u_tm[tt * 128:(tt + 1) * 128, :], sg[:], reads=[sg_b])
                elif kind in ("vd", "vf"):
                    sg, sg_b = stg_v[tt % 3]
                    kb.op("act", lambda e, sg=sg, ps=ps: e.activation(out=sg[:, :, 0:64], in_=ps[:].rearrange("p (h e) -> p h e", h=8), func=AF.Copy),
                          reads=[ps_b], writes=[sg_b])
                    dst = self.vd if kind == "vd" else self.vf
                    kb.dma("sp", dst[tt * 128:(tt + 1) * 128, :], sg[:].rearrange("p h e -> p (h e)"), reads=[sg_b])
                else:
                    isq = kind == "qd"
                    sg, sg_b = stg_r[tt % 3]
                    kb.op("act", lambda e, sg=sg, ps=ps, isq=isq: e.activation(out=sg[:], in_=ps[:], func=AF.Copy, scale=(0.125 if isq else 1.0)),
                          reads=[ps_b], writes=[sg_b])
                    cs, cs_b = self.rot["cosq" if isq else "cos"]
                    sn, sn_b = self.rot["sinq" if isq else "sin"]
                    pv = ps[:].rearrange("p (h e) -> p h e", h=8)
                    sgv = sg[:].rearrange("p (h e) -> p h e", h=8)
                    t1, t2 = pv[:, :, 0:8], pv[:, :, 8:16]
                    cb = cs[:, tt:tt + 1, :].to_broadcast([128, 8, 8])
                    sb_ = sn[:, tt:tt + 1, :].to_broadcast([128, 8, 8])
                    (ra, ra_b), (rb, rb_b), (rc, rc_b), (rd, rd_b) = rtmp
                    kb.op("dve", lambda e, ra=ra, t1=t1, cb=cb: e.tensor_tensor(out=ra[:], in0=t1, in1=cb, op=ALU.mult), reads=[ps_b, cs_b], writes=[ra_b])
                    kb.op("dve", lambda e, rb=rb, t2=t2, sb_=sb_: e.tensor_tensor(out=rb[:], in0=t2, in1=sb_, op=ALU.mult), reads=[ps_b, sn_b], writes=[rb_b])
                    kb.op("dve", lambda e, rc=rc, t2=t2, cb=cb: e.tensor_tensor(out=rc[:], in0=t2, in1=cb, op=ALU.mult), reads=[ps_b, cs_b], writes=[rc_b])
                    kb.op("dve", lambda e, rd=rd, t1=t1, sb_=sb_: e.tensor_tensor(out=rd[:], in0=t1, in1=sb_, op=ALU.mult), reads=[ps_b, sn_b], writes=[rd_b])
                    kb.op("dve", lambda e, sgv=sgv, ra=ra, rb=rb: e.tensor_tensor(out=sgv[:, :, 0:8], in0=ra[:], in1=rb[:], op=ALU.subtract),
                          reads=[ra_b, rb_b, sg_b], writes=[sg_b])
                    kb.op("dve", lambda e, sgv=sgv, rc=rc, rd=rd: e.tensor_tensor(out=sgv[:, :, 8:16], in0=rc[:], in1=rd[:], op=ALU.add),
                          reads=[rc_b, rd_b, sg_b], writes=[sg_b])
                    ps2, ps2_b = kb.psum()
                    for j in range(4):
                        kb.op("pe", lambda e, ps2=ps2, sg=sg, j=j: e.matmul(ps2[:, j * 128:(j + 1) * 128], sg[:, j * 128:(j + 1) * 128], self.identb[:],
                                                                          start=True, stop=True),
                              reads=[sg_b, self.identb_b], writes=[ps2_b])
                    g4 = tt // 4
                    tq = tt % 4
                    tg, tg_b = stg_t[g4 % 2]
                    kb.op("act", lambda e, tg=tg, ps2=ps2, tq=tq: e.activation(out=tg[:, :, tq * 128:(tq + 1) * 128], in_=ps2[:].rearrange("p (a b) -> p a b", a=4), func=AF.Copy),
                          reads=[ps2_b], writes=[tg_b])
                    if tq == 3:
                        dst = self.qdT if isq else self.kdT
                        kb.dma("sp", dst.rearrange("(a p) t -> p a t", p=128)[:, :, g4 * 512:(g4 + 1) * 512], tg[:], reads=[tg_b])

        if "u" in self.blocksA:
            tok_block(O_U, "u")
        if "qd" in self.blocksA:
            tok_block(O_QD, "qd")
            tok_block(O_KD, "kd")
        if "vd" in self.blocksA:
            tok_block(O_VD, "vd")
        if "vf" in self.blocksA:
            tok_block(O_VF, "vf")

        stg_f = [kb.sb("stgf%d" % i, [128, S], BF16) for i in range(3)]
        self._fi = 0

        def feat_block(c0, kind, dst):
            wb, wb_b = load_w(c0)
            for j in range(4):
                sg, sg_b = stg_f[self._fi % 3]
                self._fi += 1
                for tg in range(8):
                    ps, ps_b = kb.psum()
                    for kt in range(8):
                        kb.op("pe", lambda e, ps=ps, wb=wb, kt=kt, tg=tg, j=j: e.matmul(ps[:], wb[:, kt, j * 128:(j + 1) * 128], xT[:, kt, tg * 512:(tg + 1) * 512],
                                                                                      start=(kt == 0), stop=(kt == 7)),
                              reads=[xT_b, wb_b], writes=[ps_b])
                    if kind == "g":
                        kb.op("act", lambda e, sg=sg, ps=ps, tg=tg: e.activation(out=sg[:, tg * 512:(tg + 1) * 512], in_=ps[:], func=AF.Sigmoid),
                              reads=[ps_b], writes=[sg_b])
                    else:
                        sc = 0.125 if kind == "qf" else 1.0
                        if tg % 2 == 0:
                            kb.op("act", lambda e, sg=sg, ps=ps, tg=tg, sc=sc: e.activation(out=sg[:, tg * 512:(tg + 1) * 512], in_=ps[:], func=AF.Copy, scale=sc),
                                  reads=[ps_b], writes=[sg_b])
                        else:
                            kb.op("dve", lambda e, sg=sg, ps=ps, tg=tg, sc=sc: e.tensor_scalar(out=sg[:, tg * 512:(tg + 1) * 512], in0=ps[:], scalar1=sc, scalar2=None, op0=ALU.mult),
                                  reads=[ps_b], writes=[sg_b])
                kb.dma("sp", dst[j * 128:(j + 1) * 128, :], sg[:], reads=[sg_b])

        if "qf" in self.blocksA:
            feat_block(O_QF, "qf", self.qfT)
            feat_block(O_KF, "kf", self.kfT)
        if "g" in self.blocksA:
            for gb in range(6):
                feat_block(O_G + gb * 512, "g", self.gT[gb * 512:(gb + 1) * 512, :])
        if "f" in self.blocksA:
            kb.dma("pool", wb8[:], wv[:, :, O_F:O_F + 8], writes=[wb8_b])
            fs, fs_b = kb.sb("fstg", [8, S], F32)
            for tg in range(8):
                ps, ps_b = kb.psum()
                for kt in range(8):
                    kb.op("pe", lambda e, ps=ps, kt=kt, tg=tg: e.matmul(ps[0:8, :], wb8[:, kt, :], xT[:, kt, tg * 512:(tg + 1) * 512], start=(kt == 0), stop=(kt == 7)),
                          reads=[xT_b, wb8_b], writes=[ps_b])
                kb.op("dve", lambda e, ps=ps, tg=tg: e.tensor_copy(out=fs[:, tg * 512:(tg + 1) * 512], in_=ps[0:8, :]), reads=[ps_b], writes=[fs_b])
            kb.dma("sp", self.fl[:, :], fs[:], reads=[fs_b])
        kb.barrier()

    blocksA = ("u", "qd", "vd", "vf", "qf", "g", "f")

    def convert_weights(self, l):
        kb = self.kb
        li2 = l // 2

        def conv(dst, src):
            K_ = src.shape[0]
            a_n = K_ // 128
            dv = dst.rearrange("(a p) n -> p a n", p=128)
            sv = src.rearrange("(a p) n -> p a n", p=128)
            for a0 in range(0, a_n, 8):
                a1 = min(a_n, a0 + 8)
                kb.dma("pool", dv[:, a0:a1, :], sv[:, a0:a1, :], writes=[self.wcb])
        for n_ in range(3):
            conv(self.wc["w_branch"][n_], self.I("w_branch")[l][n_])
        for nm in ("w_mix_out", "w_xq", "w_xk", "w_xv", "w_xo"):
            conv(self.wc[nm], self.I(nm)[l])
        if l % 2 == 0:
            for nm in ("ffn_w_gate", "ffn_w_up", "ffn_w_down"):
                conv(self.wc[nm], self.I(nm)[li2])
        else:
            for nm in ("moe_w_gate", "moe_w_up", "moe_w_down"):
                for ex in range(NEXP):
                    conv(self.wc[nm][ex], self.I(nm)[li2][ex])

    def build(self):
        self.declare()
        self.setup()
        if "N" not in self.stages:
            self.stage0()
        if "Z" in self.stages:
            kb = self.kb
            kb.reset(self.arena0)
            zt, zt_b = kb.sb("zfill", [128, S], BF16)
            kb.op("pool", lambda e: e.memset(zt[:], 0.0), writes=[zt_b])
            for a in range(12):
                kb.dma("sp", self.ysT[a * 128:(a + 1) * 128, :], zt[:], reads=[zt_b])
            kb.barrier()
        for l in self.layers:
            if "C" in self.stages:
                self.convert_weights(l)
            if "A" in self.stages:
                self.stageA(l)
            if "1" in self.stages:
                self.stageB1(l)
            if "2" in self.stages:
                self.stageB2(l)
            if "3" in self.stages:
                self.stageB3(l)
            if "C" in self.stages:
                self.stageC(l, l == DEPTH - 1 or l == self.layers[-1])
        self.kb.barrier()
        return self.kb.finish(self.stack)


def build_program(layers=range(DEPTH), stages="A123C", debug=()):
    nc = bass.Bass("TRN2", target_bir_lowering=False)
    stack = contextlib.ExitStack()
    with stack:
        p = Prog(nc, stack, layers, stages, debug)
        n = p.build()
    return nc, p, n


INPUT_NAMES = ["x", "mem", "positions", "w_in", "b_forget", "ssm_lambda_re", "ssm_lambda_im", "ssm_log_dt",
               "ssm_b_re", "ssm_b_im", "ssm_c_re", "ssm_c_im", "ssm_d", "w_glu", "w_branch", "w_mix_out",
               "ln_mix_g", "ln_mix_b", "w_xq", "w_xk", "w_xv", "w_xo", "ln_x_g", "ln_x_b",
               "ffn_w_gate", "ffn_w_up", "ffn_w_down", "moe_w_router", "moe_b_router",
               "moe_w_gate", "moe_w_up", "moe_w_down", "ln_ffn_g", "ln_ffn_b"]


def make_in_maps(inputs, n=8, names=None):
    maps = []
    names = names or INPUT_NAMES
    shared = {k: np.ascontiguousarray(np.asarray(inputs[k])) for k in names if k not in ("x", "mem", "positions")}
    x = np.asarray(inputs["x"])
    mem = np.asarray(inputs["mem"])
    pos = np.asarray(inputs["positions"])
    for i in range(n):
        m = dict(shared)
        if "x" in names:
            m["x"] = np.ascontiguousarray(x[i])
        if "mem" in names:
            m["mem"] = np.ascontiguousarray(mem[i])
        if "positions" in names:
            m["positions"] = np.ascontiguousarray(pos[i].astype(np.int32).reshape(32, 128).T)
        maps.append(m)
    return maps


def kernel(**inputs):
    nc, p, n = build_program()
    in_maps = make_in_maps(inputs, names=list(p.inp.keys()))
    res = run_bass_kernel_spmd(nc, in_maps, core_ids=list(range(8)))
    return np.stack([np.asarray(r["out"]) for r in res.results], axis=0).astype(np.float32)


def _stageB3(self, l):
    kb = self.kb
    kb.reset(self.arena0)
    NEG = -30000.0
    mk, mk_b = kb.sb("fmask", [128, 4, 512], BF16)
    mkf, mkf_b = kb.sb("fmaskf", [128, 512], F32)
    for j in range(4):
        kb.op("pool", lambda e: e.memset(mkf[:], 0.0), writes=[mkf_b])
        kb.op("pool", lambda e, j=j: e.affine_select(out=mkf[:], in_=mkf[:], pattern=[[1, 512]], compare_op=ALU.is_ge,
                                                     fill=NEG, base=-j * 128, channel_multiplier=-1),
              reads=[mkf_b], writes=[mkf_b])
        kb.op("dve", lambda e, j=j: e.tensor_copy(out=mk[:, j, :], in_=mkf[:]), reads=[mkf_b], writes=[mk_b])
    ones, ones_b = kb.sb("fones", [128, 64], BF16)
    kb.op("pool", lambda e: e.memset(ones[:], 1.0), writes=[ones_b])
    flt, flt_b = kb.sb("flt", [8, S], F32)
    kb.dma("sp", flt[:], self.fl[:, :], writes=[flt_b])
    bf_, bf_b = kb.sb("bfg", [8, 1], F32)
    kb.dma("sp", bf_[:], self.I("b_forget")[l].rearrange("(h o) -> h o", o=1), writes=[bf_b])
    nb, nb_b = kb.sb("nbfg", [8, 1], F32)
    kb.op("dve", lambda e: e.tensor_scalar(out=nb[:], in0=bf_[:], scalar1=-1.0, scalar2=None, op0=ALU.mult), reads=[bf_b], writes=[nb_b])
    kb.op("act", lambda e: e.activation(out=flt[:], in_=flt[:], func=AF.Exp, scale=-1.0, bias=nb[:]), reads=[flt_b, nb_b], writes=[flt_b])
    kb.op("act", lambda e: e.activation(out=flt[:], in_=flt[:], func=AF.Ln, bias=1.0), reads=[flt_b], writes=[flt_b])
    onesf, onesf_b = kb.sb("onesf", [8, S], F32)
    kb.op("pool", lambda e: e.memset(onesf[:], 1.0), writes=[onesf_b])
    ncum, ncum_b = kb.sb("ncum", [8, S], F32)
    kb.op("dve", lambda e: e.tensor_tensor_scan(out=ncum[:], data0=onesf[:], data1=flt[:], initial=0.0, op0=ALU.mult, op1=ALU.add),
          reads=[onesf_b, flt_b], writes=[ncum_b])
    augk, augk_b = kb.sb("augk", [8, 6, S], BF16)
    augq, augq_b = kb.sb("augq", [8, 6, S], BF16)
    kb.op("pool", lambda e: e.memset(augk[:, 0:3, :], 1.0), writes=[augk_b])
    kb.op("pool", lambda e: e.memset(augq[:, 3:6, :], 1.0), writes=[augq_b])
    rem, rem_b = kb.sb("rem", [8, S], F32)
    kb.op("dve", lambda e: e.tensor_copy(out=augk[:, 3, :], in_=ncum[:]), reads=[ncum_b, augk_b], writes=[augk_b])
    kb.op("dve", lambda e: e.tensor_tensor(out=rem[:], in0=ncum[:], in1=augk[:, 3, :], op=ALU.subtract), reads=[ncum_b, augk_b], writes=[rem_b])
    kb.op("dve", lambda e: e.tensor_copy(out=augk[:, 4, :], in_=rem[:]), reads=[rem_b, augk_b], writes=[augk_b])
    kb.op("dve", lambda e: e.tensor_tensor(out=rem[:], in0=rem[:], in1=augk[:, 4, :], op=ALU.subtract), reads=[rem_b, augk_b], writes=[rem_b])
    kb.op("dve", lambda e: e.tensor_copy(out=augk[:, 5, :], in_=rem[:]), reads=[rem_b, augk_b], writes=[augk_b])
    kb.op("dve", lambda e: e.tensor_scalar(out=augq[:, 0:3, :], in0=augk[:, 3:6, :], scalar1=-1.0, scalar2=None, op0=ALU.mult),
          reads=[augk_b, augq_b], writes=[augq_b])
    aqd = self.scr["augq"]
    akd = self.scr["augk"]
    kb.dma("sp", aqd[:, :, :], augq[:], reads=[augq_b])
    kb.dma("sp", akd[:, :, :], augk[:], reads=[augk_b])
    kb.barrier()
    kb.reset(self.arena0 + 4 * 1024 + 2048 + 256)
    vall, vall_b = kb.sb("fvall", [128, 32, VA], BF16)
    for k0 in range(0, 32, 8):
        kb.dma("sp", vall[:, k0:k0 + 8, :], self.vf.rearrange("(kt p) c -> p kt c", p=128)[:, k0:k0 + 8, :], writes=[vall_b])
    qk = [(kb.sb("fq%d" % i, [70, S], BF16), kb.sb("fk%d" % i, [70, S], BF16)) for i in range(2)]
    pts = [kb.sb("fpt%d" % i, [128, 512], BF16) for i in range(6)]
    osb = [kb.sb("fosb%d" % i, [65, 512], F32) for i in range(2)]
    rds = [kb.sb("frd%d" % i, [65, 512], F32) for i in range(2)]
    rhl = [kb.sb("frhl%d" % i, [65, 2, 512], BF16) for i in range(2)]
    yh = [kb.sb("fyh%d" % i, [64, S], BF16) for i in range(2)]
    self._it = 0
    for h in range(8):
        (qh, qh_b), (kh, kh_b) = qk[h % 2]
        kb.dma("sp", qh[0:64, :], self.qfT[h * 64:(h + 1) * 64, :], writes=[qh_b])
        kb.dma("sp", qh[64:70, :], aqd[h], writes=[qh_b])
        kb.dma("sp", kh[0:64, :], self.kfT[h * 64:(h + 1) * 64, :], writes=[kh_b])
        kb.dma("sp", kh[64:70, :], akd[h], writes=[kh_b])
        yo, yo_b = yh[h % 2]
        items = [(g, kbi) for g in range(8) for kbi in range(4 * g + 4)]
        pend = {}
        pos = {}
        defer = []

        def emit_qk(i, qh=qh, kh=kh, qh_b=qh_b, kh_b=kh_b):
            g, kbi = items[i]
            ps, ps_b = kb.psum()
            diag = kbi >= 4 * g
            kb.op("pe", lambda e, ps=ps, kh=kh, qh=qh, kbi=kbi, g=g, diag=diag: e.matmul(ps[:], kh[0:70, kbi * 128:(kbi + 1) * 128], qh[0:70, g * 512:(g + 1) * 512],
                                                                                     start=True, stop=not diag),
                  reads=[kh_b, qh_b], writes=[ps_b])
            if diag:
                j = kbi - 4 * g
                kb.op("pe", lambda e, ps=ps, j=j: e.matmul(ps[:], self.identb[:], mk[:, j, :], start=False, stop=True),
                      reads=[self.identb_b, mk_b], writes=[ps_b])
            pt, pt_b = pts[self._it % len(pts)]
            self._it += 1
            kb.op("act", lambda e, pt=pt, ps=ps: e.activation(out=pt[:], in_=ps[:], func=AF.Exp), reads=[ps_b], writes=[pt_b])
            pend[i] = (pt, pt_b)

        def emit_pv(i, h=h, yo=yo, yo_b=yo_b):
            g, kbi = items[i]
            nkb = 4 * g + 4
            if kbi == 0:
                pos[g] = kb.psum_acc()
            po, po_b = pos[g]
            pt, pt_b = pend.pop(i)
            kb.op("pe", lambda e, po=po, pt=pt, kbi=kbi, h=h, nkb=nkb: e.matmul(po[0:65, :], vall[:, kbi, h * 65:(h + 1) * 65], pt[:],
                                                                                start=(kbi == 0), stop=(kbi == nkb - 1)),
                  reads=[vall_b, pt_b], writes=[po_b])
            if kbi == nkb - 1:
                fin = self._attn_finalize_split(po, po_b, osb[g % 2], rds[g % 2], rhl[g % 2], ones, ones_b, yo, yo_b, g)
                defer.append([3, fin])
        LA = 3
        for i in range(min(LA, len(items))):
            emit_qk(i)
        for i in range(len(items)):
            if i + LA < len(items):
                emit_qk(i + LA)
            emit_pv(i)
            for dref in list(defer):
                dref[0] -= 1
                if dref[0] <= 0:
                    dref[1]()
                    defer.remove(dref)
        for dref in defer:
            dref[1]()
        kb.dma("sp", self.ysT[2 * BW + h * 64:2 * BW + (h + 1) * 64, :], yo[:], reads=[yo_b])
    kb.barrier()


def _attn_finalize(self, po, po_b, osb_, rds_, rhl_, ones, ones_b, yo, yo_b, g):
    kb = self.kb
    (ob, ob_b), (rd, rd_b), (rh, rh_b) = osb_, rds_, rhl_
    kb.op("act", lambda e: e.activation(out=ob[:], in_=po[0:65, :], func=AF.Copy), reads=[po_b], writes=[ob_b])
    kb.op("dve", lambda e: e.reciprocal(out=rd[64:65, :], in_=ob[64:65, :]), reads=[ob_b], writes=[rd_b])
    kb.op("dve", lambda e: e.tensor_copy(out=rh[64:65, 0, :], in_=rd[64:65, :]), reads=[rd_b], writes=[rh_b])
    kb.op("dve", lambda e: e.tensor_tensor(out=rd[64:65, :], in0=rd[64:65, :], in1=rh[64:65, 0, :], op=ALU.subtract), reads=[rd_b, rh_b], writes=[rd_b])
    kb.op("dve", lambda e: e.tensor_copy(out=rh[64:65, 1, :], in_=rd[64:65, :]), reads=[rd_b, rh_b], writes=[rh_b])
    pb, pb_b = kb.psum()
    kb.op("pe", lambda e: e.matmul(pb[0:64, :], ones[64:65, 0:64], rh[64:65, 0, :], start=True, stop=False), reads=[ones_b, rh_b], writes=[pb_b])
    kb.op("pe", lambda e: e.matmul(pb[0:64, :], ones[64:65, 0:64], rh[64:65, 1, :], start=False, stop=True), reads=[ones_b, rh_b], writes=[pb_b])
    kb.op("dve", lambda e: e.tensor_tensor(out=yo[0:64, g * 512:(g + 1) * 512], in0=ob[0:64, :], in1=pb[0:64, :], op=ALU.mult),
          reads=[ob_b, pb_b], writes=[yo_b])


def _attn_finalize_split(self, po, po_b, osb_, rds_, rhl_, ones, ones_b, yo, yo_b, g):
    kb = self.kb
    (ob, ob_b), (rd, rd_b), (rh, rh_b) = osb_, rds_, rhl_
    kb.op("act", lambda e: e.activation(out=ob[:], in_=po[0:65, :], func=AF.Copy), reads=[po_b], writes=[ob_b])
    kb.op("dve", lambda e: e.reciprocal(out=rd[64:65, :], in_=ob[64:65, :]), reads=[ob_b], writes=[rd_b])
    kb.op("dve", lambda e: e.tensor_copy(out=rh[64:65, 0, :], in_=rd[64:65, :]), reads=[rd_b], writes=[rh_b])
    kb.op("dve", lambda e: e.tensor_tensor(out=rd[64:65, :], in0=rd[64:65, :], in1=rh[64:65, 0, :], op=ALU.subtract), reads=[rd_b, rh_b], writes=[rd_b])
    kb.op("dve", lambda e: e.tensor_copy(out=rh[64:65, 1, :], in_=rd[64:65, :]), reads=[rd_b, rh_b], writes=[rh_b])

    def part2():
        pb, pb_b = kb.psum()
        kb.op("pe", lambda e: e.matmul(pb[0:64, :], ones[64:65, 0:64], rh[64:65, 0, :], start=True, stop=False), reads=[ones_b, rh_b], writes=[pb_b])
        kb.op("pe", lambda e: e.matmul(pb[0:64, :], ones[64:65, 0:64], rh[64:65, 1, :], start=False, stop=True), reads=[ones_b, rh_b], writes=[pb_b])
        kb.op("dve", lambda e: e.tensor_tensor(out=yo[0:64, g * 512:(g + 1) * 512], in0=ob[0:64, :], in1=pb[0:64, :], op=ALU.mult),
              reads=[ob_b, pb_b], writes=[yo_b])
    return part2


Prog.stageB3 = _stageB3
Prog._attn_finalize = _attn_finalize
Prog._attn_finalize_split = _attn_finalize_split


def _stageB2(self, l):
    kb = self.kb
    kb.reset(self.arena0)
    NEG = -30000.0
    mk, mk_b = kb.sb("dmask", [128, 256], BF16)
    mkf, mkf_b = kb.sb("dmaskf", [128, 256], F32)
    kb.op("pool", lambda e: e.memset(mkf[:], 0.0), writes=[mkf_b])
    kb.op("pool", lambda e: e.affine_select(out=mkf[:, 0:128], in_=mkf[:, 0:128], pattern=[[1, 128]], compare_op=ALU.is_ge,
                                            fill=NEG, base=0, channel_multiplier=-1), reads=[mkf_b], writes=[mkf_b])
    kb.op("pool", lambda e: e.affine_select(out=mkf[:, 128:256], in_=mkf[:, 128:256], pattern=[[-1, 128]], compare_op=ALU.is_ge,
                                            fill=NEG, base=0, channel_multiplier=1), reads=[mkf_b], writes=[mkf_b])
    kb.op("dve", lambda e: e.tensor_copy(out=mk[:], in_=mkf[:]), reads=[mkf_b], writes=[mk_b])
    ones, ones_b = kb.sb("dones", [128, 64], BF16)
    kb.op("pool", lambda e: e.memset(ones[:], 1.0), writes=[ones_b])
    DILS = (1, 4, 16)
    vall = {}
    for d in DILS:
        t, t_b = kb.sb("dv%d" % d, [128, 32, VA], BF16)
        nm = 32 // d
        if d == 1:
            for k0 in range(0, 32, 8):
                kb.dma("sp", t[:, k0:k0 + 8, :], self.vd.rearrange("(m p) c -> p m c", p=128)[:, k0:k0 + 8, :], writes=[t_b])
        else:
            src = self.vd.rearrange("(m p r) c -> p r m c", p=128, r=d)
            for r in range(d):
                kb.dma("sp", t[:, r * nm:(r + 1) * nm, :], src[:, r, :, :], writes=[t_b])
        vall[d] = (t, t_b)
    qk = [(kb.sb("dq%d" % i, [128, S], BF16), kb.sb("dk%d" % i, [128, S], BF16)) for i in range(2)]
    pts = [kb.sb("dpt%d" % i, [128, 256], BF16) for i in range(6)]
    acc = [kb.sb("dacc%d" % i, [65, S], F32) for i in range(2)]
    osb = [kb.sb("dosb%d" % i, [65, 512], F32) for i in range(2)]
    rds = [kb.sb("drd%d" % i, [65, 512], F32) for i in range(2)]
    rhl = [kb.sb("drhl%d" % i, [65, 2, 512], BF16) for i in range(2)]
    yh = [kb.sb("dyh%d" % i, [64, S], BF16) for i in range(2)]
    self._it = 0
    for hp in range(4):
        (qt, qt_b), (kt_, kt_b) = qk[hp % 2]
        kb.dma("sp", qt[:], self.qdT[hp * 128:(hp + 1) * 128, :], writes=[qt_b])
        kb.dma("sp", kt_[:], self.kdT[hp * 128:(hp + 1) * 128, :], writes=[kt_b])
        for hh in range(2):
            h = hp * 2 + hh
            pb0 = hh * 64
            ac, ac_b = acc[h % 2]
            items = []
            for d in DILS:
                nm = 32 // d
                for r in range(d):
                    for m in range(nm):
                        items.append((d, r, m, nm))
            pend = {}
            pos = {}

            def emit_qk(i, pb0=pb0, kt_=kt_, qt=qt, kt_b=kt_b, qt_b=qt_b):
                d, r, m, nm = items[i]
                nq = 256 if m < nm - 1 else 128
                t0 = m * 128 * d + r
                ksl = slice(t0, t0 + 127 * d + 1, d)
                qsl = slice(t0, t0 + (nq - 1) * d + 1, d)
                ps, ps_b = kb.psum()
                kb.op("pe", lambda e, ps=ps, ksl=ksl, qsl=qsl, nq=nq, pb0=pb0, kt_=kt_, qt=qt: e.matmul(ps[:, 0:nq], kt_[pb0:pb0 + 64, ksl], qt[pb0:pb0 + 64, qsl], start=True, stop=False),
                      reads=[kt_b, qt_b], writes=[ps_b])
                kb.op("pe", lambda e, ps=ps, nq=nq: e.matmul(ps[:, 0:nq], self.identb[:], mk[:, 0:nq], start=False, stop=True),
                      reads=[self.identb_b, mk_b], writes=[ps_b])
                pt, pt_b = pts[self._it % len(pts)]
                self._it += 1
                kb.op("act", lambda e, pt=pt, ps=ps, nq=nq: e.activation(out=pt[:, 0:nq], in_=ps[:, 0:nq], func=AF.Exp), reads=[ps_b], writes=[pt_b])
                pend[i] = (pt, pt_b, nq, t0)

            def emit_pv(i, h=h, ac=ac, ac_b=ac_b):
                d, r, m, nm = items[i]
                vt, vt_b = vall[d]
                b = r * nm + m
                pt, pt_b, nq, t0 = pend.pop(i)
                if m == 0:
                    pos[(d, r, 0)] = kb.psum_acc()
                po, po_b = pos.pop((d, r, m))
                kb.op("pe", lambda e, po=po, vt=vt, b=b, h=h, pt=pt, m=m: e.matmul(po[0:65, 0:128], vt[:, b, h * 65:(h + 1) * 65], pt[:, 0:128], start=(m == 0), stop=True),
                      reads=[vt_b, pt_b], writes=[po_b])
                if nq == 256:
                    pos[(d, r, m + 1)] = kb.psum_acc()
                    po2, po2_b = pos[(d, r, m + 1)]
                    kb.op("pe", lambda e, po2=po2, vt=vt, b=b, h=h, pt=pt: e.matmul(po2[0:65, 0:128], vt[:, b, h * 65:(h + 1) * 65], pt[:, 128:256], start=True, stop=False),
                          reads=[vt_b, pt_b], writes=[po2_b])
                if d == 1:
                    kb.op("dve", lambda e, ac=ac, po=po, qs=slice(t0, t0 + 128): e.tensor_copy(out=ac[:, qs], in_=po[0:65, 0:128]),
                          reads=[po_b], writes=[ac_b])
                else:
                    qs = slice(t0, t0 + 127 * d + 1, d)
                    kb.op("dve", lambda e, ac=ac, po=po, qs=qs: e.tensor_tensor(out=ac[:, qs], in0=ac[:, qs], in1=po[0:65, 0:128], op=ALU.add),
                          reads=[po_b, ac_b], writes=[ac_b])
            LA = 3
            for i in range(min(LA, len(items))):
                emit_qk(i)
            for i in range(len(items)):
                if i + LA < len(items):
                    emit_qk(i + LA)
                emit_pv(i)
            yo, yo_b = yh[h % 2]
            for g in range(8):
                self._attn_finalize2(ac, ac_b, rds[g % 2], rhl[g % 2], ones, ones_b, yo, yo_b, g)
            kb.dma("sp", self.ysT[BW + h * 64:BW + (h + 1) * 64, :], yo[:], reads=[yo_b])
    kb.barrier()


def _attn_finalize2(self, ac, ac_b, rds_, rhl_, ones, ones_b, yo, yo_b, g):
    kb = self.kb
    (rd, rd_b), (rh, rh_b) = rds_, rhl_
    sl = slice(g * 512, (g + 1) * 512)
    kb.op("dve", lambda e: e.reciprocal(out=rd[64:65, :], in_=ac[64:65, sl]), reads=[ac_b], writes=[rd_b])
    kb.op("dve", lambda e: e.tensor_copy(out=rh[64:65, 0, :], in_=rd[64:65, :]), reads=[rd_b], writes=[rh_b])
    kb.op("dve", lambda e: e.tensor_tensor(out=rd[64:65, :], in0=rd[64:65, :], in1=rh[64:65, 0, :], op=ALU.subtract), reads=[rd_b, rh_b], writes=[rd_b])
    kb.op("dve", lambda e: e.tensor_copy(out=rh[64:65, 1, :], in_=rd[64:65, :]), reads=[rd_b, rh_b], writes=[rh_b])
    pb, pb_b = kb.psum()
    kb.op("pe", lambda e: e.matmul(pb[0:64, :], ones[64:65, 0:64], rh[64:65, 0, :], start=True, stop=False), reads=[ones_b, rh_b], writes=[pb_b])
    kb.op("pe", lambda e: e.matmul(pb[0:64, :], ones[64:65, 0:64], rh[64:65, 1, :], start=False, stop=True), reads=[ones_b, rh_b], writes=[pb_b])
    kb.op("dve", lambda e: e.tensor_tensor(out=yo[0:64, sl], in0=ac[0:64, sl], in1=pb[0:64, :], op=ALU.mult),
          reads=[ac_b, pb_b], writes=[yo_b])


Prog.stageB2 = _stageB2
Prog._attn_finalize2 = _attn_finalize2


def _stageB1(self, l):
    kb = self.kb
    kb.reset(self.arena0)
    PB = Buf("ssm_prep")
    idf, idf_b = self.identf, self.identf_b
    BSt, _ = kb.sb("BSt", [128, 2, 16, 2, 128], BF16)
    Gt, _ = kb.sb("Gt", [128, 2, 16, 2, 128], BF16)
    Tt, _ = kb.sb("Tt", [128, 32, 128], BF16)
    Dr, _ = kb.sb("Dr", [128, 16, 9], F32)
    Di, _ = kb.sb("Di", [128, 16, 9], F32)
    nDi, _ = kb.sb("nDi", [128, 16, 9], F32)
    m_keep = kb.mark()

    def T_(name, shape, dt_=F32):
        t, _ = kb.sb(name, shape, dt_)
        return t

    def dve(fn, extra_r=(), extra_w=()):
        kb.op("dve", fn, reads=[PB] + list(extra_r), writes=[PB] + list(extra_w))

    def tt(out, a, b, op):
        dve(lambda e: e.tensor_tensor(out=out, in0=a, in1=b, op=op))

    def ts(out, a, s1, op0, s2=None, op1=None):
        if op1 is None:
            dve(lambda e: e.tensor_scalar(out=out, in0=a, scalar1=s1, scalar2=None, op0=op0))
        else:
            dve(lambda e: e.tensor_scalar(out=out, in0=a, scalar1=s1, scalar2=s2, op0=op0, op1=op1))

    def cp(out, a):
        dve(lambda e: e.tensor_copy(out=out, in_=a))

    pp = T_("pp", [16, 3, 128])
    ld = T_("ld", [16, 2])
    kb.dma("sp", pp[:, 0, :], self.I("ssm_lambda_re")[l].rearrange("(q a) p -> q (a p)", a=2), writes=[PB])
    kb.dma("sp", pp[:, 1, :], self.I("ssm_lambda_im")[l].rearrange("(q a) p -> q (a p)", a=2), writes=[PB])
    kb.dma("sp", ld[:], self.I("ssm_log_dt")[l].rearrange("(q a) -> q a", a=2), writes=[PB])
    cp(pp[:, 2, :].rearrange("q (a p) -> q a p", a=2), ld[:].unsqueeze(2).to_broadcast([16, 2, 64]))
    par = T_("par", [128, 3, 16])
    ps, ps_b = kb.psum()
    for i in range(3):
        kb.op("pe", lambda e, i=i, ps=ps: e.transpose(ps[:, i * 16:(i + 1) * 16], pp[:, i, :], idf[0:16, 0:16]), reads=[PB, idf_b], writes=[ps_b])
    dve(lambda e, ps=ps: e.tensor_copy(out=par[:].rearrange("p a q -> p (a q)"), in_=ps[:, 0:48]), extra_r=[ps_b])
    lr, li, ldt = par[:, 0, :], par[:, 1, :], par[:, 2, :]
    Bre = T_("Bre", [128, 16, 16])
    Bim = T_("Bim", [128, 16, 16])
    for (dst, nm) in ((Bre, "ssm_b_re"), (Bim, "ssm_b_im")):
        src = self.I(nm)[l].rearrange("(q a) p c -> (a p) q c", a=2)
        for q0 in range(0, 16, 4):
            kb.dma("sp", dst[:, q0:q0 + 4, :], src[:, q0:q0 + 4, :], writes=[PB])
    Cre = T_("Cre", [128, 16, 16])
    Cim = T_("Cim", [128, 16, 16])
    Z = T_("Z", [32, 16, 128])
    zt = T_("zt", [128, 256])
    for (dst, nm) in ((Cre, "ssm_c_re"), (Cim, "ssm_c_im")):
        dve(lambda e: e.memset(Z[:], 0.0))
        src = self.I(nm)[l].rearrange("(q a) c p -> a c q p", a=2)
        kb.dma("sp", Z[0:16, :, 0:64], src[0], reads=[PB], writes=[PB])
        kb.dma("sp", Z[16:32, :, 64:128], src[1], reads=[PB], writes=[PB])
        for q in range(16):
            if q % 8 == 0:
                ps, ps_b = kb.psum()
            kb.op("pe", lambda e, ps=ps, q=q: e.transpose(ps[:, (q % 8) * 32:(q % 8) * 32 + 32], Z[:, q, :], idf[0:32, 0:32]), reads=[PB, idf_b], writes=[ps_b])
            if q % 8 == 7:
                q0 = q - 7
                dve(lambda e, ps=ps: e.tensor_copy(out=zt[:], in_=ps[:, 0:256]), extra_r=[ps_b])
                pv = zt[:].rearrange("p (q a c) -> p q a c", q=8, a=2)
                dve(lambda e, dst=dst, pv=pv, q0=q0: e.tensor_tensor(out=dst[:, q0:q0 + 8, :], in0=pv[:, :, 0, :], in1=pv[:, :, 1, :], op=ALU.add))
    def S_(name):
        return T_(name, [128, 16])
    dt = S_("dt")
    kb.op("act", lambda e: e.activation(out=dt[:], in_=ldt, func=AF.Exp), reads=[PB], writes=[PB])
    x = S_("x")
    tt(x[:], lr, dt[:], ALU.mult)
    er = S_("er")
    ts(er[:], x[:], 1.0 / 7, ALU.mult, 1.0, ALU.add)
    for k in (6, 5, 4, 3, 2, 1):
        tt(er[:], er[:], x[:], ALU.mult)
        ts(er[:], er[:], 1.0 / k, ALU.mult, 1.0, ALU.add)
    phi = S_("phi")
    tt(phi[:], li, dt[:], ALU.mult)
    ts(phi[:], phi[:], 1.0 / 32, ALU.mult)
    z = S_("z")
    tt(z[:], phi[:], phi[:], ALU.mult)
    cr = S_("cr")
    ci_ = S_("ci")
    cc = [1.0, -1.0 / 2, 1.0 / 24, -1.0 / 720, 1.0 / 40320, -1.0 / 3628800, 1.0 / 479001600]
    sc = [1.0, -1.0 / 6, 1.0 / 120, -1.0 / 5040, 1.0 / 362880, -1.0 / 39916800, 1.0 / 6227020800]
    for (dst, co) in ((cr, cc), (ci_, sc)):
        ts(dst[:], z[:], co[6], ALU.mult, co[5], ALU.add)
        for k in (4, 3, 2, 1, 0):
            tt(dst[:], dst[:], z[:], ALU.mult)
            ts(dst[:], dst[:], co[k], ALU.add)
    tt(ci_[:], ci_[:], phi[:], ALU.mult)
    t1, t2, t3, t4 = S_("t1"), S_("t2"), S_("t3"), S_("t4")

    def cmul(or_, oi, ar, ai, br, bi, a1=t1, a2=t2, a3=t3, a4=t4):
        tt(a1, ar, br, ALU.mult)
        tt(a2, ai, bi, ALU.mult)
        tt(a3, ar, bi, ALU.mult)
        tt(a4, ai, br, ALU.mult)
        tt(or_, a1, a2, ALU.subtract)
        tt(oi, a3, a4, ALU.add)

    for _ in range(5):
        cmul(cr[:], ci_[:], cr[:], ci_[:], cr[:], ci_[:], t1[:], t2[:], t3[:], t4[:])
        tt(t1[:], cr[:], cr[:], ALU.mult)
        tt(t2[:], ci_[:], ci_[:], ALU.mult)
        tt(t1[:], t1[:], t2[:], ALU.add)
        ts(t1[:], t1[:], -0.5, ALU.mult, 1.5, ALU.add)
        tt(cr[:], cr[:], t1[:], ALU.mult)
        tt(ci_[:], ci_[:], t1[:], ALU.mult)
    Pl_r = T_("Plr", [128, 9, 16])
    Pl_i = T_("Pli", [128, 9, 16])
    dve(lambda e: e.memset(Pl_r[:, 0, :], 1.0))
    dve(lambda e: e.memset(Pl_i[:, 0, :], 0.0))
    tt(Pl_r[:, 1, :], cr[:], er[:], ALU.mult)
    tt(Pl_i[:, 1, :], ci_[:], er[:], ALU.mult)
    ar, ai = Pl_r[:, 1, :], Pl_i[:, 1, :]
    for k in range(2, 9):
        cmul(Pl_r[:, k, :], Pl_i[:, k, :], Pl_r[:, k - 1, :], Pl_i[:, k - 1, :], ar, ai, t1[:], t2[:], t3[:], t4[:])
    Nl_r = T_("Nlr", [128, 8, 16])
    Nl_i = T_("Nli", [128, 8, 16])
    dve(lambda e: e.memset(Nl_r[:, 0, :], 1.0))
    dve(lambda e: e.memset(Nl_i[:, 0, :], 0.0))
    tt(t1[:], ar, ar, ALU.mult)
    tt(t2[:], ai, ai, ALU.mult)
    tt(t1[:], t1[:], t2[:], ALU.add)
    dve(lambda e: e.reciprocal(out=t1[:], in_=t1[:]))
    tt(Nl_r[:, 1, :], ar, t1[:], ALU.mult)
    tt(Nl_i[:, 1, :], ai, t1[:], ALU.mult)
    ts(Nl_i[:, 1, :], Nl_i[:, 1, :], -1.0, ALU.mult)
    for k in range(2, 8):
        cmul(Nl_r[:, k, :], Nl_i[:, k, :], Nl_r[:, k - 1, :], Nl_i[:, k - 1, :], Nl_r[:, 1, :], Nl_i[:, 1, :], t1[:], t2[:], t3[:], t4[:])
    cp(Dr[:, :, 0], Pl_r[:, 8, :])
    cp(Di[:, :, 0], Pl_i[:, 8, :])
    for k in range(1, 9):
        cmul(Dr[:, :, k], Di[:, :, k], Dr[:, :, k - 1], Di[:, :, k - 1], Dr[:, :, k - 1], Di[:, :, k - 1], t1[:], t2[:], t3[:], t4[:])
    ts(nDi[:], Di[:], -1.0, ALU.mult)
    qr, qi = S_("qr"), S_("qi")
    nr = S_("nr")
    ts(nr[:], ar, -1.0, ALU.add)
    tt(t1[:], lr, lr, ALU.mult)
    tt(t2[:], li, li, ALU.mult)
    tt(t1[:], t1[:], t2[:], ALU.add)
    dve(lambda e: e.reciprocal(out=t1[:], in_=t1[:]))
    tt(t2[:], nr[:], lr, ALU.mult)
    tt(t3[:], ai, li, ALU.mult)
    tt(t2[:], t2[:], t3[:], ALU.add)
    tt(qr[:], t2[:], t1[:], ALU.mult)
    tt(t2[:], ai, lr, ALU.mult)
    tt(t3[:], nr[:], li, ALU.mult)
    tt(t2[:], t2[:], t3[:], ALU.subtract)
    tt(qi[:], t2[:], t1[:], ALU.mult)
    Br2, Bi2 = T_("Br2", [128, 16, 16]), T_("Bi2", [128, 16, 16])
    u1, u2, u3, u4 = (T_("u%d" % i, [128, 16, 16]) for i in range(4))

    def bq(t):
        return t.unsqueeze(2).to_broadcast([128, 16, 16])
    cmul(Br2[:], Bi2[:], Bre[:], Bim[:], bq(qr[:]), bq(qi[:]), u1[:], u2[:], u3[:], u4[:])
    def Bg(name):
        return T_(name, [128, 16, 8, 16])
    g1, g2, g3, g4 = Bg("g1"), Bg("g2"), Bg("g3"), Bg("g4")
    Wm_r, Wm_i = Bg("Wmr"), Bg("Wmi")

    def over_k(t):
        return t.unsqueeze(2).to_broadcast([128, 16, 8, 16])

    def over_c(t):
        return t.rearrange("p k q -> p q k").unsqueeze(3).to_broadcast([128, 16, 8, 16])

    def over_kc(t):
        return t.unsqueeze(2).unsqueeze(3).to_broadcast([128, 16, 8, 16])

    cmul(Wm_r[:], Wm_i[:], over_k(Br2[:]), over_k(Bi2[:]), over_c(Nl_r[:, 0:8, :]), over_c(Nl_i[:, 0:8, :]), g1[:], g2[:], g3[:], g4[:])
    WmM = T_("WmM", [128, 2, 2, 16, 128], BF16)
    dve(lambda e: e.memset(WmM[:].rearrange("p a b q n -> p (a b q n)"), 0.0))
    for a in range(2):
        for part, src in ((0, Wm_r), (1, Wm_i)):
            cp(WmM[a * 64:(a + 1) * 64, a, part, :, :], src[a * 64:(a + 1) * 64].rearrange("p q k c -> p q (k c)"))
    W7_r, W7_i = Bg("W7r"), Bg("W7i")
    cmul(W7_r[:], W7_i[:], Wm_r[:], Wm_i[:], over_kc(Pl_r[:, 7, :]), over_kc(Pl_i[:, 7, :]), g1[:], g2[:], g3[:], g4[:])
    BSt_b = Buf("BSt")
    kb.op("pool", lambda e: e.memset(BSt[:].rearrange("p a q b n -> p (a q b n)"), 0.0), writes=[BSt_b])
    for part, src in ((0, W7_r), (1, W7_i)):
        for q in range(16):
            if q % 4 == 0:
                ps, ps_b = kb.psum()
            kb.op("pe", lambda e, ps=ps, q=q, src=src: e.transpose(ps[:, (q % 4) * 128:(q % 4) * 128 + 128], src[:, q].rearrange("p k c -> p (k c)"), idf[:]),
                  reads=[PB, idf_b], writes=[ps_b])
            if q % 4 == 3:
                for a in range(2):
                    pv = ps[:].rearrange("p (q n) -> p q n", q=4)[:, :, a * 64:(a + 1) * 64]
                    kb.op("act", lambda e, pv=pv, part=part, q=q, a=a: e.activation(out=BSt[:, part, q - 3:q + 1, a, a * 64:(a + 1) * 64], in_=pv, func=AF.Copy),
                          reads=[ps_b, BSt_b], writes=[BSt_b])
    Cp_r, Cp_i = Bg("Cpr"), Bg("Cpi")
    cmul(Cp_r[:], Cp_i[:], over_k(Cre[:]), over_k(Cim[:]), over_c(Pl_r[:, 0:8, :]), over_c(Pl_i[:, 0:8, :]), g1[:], g2[:], g3[:], g4[:])
    CpB = T_("CpB", [128, 2, 16, 128], BF16)
    cp(CpB[:, 0], Cp_r[:].rearrange("p q k c -> p q (k c)"))
    ts(CpB[:, 1], Cp_i[:].rearrange("p q k c -> p q (k c)"), -1.0, ALU.mult)
    G_r, G_i = W7_r, W7_i
    kb.op("dve", lambda e: e.memset(t1[:], 0.0), reads=[PB, BSt_b], writes=[PB])
    cmul(G_r[:], G_i[:], Cp_r[:], Cp_i[:], over_kc(ar), over_kc(ai), g1[:], g2[:], g3[:], g4[:])
    dve(lambda e: e.memset(Gt[:].rearrange("p a q b n -> p (a q b n)"), 0.0))
    for a in range(2):
        cp(Gt[a * 64:(a + 1) * 64, 0, :, a, :], G_r[a * 64:(a + 1) * 64].rearrange("p q k c -> p q (k c)"))
        ts(Gt[a * 64:(a + 1) * 64, 1, :, a, :], G_i[a * 64:(a + 1) * 64].rearrange("p q k c -> p q (k c)"), -1.0, ALU.mult)
    kidx_i = T_("kidx_i", [128, 1], I32)
    kidx = T_("kidx", [128, 1])
    jidx_i = T_("jidx_i", [128, 8, 16], I32)
    jidx = T_("jidx", [128, 128])
    cmask = T_("cmask", [128, 128])
    kb.op("pool", lambda e: e.iota(kidx_i[:], pattern=[[0, 1]], base=0, channel_multiplier=1), reads=[PB], writes=[PB])
    kb.op("pool", lambda e: e.iota(jidx_i[:], pattern=[[1, 8], [0, 16]], base=0, channel_multiplier=0), reads=[PB], writes=[PB])
    dve(lambda e: e.tensor_single_scalar(out=kidx_i[:], in_=kidx_i[:], scalar=4, op=ALU.arith_shift_right))
    cp(kidx[:], kidx_i[:])
    cp(jidx[:], jidx_i[:].rearrange("p a b -> p (a b)"))
    ts(cmask[:], jidx[:], kidx[:, 0:1], ALU.is_ge)
    dB = T_("dB", [128, 512])
    kb.dma("sp", dB[:], self.I("ssm_d")[l].partition_broadcast(128), writes=[PB])
    IDd = T_("IDd", [128, 32, 128], BF16)
    for g in range(32):
        dve(lambda e, g=g: e.tensor_tensor(out=IDd[:, g, :].rearrange("p (j c) -> p j c", j=8), in0=idf[:].rearrange("p (j c) -> p j c", j=8),
                                           in1=dB[:, g * 16:(g + 1) * 16].unsqueeze(1).to_broadcast([128, 8, 16]), op=ALU.mult), extra_r=[idf_b])
    Tt_b = Buf("Tt")
    for g in range(32):
        q, a = g // 2, g % 2
        if g % 4 == 0:
            ps, ps_b = kb.psum()
        sl = slice((g % 4) * 128, (g % 4) * 128 + 128)
        kb.op("pe", lambda e, ps=ps, sl=sl, q=q, a=a: e.matmul(ps[:, sl], WmM[:, a, 0, q, :], CpB[:, 0, q, :], start=True, stop=False), reads=[PB], writes=[ps_b])
        kb.op("pe", lambda e, ps=ps, sl=sl, q=q, a=a: e.matmul(ps[:, sl], WmM[:, a, 1, q, :], CpB[:, 1, q, :], start=False, stop=False), reads=[PB], writes=[ps_b])
        kb.op("pe", lambda e, ps=ps, sl=sl, g=g: e.matmul(ps[:, sl], self.identb[:], IDd[:, g, :], start=False, stop=True), reads=[PB, self.identb_b], writes=[ps_b])
        if g % 4 == 3:
            kb.op("dve", lambda e, ps=ps, g=g: e.tensor_tensor(out=Tt[:, g - 3:g + 1, :], in0=ps[:].rearrange("p (g n) -> p g n", g=4),
                                                               in1=cmask[:].unsqueeze(1).to_broadcast([128, 4, 128]), op=ALU.mult),
                  reads=[ps_b, PB], writes=[Tt_b])
    kb.barrier()
    kb.reset(m_keep)
    self._ssm_main(l, dict(BSt=BSt, Gt=Gt, Tt=Tt, Dr=Dr, Di=Di, nDi=nDi), m_keep)


def _ssm_main(self, l, tb, m0):
    kb = self.kb
    BSt, Gt, Tt, Dr, Di, nDi = tb["BSt"], tb["Gt"], tb["Tt"], tb["Dr"], tb["Di"], tb["nDi"]
    TB = Buf("ssm_tables")
    U, U_b = kb.sb("U", [128, 32, 512], BF16)
    m_x = kb.mark()
    xts = [kb.sb("Xc%d" % i, [128, 8, 512], BF16) for i in range(2)]
    x2s = [kb.sb("X2c%d" % i, [128, 32, 128], BF16) for i in range(2)]
    kb.reset(m_x)
    ygT, ygT_b = kb.sb("ygT", [128, 4, S], BF16)
    for ct in range(4):
        xt0, xt0_b = xts[ct % 2]
        kb.dma("sp", xt0[:].rearrange("p k c -> p (k c)"), self.u_tm[ct * 1024:(ct + 1) * 1024, :].rearrange("(p k) c -> p (k c)", k=8), writes=[xt0_b])
        xt, xt_b = x2s[ct % 2]
        kb.op("pool", lambda e, xt=xt, xt0=xt0: e.tensor_copy(out=xt[:].rearrange("p g (k c) -> p g k c", k=8),
                                                              in_=xt0[:].rearrange("p k (g c) -> p g k c", g=32)),
              reads=[xt0_b], writes=[xt_b])
        for g in range(32):
            if g % 4 == 0:
                ps, ps_b = kb.psum()
            kb.op("pe", lambda e, ps=ps, g=g, xt=xt: e.matmul(ps[:, (g % 4) * 128:(g % 4) * 128 + 128], xt[:, g, :], self.identb[:], start=True, stop=True),
                  reads=[xt_b, self.identb_b], writes=[ps_b])
            if g % 4 == 3:
                eng = "act" if (g // 4) % 2 == 0 else "dve"
                pv = ps[:].rearrange("p (g n) -> p g n", g=4)
                if eng == "act":
                    kb.op("act", lambda e, pv=pv, g=g, ct=ct: e.activation(out=U[:, g - 3:g + 1, ct * 128:(ct + 1) * 128], in_=pv, func=AF.Copy), reads=[ps_b], writes=[U_b])
                else:
                    kb.op("dve", lambda e, pv=pv, g=g, ct=ct: e.tensor_copy(out=U[:, g - 3:g + 1, ct * 128:(ct + 1) * 128], in_=pv), reads=[ps_b], writes=[U_b])
    Yg, Yg_b = kb.sb("Yg", [128, 4, 8, 512], BF16)
    SA = [kb.sb("SA%d" % i, [128, 2, 512], F32) for i in range(2)]
    SB_ = [kb.sb("SB%d" % i, [128, 2, 512], F32) for i in range(2)]
    Ssh = [kb.sb("Ssh%d" % i, [128, 2, 512], BF16) for i in range(2)]
    for q in range(16):
        (sa, sa_b), (sb2, sb2_b), (ssh, ssh_b) = SA[q % 2], SB_[q % 2], Ssh[q % 2]
        for part in range(2):
            ps, ps_b = kb.psum()
            for a in range(2):
                kb.op("pe", lambda e, ps=ps, part=part, a=a, q=q: e.matmul(ps[:], BSt[:, part, q, a, :], U[:, 2 * q + a, :], start=(a == 0), stop=(a == 1)),
                      reads=[U_b, TB], writes=[ps_b])
            kb.op("act", lambda e, ps=ps, part=part, sa=sa: e.activation(out=sa[:, part, :], in_=ps[:], func=AF.Copy), reads=[ps_b], writes=[sa_b])
        cur, cur_b, nxt, nxt_b = sa, sa_b, sb2, sb2_b
        for k in range(9):
            m = 1 << k
            dr, di, ndi = Dr[:, q, k:k + 1], Di[:, q, k:k + 1], nDi[:, q, k:k + 1]
            kb.op("pool", lambda e, cur=cur, nxt=nxt, m=m: e.tensor_copy(out=nxt[:, :, 0:m], in_=cur[:, :, 0:m]), reads=[cur_b], writes=[nxt_b])
            kb.op("dve", lambda e, cur=cur, nxt=nxt, m=m, dr=dr: e.scalar_tensor_tensor(out=nxt[:, 0, m:512], in0=cur[:, 0, 0:512 - m], scalar=dr, in1=cur[:, 0, m:512], op0=ALU.mult, op1=ALU.add),
                  reads=[cur_b, TB], writes=[nxt_b])
            kb.op("dve", lambda e, cur=cur, nxt=nxt, m=m, ndi=ndi: e.scalar_tensor_tensor(out=nxt[:, 0, m:512], in0=cur[:, 1, 0:512 - m], scalar=ndi, in1=nxt[:, 0, m:512], op0=ALU.mult, op1=ALU.add),
                  reads=[cur_b, nxt_b, TB], writes=[nxt_b])
            kb.op("dve", lambda e, cur=cur, nxt=nxt, m=m, di=di: e.scalar_tensor_tensor(out=nxt[:, 1, m:512], in0=cur[:, 0, 0:512 - m], scalar=di, in1=cur[:, 1, m:512], op0=ALU.mult, op1=ALU.add),
                  reads=[cur_b, TB], writes=[nxt_b])
            kb.op("dve", lambda e, cur=cur, nxt=nxt, m=m, dr=dr: e.scalar_tensor_tensor(out=nxt[:, 1, m:512], in0=cur[:, 1, 0:512 - m], scalar=dr, in1=nxt[:, 1, m:512], op0=ALU.mult, op1=ALU.add),
                  reads=[cur_b, nxt_b, TB], writes=[nxt_b])
            cur, cur_b, nxt, nxt_b = nxt, nxt_b, cur, cur_b
        kb.op("pool", lambda e, ssh=ssh: e.memset(ssh[:, :, 0:1], 0.0), writes=[ssh_b])
        kb.op("act", lambda e, ssh=ssh, cur=cur: e.activation(out=ssh[:, :, 1:512], in_=cur[:, :, 0:511], func=AF.Copy), reads=[cur_b, ssh_b], writes=[ssh_b])
        for ct in range(4):
            if ct % 2 == 0:
                ps, ps_b = kb.psum()
            o0 = (ct % 2) * 256
            csl = slice(ct * 128, (ct + 1) * 128)
            kb.op("pe", lambda e, ps=ps, o0=o0, csl=csl, ssh=ssh, q=q: e.matmul(ps[:, o0:o0 + 256], ssh[:, 0, csl], Gt[:, 0, q].rearrange("p a n -> p (a n)"), start=True, stop=False),
                  reads=[ssh_b, TB], writes=[ps_b])
            kb.op("pe", lambda e, ps=ps, o0=o0, csl=csl, ssh=ssh, q=q: e.matmul(ps[:, o0:o0 + 256], ssh[:, 1, csl], Gt[:, 1, q].rearrange("p a n -> p (a n)"), start=False, stop=False),
                  reads=[ssh_b, TB], writes=[ps_b])
            for a in range(2):
                kb.op("pe", lambda e, ps=ps, o0=o0, csl=csl, a=a, q=q: e.matmul(ps[:, o0 + a * 128:o0 + (a + 1) * 128], U[:, 2 * q + a, csl], Tt[:, 2 * q + a, :], start=False, stop=(a == 1)),
                      reads=[U_b, TB], writes=[ps_b])
            kb.op("act", lambda e, ps=ps, o0=o0, ct=ct, q=q: e.activation(out=Yg[:, ct, :, q * 32:(q + 1) * 32].rearrange("p j (a c) -> p a j c", a=2),
                                                                           in_=ps[:, o0:o0 + 256].rearrange("p (a j c) -> p a j c", a=2, j=8), func=AF.Gelu_apprx_tanh),
                  reads=[ps_b], writes=[Yg_b])
    ev = 0
    for ct in range(4):
        for cht in range(4):
            for jh in range(2):
                ps, ps_b = kb.psum()
                for jj in range(4):
                    j = jh * 4 + jj
                    kb.op("pe", lambda e, ps=ps, jj=jj, j=j, ct=ct, cht=cht: e.matmul(ps[:, jj * 128:(jj + 1) * 128], Yg[:, ct, j, cht * 128:(cht + 1) * 128], self.identb[:], start=True, stop=True),
                          reads=[Yg_b, self.identb_b], writes=[ps_b])
                t0 = ct * 1024 + jh * 4
                dst = ygT[:, cht, ct * 1024:(ct + 1) * 1024].rearrange("p (c j) -> p j c", j=8)[:, jh * 4:jh * 4 + 4, :]
                pv = ps[:].rearrange("p (j c) -> p j c", j=4)
                if ev % 2 == 0:
                    kb.op("act", lambda e, dst=dst, pv=pv: e.activation(out=dst, in_=pv, func=AF.Copy), reads=[ps_b], writes=[ygT_b])
                else:
                    kb.op("dve", lambda e, dst=dst, pv=pv: e.tensor_copy(out=dst, in_=pv), reads=[ps_b], writes=[ygT_b])
                ev += 1
    wg, wg_b = kb.sb("wglu", [128, 4, 512], BF16)
    kb.dma("pool", wg[:], self.I("w_glu")[l].rearrange("(kt p) n -> p kt n", p=128), writes=[wg_b])
    sgs = [kb.sb("sg%d" % i, [128, 512], BF16) for i in range(3)]
    yos = [kb.sb("yo%d" % i, [128, S], BF16) for i in range(2)]
    it = 0
    for mo in range(4):
        yo, yo_b = yos[mo % 2]
        for tg in range(8):
            ps, ps_b = kb.psum()
            tsl = slice(tg * 512, (tg + 1) * 512)
            for kt in range(4):
                kb.op("pe", lambda e, ps=ps, kt=kt, mo=mo, tsl=tsl: e.matmul(ps[:], wg[:, kt, mo * 128:(mo + 1) * 128], ygT[:, kt, tsl], start=(kt == 0), stop=(kt == 3)),
                      reads=[wg_b, ygT_b], writes=[ps_b])
            sg, sg_b = sgs[it % 3]
            it += 1
            kb.op("act", lambda e, sg=sg, ps=ps: e.activation(out=sg[:], in_=ps[:], func=AF.Sigmoid), reads=[ps_b], writes=[sg_b])
            kb.op("pool", lambda e, yo=yo, sg=sg, mo=mo, tsl=tsl: e.tensor_tensor(out=yo[:, tsl], in0=ygT[:, mo, tsl], in1=sg[:], op=ALU.mult),
                  reads=[ygT_b, sg_b], writes=[yo_b])
        kb.dma("sp", self.ysT[mo * 128:(mo + 1) * 128, :], yo[:], reads=[yo_b])
    kb.barrier()


Prog.stageB1 = _stageB1
Prog._ssm_main = _ssm_main


def _stageC(self, l, last):
    kb = self.kb
    kb.reset(self.arena0)
    idf, idf_b = self.identf, self.identf_b
    moe = (l % 2 == 1)
    li2 = l // 2
    ones_m, ones_m_b = kb.sb("ones_m", [128, 128], BF16)
    ones_1, ones_1_b = kb.sb("ones_1", [128, 128], BF16)
    kb.op("pool", lambda e: e.memset(ones_m[:], 1.0 / 1024), writes=[ones_m_b])
    kb.op("pool", lambda e: e.memset(ones_1[:], 1.0), writes=[ones_1_b])
    lnp, lnp_b = kb.sb("lnp", [8, 6, 128], F32)
    for i, nm in enumerate(["ln_mix_g", "ln_mix_b", "ln_x_g", "ln_x_b", "ln_ffn_g", "ln_ffn_b"]):
        kb.dma("sp", lnp[:, i, :], self.I(nm)[l].rearrange("(kt p) -> kt p", p=128), writes=[lnp_b])
    lnT, lnT_b = kb.sb("lnT", [128, 6, 8], F32)
    ps, ps_b = kb.psum()
    for i in range(6):
        kb.op("pe", lambda e, i=i, ps=ps: e.transpose(ps[:, i * 8:(i + 1) * 8], lnp[:, i, :], idf[0:8, 0:8]), reads=[lnp_b, idf_b], writes=[ps_b])
    kb.op("dve", lambda e, ps=ps: e.tensor_copy(out=lnT[:].rearrange("p a b -> p (a b)"), in_=ps[:, 0:48]), reads=[ps_b], writes=[lnT_b])
    NW = 4 if moe else 6
    wraw = [kb.sb("wC%d" % i, [128, 4096], BF16) for i in range(NW)]
    self._wc = 0

    def wbuf():
        t = wraw[self._wc % NW]
        self._wc += 1
        return t

    def load_w(src, kt_n, ncols):
        t, t_b = wbuf()
        v = t[:, 0:kt_n * ncols].rearrange("p (k n) -> p k n", k=kt_n)
        sv = src.rearrange("(k p) n -> p k n", p=128)
        for k0 in range(0, kt_n, 8):
            k1 = min(kt_n, k0 + 8)
            kb.dma("sp", v[:, k0:k1, :], sv[:, k0:k1, :], reads=[self.wcb], writes=[t_b])
        return v, t_b

    def linear(W, kt_n, n_out, rhs_fn, rhs_bufs, consume):
        cpc = 512
        while kt_n * cpc * 2 > 8192:
            cpc //= 2
        c0 = 0
        while c0 < n_out:
            nc_ = min(cpc, n_out - c0)
            wv, w_b = load_w(W[:, c0:c0 + nc_], kt_n, nc_)
            for j in range(nc_ // 128):
                ps, ps_b = kb.psum()
                for kt in range(kt_n):
                    kb.op("pe", lambda e, ps=ps, wv=wv, kt=kt, j=j: e.matmul(ps[:], wv[:, kt, j * 128:(j + 1) * 128], rhs_fn(kt), start=(kt == 0), stop=(kt == kt_n - 1)),
                          reads=[w_b] + rhs_bufs, writes=[ps_b])
                consume((c0 // 128) + j, ps, ps_b)
            c0 += nc_

    memT, memT_b = self.memT, self.memT_b
    KT, KT_b = kb.sb("KT", [128, 8, NMEM], BF16)
    Vm, Vm_b = kb.sb("Vm", [128, 2, D], BF16)

    def cons_k(ot, ps, ps_b):
        kb.op("act", lambda e: e.activation(out=KT[:, ot, :], in_=ps[:, 0:NMEM], func=AF.Copy), reads=[ps_b], writes=[KT_b])
    def lin_k():
        W = self.wc["w_xk"]
        for c0 in (0, 512):
            wv, w_b = load_w(W[:, c0:c0 + 512], 8, 512)
            for j in range(4):
                ps, ps_b = kb.psum()
                for kt in range(8):
                    kb.op("pe", lambda e, ps=ps, wv=wv, kt=kt, j=j: e.matmul(ps[:, 0:NMEM], wv[:, kt, j * 128:(j + 1) * 128], memT[:, kt, :], start=(kt == 0), stop=(kt == 7)),
                          reads=[w_b, memT_b], writes=[ps_b])
                cons_k(c0 // 128 + j, ps, ps_b)
    lin_k()
    Wv = self.wc["w_xv"]
    for c0 in (0, 512):
        wv, w_b = load_w(Wv[:, c0:c0 + 512], 8, 512)
        for mt in range(2):
            ps, ps_b = kb.psum()
            for kt in range(8):
                kb.op("pe", lambda e, ps=ps, wv=wv, kt=kt, mt=mt: e.matmul(ps[:], memT[:, kt, mt * 128:(mt + 1) * 128], wv[:, kt, :], start=(kt == 0), stop=(kt == 7)),
                      reads=[w_b, memT_b], writes=[ps_b])
            kb.op("act", lambda e, ps=ps, mt=mt, c0=c0: e.activation(out=Vm[:, mt, c0:c0 + 512], in_=ps[:], func=AF.Copy), reads=[ps_b], writes=[Vm_b])
    if moe:
        wr32, wr32_b = kb.sb("wr32", [128, 8, 8], F32)
        kb.dma("sp", wr32[:], self.I("moe_w_router")[li2].rearrange("(k p) e -> p k e", p=128), writes=[wr32_b])
        wrh, wrh_b = kb.sb("wrh", [128, 2, 8, 8], BF16)
        wrr, wrr_b = kb.sb("wrr", [128, 8, 8], F32)
        kb.op("dve", lambda e: e.tensor_copy(out=wrh[:, 0], in_=wr32[:]), reads=[wr32_b], writes=[wrh_b])
        kb.op("dve", lambda e: e.tensor_tensor(out=wrr[:], in0=wr32[:], in1=wrh[:, 0], op=ALU.subtract), reads=[wr32_b, wrh_b], writes=[wrr_b])
        kb.op("dve", lambda e: e.tensor_copy(out=wrh[:, 1], in_=wrr[:]), reads=[wrr_b, wrh_b], writes=[wrh_b])
        brB, brB_b = kb.sb("brB", [128, 8], F32)
        kb.dma("sp", brB[:], self.I("moe_b_router")[li2].partition_broadcast(128), writes=[brB_b])
        sel, sel_b = kb.sb("sel", [8, 8, 128], BF16)
        self_f, self_f_b = kb.sb("self", [8, 8, 128], F32)
        kb.op("pool", lambda e: e.memset(self_f[:], 1.0), writes=[self_f_b])
        kb.op("pool", lambda e: e.affine_select(out=self_f[:], in_=self_f[:], pattern=[[1, 8], [0, 128]], compare_op=ALU.is_equal, fill=0.0, base=0, channel_multiplier=-1),
              reads=[self_f_b], writes=[self_f_b])
        kb.op("dve", lambda e: e.tensor_copy(out=sel[:], in_=self_f[:]), reads=[self_f_b], writes=[sel_b])
    xr, xr_b = kb.sb("xr", [128, 8, 512], F32)
    vv, vv_b = kb.sb("vv", [128, 8, 512], F32)
    GB = [kb.sb("G%d" % i, [128, 8, 512], BF16) for i in range(4)]
    m_u = kb.mark()
    ys, ys_b = kb.sb("ysg", [128, 12, 512], BF16)
    gt, gt_b = kb.sb("gtg", [128, 24, 512], BF16)
    kb.reset(m_u)
    nft = 11 if moe else 22
    hh, hh_b = kb.sb("hh", [128, nft, 512], BF16)
    if moe:
        macc, macc_b = kb.sb("macc", [128, 8, 512], F32)
    kb.reset(m_u + 36 * 1024)
    PT = [kb.sb("PT%d" % i, [128, 2, 512], BF16) for i in range(2)]
    NTF = 4 if moe else 6
    tmpf = [kb.sb("tmpf%d" % i, [128, 512], F32) for i in range(NTF)]
    tmpb = [kb.sb("tmpb%d" % i, [128, 512], BF16) for i in range(3)]
    st_mean, st_mean_b = kb.sb("st_mean", [128, 512], F32)
    st_rstd, st_rstd_b = kb.sb("st_rstd", [128, 512], F32)
    if moe:
        lg, lg_b = kb.sb("lg", [128, 4, 8], F32)
        gsm = {n_: kb.sb("g_" + n_, [128, 4, 8], F32) for n_ in ("eq1", "eq2", "lg2", "gate", "gr")}
        gs1 = {n_: kb.sb("s_" + n_, [128, 4], F32) for n_ in ("m1", "m2", "w1", "w2")}
        gTs, gTs_b = kb.sb("gTs", [8, 512], F32)
        gTh, gTh_b = kb.sb("gTh", [8, 2, 512], BF16)
        gTr, gTr_b = kb.sb("gTr", [8, 512], F32)
        xlo, xlo_b = kb.sb("xlo", [128, 8, 512], BF16)
        gbs, gbs_b = kb.sb("gbs", [128, 512], F32)
    if last:
        ostg = [kb.sb("ostg%d" % i, [128, D], F32) for i in range(2)]
    self._ti = 0

    def tf():
        t = tmpf[self._ti % NTF]
        self._ti += 1
        return t

    self._tb = 0

    def tbf():
        t = tmpb[self._tb % 3]
        self._tb += 1
        return t

    def layer_norm(gi):
        (vb, vb_b), (vsq, vsq_b) = GB[1], GB[2]
        kb.op("act", lambda e: e.activation(out=vb[:], in_=vv[:], func=AF.Copy), reads=[vv_b], writes=[vb_b])
        kb.op("act", lambda e: e.activation(out=vsq[:], in_=vv[:], func=AF.Square), reads=[vv_b], writes=[vsq_b])
        pm, pm_b = kb.psum()
        for kt in range(8):
            kb.op("pe", lambda e, kt=kt: e.matmul(pm[:], ones_m[:], vb[:, kt, :], start=(kt == 0), stop=(kt == 7)), reads=[ones_m_b, vb_b], writes=[pm_b])
        pq, pq_b = kb.psum()
        for kt in range(8):
            kb.op("pe", lambda e, kt=kt: e.matmul(pq[:], ones_m[:], vsq[:, kt, :], start=(kt == 0), stop=(kt == 7)), reads=[ones_m_b, vsq_b], writes=[pq_b])
        kb.op("act", lambda e: e.activation(out=st_mean[:], in_=pm[:], func=AF.Copy), reads=[pm_b], writes=[st_mean_b])
        (m2, m2_b) = tf()
        kb.op("dve", lambda e: e.tensor_tensor(out=m2[:], in0=st_mean[:], in1=st_mean[:], op=ALU.mult), reads=[st_mean_b], writes=[m2_b])
        kb.op("dve", lambda e: e.tensor_tensor(out=m2[:], in0=pq[:], in1=m2[:], op=ALU.subtract), reads=[pq_b, m2_b], writes=[m2_b])
        kb.op("dve", lambda e: e.tensor_scalar(out=m2[:], in0=m2[:], scalar1=0.0, scalar2=LN_EPS, op0=ALU.max, op1=ALU.add), reads=[m2_b], writes=[m2_b])
        kb.op("act", lambda e: e.activation(out=m2[:], in_=m2[:], func=AF.Sqrt), reads=[m2_b], writes=[m2_b])
        kb.op("dve", lambda e: e.reciprocal(out=st_rstd[:], in_=m2[:]), reads=[m2_b], writes=[st_rstd_b])
        for kt in range(8):
            eng = "dve" if kt % 2 == 0 else "pool"
            kb.op(eng, lambda e, kt=kt: e.tensor_tensor(out=vv[:, kt, :], in0=vv[:, kt, :], in1=st_mean[:], op=ALU.subtract), reads=[vv_b, st_mean_b], writes=[vv_b])
            kb.op(eng, lambda e, kt=kt: e.tensor_tensor(out=vv[:, kt, :], in0=vv[:, kt, :], in1=st_rstd[:], op=ALU.mult), reads=[vv_b, st_rstd_b], writes=[vv_b])
            kb.op(eng, lambda e, kt=kt: e.tensor_scalar(out=xr[:, kt, :], in0=vv[:, kt, :], scalar1=lnT[:, gi, kt:kt + 1], scalar2=lnT[:, gi + 1, kt:kt + 1], op0=ALU.mult, op1=ALU.add),
                  reads=[vv_b, lnT_b], writes=[xr_b])

    def resid_consume(ot, ps, ps_b):
        kb.op("dve", lambda e: e.scalar_tensor_tensor(out=vv[:, ot, :], in0=xr[:, ot, :], scalar=float(ALPHA), in1=ps[:], op0=ALU.mult, op1=ALU.add),
              reads=[xr_b, ps_b], writes=[vv_b])

    xres_v = self.xres.rearrange("(kt p) t -> p kt t", p=128)
    xTd_v = self.xTd.rearrange("(kt p) t -> p kt t", p=128)
    for tg in range(8):
        tsl = slice(tg * 512, (tg + 1) * 512)
        kb.dma("pool", xr[:], xres_v[:, :, tsl], writes=[xr_b])
        ysv = self.ysT.rearrange("(a p) t -> p a t", p=128)
        for a0 in range(0, 12, 6):
            kb.dma("pool", ys[:, a0:a0 + 6, :], ysv[:, a0:a0 + 6, tsl], writes=[ys_b, hh_b])
        gv = self.gT.rearrange("(a p) t -> p a t", p=128)
        for a0 in range(0, 24, 6):
            kb.dma("pool", gt[:, a0:a0 + 6, :], gv[:, a0:a0 + 6, tsl], writes=[gt_b, hh_b] + ([macc_b] if moe else []))
        mg, mg_b = GB[0]
        wbr = []
        for n_ in range(3):
            wbr.append(load_w(self.wc["w_branch"][n_], 4, D))
        for dt_ in range(8):
            pss = []
            for n_ in range(3):
                wv, w_b = wbr[n_]
                ps, ps_b = kb.psum()
                for ck in range(4):
                    kb.op("pe", lambda e, ps=ps, wv=wv, ck=ck, n_=n_, dt_=dt_: e.matmul(ps[:], wv[:, ck, dt_ * 128:(dt_ + 1) * 128], ys[:, n_ * 4 + ck, :], start=(ck == 0), stop=(ck == 3)),
                          reads=[w_b, ys_b], writes=[ps_b])
                pss.append((ps, ps_b))
            (a0_, a0_b), (a1_, a1_b), (a2_, a2_b) = tf(), tf(), tf()
            kb.op("dve", lambda e, p=pss[0][0], dt_=dt_, a0_=a0_: e.tensor_tensor(out=a0_[:], in0=p[:], in1=gt[:, dt_, :], op=ALU.mult), reads=[pss[0][1], gt_b], writes=[a0_b])
            kb.op("dve", lambda e, p=pss[1][0], dt_=dt_, a1_=a1_: e.tensor_tensor(out=a1_[:], in0=p[:], in1=gt[:, 8 + dt_, :], op=ALU.mult), reads=[pss[1][1], gt_b], writes=[a1_b])
            kb.op("dve", lambda e, p=pss[2][0], dt_=dt_, a2_=a2_: e.tensor_tensor(out=a2_[:], in0=p[:], in1=gt[:, 16 + dt_, :], op=ALU.mult), reads=[pss[2][1], gt_b], writes=[a2_b])
            kb.op("pool", lambda e, a0_=a0_, a1_=a1_: e.tensor_tensor(out=a0_[:], in0=a0_[:], in1=a1_[:], op=ALU.add), reads=[a0_b, a1_b], writes=[a0_b])
            kb.op("pool", lambda e, a0_=a0_, a2_=a2_, dt_=dt_: e.tensor_tensor(out=mg[:, dt_, :], in0=a0_[:], in1=a2_[:], op=ALU.add), reads=[a0_b, a2_b], writes=[mg_b])
        linear(self.wc["w_mix_out"], 8, D, lambda kt: mg[:, kt, :], [mg_b], resid_consume)
        layer_norm(0)
        xb, xb_b = GB[3]
        kb.op("act", lambda e: e.activation(out=xb[:], in_=xr[:], func=AF.Copy), reads=[xr_b], writes=[xb_b])
        qT, qT_b = GB[0]

        def cons_q(ot, ps, ps_b):
            kb.op("act", lambda e: e.activation(out=qT[:, ot, :], in_=ps[:], func=AF.Copy, scale=1.0 / 16), reads=[ps_b], writes=[qT_b])
        linear(self.wc["w_xq"], 8, D, lambda kt: xb[:, kt, :], [xb_b], cons_q)
        oT, oT_b = GB[1]
        for h in range(4):
            pt, pt_b = PT[h % 2]
            for mt in range(2):
                ps, ps_b = kb.psum()
                for ee in range(2):
                    et = 2 * h + ee
                    kb.op("pe", lambda e, ps=ps, et=et, mt=mt, ee=ee: e.matmul(ps[:], KT[:, et, mt * 128:(mt + 1) * 128], qT[:, et, :], start=(ee == 0), stop=(ee == 1)),
                          reads=[KT_b, qT_b], writes=[ps_b])
                kb.op("act", lambda e, ps=ps, pt=pt, mt=mt: e.activation(out=pt[:, mt, :], in_=ps[:], func=AF.Exp), reads=[ps_b], writes=[pt_b])
            pd, pd_b = kb.psum()
            for mt in range(2):
                kb.op("pe", lambda e, pt=pt, mt=mt, pd=pd: e.matmul(pd[:], ones_1[:], pt[:, mt, :], start=(mt == 0), stop=(mt == 1)), reads=[ones_1_b, pt_b], writes=[pd_b])
            rd, rd_b = tf()
            kb.op("dve", lambda e, rd=rd, pd=pd: e.reciprocal(out=rd[:], in_=pd[:]), reads=[pd_b], writes=[rd_b])
            for ee in range(2):
                et = 2 * h + ee
                ps, ps_b = kb.psum()
                for mt in range(2):
                    kb.op("pe", lambda e, ps=ps, et=et, mt=mt, pt=pt: e.matmul(ps[:], Vm[:, mt, et * 128:(et + 1) * 128], pt[:, mt, :], start=(mt == 0), stop=(mt == 1)),
                          reads=[Vm_b, pt_b], writes=[ps_b])
                kb.op("dve", lambda e, ps=ps, et=et, rd=rd: e.tensor_tensor(out=oT[:, et, :], in0=ps[:], in1=rd[:], op=ALU.mult), reads=[ps_b, rd_b], writes=[oT_b])
        linear(self.wc["w_xo"], 8, D, lambda kt: oT[:, kt, :], [oT_b], resid_consume)
        layer_norm(2)
        kb.op("act", lambda e: e.activation(out=xb[:], in_=xr[:], func=AF.Copy), reads=[xr_b], writes=[xb_b])
        def swiglu(Wg, Wu, Wd, nft_, gate_sb=None, down_consume=None):
            gl = {}

            def cons_g(ot, ps, ps_b):
                sg, sg_b = tbf()
                kb.op("act", lambda e: e.activation(out=sg[:], in_=ps[:], func=AF.Silu), reads=[ps_b], writes=[sg_b])
                gl[ot] = (sg, sg_b)

            def cons_u(ot, ps, ps_b):
                sg, sg_b = gl[ot]
                if gate_sb is None:
                    kb.op("dve", lambda e: e.tensor_tensor(out=hh[:, ot, :], in0=ps[:], in1=sg[:], op=ALU.mult), reads=[ps_b, sg_b], writes=[hh_b])
                else:
                    t_, t_b = tf()
                    kb.op("dve", lambda e: e.tensor_tensor(out=t_[:], in0=ps[:], in1=sg[:], op=ALU.mult), reads=[ps_b, sg_b], writes=[t_b])
                    kb.op("pool", lambda e: e.tensor_tensor(out=hh[:, ot, :], in0=t_[:], in1=gate_sb[0][:], op=ALU.mult), reads=[t_b, gate_sb[1]], writes=[hh_b])
            for f0 in range(0, nft_, 3):
                f1 = min(nft_, f0 + 3)
                n_c = (f1 - f0) * 128
                for (W, cons) in ((Wg, cons_g), (Wu, cons_u)):
                    wv, w_b = load_w(W[:, f0 * 128:f0 * 128 + n_c], 8, n_c)
                    for j in range(f1 - f0):
                        ps, ps_b = kb.psum()
                        for kt in range(8):
                            kb.op("pe", lambda e, ps=ps, wv=wv, kt=kt, j=j: e.matmul(ps[:], wv[:, kt, j * 128:(j + 1) * 128], xb[:, kt, :], start=(kt == 0), stop=(kt == 7)),
                                  reads=[w_b, xb_b], writes=[ps_b])
                        cons(f0 + j, ps, ps_b)
            linear(Wd, nft_, D, lambda kt: hh[:, kt, :], [hh_b], down_consume)

        if not moe:
            swiglu(self.wc["ffn_w_gate"], self.wc["ffn_w_up"], self.wc["ffn_w_down"], 22, None, resid_consume)
        else:
            kb.op("dve", lambda e: e.tensor_tensor(out=vv[:], in0=xr[:], in1=xb[:], op=ALU.subtract), reads=[xr_b, xb_b], writes=[vv_b])
            kb.op("act", lambda e: e.activation(out=xlo[:], in_=vv[:], func=AF.Copy), reads=[vv_b], writes=[xlo_b])
            pl, pl_b = kb.psum()
            for tt in range(4):
                tk = slice(tt * 128, (tt + 1) * 128)
                combos = [(xb, xb_b, 0), (xb, xb_b, 1), (xlo, xlo_b, 0)]
                n_mm = 0
                for (xs, xs_b, wpart) in combos:
                    for kt in range(8):
                        kb.op("pe", lambda e, xs=xs, wpart=wpart, kt=kt, tk=tk, tt=tt, n_mm=n_mm, pl=pl: e.matmul(pl[:, tt * 8:(tt + 1) * 8], xs[:, kt, tk], wrh[:, wpart, kt, :], start=(n_mm == 0), stop=(n_mm == 23)),
                              reads=[xs_b, wrh_b], writes=[pl_b])
                        n_mm += 1
            kb.op("dve", lambda e, pl=pl: e.tensor_tensor(out=lg[:], in0=pl[:, 0:32].rearrange("p (t e) -> p t e", t=4), in1=brB[:].unsqueeze(1).to_broadcast([128, 4, 8]), op=ALU.add),
                  reads=[pl_b, brB_b], writes=[lg_b])
            GQ = Buf("gateq")
            eq1, eq2, lg2, gate, gr = (gsm[n_][0] for n_ in ("eq1", "eq2", "lg2", "gate", "gr"))
            m1, m2_, w1, w2 = (gs1[n_][0] for n_ in ("m1", "m2", "w1", "w2"))

            def gd(fn, eng="dve"):
                kb.op(eng, fn, reads=[GQ, lg_b], writes=[GQ])
            gd(lambda e: e.tensor_reduce(out=m1[:], in_=lg[:], axis=AX.X, op=ALU.max))
            gd(lambda e: e.tensor_tensor(out=eq1[:], in0=lg[:], in1=m1[:].unsqueeze(2).to_broadcast([128, 4, 8]), op=ALU.is_equal))
            gd(lambda e: e.scalar_tensor_tensor(out=lg2[:], in0=eq1[:], scalar=-1.0e30, in1=lg[:], op0=ALU.mult, op1=ALU.add))
            gd(lambda e: e.tensor_reduce(out=m2_[:], in_=lg2[:], axis=AX.X, op=ALU.max))
            gd(lambda e: e.tensor_tensor(out=eq2[:], in0=lg2[:], in1=m2_[:].unsqueeze(2).to_broadcast([128, 4, 8]), op=ALU.is_equal))
            gd(lambda e: e.tensor_tensor(out=w2[:], in0=m1[:], in1=m2_[:], op=ALU.subtract))
            gd(lambda e: e.activation(out=w1[:], in_=w2[:], func=AF.Sigmoid), "act")
            gd(lambda e: e.tensor_scalar(out=w2[:], in0=w1[:], scalar1=-1.0, scalar2=1.0, op0=ALU.mult, op1=ALU.add))
            gd(lambda e: e.tensor_tensor(out=gate[:], in0=eq1[:], in1=w1[:].unsqueeze(2).to_broadcast([128, 4, 8]), op=ALU.mult))
            gd(lambda e: e.tensor_tensor(out=gr[:], in0=eq2[:], in1=w2[:].unsqueeze(2).to_broadcast([128, 4, 8]), op=ALU.mult))
            gd(lambda e: e.tensor_tensor(out=gate[:], in0=gate[:], in1=gr[:], op=ALU.add))
            pg, pg_b = kb.psum()
            for tt in range(4):
                kb.op("pe", lambda e, tt=tt, pg=pg: e.transpose(pg[0:8, tt * 128:(tt + 1) * 128], gate[:, tt, :], idf[:]), reads=[GQ, idf_b], writes=[pg_b])
            kb.op("dve", lambda e, pg=pg: e.tensor_copy(out=gTs[:], in_=pg[0:8, :]), reads=[pg_b], writes=[gTs_b])
            kb.op("dve", lambda e: e.tensor_copy(out=gTh[:, 0, :], in_=gTs[:]), reads=[gTs_b], writes=[gTh_b])
            kb.op("dve", lambda e: e.tensor_tensor(out=gTr[:], in0=gTs[:], in1=gTh[:, 0, :], op=ALU.subtract), reads=[gTs_b, gTh_b], writes=[gTr_b])
            kb.op("dve", lambda e: e.tensor_copy(out=gTh[:, 1, :], in_=gTr[:]), reads=[gTr_b, gTh_b], writes=[gTh_b])
            for ex in range(NEXP):
                pgb, pgb_b = kb.psum()
                for part in range(2):
                    kb.op("pe", lambda e, ex=ex, part=part, pgb=pgb: e.matmul(pgb[:], sel[:, ex, :], gTh[:, part, :], start=(part == 0), stop=(part == 1)),
                          reads=[sel_b, gTh_b], writes=[pgb_b])
                kb.op("act", lambda e, pgb=pgb: e.activation(out=gbs[:], in_=pgb[:], func=AF.Copy), reads=[pgb_b], writes=[gbs_b])

                def dcons(ot, ps, ps_b, ex=ex):
                    if ex == 0:
                        kb.op("dve", lambda e: e.tensor_copy(out=macc[:, ot, :], in_=ps[:]), reads=[ps_b], writes=[macc_b])
                    elif ex < NEXP - 1:
                        kb.op("dve", lambda e: e.tensor_tensor(out=macc[:, ot, :], in0=macc[:, ot, :], in1=ps[:], op=ALU.add), reads=[ps_b, macc_b], writes=[macc_b])
                    else:
                        t_, t_b = tf()
                        kb.op("dve", lambda e: e.tensor_tensor(out=t_[:], in0=macc[:, ot, :], in1=ps[:], op=ALU.add), reads=[ps_b, macc_b], writes=[t_b])
                        kb.op("dve", lambda e: e.scalar_tensor_tensor(out=vv[:, ot, :], in0=xr[:, ot, :], scalar=float(ALPHA), in1=t_[:], op0=ALU.mult, op1=ALU.add),
                              reads=[xr_b, t_b], writes=[vv_b])
                swiglu(self.wc["moe_w_gate"][ex], self.wc["moe_w_up"][ex], self.wc["moe_w_down"][ex], 11, (gbs, gbs_b), dcons)
        layer_norm(4)
        kb.dma("pool", xres_v[:, :, tsl], xr[:], reads=[xr_b])
        kb.op("act", lambda e: e.activation(out=xb[:], in_=xr[:], func=AF.Copy), reads=[xr_b], writes=[xb_b])
        kb.dma("pool", xTd_v[:, :, tsl], xb[:], reads=[xb_b])
        if last:
            for tt in range(4):
                og, og_b = ostg[tt % 2]
                for half in range(2):
                    ps, ps_b = kb.psum()
                    for j in range(4):
                        kt = half * 4 + j
                        kb.op("pe", lambda e, ps=ps, kt=kt, j=j, tt=tt: e.transpose(ps[:, j * 128:(j + 1) * 128], xr[:, kt, tt * 128:(tt + 1) * 128], idf[:]),
                              reads=[xr_b, idf_b], writes=[ps_b])
                    kb.op("act", lambda e, ps=ps, og=og, half=half: e.activation(out=og[:, half * 512:(half + 1) * 512], in_=ps[:], func=AF.Copy), reads=[ps_b], writes=[og_b])
                r0 = tg * 512 + tt * 128
                kb.dma("sp", self.out[r0:r0 + 128, :], og[:], reads=[og_b])
    kb.barrier()


Prog.stageC = _stageC
```

```python
import contextlib
import math

import numpy as np
import concourse.bass as bass
import concourse.mybir as mybir
from concourse.bass_utils import run_bass_kernel_spmd

F32 = mybir.dt.float32
BF16 = mybir.dt.bfloat16
I32 = mybir.dt.int32
AF = mybir.ActivationFunctionType
ALU = mybir.AluOpType
AX = mybir.AxisListType

CENG = ("pe", "act", "dve", "pool", "sp")

D = 1024
S = 4096
DEPTH = 4
NIN = 6664
BW = 512
DFF = 2816
NEXP = 8
DFE = 1408
NMEM = 256
ALPHA = (2 * DEPTH) ** 0.25
LN_EPS = 1e-5
ROPE_THETA = 500000.0
O_U, O_QD, O_KD, O_VD, O_QF, O_KF, O_VF, O_F, O_G = 0, 512, 1024, 1536, 2048, 2560, 3072, 3584, 3592
VA = 520


class Buf:
    __slots__ = ("name", "w", "r", "excl")

    def __init__(self, name="", excl=False):
        self.name = name
        self.w = None
        self.r = {}
        self.excl = excl


class Op:
    __slots__ = ("eng", "fn", "waits", "sig", "count", "dma")

    def __init__(self, eng, fn, dma=None):
        self.eng = eng
        self.fn = fn
        self.waits = []
        self.sig = False
        self.count = 0
        self.dma = dma


class KB:
    SB_LO = 16640
    SB_HI = 229376

    def __init__(self, nc, n_dma_slots=12):
        self.nc = nc
        self.ops = {e: [] for e in CENG}
        self.seen_c = {e: {s: -1 for s in CENG} for e in CENG}
        self.seen_d = {e: {} for e in CENG}
        self.dq = {"sp": [], "pool": [], "act": []}
        self.slot_cnt = {}
        self.slot_rr = {"sp": 0, "pool": 0, "act": 0}
        for q in self.dq:
            for i in range(n_dma_slots if q != "act" else 4):
                sid = (q, i)
                self.dq[q].append(sid)
                self.slot_cnt[sid] = 0
        self.sb_off = self.SB_LO
        self.n_alloc = 0
        self.ps = []
        self.ps_rr = 0
        self.pa_rr = 0

    def sb(self, name, shape, dtype):
        esz = {F32: 4, BF16: 2, I32: 4}[dtype]
        n = esz
        for d in shape[1:]:
            n *= d
        n = (n + 63) // 64 * 64
        off = self.sb_off
        assert off + n <= self.SB_HI, "SBUF overflow at %s: %d + %d" % (name, off, n)
        self.sb_off += n
        self.n_alloc += 1
        t = self.nc.alloc_sbuf_tensor_at("%s_%d" % (name, self.n_alloc), list(shape), dtype, offset=off)
        return t, Buf(name)

    def mark(self):
        return self.sb_off

    def reset(self, m):
        self.sb_off = m

    def psum(self):
        p = self.ps[self.ps_rr % 5]
        self.ps_rr += 1
        return p

    def psum_acc(self):
        p = self.ps[5 + self.pa_rr % 3]
        self.pa_rr += 1
        return p

    def _collect(self, eng, reads, writes, is_dma):
        ev = []
        for b in reads:
            if b.w is not None:
                ev.append(b.w)
            if b.excl:
                for k, e in b.r.items():
                    if e[0] == "d" or e[1] != eng:
                        ev.append(e)
        for b in writes:
            if b.w is not None:
                if is_dma or b.w[0] == "d" or b.w[1] != eng or eng != "pe":
                    ev.append(b.w)
            for k, e in b.r.items():
                if is_dma or e[0] == "d" or e[1] != eng or eng != "pe":
                    ev.append(e)
        return ev

    def _reduce(self, eng, evs):
        best_c = {}
        best_d = {}
        for e in evs:
            if e[0] == "c":
                if e[2] > best_c.get(e[1], -1):
                    best_c[e[1]] = e[2]
            else:
                if e[2] > best_d.get(e[1], -1):
                    best_d[e[1]] = e[2]
        out = []
        for s, i in best_c.items():
            if i > self.seen_c[eng][s]:
                self.seen_c[eng][s] = i
                out.append(("c", s, i))
                self.ops[s][i].sig = True
        for sl, v in best_d.items():
            if v > self.seen_d[eng].get(sl, 0):
                self.seen_d[eng][sl] = v
                out.append(("d", sl, v))
        return out

    def _update(self, ev, reads, writes, rkey):
        for b in reads:
            b.r[rkey] = ev
        for b in writes:
            b.w = ev
            b.r = {}

    def op(self, eng, fn, reads=(), writes=()):
        o = Op(eng, fn)
        evs = self._collect(eng, reads, writes, False)
        o.waits = self._reduce(eng, evs)
        idx = len(self.ops[eng])
        self.ops[eng].append(o)
        self._update(("c", eng, idx), reads, writes, eng)
        return o

    def dma(self, q, out, in_, reads=(), writes=(), **kw):
        slots = self.dq[q]
        sid = slots[self.slot_rr[q] % len(slots)]
        self.slot_rr[q] += 1
        evs = self._collect(q, reads, writes, True)
        if self.slot_cnt[sid] > 0:
            evs.append(("d", sid, self.slot_cnt[sid] * 16))
        self.slot_cnt[sid] += 1
        val = self.slot_cnt[sid] * 16
        o = Op(q, lambda e: e.dma_start(out=out, in_=in_, **kw), dma=(sid, val))
        o.waits = self._reduce(q, evs)
        self.ops[q].append(o)
        self._update(("d", sid, val), reads, writes, ("d", sid))
        return o

    def barrier(self):
        evs = []
        for e in CENG:
            if e != "dve":
                for i in range(len(self.ops[e]) - 1, -1, -1):
                    if self.ops[e][i].dma is None and self.ops[e][i].fn is not None:
                        evs.append(("c", e, i))
                        break
        dev = [("d", sid, c * 16) for sid, c in self.slot_cnt.items() if c > 0]
        tok = self.bar_tile
        o = Op("dve", lambda e: e.memset(tok[:], 0.0))
        evs += self._collect("dve", [], [self.bar_buf], False)
        o.waits = self._reduce("dve", evs + dev)
        idx = len(self.ops["dve"])
        self.ops["dve"].append(o)
        self._update(("c", "dve", idx), [], [self.bar_buf], "dve")
        for e in CENG:
            if e == "dve":
                continue
            o2 = Op(e, None)
            o2.waits = self._reduce(e, [("c", "dve", idx)] + dev)
            self.ops[e].append(o2)

    def finish(self, stack):
        nc = self.nc
        for e in CENG:
            c = 0
            for o in self.ops[e]:
                if o.sig:
                    c += 1
                    o.count = c
        sems = {e: stack.enter_context(nc.semaphore("s_" + e)) for e in CENG}
        dsems = {}
        for q, slots in self.dq.items():
            for sid in slots:
                if self.slot_cnt[sid] > 0:
                    dsems[sid] = stack.enter_context(nc.semaphore("d_%s%d" % sid))
        block = stack.enter_context(nc.Block())
        ops = self.ops

        def replay(ename):
            def run(eng):
                for o in ops[ename]:
                    for w in o.waits:
                        if w[0] == "c":
                            eng.wait_ge(sems[w[1]], ops[w[1]][w[2]].count)
                        else:
                            eng.wait_ge(dsems[w[1]], w[2])
                    if o.fn is None:
                        continue
                    ins = o.fn(eng)
                    if o.dma is not None:
                        ins.then_inc(dsems[o.dma[0]], 16)
                    elif o.sig:
                        ins.then_inc(sems[ename], 1)
            return run

        block.tensor(replay("pe"))
        block.scalar(replay("act"))
        block.vector(replay("dve"))
        block.gpsimd(replay("pool"))
        block.sync(replay("sp"))
        return {e: len(ops[e]) for e in CENG}


class Prog:
    def __init__(self, nc, stack, layers=range(DEPTH), stages="ABC", debug=()):
        self.nc = nc
        self.stack = stack
        self.kb = KB(nc)
        self.layers = list(layers)
        self.stages = stages
        self.debug = set(debug)
        self.inp = {}
        self.scr = {}

    def din(self, name, shape, dtype=F32):
        t = self.nc.dram_tensor(name, list(shape), dtype, kind="ExternalInput").ap()
        self.inp[name] = t
        return t

    def dscr(self, name, shape, dtype):
        kind = "ExternalOutput" if name in self.debug else "Internal"
        t = self.nc.dram_tensor(name, list(shape), dtype, kind=kind).ap()
        self.scr[name] = t
        return t

    SHAPES = {
        "x": ([S, D], F32), "mem": ([NMEM, D], F32), "positions": ([128, 32], I32),
        "w_in": ([DEPTH, D, NIN], F32), "b_forget": ([DEPTH, 8], F32),
        "ssm_lambda_re": ([DEPTH, 32, 64], F32), "ssm_lambda_im": ([DEPTH, 32, 64], F32), "ssm_log_dt": ([DEPTH, 32], F32),
        "ssm_b_re": ([DEPTH, 32, 64, 16], F32), "ssm_b_im": ([DEPTH, 32, 64, 16], F32),
        "ssm_c_re": ([DEPTH, 32, 16, 64], F32), "ssm_c_im": ([DEPTH, 32, 16, 64], F32),
        "ssm_d": ([DEPTH, 512], F32), "w_glu": ([DEPTH, 512, 512], F32), "w_branch": ([DEPTH, 3, 512, D], F32),
        "w_mix_out": ([DEPTH, D, D], F32), "ln_mix_g": ([DEPTH, D], F32), "ln_mix_b": ([DEPTH, D], F32),
        "w_xq": ([DEPTH, D, D], F32), "w_xk": ([DEPTH, D, D], F32), "w_xv": ([DEPTH, D, D], F32), "w_xo": ([DEPTH, D, D], F32),
        "ln_x_g": ([DEPTH, D], F32), "ln_x_b": ([DEPTH, D], F32),
        "ffn_w_gate": ([2, D, DFF], F32), "ffn_w_up": ([2, D, DFF], F32), "ffn_w_down": ([2, DFF, D], F32),
        "moe_w_router": ([2, D, NEXP], F32), "moe_b_router": ([2, NEXP], F32),
        "moe_w_gate": ([2, NEXP, D, DFE], F32), "moe_w_up": ([2, NEXP, D, DFE], F32), "moe_w_down": ([2, NEXP, DFE, D], F32),
        "ln_ffn_g": ([DEPTH, D], F32), "ln_ffn_b": ([DEPTH, D], F32),
    }

    def I(self, name):
        if name not in self.inp:
            shp, dt_ = self.SHAPES[name]
            self.din(name, shp, dt_)
        return self.inp[name]

    def declare(self):
        self.x = self.I("x")
        self.pos = self.I("positions")
        self.out = self.nc.dram_tensor("out", [S, D], F32, kind="ExternalOutput").ap()
        self.xres = self.dscr("xres", [D, S], F32)
        self.xTd = self.dscr("xTd", [D, S], BF16)
        self.u_tm = self.dscr("u_tm", [S, 512], BF16)
        self.qdT = self.dscr("qdT", [512, S], BF16)
        self.kdT = self.dscr("kdT", [512, S], BF16)
        self.vd = self.dscr("vd", [S, VA], BF16)
        self.qfT = self.dscr("qfT", [512, S], BF16)
        self.kfT = self.dscr("kfT", [512, S], BF16)
        self.vf = self.dscr("vf", [S, VA], BF16)
        self.fl = self.dscr("fl", [8, S], F32)
        self.gT = self.dscr("gT", [3 * D, S], BF16)
        self.ysT = self.dscr("ysT", [3 * BW, S], BF16)
        self.wcb = Buf("wconv")
        self.wc = {
            "w_branch": self.dscr("c_wbr", [3, 512, D], BF16), "w_mix_out": self.dscr("c_wo", [D, D], BF16),
            "w_xq": self.dscr("c_wq", [D, D], BF16), "w_xk": self.dscr("c_wk", [D, D], BF16),
            "w_xv": self.dscr("c_wv", [D, D], BF16), "w_xo": self.dscr("c_wxo", [D, D], BF16),
            "ffn_w_gate": self.dscr("c_fg", [D, DFF], BF16), "ffn_w_up": self.dscr("c_fu", [D, DFF], BF16), "ffn_w_down": self.dscr("c_fd", [DFF, D], BF16),
            "moe_w_gate": self.dscr("c_mg", [NEXP, D, DFE], BF16), "moe_w_up": self.dscr("c_mu", [NEXP, D, DFE], BF16), "moe_w_down": self.dscr("c_md", [NEXP, DFE, D], BF16),
        }
        self.dscr("augq", [8, 6, S], BF16)
        self.dscr("augk", [8, 6, S], BF16)

    def setup(self):
        kb, nc = self.kb, self.nc
        st = self.stack
        kb.ps = [(st.enter_context(nc.psum_tensor("ps%d" % i, [128, 512], F32)), Buf("ps%d" % i, True)) for i in range(8)]
        bar, bar_b = kb.sb("bar", [128, 8], F32)
        kb.bar_tile = bar
        kb.bar_buf = bar_b
        self.identf, self.identf_b = kb.sb("identf", [128, 128], F32)
        self.identb, self.identb_b = kb.sb("identb", [128, 128], BF16)
        idf, idb = self.identf, self.identb
        kb.op("pool", lambda e: e.memset(idf[:], 1.0), writes=[self.identf_b])
        kb.op("pool", lambda e: e.affine_select(out=idf[:], in_=idf[:], pattern=[[1, 128]], compare_op=ALU.is_equal,
                                                fill=0.0, base=0, channel_multiplier=-1),
              reads=[self.identf_b], writes=[self.identf_b])
        kb.op("dve", lambda e: e.tensor_copy(out=idb[:], in_=idf[:]), reads=[self.identf_b], writes=[self.identb_b])
        self.rot = {}
        for n_ in ["cos", "sin", "cosq", "sinq"]:
            self.rot[n_] = kb.sb("rot_" + n_, [128, 32, 8], F32)
        self.memT, self.memT_b = kb.sb("memT", [128, 8, NMEM], BF16)
        self._build_rot(kb.mark())
        self._build_mem()

    def _build_rot(self, m2):
        kb = self.kb
        posi, posi_b = kb.sb("posi2", [128, 32], I32)
        kb.dma("sp", posi[:], self.pos[:, :], writes=[posi_b])
        posf, posf_b = kb.sb("posf2", [128, 32], F32)
        kb.op("dve", lambda e: e.tensor_copy(out=posf[:], in_=posi[:]), reads=[posi_b], writes=[posf_b])
        ang, ang_b = kb.sb("ang2", [128, 32, 8], F32)
        for i in range(8):
            f32 = float(np.float32(ROPE_THETA ** (-(2.0 * i) / 16.0)))
            kb.op("dve", lambda e, i=i, f32=f32: e.tensor_scalar(out=ang[:, :, i], in0=posf[:], scalar1=f32, scalar2=None, op0=ALU.mult),
                  reads=[posf_b], writes=[ang_b])
        TWO_PI = 2.0 * math.pi
        C1 = 6.28125
        C2 = TWO_PI - C1

        def reduce_sin(dst, dst_b, shift, scale):
            a2, a2_b = kb.sb("a2", [128, 256], F32)
            kf, kf_b = kb.sb("kf", [128, 256], F32)
            ki, ki_b = kb.sb("ki", [128, 256], I32)
            angf = ang[:].rearrange("p a b -> p (a b)")
            kb.op("dve", lambda e: e.tensor_scalar(out=a2[:], in0=angf, scalar1=float(shift), scalar2=None, op0=ALU.add),
                  reads=[ang_b], writes=[a2_b])
            kb.op("dve", lambda e: e.tensor_scalar(out=kf[:], in0=a2[:], scalar1=float(1.0 / TWO_PI), scalar2=None, op0=ALU.mult),
                  reads=[a2_b], writes=[kf_b])
            kb.op("dve", lambda e: e.tensor_copy(out=ki[:], in_=kf[:]), reads=[kf_b], writes=[ki_b])
            kb.op("dve", lambda e: e.tensor_copy(out=kf[:], in_=ki[:]), reads=[ki_b], writes=[kf_b])
            kb.op("dve", lambda e: e.scalar_tensor_tensor(out=a2[:], in0=kf[:], scalar=-C1, in1=a2[:], op0=ALU.mult, op1=ALU.add),
                  reads=[kf_b, a2_b], writes=[a2_b])
            kb.op("dve", lambda e: e.scalar_tensor_tensor(out=a2[:], in0=kf[:], scalar=-C2, in1=a2[:], op0=ALU.mult, op1=ALU.add),
                  reads=[kf_b, a2_b], writes=[a2_b])
            kb.op("dve", lambda e: e.tensor_scalar(out=a2[:], in0=a2[:], scalar1=float(-math.pi), scalar2=float(math.pi), op0=ALU.max, op1=ALU.min),
                  reads=[a2_b], writes=[a2_b])
            d = dst[:].rearrange("p a b -> p (a b)")
            kb.op("act", lambda e: e.activation(out=d, in_=a2[:], func=AF.Sin), reads=[a2_b], writes=[dst_b])
            if scale != 1.0:
                kb.op("dve", lambda e: e.tensor_scalar(out=d, in0=d, scalar1=float(scale), scalar2=None, op0=ALU.mult),
                      reads=[dst_b], writes=[dst_b])

        reduce_sin(*self.rot["sin"], 0.0, 1.0)
        reduce_sin(*self.rot["cos"], math.pi / 2, 1.0)
        reduce_sin(*self.rot["sinq"], 0.0, 0.125)
        reduce_sin(*self.rot["cosq"], math.pi / 2, 0.125)
        kb.barrier()
        kb.reset(m2)
        self.arena0 = m2

    def _build_mem(self):
        kb = self.kb
        kb.reset(self.arena0)
        mem = self.I("mem")
        mi = [kb.sb("memin%d" % i, [128, D], F32) for i in range(2)]
        for mt in range(2):
            t, t_b = mi[mt]
            kb.dma("sp", t[:], mem[mt * 128:(mt + 1) * 128, :], writes=[t_b])
            for half in range(2):
                ps, ps_b = kb.psum()
                for j in range(4):
                    kt = half * 4 + j
                    kb.op("pe", lambda e, ps=ps, t=t, kt=kt, j=j: e.transpose(ps[:, j * 128:(j + 1) * 128], t[:, kt * 128:(kt + 1) * 128], self.identf[:]),
                          reads=[t_b, self.identf_b], writes=[ps_b])
                kb.op("act", lambda e, ps=ps, half=half, mt=mt: e.activation(out=self.memT[:, half * 4:half * 4 + 4, mt * 128:(mt + 1) * 128],
                                                                             in_=ps[:].rearrange("p (a b) -> p a b", a=4), func=AF.Copy),
                      reads=[ps_b], writes=[self.memT_b])
        kb.barrier()
        kb.reset(self.arena0)

    def stage0(self):
        kb = self.kb
        kb.reset(self.arena0)
        self.xT, self.xT_b = kb.sb("xT", [128, 8, S], BF16)
        xT = self.xT
        xin = [kb.sb("xin%d" % i, [128, D], F32) for i in range(2)]
        stg = [kb.sb("xstg%d" % i, [128, 8, 128], F32) for i in range(2)]
        for tt in range(32):
            xi, xi_b = xin[tt % 2]
            kb.dma("sp", xi[:], self.x[tt * 128:(tt + 1) * 128, :], writes=[xi_b])
            sg, sg_b = stg[tt % 2]
            for half in range(2):
                ps, ps_b = kb.psum()
                for j in range(4):
                    kt = half * 4 + j
                    kb.op("pe", lambda e, ps=ps, xi=xi, kt=kt, j=j: e.transpose(ps[:, j * 128:(j + 1) * 128], xi[:, kt * 128:(kt + 1) * 128],
                                                                               self.identf[:]),
                          reads=[xi_b, self.identf_b], writes=[ps_b])
                pv = ps[:].rearrange("p (a b) -> p a b", a=4)
                kb.op("act", lambda e, pv=pv, half=half, tt=tt: e.activation(out=xT[:, half * 4:half * 4 + 4, tt * 128:(tt + 1) * 128], in_=pv, func=AF.Copy),
                      reads=[ps_b], writes=[self.xT_b])
                kb.op("dve", lambda e, pv=pv, half=half, sg=sg: e.tensor_copy(out=sg[:, half * 4:half * 4 + 4, :], in_=pv),
                      reads=[ps_b], writes=[sg_b])
            kb.dma("sp", self.xres.rearrange("(kt p) t -> p kt t", p=128)[:, :, tt * 128:(tt + 1) * 128], sg[:], reads=[sg_b])
        kb.dma("sp", self.xTd.rearrange("(kt p) t -> p kt t", p=128), xT[:], reads=[self.xT_b])
        kb.barrier()

    def stageA(self, l):
        kb = self.kb
        kb.reset(self.arena0)
        self.xT, self.xT_b = kb.sb("xT", [128, 8, S], BF16)
        xT, xT_b = self.xT, self.xT_b
        kb.dma("sp", xT[:], self.xTd.rearrange("(kt p) t -> p kt t", p=128), writes=[xT_b])
        w = self.I("w_in")[l]
        wv = w.rearrange("(kt p) n -> p kt n", p=128)
        wbs = [kb.sb("wA%d" % i, [128, 8, 512], BF16) for i in range(5)]
        wb8, wb8_b = kb.sb("wA8", [128, 8, 8], BF16)
        self._wi = 0

        def load_w(c0):
            wb, wb_b = wbs[self._wi % 5]
            self._wi += 1
            kb.dma("pool", wb[:], wv[:, :, c0:c0 + 512], writes=[wb_b])
            return wb, wb_b

        stg_u = [kb.sb("stgu%d" % i, [128, 512], BF16) for i in range(3)]
        stg_v = [kb.sb("stgv%d" % i, [128, 8, 65], BF16) for i in range(3)]
        for sv, sv_b in stg_v:
            kb.op("pool", lambda e, sv=sv: e.memset(sv[:], 1.0), writes=[sv_b])
        stg_r = [kb.sb("stgr%d" % i, [128, 512], BF16) for i in range(3)]
        stg_t = [kb.sb("stgt%d" % i, [128, 4, 512], BF16) for i in range(2)]
        rtmp = [kb.sb("rtmp%d" % i, [128, 8, 8], F32) for i in range(4)]

        def tok_block(c0, kind):
            wb, wb_b = load_w(c0)
            for tt in range(32):
                ps, ps_b = kb.psum()
                for kt in range(8):
                    kb.op("pe", lambda e, ps=ps, wb=wb, kt=kt, tt=tt: e.matmul(ps[:], xT[:, kt, tt * 128:(tt + 1) * 128], wb[:, kt, :],
                                                                              start=(kt == 0), stop=(kt == 7)),
                          reads=[xT_b, wb_b], writes=[ps_b])
                if kind == "u":
                    sg, sg_b = stg_u[tt % 3]
                    kb.op("act", lambda e, sg=sg, ps=ps: e.activation(out=sg[:], in_=ps[:], func=AF.Copy), reads=[ps_b], writes=[sg_b])
                    kb.dma("sp", self.u_tm[tt * 128:(tt + 1) * 128, :], sg[:], reads=[sg_b])
                elif kind in ("vd", "vf"):
                    sg, sg_b = stg_v[tt % 3]
                    kb.op("act", lambda e, sg=sg, ps=ps: e.activation(out=sg[:, :, 0:64], in_=ps[:].rearrange("p (h e) -> p h e", h=8), func=AF.Copy),
                          reads=[ps_b], writes=[sg_b])
                    dst = self.vd if kind == "vd" else self.vf
                    kb.dma("sp", dst[tt * 128:(tt + 1) * 128, :], sg[:].rearrange("p h e -> p (h e)"), reads=[sg_b])
                else:
                    isq = kind == "qd"
                    sg, sg_b = stg_r[tt % 3]
                    kb.op("act", lambda e, sg=sg, ps=ps, isq=isq: e.activation(out=sg[:], in_=ps[:], func=AF.Copy, scale=(0.125 if isq else 1.0)),
                          reads=[ps_b], writes=[sg_b])
                    cs, cs_b = self.rot["cosq" if isq else "cos"]
                    sn, sn_b = self.rot["sinq" if isq else "sin"]
                    pv = ps[:].rearrange("p (h e) -> p h e", h=8)
                    sgv = sg[:].rearrange("p (h e) -> p h e", h=8)
                    t1, t2 = pv[:, :, 0:8], pv[:, :, 8:16]
                    cb = cs[:, tt:tt + 1, :].to_broadcast([128, 8, 8])
                    sb_ = sn[:, tt:tt + 1, :].to_broadcast([128, 8, 8])
                    (ra, ra_b), (rb, rb_b), (rc, rc_b), (rd, rd_b) = rtmp
                    kb.op("dve", lambda e, ra=ra, t1=t1, cb=cb: e.tensor_tensor(out=ra[:], in0=t1, in1=cb, op=ALU.mult), reads=[ps_b, cs_b], writes=[ra_b])
                    kb.op("dve", lambda e, rb=rb, t2=t2, sb_=sb_: e.tensor_tensor(out=rb[:], in0=t2, in1=sb_, op=ALU.mult), reads=[ps_b, sn_b], writes=[rb_b])
                    kb.op("dve", lambda e, rc=rc, t2=t2, cb=cb: e.tensor_tensor(out=rc[:], in0=t2, in1=cb, op=ALU.mult), reads=[ps_b, cs_b], writes=[rc_b])
                    kb.op("dve", lambda e, rd=rd, t1=t1, sb_=sb_: e.tensor_tensor(out=rd[:], in0=t1, in1=sb_, op=ALU.mult), reads=[ps_b, sn_b], writes=[rd_b])
                    kb.op("dve", lambda e, sgv=sgv, ra=ra, rb=rb: e.tensor_tensor(out=sgv[:, :, 0:8], in0=ra[:], in1=rb[:], op=ALU.subtract),
                          reads=[ra_b, rb_b, sg_b], writes=[sg_b])
                    kb.op("dve", lambda e, sgv=sgv, rc=rc, rd=rd: e.tensor_tensor(out=sgv[:, :, 8:16], in0=rc[:], in1=rd[:], op=ALU.add),
                          reads=[rc_b, rd_b, sg_b], writes=[sg_b])
                    ps2, ps2_b = kb.psum()
                    for j in range(4):
                        kb.op("pe", lambda e, ps2=ps2, sg=sg, j=j: e.matmul(ps2[:, j * 128:(j + 1) * 128], sg[:, j * 128:(j + 1) * 128], self.identb[:],
                                                                          start=True, stop=True),
                              reads=[sg_b, self.identb_b], writes=[ps2_b])
                    g4 = tt // 4
                    tq = tt % 4
                    tg, tg_b = stg_t[g4 % 2]
                    kb.op("act", lambda e, tg=tg, ps2=ps2, tq=tq: e.activation(out=tg[:, :, tq * 128:(tq + 1) * 128], in_=ps2[:].rearrange("p (a b) -> p a b", a=4), func=AF.Copy),
                          reads=[ps2_b], writes=[tg_b])
                    if tq == 3:
                        dst = self.qdT if isq else self.kdT
                        kb.dma("sp", dst.rearrange("(a p) t -> p a t", p=128)[:, :, g4 * 512:(g4 + 1) * 512], tg[:], reads=[tg_b])

        if "u" in self.blocksA:
            tok_block(O_U, "u")
        if "qd" in self.blocksA:
            tok_block(O_QD, "qd")
            tok_block(O_KD, "kd")
        if "vd" in self.blocksA:
            tok_block(O_VD, "vd")
        if "vf" in self.blocksA:
            tok_block(O_VF, "vf")

        stg_f = [kb.sb("stgf%d" % i, [128, S], BF16) for i in range(3)]
        self._fi = 0

        def feat_block(c0, kind, dst):
            wb, wb_b = load_w(c0)
            for j in range(4):
                sg, sg_b = stg_f[self._fi % 3]
                self._fi += 1
                for tg in range(8):
                    ps, ps_b = kb.psum()
                    for kt in range(8):
                        kb.op("pe", lambda e, ps=ps, wb=wb, kt=kt, tg=tg, j=j: e.matmul(ps[:], wb[:, kt, j * 128:(j + 1) * 128], xT[:, kt, tg * 512:(tg + 1) * 512],
                                                                                      start=(kt == 0), stop=(kt == 7)),
                              reads=[xT_b, wb_b], writes=[ps_b])
                    if kind == "g":
                        kb.op("act", lambda e, sg=sg, ps=ps, tg=tg: e.activation(out=sg[:, tg * 512:(tg + 1) * 512], in_=ps[:], func=AF.Sigmoid),
                              reads=[ps_b], writes=[sg_b])
                    else:
                        sc = 0.125 if kind == "qf" else 1.0
                        if tg % 2 == 0:
                            kb.op("act", lambda e, sg=sg, ps=ps, tg=tg, sc=sc: e.activation(out=sg[:, tg * 512:(tg + 1) * 512], in_=ps[:], func=AF.Copy, scale=sc),
                                  reads=[ps_b], writes=[sg_b])
                        else:
                            kb.op("dve", lambda e, sg=sg, ps=ps, tg=tg, sc=sc: e.tensor_scalar(out=sg[:, tg * 512:(tg + 1) * 512], in0=ps[:], scalar1=sc, scalar2=None, op0=ALU.mult),
                                  reads=[ps_b], writes=[sg_b])
                kb.dma("sp", dst[j * 128:(j + 1) * 128, :], sg[:], reads=[sg_b])

        if "qf" in self.blocksA:
            feat_block(O_QF, "qf", self.qfT)
            feat_block(O_KF, "kf", self.kfT)
        if "g" in self.blocksA:
            for gb in range(6):
                feat_block(O_G + gb * 512, "g", self.gT[gb * 512:(gb + 1) * 512, :])
        if "f" in self.blocksA:
            kb.dma("pool", wb8[:], wv[:, :, O_F:O_F + 8], writes=[wb8_b])
            fs, fs_b = kb.sb("fstg", [8, S], F32)
            for tg in range(8):
                ps, ps_b = kb.psum()
                for kt in range(8):
                    kb.op("pe", lambda e, ps=ps, kt=kt, tg=tg: e.matmul(ps[0:8, :], wb8[:, kt, :], xT[:, kt, tg * 512:(tg + 1) * 512], start=(kt == 0), stop=(kt == 7)),
                          reads=[xT_b, wb8_b], writes=[ps_b])
                kb.op("dve", lambda e, ps=ps, tg=tg: e.tensor_copy(out=fs[:, tg * 512:(tg + 1) * 512], in_=ps[0:8, :]), reads=[ps_b], writes=[fs_b])
            kb.dma("sp", self.fl[:, :], fs[:], reads=[fs_b])
        kb.barrier()

    blocksA = ("u", "qd", "vd", "vf", "qf", "g", "f")

    def convert_weights(self, l):
        kb = self.kb
        li2 = l // 2

        def conv(dst, src):
            K_ = src.shape[0]
            a_n = K_ // 128
            dv = dst.rearrange("(a p) n -> p a n", p=128)
            sv = src.rearrange("(a p) n -> p a n", p=128)
            for a0 in range(0, a_n, 8):
                a1 = min(a_n, a0 + 8)
                kb.dma("pool", dv[:, a0:a1, :], sv[:, a0:a1, :], writes=[self.wcb])
        for n_ in range(3):
            conv(self.wc["w_branch"][n_], self.I("w_branch")[l][n_])
        for nm in ("w_mix_out", "w_xq", "w_xk", "w_xv", "w_xo"):
            conv(self.wc[nm], self.I(nm)[l])
        if l % 2 == 0:
            for nm in ("ffn_w_gate", "ffn_w_up", "ffn_w_down"):
                conv(self.wc[nm], self.I(nm)[li2])
        else:
            for nm in ("moe_w_gate", "moe_w_up", "moe_w_down"):
                for ex in range(NEXP):
                    conv(self.wc[nm][ex], self.I(nm)[li2][ex])

    def build(self):
        self.declare()
        self.setup()
        if "N" not in self.stages:
            self.stage0()
        if "Z" in self.stages:
            kb = self.kb
            kb.reset(self.arena0)
            zt, zt_b = kb.sb("zfill", [128, S], BF16)
            kb.op("pool", lambda e: e.memset(zt[:], 0.0), writes=[zt_b])
            for a in range(12):
                kb.dma("sp", self.ysT[a * 128:(a + 1) * 128, :], zt[:], reads=[zt_b])
            kb.barrier()
        for l in self.layers:
            if "C" in self.stages:
                self.convert_weights(l)
            if "A" in self.stages:
                self.stageA(l)
            if "1" in self.stages:
                self.stageB1(l)
            if "2" in self.stages:
                self.stageB2(l)
            if "3" in self.stages:
                self.stageB3(l)
            if "C" in self.stages:
                self.stageC(l, l == DEPTH - 1 or l == self.layers[-1])
        self.kb.barrier()
        return self.kb.finish(self.stack)


def build_program(layers=range(DEPTH), stages="A123C", debug=()):
    nc = bass.Bass("TRN2", target_bir_lowering=False)
    stack = contextlib.ExitStack()
    with stack:
        p = Prog(nc, stack, layers, stages, debug)
        n = p.build()
    return nc, p, n


INPUT_NAMES = ["x", "mem", "positions", "w_in", "b_forget", "ssm_lambda_re", "ssm_lambda_im", "ssm_log_dt",
               "ssm_b_re", "ssm_b_im", "ssm_c_re", "ssm_c_im", "ssm_d", "w_glu", "w_branch", "w_mix_out",
               "ln_mix_g", "ln_mix_b", "w_xq", "w_xk", "w_xv", "w_xo", "ln_x_g", "ln_x_b",
               "ffn_w_gate", "ffn_w_up", "ffn_w_down", "moe_w_router", "moe_b_router",
               "moe_w_gate", "moe_w_up", "moe_w_down", "ln_ffn_g", "ln_ffn_b"]


def make_in_maps(inputs, n=8, names=None):
    maps = []
    names = names or INPUT_NAMES
    shared = {k: np.ascontiguousarray(np.asarray(inputs[k])) for k in names if k not in ("x", "mem", "positions")}
    x = np.asarray(inputs["x"])
    mem = np.asarray(inputs["mem"])
    pos = np.asarray(inputs["positions"])
    for i in range(n):
        m = dict(shared)
        if "x" in names:
            m["x"] = np.ascontiguousarray(x[i])
        if "mem" in names:
            m["mem"] = np.ascontiguousarray(mem[i])
        if "positions" in names:
            m["positions"] = np.ascontiguousarray(pos[i].astype(np.int32).reshape(32, 128).T)
        maps.append(m)
    return maps


def kernel(**inputs):
    nc, p, n = build_program()
    in_maps = make_in_maps(inputs, names=list(p.inp.keys()))
    res = run_bass_kernel_spmd(nc, in_maps, core_ids=list(range(8)))
    return np.stack([np.asarray(r["out"]) for r in res.results], axis=0).astype(np.float32)


def _stageB3(self, l):
    kb = self.kb
    kb.reset(self.arena0)
    NEG = -30000.0
    mk, mk_b = kb.sb("fmask", [128, 4, 512], BF16)
    mkf, mkf_b = kb.sb("fmaskf", [128, 512], F32)
    for j in range(4):
        kb.op("pool", lambda e: e.memset(mkf[:], 0.0), writes=[mkf_b])
        kb.op("pool", lambda e, j=j: e.affine_select(out=mkf[:], in_=mkf[:], pattern=[[1, 512]], compare_op=ALU.is_ge,
                                                     fill=NEG, base=-j * 128, channel_multiplier=-1),
              reads=[mkf_b], writes=[mkf_b])
        kb.op("dve", lambda e, j=j: e.tensor_copy(out=mk[:, j, :], in_=mkf[:]), reads=[mkf_b], writes=[mk_b])
    ones, ones_b = kb.sb("fones", [128, 64], BF16)
    kb.op("pool", lambda e: e.memset(ones[:], 1.0), writes=[ones_b])
    flt, flt_b = kb.sb("flt", [8, S], F32)
    kb.dma("sp", flt[:], self.fl[:, :], writes=[flt_b])
    bf_, bf_b = kb.sb("bfg", [8, 1], F32)
    kb.dma("sp", bf_[:], self.I("b_forget")[l].rearrange("(h o) -> h o", o=1), writes=[bf_b])
    nb, nb_b = kb.sb("nbfg", [8, 1], F32)
    kb.op("dve", lambda e: e.tensor_scalar(out=nb[:], in0=bf_[:], scalar1=-1.0, scalar2=None, op0=ALU.mult), reads=[bf_b], writes=[nb_b])
    kb.op("act", lambda e: e.activation(out=flt[:], in_=flt[:], func=AF.Exp, scale=-1.0, bias=nb[:]), reads=[flt_b, nb_b], writes=[flt_b])
    kb.op("act", lambda e: e.activation(out=flt[:], in_=flt[:], func=AF.Ln, bias=1.0), reads=[flt_b], writes=[flt_b])
    onesf, onesf_b = kb.sb("onesf", [8, S], F32)
    kb.op("pool", lambda e: e.memset(onesf[:], 1.0), writes=[onesf_b])
    ncum, ncum_b = kb.sb("ncum", [8, S], F32)
    kb.op("dve", lambda e: e.tensor_tensor_scan(out=ncum[:], data0=onesf[:], data1=flt[:], initial=0.0, op0=ALU.mult, op1=ALU.add),
          reads=[onesf_b, flt_b], writes=[ncum_b])
    augk, augk_b = kb.sb("augk", [8, 6, S], BF16)
    augq, augq_b = kb.sb("augq", [8, 6, S], BF16)
    kb.op("pool", lambda e: e.memset(augk[:, 0:3, :], 1.0), writes=[augk_b])
    kb.op("pool", lambda e: e.memset(augq[:, 3:6, :], 1.0), writes=[augq_b])
    rem, rem_b = kb.sb("rem", [8, S], F32)
    kb.op("dve", lambda e: e.tensor_copy(out=augk[:, 3, :], in_=ncum[:]), reads=[ncum_b, augk_b], writes=[augk_b])
    kb.op("dve", lambda e: e.tensor_tensor(out=rem[:], in0=ncum[:], in1=augk[:, 3, :], op=ALU.subtract), reads=[ncum_b, augk_b], writes=[rem_b])
    kb.op("dve", lambda e: e.tensor_copy(out=augk[:, 4, :], in_=rem[:]), reads=[rem_b, augk_b], writes=[augk_b])
    kb.op("dve", lambda e: e.tensor_tensor(out=rem[:], in0=rem[:], in1=augk[:, 4, :], op=ALU.subtract), reads=[rem_b, augk_b], writes=[rem_b])
    kb.op("dve", lambda e: e.tensor_copy(out=augk[:, 5, :], in_=rem[:]), reads=[rem_b, augk_b], writes=[augk_b])
    kb.op("dve", lambda e: e.tensor_scalar(out=augq[:, 0:3, :], in0=augk[:, 3:6, :], scalar1=-1.0, scalar2=None, op0=ALU.mult),
          reads=[augk_b, augq_b], writes=[augq_b])
    aqd = self.scr["augq"]
    akd = self.scr["augk"]
    kb.dma("sp", aqd[:, :, :], augq[:], reads=[augq_b])
    kb.dma("sp", akd[:, :, :], augk[:], reads=[augk_b])
    kb.barrier()
    kb.reset(self.arena0 + 4 * 1024 + 2048 + 256)
    vall, vall_b = kb.sb("fvall", [128, 32, VA], BF16)
    for k0 in range(0, 32, 8):
        kb.dma("sp", vall[:, k0:k0 + 8, :], self.vf.rearrange("(kt p) c -> p kt c", p=128)[:, k0:k0 + 8, :], writes=[vall_b])
    qk = [(kb.sb("fq%d" % i, [70, S], BF16), kb.sb("fk%d" % i, [70, S], BF16)) for i in range(2)]
    pts = [kb.sb("fpt%d" % i, [128, 512], BF16) for i in range(6)]
    osb = [kb.sb("fosb%d" % i, [65, 512], F32) for i in range(2)]
    rds = [kb.sb("frd%d" % i, [65, 512], F32) for i in range(2)]
    rhl = [kb.sb("frhl%d" % i, [65, 2, 512], BF16) for i in range(2)]
    yh = [kb.sb("fyh%d" % i, [64, S], BF16) for i in range(2)]
    self._it = 0
    for h in range(8):
        (qh, qh_b), (kh, kh_b) = qk[h % 2]
        kb.dma("sp", qh[0:64, :], self.qfT[h * 64:(h + 1) * 64, :], writes=[qh_b])
        kb.dma("sp", qh[64:70, :], aqd[h], writes=[qh_b])
        kb.dma("sp", kh[0:64, :], self.kfT[h * 64:(h + 1) * 64, :], writes=[kh_b])
        kb.dma("sp", kh[64:70, :], akd[h], writes=[kh_b])
        yo, yo_b = yh[h % 2]
        items = [(g, kbi) for g in range(8) for kbi in range(4 * g + 4)]
        pend = {}
        pos = {}
        defer = []

        def emit_qk(i, qh=qh, kh=kh, qh_b=qh_b, kh_b=kh_b):
            g, kbi = items[i]
            ps, ps_b = kb.psum()
            diag = kbi >= 4 * g
            kb.op("pe", lambda e, ps=ps, kh=kh, qh=qh, kbi=kbi, g=g, diag=diag: e.matmul(ps[:], kh[0:70, kbi * 128:(kbi + 1) * 128], qh[0:70, g * 512:(g + 1) * 512],
                                                                                     start=True, stop=not diag),
                  reads=[kh_b, qh_b], writes=[ps_b])
            if diag:
                j = kbi - 4 * g
                kb.op("pe", lambda e, ps=ps, j=j: e.matmul(ps[:], self.identb[:], mk[:, j, :], start=False, stop=True),
                      reads=[self.identb_b, mk_b], writes=[ps_b])
            pt, pt_b = pts[self._it % len(pts)]
            self._it += 1
            kb.op("act", lambda e, pt=pt, ps=ps: e.activation(out=pt[:], in_=ps[:], func=AF.Exp), reads=[ps_b], writes=[pt_b])
            pend[i] = (pt, pt_b)

        def emit_pv(i, h=h, yo=yo, yo_b=yo_b):
            g, kbi = items[i]
            nkb = 4 * g + 4
            if kbi == 0:
                pos[g] = kb.psum_acc()
            po, po_b = pos[g]
            pt, pt_b = pend.pop(i)
            kb.op("pe", lambda e, po=po, pt=pt, kbi=kbi, h=h, nkb=nkb: e.matmul(po[0:65, :], vall[:, kbi, h * 65:(h + 1) * 65], pt[:],
                                                                                start=(kbi == 0), stop=(kbi == nkb - 1)),
                  reads=[vall_b, pt_b], writes=[po_b])
            if kbi == nkb - 1:
                fin = self._attn_finalize_split(po, po_b, osb[g % 2], rds[g % 2], rhl[g % 2], ones, ones_b, yo, yo_b, g)
                defer.append([3, fin])
        LA = 3
        for i in range(min(LA, len(items))):
            emit_qk(i)
        for i in range(len(items)):
            if i + LA < len(items):
                emit_qk(i + LA)
            emit_pv(i)
            for dref in list(defer):
                dref[0] -= 1
                if dref[0] <= 0:
                    dref[1]()
                    defer.remove(dref)
        for dref in defer:
            dref[1]()
        kb.dma("sp", self.ysT[2 * BW + h * 64:2 * BW + (h + 1) * 64, :], yo[:], reads=[yo_b])
    kb.barrier()


def _attn_finalize(self, po, po_b, osb_, rds_, rhl_, ones, ones_b, yo, yo_b, g):
    kb = self.kb
    (ob, ob_b), (rd, rd_b), (rh, rh_b) = osb_, rds_, rhl_
    kb.op("act", lambda e: e.activation(out=ob[:], in_=po[0:65, :], func=AF.Copy), reads=[po_b], writes=[ob_b])
    kb.op("dve", lambda e: e.reciprocal(out=rd[64:65, :], in_=ob[64:65, :]), reads=[ob_b], writes=[rd_b])
    kb.op("dve", lambda e: e.tensor_copy(out=rh[64:65, 0, :], in_=rd[64:65, :]), reads=[rd_b], writes=[rh_b])
    kb.op("dve", lambda e: e.tensor_tensor(out=rd[64:65, :], in0=rd[64:65, :], in1=rh[64:65, 0, :], op=ALU.subtract), reads=[rd_b, rh_b], writes=[rd_b])
    kb.op("dve", lambda e: e.tensor_copy(out=rh[64:65, 1, :], in_=rd[64:65, :]), reads=[rd_b, rh_b], writes=[rh_b])
    pb, pb_b = kb.psum()
    kb.op("pe", lambda e: e.matmul(pb[0:64, :], ones[64:65, 0:64], rh[64:65, 0, :], start=True, stop=False), reads=[ones_b, rh_b], writes=[pb_b])
    kb.op("pe", lambda e: e.matmul(pb[0:64, :], ones[64:65, 0:64], rh[64:65, 1, :], start=False, stop=True), reads=[ones_b, rh_b], writes=[pb_b])
    kb.op("dve", lambda e: e.tensor_tensor(out=yo[0:64, g * 512:(g + 1) * 512], in0=ob[0:64, :], in1=pb[0:64, :], op=ALU.mult),
          reads=[ob_b, pb_b], writes=[yo_b])


def _attn_finalize_split(self, po, po_b, osb_, rds_, rhl_, ones, ones_b, yo, yo_b, g):
    kb = self.kb
    (ob, ob_b), (rd, rd_b), (rh, rh_b) = osb_, rds_, rhl_
    kb.op("act", lambda e: e.activation(out=ob[:], in_=po[0:65, :], func=AF.Copy), reads=[po_b], writes=[ob_b])
    kb.op("dve", lambda e: e.reciprocal(out=rd[64:65, :], in_=ob[64:65, :]), reads=[ob_b], writes=[rd_b])
    kb.op("dve", lambda e: e.tensor_copy(out=rh[64:65, 0, :], in_=rd[64:65, :]), reads=[rd_b], writes=[rh_b])
    kb.op("dve", lambda e: e.tensor_tensor(out=rd[64:65, :], in0=rd[64:65, :], in1=rh[64:65, 0, :], op=ALU.subtract), reads=[rd_b, rh_b], writes=[rd_b])
    kb.op("dve", lambda e: e.tensor_copy(out=rh[64:65, 1, :], in_=rd[64:65, :]), reads=[rd_b, rh_b], writes=[rh_b])

    def part2():
        pb, pb_b = kb.psum()
        kb.op("pe", lambda e: e.matmul(pb[0:64, :], ones[64:65, 0:64], rh[64:65, 0, :], start=True, stop=False), reads=[ones_b, rh_b], writes=[pb_b])
        kb.op("pe", lambda e: e.matmul(pb[0:64, :], ones[64:65, 0:64], rh[64:65, 1, :], start=False, stop=True), reads=[ones_b, rh_b], writes=[pb_b])
        kb.op("dve", lambda e: e.tensor_tensor(out=yo[0:64, g * 512:(g + 1) * 512], in0=ob[0:64, :], in1=pb[0:64, :], op=ALU.mult),
              reads=[ob_b, pb_b], writes=[yo_b])
    return part2


Prog.stageB3 = _stageB3
Prog._attn_finalize = _attn_finalize
Prog._attn_finalize_split = _attn_finalize_split


def _stageB2(self, l):
    kb = self.kb
    kb.reset(self.arena0)
    NEG = -30000.0
    mk, mk_b = kb.sb("dmask", [128, 256], BF16)
    mkf, mkf_b = kb.sb("dmaskf", [128, 256], F32)
    kb.op("pool", lambda e: e.memset(mkf[:], 0.0), writes=[mkf_b])
    kb.op("pool", lambda e: e.affine_select(out=mkf[:, 0:128], in_=mkf[:, 0:128], pattern=[[1, 128]], compare_op=ALU.is_ge,
                                            fill=NEG, base=0, channel_multiplier=-1), reads=[mkf_b], writes=[mkf_b])
    kb.op("pool", lambda e: e.affine_select(out=mkf[:, 128:256], in_=mkf[:, 128:256], pattern=[[-1, 128]], compare_op=ALU.is_ge,
                                            fill=NEG, base=0, channel_multiplier=1), reads=[mkf_b], writes=[mkf_b])
    kb.op("dve", lambda e: e.tensor_copy(out=mk[:], in_=mkf[:]), reads=[mkf_b], writes=[mk_b])
    ones, ones_b = kb.sb("dones", [128, 64], BF16)
    kb.op("pool", lambda e: e.memset(ones[:], 1.0), writes=[ones_b])
    DILS = (1, 4, 16)
    vall = {}
    for d in DILS:
        t, t_b = kb.sb("dv%d" % d, [128, 32, VA], BF16)
        nm = 32 // d
        if d == 1:
            for k0 in range(0, 32, 8):
                kb.dma("sp", t[:, k0:k0 + 8, :], self.vd.rearrange("(m p) c -> p m c", p=128)[:, k0:k0 + 8, :], writes=[t_b])
        else:
            src = self.vd.rearrange("(m p r) c -> p r m c", p=128, r=d)
            for r in range(d):
                kb.dma("sp", t[:, r * nm:(r + 1) * nm, :], src[:, r, :, :], writes=[t_b])
        vall[d] = (t, t_b)
    qk = [(kb.sb("dq%d" % i, [128, S], BF16), kb.sb("dk%d" % i, [128, S], BF16)) for i in range(2)]
    pts = [kb.sb("dpt%d" % i, [128, 256], BF16) for i in range(6)]
    acc = [kb.sb("dacc%d" % i, [65, S], F32) for i in range(2)]
    osb = [kb.sb("dosb%d" % i, [65, 512], F32) for i in range(2)]
    rds = [kb.sb("drd%d" % i, [65, 512], F32) for i in range(2)]
    rhl = [kb.sb("drhl%d" % i, [65, 2, 512], BF16) for i in range(2)]
    yh = [kb.sb("dyh%d" % i, [64, S], BF16) for i in range(2)]
    self._it = 0
    for hp in range(4):
        (qt, qt_b), (kt_, kt_b) = qk[hp % 2]
        kb.dma("sp", qt[:], self.qdT[hp * 128:(hp + 1) * 128, :], writes=[qt_b])
        kb.dma("sp", kt_[:], self.kdT[hp * 128:(hp + 1) * 128, :], writes=[kt_b])
        for hh in range(2):
            h = hp * 2 + hh
            pb0 = hh * 64
            ac, ac_b = acc[h % 2]
            items = []
            for d in DILS:
                nm = 32 // d
                for r in range(d):
                    for m in range(nm):
                        items.append((d, r, m, nm))
            pend = {}
            pos = {}

            def emit_qk(i, pb0=pb0, kt_=kt_, qt=qt, kt_b=kt_b, qt_b=qt_b):
                d, r, m, nm = items[i]
                nq = 256 if m < nm - 1 else 128
                t0 = m * 128 * d + r
                ksl = slice(t0, t0 + 127 * d + 1, d)
                qsl = slice(t0, t0 + (nq - 1) * d + 1, d)
                ps, ps_b = kb.psum()
                kb.op("pe", lambda e, ps=ps, ksl=ksl, qsl=qsl, nq=nq, pb0=pb0, kt_=kt_, qt=qt: e.matmul(ps[:, 0:nq], kt_[pb0:pb0 + 64, ksl], qt[pb0:pb0 + 64, qsl], start=True, stop=False),
                      reads=[kt_b, qt_b], writes=[ps_b])
                kb.op("pe", lambda e, ps=ps, nq=nq: e.matmul(ps[:, 0:nq], self.identb[:], mk[:, 0:nq], start=False, stop=True),
                      reads=[self.identb_b, mk_b], writes=[ps_b])
                pt, pt_b = pts[self._it % len(pts)]
                self._it += 1
                kb.op("act", lambda e, pt=pt, ps=ps, nq=nq: e.activation(out=pt[:, 0:nq], in_=ps[:, 0:nq], func=AF.Exp), reads=[ps_b], writes=[pt_b])
                pend[i] = (pt, pt_b, nq, t0)

            def emit_pv(i, h=h, ac=ac, ac_b=ac_b):
                d, r, m, nm = items[i]
                vt, vt_b = vall[d]
                b = r * nm + m
                pt, pt_b, nq, t0 = pend.pop(i)
                if m == 0:
                    pos[(d, r, 0)] = kb.psum_acc()
                po, po_b = pos.pop((d, r, m))
                kb.op("pe", lambda e, po=po, vt=vt, b=b, h=h, pt=pt, m=m: e.matmul(po[0:65, 0:128], vt[:, b, h * 65:(h + 1) * 65], pt[:, 0:128], start=(m == 0), stop=True),
                      reads=[vt_b, pt_b], writes=[po_b])
                if nq == 256:
                    pos[(d, r, m + 1)] = kb.psum_acc()
                    po2, po2_b = pos[(d, r, m + 1)]
                    kb.op("pe", lambda e, po2=po2, vt=vt, b=b, h=h, pt=pt: e.matmul(po2[0:65, 0:128], vt[:, b, h * 65:(h + 1) * 65], pt[:, 128:256], start=True, stop=False),
                          reads=[vt_b, pt_b], writes=[po2_b])
                if d == 1:
                    kb.op("dve", lambda e, ac=ac, po=po, qs=slice(t0, t0 + 128): e.tensor_copy(out=ac[:, qs], in_=po[0:65, 0:128]),
                          reads=[po_b], writes=[ac_b])
                else:
                    qs = slice(t0, t0 + 127 * d + 1, d)
                    kb.op("dve", lambda e, ac=ac, po=po, qs=qs: e.tensor_tensor(out=ac[:, qs], in0=ac[:, qs], in1=po[0:65, 0:128], op=ALU.add),
                          reads=[po_b, ac_b], writes=[ac_b])
            LA = 3
            for i in range(min(LA, len(items))):
                emit_qk(i)
            for i in range(len(items)):
                if i + LA < len(items):
                    emit_qk(i + LA)
                emit_pv(i)
            yo, yo_b = yh[h % 2]
            for g in range(8):
                self._attn_finalize2(ac, ac_b, rds[g % 2], rhl[g % 2], ones, ones_b, yo, yo_b, g)
            kb.dma("sp", self.ysT[BW + h * 64:BW + (h + 1) * 64, :], yo[:], reads=[yo_b])
    kb.barrier()


def _attn_finalize2(self, ac, ac_b, rds_, rhl_, ones, ones_b, yo, yo_b, g):
    kb = self.kb
    (rd, rd_b), (rh, rh_b) = rds_, rhl_
    sl = slice(g * 512, (g + 1) * 512)
    kb.op("dve", lambda e: e.reciprocal(out=rd[64:65, :], in_=ac[64:65, sl]), reads=[ac_b], writes=[rd_b])
    kb.op("dve", lambda e: e.tensor_copy(out=rh[64:65, 0, :], in_=rd[64:65, :]), reads=[rd_b], writes=[rh_b])
    kb.op("dve", lambda e: e.tensor_tensor(out=rd[64:65, :], in0=rd[64:65, :], in1=rh[64:65, 0, :], op=ALU.subtract), reads=[rd_b, rh_b], writes=[rd_b])
    kb.op("dve", lambda e: e.tensor_copy(out=rh[64:65, 1, :], in_=rd[64:65, :]), reads=[rd_b, rh_b], writes=[rh_b])
    pb, pb_b = kb.psum()
    kb.op("pe", lambda e: e.matmul(pb[0:64, :], ones[64:65, 0:64], rh[64:65, 0, :], start=True, stop=False), reads=[ones_b, rh_b], writes=[pb_b])
    kb.op("pe", lambda e: e.matmul(pb[0:64, :], ones[64:65, 0:64], rh[64:65, 1, :], start=False, stop=True), reads=[ones_b, rh_b], writes=[pb_b])
    kb.op("dve", lambda e: e.tensor_tensor(out=yo[0:64, sl], in0=ac[0:64, sl], in1=pb[0:64, :], op=ALU.mult),
          reads=[ac_b, pb_b], writes=[yo_b])


Prog.stageB2 = _stageB2
Prog._attn_finalize2 = _attn_finalize2


def _stageB1(self, l):
    kb = self.kb
    kb.reset(self.arena0)
    PB = Buf("ssm_prep")
    idf, idf_b = self.identf, self.identf_b
    BSt, _ = kb.sb("BSt", [128, 2, 16, 2, 128], BF16)
    Gt, _ = kb.sb("Gt", [128, 2, 16, 2, 128], BF16)
    Tt, _ = kb.sb("Tt", [128, 32, 128], BF16)
    Dr, _ = kb.sb("Dr", [128, 16, 9], F32)
    Di, _ = kb.sb("Di", [128, 16, 9], F32)
    nDi, _ = kb.sb("nDi", [128, 16, 9], F32)
    m_keep = kb.mark()

    def T_(name, shape, dt_=F32):
        t, _ = kb.sb(name, shape, dt_)
        return t

    def dve(fn, extra_r=(), extra_w=()):
        kb.op("dve", fn, reads=[PB] + list(extra_r), writes=[PB] + list(extra_w))

    def tt(out, a, b, op):
        dve(lambda e: e.tensor_tensor(out=out, in0=a, in1=b, op=op))

    def ts(out, a, s1, op0, s2=None, op1=None):
        if op1 is None:
            dve(lambda e: e.tensor_scalar(out=out, in0=a, scalar1=s1, scalar2=None, op0=op0))
        else:
            dve(lambda e: e.tensor_scalar(out=out, in0=a, scalar1=s1, scalar2=s2, op0=op0, op1=op1))

    def cp(out, a):
        dve(lambda e: e.tensor_copy(out=out, in_=a))

    pp = T_("pp", [16, 3, 128])
    ld = T_("ld", [16, 2])
    kb.dma("sp", pp[:, 0, :], self.I("ssm_lambda_re")[l].rearrange("(q a) p -> q (a p)", a=2), writes=[PB])
    kb.dma("sp", pp[:, 1, :], self.I("ssm_lambda_im")[l].rearrange("(q a) p -> q (a p)", a=2), writes=[PB])
    kb.dma("sp", ld[:], self.I("ssm_log_dt")[l].rearrange("(q a) -> q a", a=2), writes=[PB])
    cp(pp[:, 2, :].rearrange("q (a p) -> q a p", a=2), ld[:].unsqueeze(2).to_broadcast([16, 2, 64]))
    par = T_("par", [128, 3, 16])
    ps, ps_b = kb.psum()
    for i in range(3):
        kb.op("pe", lambda e, i=i, ps=ps: e.transpose(ps[:, i * 16:(i + 1) * 16], pp[:, i, :], idf[0:16, 0:16]), reads=[PB, idf_b], writes=[ps_b])
    dve(lambda e, ps=ps: e.tensor_copy(out=par[:].rearrange("p a q -> p (a q)"), in_=ps[:, 0:48]), extra_r=[ps_b])
    lr, li, ldt = par[:, 0, :], par[:, 1, :], par[:, 2, :]
    Bre = T_("Bre", [128, 16, 16])
    Bim = T_("Bim", [128, 16, 16])
    for (dst, nm) in ((Bre, "ssm_b_re"), (Bim, "ssm_b_im")):
        src = self.I(nm)[l].rearrange("(q a) p c -> (a p) q c", a=2)
        for q0 in range(0, 16, 4):
            kb.dma("sp", dst[:, q0:q0 + 4, :], src[:, q0:q0 + 4, :], writes=[PB])
    Cre = T_("Cre", [128, 16, 16])
    Cim = T_("Cim", [128, 16, 16])
    Z = T_("Z", [32, 16, 128])
    zt = T_("zt", [128, 256])
    for (dst, nm) in ((Cre, "ssm_c_re"), (Cim, "ssm_c_im")):
        dve(lambda e: e.memset(Z[:], 0.0))
        src = self.I(nm)[l].rearrange("(q a) c p -> a c q p", a=2)
        kb.dma("sp", Z[0:16, :, 0:64], src[0], reads=[PB], writes=[PB])
        kb.dma("sp", Z[16:32, :, 64:128], src[1], reads=[PB], writes=[PB])
        for q in range(16):
            if q % 8 == 0:
                ps, ps_b = kb.psum()
            kb.op("pe", lambda e, ps=ps, q=q: e.transpose(ps[:, (q % 8) * 32:(q % 8) * 32 + 32], Z[:, q, :], idf[0:32, 0:32]), reads=[PB, idf_b], writes=[ps_b])
            if q % 8 == 7:
                q0 = q - 7
                dve(lambda e, ps=ps: e.tensor_copy(out=zt[:], in_=ps[:, 0:256]), extra_r=[ps_b])
                pv = zt[:].rearrange("p (q a c) -> p q a c", q=8, a=2)
                dve(lambda e, dst=dst, pv=pv, q0=q0: e.tensor_tensor(out=dst[:, q0:q0 + 8, :], in0=pv[:, :, 0, :], in1=pv[:, :, 1, :], op=ALU.add))
    def S_(name):
        return T_(name, [128, 16])
    dt = S_("dt")
    kb.op("act", lambda e: e.activation(out=dt[:], in_=ldt, func=AF.Exp), reads=[PB], writes=[PB])
    x = S_("x")
    tt(x[:], lr, dt[:], ALU.mult)
    er = S_("er")
    ts(er[:], x[:], 1.0 / 7, ALU.mult, 1.0, ALU.add)
    for k in (6, 5, 4, 3, 2, 1):
        tt(er[:], er[:], x[:], ALU.mult)
        ts(er[:], er[:], 1.0 / k, ALU.mult, 1.0, ALU.add)
    phi = S_("phi")
    tt(phi[:], li, dt[:], ALU.mult)
    ts(phi[:], phi[:], 1.0 / 32, ALU.mult)
    z = S_("z")
    tt(z[:], phi[:], phi[:], ALU.mult)
    cr = S_("cr")
    ci_ = S_("ci")
    cc = [1.0, -1.0 / 2, 1.0 / 24, -1.0 / 720, 1.0 / 40320, -1.0 / 3628800, 1.0 / 479001600]
    sc = [1.0, -1.0 / 6, 1.0 / 120, -1.0 / 5040, 1.0 / 362880, -1.0 / 39916800, 1.0 / 6227020800]
    for (dst, co) in ((cr, cc), (ci_, sc)):
        ts(dst[:], z[:], co[6], ALU.mult, co[5], ALU.add)
        for k in (4, 3, 2, 1, 0):
            tt(dst[:], dst[:], z[:], ALU.mult)
            ts(dst[:], dst[:], co[k], ALU.add)
    tt(ci_[:], ci_[:], phi[:], ALU.mult)
    t1, t2, t3, t4 = S_("t1"), S_("t2"), S_("t3"), S_("t4")

    def cmul(or_, oi, ar, ai, br, bi, a1=t1, a2=t2, a3=t3, a4=t4):
        tt(a1, ar, br, ALU.mult)
        tt(a2, ai, bi, ALU.mult)
        tt(a3, ar, bi, ALU.mult)
        tt(a4, ai, br, ALU.mult)
        tt(or_, a1, a2, ALU.subtract)
        tt(oi, a3, a4, ALU.add)

    for _ in range(5):
        cmul(cr[:], ci_[:], cr[:], ci_[:], cr[:], ci_[:], t1[:], t2[:], t3[:], t4[:])
        tt(t1[:], cr[:], cr[:], ALU.mult)
        tt(t2[:], ci_[:], ci_[:], ALU.mult)
        tt(t1[:], t1[:], t2[:], ALU.add)
        ts(t1[:], t1[:], -0.5, ALU.mult, 1.5, ALU.add)
        tt(cr[:], cr[:], t1[:], ALU.mult)
        tt(ci_[:], ci_[:], t1[:], ALU.mult)
    Pl_r = T_("Plr", [128, 9, 16])
    Pl_i = T_("Pli", [128, 9, 16])
    dve(lambda e: e.memset(Pl_r[:, 0, :], 1.0))
    dve(lambda e: e.memset(Pl_i[:, 0, :], 0.0))
    tt(Pl_r[:, 1, :], cr[:], er[:], ALU.mult)
    tt(Pl_i[:, 1, :], ci_[:], er[:], ALU.mult)
    ar, ai = Pl_r[:, 1, :], Pl_i[:, 1, :]
    for k in range(2, 9):
        cmul(Pl_r[:, k, :], Pl_i[:, k, :], Pl_r[:, k - 1, :], Pl_i[:, k - 1, :], ar, ai, t1[:], t2[:], t3[:], t4[:])
    Nl_r = T_("Nlr", [128, 8, 16])
    Nl_i = T_("Nli", [128, 8, 16])
    dve(lambda e: e.memset(Nl_r[:, 0, :], 1.0))
    dve(lambda e: e.memset(Nl_i[:, 0, :], 0.0))
    tt(t1[:], ar, ar, ALU.mult)
    tt(t2[:], ai, ai, ALU.mult)
    tt(t1[:], t1[:], t2[:], ALU.add)
    dve(lambda e: e.reciprocal(out=t1[:], in_=t1[:]))
    tt(Nl_r[:, 1, :], ar, t1[:], ALU.mult)
    tt(Nl_i[:, 1, :], ai, t1[:], ALU.mult)
    ts(Nl_i[:, 1, :], Nl_i[:, 1, :], -1.0, ALU.mult)
    for k in range(2, 8):
        cmul(Nl_r[:, k, :], Nl_i[:, k, :], Nl_r[:, k - 1, :], Nl_i[:, k - 1, :], Nl_r[:, 1, :], Nl_i[:, 1, :], t1[:], t2[:], t3[:], t4[:])
    cp(Dr[:, :, 0], Pl_r[:, 8, :])
    cp(Di[:, :, 0], Pl_i[:, 8, :])
    for k in range(1, 9):
        cmul(Dr[:, :, k], Di[:, :, k], Dr[:, :, k - 1], Di[:, :, k - 1], Dr[:, :, k - 1], Di[:, :, k - 1], t1[:], t2[:], t3[:], t4[:])
    ts(nDi[:], Di[:], -1.0, ALU.mult)
    qr, qi = S_("qr"), S_("qi")
    nr = S_("nr")
    ts(nr[:], ar, -1.0, ALU.add)
    tt(t1[:], lr, lr, ALU.mult)
    tt(t2[:], li, li, ALU.mult)
    tt(t1[:], t1[:], t2[:], ALU.add)
    dve(lambda e: e.reciprocal(out=t1[:], in_=t1[:]))
    tt(t2[:], nr[:], lr, ALU.mult)
    tt(t3[:], ai, li, ALU.mult)
    tt(t2[:], t2[:], t3[:], ALU.add)
    tt(qr[:], t2[:], t1[:], ALU.mult)
    tt(t2[:], ai, lr, ALU.mult)
    tt(t3[:], nr[:], li, ALU.mult)
    tt(t2[:], t2[:], t3[:], ALU.subtract)
    tt(qi[:], t2[:], t1[:], ALU.mult)
    Br2, Bi2 = T_("Br2", [128, 16, 16]), T_("Bi2", [128, 16, 16])
    u1, u2, u3, u4 = (T_("u%d" % i, [128, 16, 16]) for i in range(4))

    def bq(t):
        return t.unsqueeze(2).to_broadcast([128, 16, 16])
    cmul(Br2[:], Bi2[:], Bre[:], Bim[:], bq(qr[:]), bq(qi[:]), u1[:], u2[:], u3[:], u4[:])
    def Bg(name):
        return T_(name, [128, 16, 8, 16])
    g1, g2, g3, g4 = Bg("g1"), Bg("g2"), Bg("g3"), Bg("g4")
    Wm_r, Wm_i = Bg("Wmr"), Bg("Wmi")

    def over_k(t):
        return t.unsqueeze(2).to_broadcast([128, 16, 8, 16])

    def over_c(t):
        return t.rearrange("p k q -> p q k").unsqueeze(3).to_broadcast([128, 16, 8, 16])

    def over_kc(t):
        return t.unsqueeze(2).unsqueeze(3).to_broadcast([128, 16, 8, 16])

    cmul(Wm_r[:], Wm_i[:], over_k(Br2[:]), over_k(Bi2[:]), over_c(Nl_r[:, 0:8, :]), over_c(Nl_i[:, 0:8, :]), g1[:], g2[:], g3[:], g4[:])
    WmM = T_("WmM", [128, 2, 2, 16, 128], BF16)
    dve(lambda e: e.memset(WmM[:].rearrange("p a b q n -> p (a b q n)"), 0.0))
    for a in range(2):
        for part, src in ((0, Wm_r), (1, Wm_i)):
            cp(WmM[a * 64:(a + 1) * 64, a, part, :, :], src[a * 64:(a + 1) * 64].rearrange("p q k c -> p q (k c)"))
    W7_r, W7_i = Bg("W7r"), Bg("W7i")
    cmul(W7_r[:], W7_i[:], Wm_r[:], Wm_i[:], over_kc(Pl_r[:, 7, :]), over_kc(Pl_i[:, 7, :]), g1[:], g2[:], g3[:], g4[:])
    BSt_b = Buf("BSt")
    kb.op("pool", lambda e: e.memset(BSt[:].rearrange("p a q b n -> p (a q b n)"), 0.0), writes=[BSt_b])
    for part, src in ((0, W7_r), (1, W7_i)):
        for q in range(16):
            if q % 4 == 0:
                ps, ps_b = kb.psum()
            kb.op("pe", lambda e, ps=ps, q=q, src=src: e.transpose(ps[:, (q % 4) * 128:(q % 4) * 128 + 128], src[:, q].rearrange("p k c -> p (k c)"), idf[:]),
                  reads=[PB, idf_b], writes=[ps_b])
            if q % 4 == 3:
                for a in range(2):
                    pv = ps[:].rearrange("p (q n) -> p q n", q=4)[:, :, a * 64:(a + 1) * 64]
                    kb.op("act", lambda e, pv=pv, part=part, q=q, a=a: e.activation(out=BSt[:, part, q - 3:q + 1, a, a * 64:(a + 1) * 64], in_=pv, func=AF.Copy),
                          reads=[ps_b, BSt_b], writes=[BSt_b])
    Cp_r, Cp_i = Bg("Cpr"), Bg("Cpi")
    cmul(Cp_r[:], Cp_i[:], over_k(Cre[:]), over_k(Cim[:]), over_c(Pl_r[:, 0:8, :]), over_c(Pl_i[:, 0:8, :]), g1[:], g2[:], g3[:], g4[:])
    CpB = T_("CpB", [128, 2, 16, 128], BF16)
    cp(CpB[:, 0], Cp_r[:].rearrange("p q k c -> p q (k c)"))
    ts(CpB[:, 1], Cp_i[:].rearrange("p q k c -> p q (k c)"), -1.0, ALU.mult)
    G_r, G_i = W7_r, W7_i
    kb.op("dve", lambda e: e.memset(t1[:], 0.0), reads=[PB, BSt_b], writes=[PB])
    cmul(G_r[:], G_i[:], Cp_r[:], Cp_i[:], over_kc(ar), over_kc(ai), g1[:], g2[:], g3[:], g4[:])
    dve(lambda e: e.memset(Gt[:].rearrange("p a q b n -> p (a q b n)"), 0.0))
    for a in range(2):
        cp(Gt[a * 64:(a + 1) * 64, 0, :, a, :], G_r[a * 64:(a + 1) * 64].rearrange("p q k c -> p q (k c)"))
        ts(Gt[a * 64:(a + 1) * 64, 1, :, a, :], G_i[a * 64:(a + 1) * 64].rearrange("p q k c -> p q (k c)"), -1.0, ALU.mult)
    kidx_i = T_("kidx_i", [128, 1], I32)
    kidx = T_("kidx", [128, 1])
    jidx_i = T_("jidx_i", [128, 8, 16], I32)
    jidx = T_("jidx", [128, 128])
    cmask = T_("cmask", [128, 128])
    kb.op("pool", lambda e: e.iota(kidx_i[:], pattern=[[0, 1]], base=0, channel_multiplier=1), reads=[PB], writes=[PB])
    kb.op("pool", lambda e: e.iota(jidx_i[:], pattern=[[1, 8], [0, 16]], base=0, channel_multiplier=0), reads=[PB], writes=[PB])
    dve(lambda e: e.tensor_single_scalar(out=kidx_i[:], in_=kidx_i[:], scalar=4, op=ALU.arith_shift_right))
    cp(kidx[:], kidx_i[:])
    cp(jidx[:], jidx_i[:].rearrange("p a b -> p (a b)"))
    ts(cmask[:], jidx[:], kidx[:, 0:1], ALU.is_ge)
    dB = T_("dB", [128, 512])
    kb.dma("sp", dB[:], self.I("ssm_d")[l].partition_broadcast(128), writes=[PB])
    IDd = T_("IDd", [128, 32, 128], BF16)
    for g in range(32):
        dve(lambda e, g=g: e.tensor_tensor(out=IDd[:, g, :].rearrange("p (j c) -> p j c", j=8), in0=idf[:].rearrange("p (j c) -> p j c", j=8),
                                           in1=dB[:, g * 16:(g + 1) * 16].unsqueeze(1).to_broadcast([128, 8, 16]), op=ALU.mult), extra_r=[idf_b])
    Tt_b = Buf("Tt")
    for g in range(32):
        q, a = g // 2, g % 2
        if g % 4 == 0:
            ps, ps_b = kb.psum()
        sl = slice((g % 4) * 128, (g % 4) * 128 + 128)
        kb.op("pe", lambda e, ps=ps, sl=sl, q=q, a=a: e.matmul(ps[:, sl], WmM[:, a, 0, q, :], CpB[:, 0, q, :], start=True, stop=False), reads=[PB], writes=[ps_b])
        kb.op("pe", lambda e, ps=ps, sl=sl, q=q, a=a: e.matmul(ps[:, sl], WmM[:, a, 1, q, :], CpB[:, 1, q, :], start=False, stop=False), reads=[PB], writes=[ps_b])
        kb.op("pe", lambda e, ps=ps, sl=sl, g=g: e.matmul(ps[:, sl], self.identb[:], IDd[:, g, :], start=False, stop=True), reads=[PB, self.identb_b], writes=[ps_b])
        if g % 4 == 3:
            kb.op("dve", lambda e, ps=ps, g=g: e.tensor_tensor(out=Tt[:, g - 3:g + 1, :], in0=ps[:].rearrange("p (g n) -> p g n", g=4),
                                                               in1=cmask[:].unsqueeze(1).to_broadcast([128, 4, 128]), op=ALU.mult),
                  reads=[ps_b, PB], writes=[Tt_b])
    kb.barrier()
    kb.reset(m_keep)
    self._ssm_main(l, dict(BSt=BSt, Gt=Gt, Tt=Tt, Dr=Dr, Di=Di, nDi=nDi), m_keep)


def _ssm_main(self, l, tb, m0):
    kb = self.kb
    BSt, Gt, Tt, Dr, Di, nDi = tb["BSt"], tb["Gt"], tb["Tt"], tb["Dr"], tb["Di"], tb["nDi"]
    TB = Buf("ssm_tables")
    U, U_b = kb.sb("U", [128, 32, 512], BF16)
    m_x = kb.mark()
    xts = [kb.sb("Xc%d" % i, [128, 8, 512], BF16) for i in range(2)]
    x2s = [kb.sb("X2c%d" % i, [128, 32, 128], BF16) for i in range(2)]
    kb.reset(m_x)
    ygT, ygT_b = kb.sb("ygT", [128, 4, S], BF16)
    for ct in range(4):
        xt0, xt0_b = xts[ct % 2]
        kb.dma("sp", xt0[:].rearrange("p k c -> p (k c)"), self.u_tm[ct * 1024:(ct + 1) * 1024, :].rearrange("(p k) c -> p (k c)", k=8), writes=[xt0_b])
        xt, xt_b = x2s[ct % 2]
        kb.op("pool", lambda e, xt=xt, xt0=xt0: e.tensor_copy(out=xt[:].rearrange("p g (k c) -> p g k c", k=8),
                                                              in_=xt0[:].rearrange("p k (g c) -> p g k c", g=32)),
              reads=[xt0_b], writes=[xt_b])
        for g in range(32):
            if g % 4 == 0:
                ps, ps_b = kb.psum()
            kb.op("pe", lambda e, ps=ps, g=g, xt=xt: e.matmul(ps[:, (g % 4) * 128:(g % 4) * 128 + 128], xt[:, g, :], self.identb[:], start=True, stop=True),
                  reads=[xt_b, self.identb_b], writes=[ps_b])
            if g % 4 == 3:
                eng = "act" if (g // 4) % 2 == 0 else "dve"
                pv = ps[:].rearrange("p (g n) -> p g n", g=4)
                if eng == "act":
                    kb.op("act", lambda e, pv=pv, g=g, ct=ct: e.activation(out=U[:, g - 3:g + 1, ct * 128:(ct + 1) * 128], in_=pv, func=AF.Copy), reads=[ps_b], writes=[U_b])
                else:
                    kb.op("dve", lambda e, pv=pv, g=g, ct=ct: e.tensor_copy(out=U[:, g - 3:g + 1, ct * 128:(ct + 1) * 128], in_=pv), reads=[ps_b], writes=[U_b])
    Yg, Yg_b = kb.sb("Yg", [128, 4, 8, 512], BF16)
    SA = [kb.sb("SA%d" % i, [128, 2, 512], F32) for i in range(2)]
    SB_ = [kb.sb("SB%d" % i, [128, 2, 512], F32) for i in range(2)]
    Ssh = [kb.sb("Ssh%d" % i, [128, 2, 512], BF16) for i in range(2)]
    for q in range(16):
        (sa, sa_b), (sb2, sb2_b), (ssh, ssh_b) = SA[q % 2], SB_[q % 2], Ssh[q % 2]
        for part in range(2):
            ps, ps_b = kb.psum()
            for a in range(2):
                kb.op("pe", lambda e, ps=ps, part=part, a=a, q=q: e.matmul(ps[:], BSt[:, part, q, a, :], U[:, 2 * q + a, :], start=(a == 0), stop=(a == 1)),
                      reads=[U_b, TB], writes=[ps_b])
            kb.op("act", lambda e, ps=ps, part=part, sa=sa: e.activation(out=sa[:, part, :], in_=ps[:], func=AF.Copy), reads=[ps_b], writes=[sa_b])
        cur, cur_b, nxt, nxt_b = sa, sa_b, sb2, sb2_b
        for k in range(9):
            m = 1 << k
            dr, di, ndi = Dr[:, q, k:k + 1], Di[:, q, k:k + 1], nDi[:, q, k:k + 1]
            kb.op("pool", lambda e, cur=cur, nxt=nxt, m=m: e.tensor_copy(out=nxt[:, :, 0:m], in_=cur[:, :, 0:m]), reads=[cur_b], writes=[nxt_b])
            kb.op("dve", lambda e, cur=cur, nxt=nxt, m=m, dr=dr: e.scalar_tensor_tensor(out=nxt[:, 0, m:512], in0=cur[:, 0, 0:512 - m], scalar=dr, in1=cur[:, 0, m:512], op0=ALU.mult, op1=ALU.add),
                  reads=[cur_b, TB], writes=[nxt_b])
            kb.op("dve", lambda e, cur=cur, nxt=nxt, m=m, ndi=ndi: e.scalar_tensor_tensor(out=nxt[:, 0, m:512], in0=cur[:, 1, 0:512 - m], scalar=ndi, in1=nxt[:, 0, m:512], op0=ALU.mult, op1=ALU.add),
                  reads=[cur_b, nxt_b, TB], writes=[nxt_b])
            kb.op("dve", lambda e, cur=cur, nxt=nxt, m=m, di=di: e.scalar_tensor_tensor(out=nxt[:, 1, m:512], in0=cur[:, 0, 0:512 - m], scalar=di, in1=cur[:, 1, m:512], op0=ALU.mult, op1=ALU.add),
                  reads=[cur_b, TB], writes=[nxt_b])
            kb.op("dve", lambda e, cur=cur, nxt=nxt, m=m, dr=dr: e.scalar_tensor_tensor(out=nxt[:, 1, m:512], in0=cur[:, 1, 0:512 - m], scalar=dr, in1=nxt[:, 1, m:512], op0=ALU.mult, op1=ALU.add),
                  reads=[cur_b, nxt_b, TB], writes=[nxt_b])
            cur, cur_b, nxt, nxt_b = nxt, nxt_b, cur, cur_b
        kb.op("pool", lambda e, ssh=ssh: e.memset(ssh[:, :, 0:1], 0.0), writes=[ssh_b])
        kb.op("act", lambda e, ssh=ssh, cur=cur: e.activation(out=ssh[:, :, 1:512], in_=cur[:, :, 0:511], func=AF.Copy), reads=[cur_b, ssh_b], writes=[ssh_b])
        for ct in range(4):
            if ct % 2 == 0:
                ps, ps_b = kb.psum()
            o0 = (ct % 2) * 256
            csl = slice(ct * 128, (ct + 1) * 128)
            kb.op("pe", lambda e, ps=ps, o0=o0, csl=csl, ssh=ssh, q=q: e.matmul(ps[:, o0:o0 + 256], ssh[:, 0, csl], Gt[:, 0, q].rearrange("p a n -> p (a n)"), start=True, stop=False),
                  reads=[ssh_b, TB], writes=[ps_b])
            kb.op("pe", lambda e, ps=ps, o0=o0, csl=csl, ssh=ssh, q=q: e.matmul(ps[:, o0:o0 + 256], ssh[:, 1, csl], Gt[:, 1, q].rearrange("p a n -> p (a n)"), start=False, stop=False),
                  reads=[ssh_b, TB], writes=[ps_b])
            for a in range(2):
                kb.op("pe", lambda e, ps=ps, o0=o0, csl=csl, a=a, q=q: e.matmul(ps[:, o0 + a * 128:o0 + (a + 1) * 128], U[:, 2 * q + a, csl], Tt[:, 2 * q + a, :], start=False, stop=(a == 1)),
                      reads=[U_b, TB], writes=[ps_b])
            kb.op("act", lambda e, ps=ps, o0=o0, ct=ct, q=q: e.activation(out=Yg[:, ct, :, q * 32:(q + 1) * 32].rearrange("p j (a c) -> p a j c", a=2),
                                                                           in_=ps[:, o0:o0 + 256].rearrange("p (a j c) -> p a j c", a=2, j=8), func=AF.Gelu_apprx_tanh),
                  reads=[ps_b], writes=[Yg_b])
    ev = 0
    for ct in range(4):
        for cht in range(4):
            for jh in range(2):
                ps, ps_b = kb.psum()
                for jj in range(4):
                    j = jh * 4 + jj
                    kb.op("pe", lambda e, ps=ps, jj=jj, j=j, ct=ct, cht=cht: e.matmul(ps[:, jj * 128:(jj + 1) * 128], Yg[:, ct, j, cht * 128:(cht + 1) * 128], self.identb[:], start=True, stop=True),
                          reads=[Yg_b, self.identb_b], writes=[ps_b])
                t0 = ct * 1024 + jh * 4
                dst = ygT[:, cht, ct * 1024:(ct + 1) * 1024].rearrange("p (c j) -> p j c", j=8)[:, jh * 4:jh * 4 + 4, :]
                pv = ps[:].rearrange("p (j c) -> p j c", j=4)
                if ev % 2 == 0:
                    kb.op("act", lambda e, dst=dst, pv=pv: e.activation(out=dst, in_=pv, func=AF.Copy), reads=[ps_b], writes=[ygT_b])
                else:
                    kb.op("dve", lambda e, dst=dst, pv=pv: e.tensor_copy(out=dst, in_=pv), reads=[ps_b], writes=[ygT_b])
                ev += 1
    wg, wg_b = kb.sb("wglu", [128, 4, 512], BF16)
    kb.dma("pool", wg[:], self.I("w_glu")[l].rearrange("(kt p) n -> p kt n", p=128), writes=[wg_b])
    sgs = [kb.sb("sg%d" % i, [128, 512], BF16) for i in range(3)]
    yos = [kb.sb("yo%d" % i, [128, S], BF16) for i in range(2)]
    it = 0
    for mo in range(4):
        yo, yo_b = yos[mo % 2]
        for tg in range(8):
            ps, ps_b = kb.psum()
            tsl = slice(tg * 512, (tg + 1) * 512)
            for kt in range(4):
                kb.op("pe", lambda e, ps=ps, kt=kt, mo=mo, tsl=tsl: e.matmul(ps[:], wg[:, kt, mo * 128:(mo + 1) * 128], ygT[:, kt, tsl], start=(kt == 0), stop=(kt == 3)),
                      reads=[wg_b, ygT_b], writes=[ps_b])
            sg, sg_b = sgs[it % 3]
            it += 1
            kb.op("act", lambda e, sg=sg, ps=ps: e.activation(out=sg[:], in_=ps[:], func=AF.Sigmoid), reads=[ps_b], writes=[sg_b])
            kb.op("pool", lambda e, yo=yo, sg=sg, mo=mo, tsl=tsl: e.tensor_tensor(out=yo[:, tsl], in0=ygT[:, mo, tsl], in1=sg[:], op=ALU.mult),
                  reads=[ygT_b, sg_b], writes=[yo_b])
        kb.dma("sp", self.ysT[mo * 128:(mo + 1) * 128, :], yo[:], reads=[yo_b])
    kb.barrier()


Prog.stageB1 = _stageB1
Prog._ssm_main = _ssm_main


def _stageC(self, l, last):
    kb = self.kb
    kb.reset(self.arena0)
    idf, idf_b = self.identf, self.identf_b
    moe = (l % 2 == 1)
    li2 = l // 2
    ones_m, ones_m_b = kb.sb("ones_m", [128, 128], BF16)
    ones_1, ones_1_b = kb.sb("ones_1", [128, 128], BF16)
    kb.op("pool", lambda e: e.memset(ones_m[:], 1.0 / 1024), writes=[ones_m_b])
    kb.op("pool", lambda e: e.memset(ones_1[:], 1.0), writes=[ones_1_b])
    lnp, lnp_b = kb.sb("lnp", [8, 6, 128], F32)
    for i, nm in enumerate(["ln_mix_g", "ln_mix_b", "ln_x_g", "ln_x_b", "ln_ffn_g", "ln_ffn_b"]):
        kb.dma("sp", lnp[:, i, :], self.I(nm)[l].rearrange("(kt p) -> kt p", p=128), writes=[lnp_b])
    lnT, lnT_b = kb.sb("lnT", [128, 6, 8], F32)
    ps, ps_b = kb.psum()
    for i in range(6):
        kb.op("pe", lambda e, i=i, ps=ps: e.transpose(ps[:, i * 8:(i + 1) * 8], lnp[:, i, :], idf[0:8, 0:8]), reads=[lnp_b, idf_b], writes=[ps_b])
    kb.op("dve", lambda e, ps=ps: e.tensor_copy(out=lnT[:].rearrange("p a b -> p (a b)"), in_=ps[:, 0:48]), reads=[ps_b], writes=[lnT_b])
    NW = 4 if moe else 6
    wraw = [kb.sb("wC%d" % i, [128, 4096], BF16) for i in range(NW)]
    self._wc = 0

    def wbuf():
        t = wraw[self._wc % NW]
        self._wc += 1
        return t

    def load_w(src, kt_n, ncols):
        t, t_b = wbuf()
        v = t[:, 0:kt_n * ncols].rearrange("p (k n) -> p k n", k=kt_n)
        sv = src.rearrange("(k p) n -> p k n", p=128)
        for k0 in range(0, kt_n, 8):
            k1 = min(kt_n, k0 + 8)
            kb.dma("sp", v[:, k0:k1, :], sv[:, k0:k1, :], reads=[self.wcb], writes=[t_b])
        return v, t_b

    def linear(W, kt_n, n_out, rhs_fn, rhs_bufs, consume):
        cpc = 512
        while kt_n * cpc * 2 > 8192:
            cpc //= 2
        c0 = 0
        while c0 < n_out:
            nc_ = min(cpc, n_out - c0)
            wv, w_b = load_w(W[:, c0:c0 + nc_], kt_n, nc_)
            for j in range(nc_ // 128):
                ps, ps_b = kb.psum()
                for kt in range(kt_n):
                    kb.op("pe", lambda e, ps=ps, wv=wv, kt=kt, j=j: e.matmul(ps[:], wv[:, kt, j * 128:(j + 1) * 128], rhs_fn(kt), start=(kt == 0), stop=(kt == kt_n - 1)),
                          reads=[w_b] + rhs_bufs, writes=[ps_b])
                consume((c0 // 128) + j, ps, ps_b)
            c0 += nc_

    memT, memT_b = self.memT, self.memT_b
    KT, KT_b = kb.sb("KT", [128, 8, NMEM], BF16)
    Vm, Vm_b = kb.sb("Vm", [128, 2, D], BF16)

    def cons_k(ot, ps, ps_b):
        kb.op("act", lambda e: e.activation(out=KT[:, ot, :], in_=ps[:, 0:NMEM], func=AF.Copy), reads=[ps_b], writes=[KT_b])
    def lin_k():
        W = self.wc["w_xk"]
        for c0 in (0, 512):
            wv, w_b = load_w(W[:, c0:c0 + 512], 8, 512)
            for j in range(4):
                ps, ps_b = kb.psum()
                for kt in range(8):
                    kb.op("pe", lambda e, ps=ps, wv=wv, kt=kt, j=j: e.matmul(ps[:, 0:NMEM], wv[:, kt, j * 128:(j + 1) * 128], memT[:, kt, :], start=(kt == 0), stop=(kt == 7)),
                          reads=[w_b, memT_b], writes=[ps_b])
                cons_k(c0 // 128 + j, ps, ps_b)
    lin_k()
    Wv = self.wc["w_xv"]
    for c0 in (0, 512):
        wv, w_b = load_w(Wv[:, c0:c0 + 512], 8, 512)
        for mt in range(2):
            ps, ps_b = kb.psum()
            for kt in range(8):
                kb.op("pe", lambda e, ps=ps, wv=wv, kt=kt, mt=mt: e.matmul(ps[:], memT[:, kt, mt * 128:(mt + 1) * 128], wv[:, kt, :], start=(kt == 0), stop=(kt == 7)),
                      reads=[w_b, memT_b], writes=[ps_b])
            kb.op("act", lambda e, ps=ps, mt=mt, c0=c0: e.activation(out=Vm[:, mt, c0:c0 + 512], in_=ps[:], func=AF.Copy), reads=[ps_b], writes=[Vm_b])
    if moe:
        wr32, wr32_b = kb.sb("wr32", [128, 8, 8], F32)
        kb.dma("sp", wr32[:], self.I("moe_w_router")[li2].rearrange("(k p) e -> p k e", p=128), writes=[wr32_b])
        wrh, wrh_b = kb.sb("wrh", [128, 2, 8, 8], BF16)
        wrr, wrr_b = kb.sb("wrr", [128, 8, 8], F32)
        kb.op("dve", lambda e: e.tensor_copy(out=wrh[:, 0], in_=wr32[:]), reads=[wr32_b], writes=[wrh_b])
        kb.op("dve", lambda e: e.tensor_tensor(out=wrr[:], in0=wr32[:], in1=wrh[:, 0], op=ALU.subtract), reads=[wr32_b, wrh_b], writes=[wrr_b])
        kb.op("dve", lambda e: e.tensor_copy(out=wrh[:, 1], in_=wrr[:]), reads=[wrr_b, wrh_b], writes=[wrh_b])
        brB, brB_b = kb.sb("brB", [128, 8], F32)
        kb.dma("sp", brB[:], self.I("moe_b_router")[li2].partition_broadcast(128), writes=[brB_b])
        sel, sel_b = kb.sb("sel", [8, 8, 128], BF16)
        self_f, self_f_b = kb.sb("self", [8, 8, 128], F32)
        kb.op("pool", lambda e: e.memset(self_f[:], 1.0), writes=[self_f_b])
        kb.op("pool", lambda e: e.affine_select(out=self_f[:], in_=self_f[:], pattern=[[1, 8], [0, 128]], compare_op=ALU.is_equal, fill=0.0, base=0, channel_multiplier=-1),
              reads=[self_f_b], writes=[self_f_b])
        kb.op("dve", lambda e: e.tensor_copy(out=sel[:], in_=self_f[:]), reads=[self_f_b], writes=[sel_b])
    xr, xr_b = kb.sb("xr", [128, 8, 512], F32)
    vv, vv_b = kb.sb("vv", [128, 8, 512], F32)
    GB = [kb.sb("G%d" % i, [128, 8, 512], BF16) for i in range(4)]
    m_u = kb.mark()
    ys, ys_b = kb.sb("ysg", [128, 12, 512], BF16)
    gt, gt_b = kb.sb("gtg", [128, 24, 512], BF16)
    kb.reset(m_u)
    nft = 11 if moe else 22
    hh, hh_b = kb.sb("hh", [128, nft, 512], BF16)
    if moe:
        macc, macc_b = kb.sb("macc", [128, 8, 512], F32)
    kb.reset(m_u + 36 * 1024)
    PT = [kb.sb("PT%d" % i, [128, 2, 512], BF16) for i in range(2)]
    NTF = 4 if moe else 6
    tmpf = [kb.sb("tmpf%d" % i, [128, 512], F32) for i in range(NTF)]
    tmpb = [kb.sb("tmpb%d" % i, [128, 512], BF16) for i in range(3)]
    st_mean, st_mean_b = kb.sb("st_mean", [128, 512], F32)
    st_rstd, st_rstd_b = kb.sb("st_rstd", [128, 512], F32)
    if moe:
        lg, lg_b = kb.sb("lg", [128, 4, 8], F32)
        gsm = {n_: kb.sb("g_" + n_, [128, 4, 8], F32) for n_ in ("eq1", "eq2", "lg2", "gate", "gr")}
        gs1 = {n_: kb.sb("s_" + n_, [128, 4], F32) for n_ in ("m1", "m2", "w1", "w2")}
        gTs, gTs_b = kb.sb("gTs", [8, 512], F32)
        gTh, gTh_b = kb.sb("gTh", [8, 2, 512], BF16)
        gTr, gTr_b = kb.sb("gTr", [8, 512], F32)
        xlo, xlo_b = kb.sb("xlo", [128, 8, 512], BF16)
        gbs, gbs_b = kb.sb("gbs", [128, 512], F32)
    if last:
        ostg = [kb.sb("ostg%d" % i, [128, D], F32) for i in range(2)]
    self._ti = 0

    def tf():
        t = tmpf[self._ti % NTF]
        self._ti += 1
        return t

    self._tb = 0

    def tbf():
        t = tmpb[self._tb % 3]
        self._tb += 1
        return t

    def layer_norm(gi):
        (vb, vb_b), (vsq, vsq_b) = GB[1], GB[2]
        kb.op("act", lambda e: e.activation(out=vb[:], in_=vv[:], func=AF.Copy), reads=[vv_b], writes=[vb_b])
        kb.op("act", lambda e: e.activation(out=vsq[:], in_=vv[:], func=AF.Square), reads=[vv_b], writes=[vsq_b])
        pm, pm_b = kb.psum()
        for kt in range(8):
            kb.op("pe", lambda e, kt=kt: e.matmul(pm[:], ones_m[:], vb[:, kt, :], start=(kt == 0), stop=(kt == 7)), reads=[ones_m_b, vb_b], writes=[pm_b])
        pq, pq_b = kb.psum()
        for kt in range(8):
            kb.op("pe", lambda e, kt=kt: e.matmul(pq[:], ones_m[:], vsq[:, kt, :], start=(kt == 0), stop=(kt == 7)), reads=[ones_m_b, vsq_b], writes=[pq_b])
        kb.op("act", lambda e: e.activation(out=st_mean[:], in_=pm[:], func=AF.Copy), reads=[pm_b], writes=[st_mean_b])
        (m2, m2_b) = tf()
        kb.op("dve", lambda e: e.tensor_tensor(out=m2[:], in0=st_mean[:], in1=st_mean[:], op=ALU.mult), reads=[st_mean_b], writes=[m2_b])
        kb.op("dve", lambda e: e.tensor_tensor(out=m2[:], in0=pq[:], in1=m2[:], op=ALU.subtract), reads=[pq_b, m2_b], writes=[m2_b])
        kb.op("dve", lambda e: e.tensor_scalar(out=m2[:], in0=m2[:], scalar1=0.0, scalar2=LN_EPS, op0=ALU.max, op1=ALU.add), reads=[m2_b], writes=[m2_b])
        kb.op("act", lambda e: e.activation(out=m2[:], in_=m2[:], func=AF.Sqrt), reads=[m2_b], writes=[m2_b])
        kb.op("dve", lambda e: e.reciprocal(out=st_rstd[:], in_=m2[:]), reads=[m2_b], writes=[st_rstd_b])
        for kt in range(8):
            eng = "dve" if kt % 2 == 0 else "pool"
            kb.op(eng, lambda e, kt=kt: e.tensor_tensor(out=vv[:, kt, :], in0=vv[:, kt, :], in1=st_mean[:], op=ALU.subtract), reads=[vv_b, st_mean_b], writes=[vv_b])
            kb.op(eng, lambda e, kt=kt: e.tensor_tensor(out=vv[:, kt, :], in0=vv[:, kt, :], in1=st_rstd[:], op=ALU.mult), reads=[vv_b, st_rstd_b], writes=[vv_b])
            kb.op(eng, lambda e, kt=kt: e.tensor_scalar(out=xr[:, kt, :], in0=vv[:, kt, :], scalar1=lnT[:, gi, kt:kt + 1], scalar2=lnT[:, gi + 1, kt:kt + 1], op0=ALU.mult, op1=ALU.add),
                  reads=[vv_b, lnT_b], writes=[xr_b])

    def resid_consume(ot, ps, ps_b):
        kb.op("dve", lambda e: e.scalar_tensor_tensor(out=vv[:, ot, :], in0=xr[:, ot, :], scalar=float(ALPHA), in1=ps[:], op0=ALU.mult, op1=ALU.add),
              reads=[xr_b, ps_b], writes=[vv_b])

    xres_v = self.xres.rearrange("(kt p) t -> p kt t", p=128)
    xTd_v = self.xTd.rearrange("(kt p) t -> p kt t", p=128)
    for tg in range(8):
        tsl = slice(tg * 512, (tg + 1) * 512)
        kb.dma("pool", xr[:], xres_v[:, :, tsl], writes=[xr_b])
        ysv = self.ysT.rearrange("(a p) t -> p a t", p=128)
        for a0 in range(0, 12, 6):
            kb.dma("pool", ys[:, a0:a0 + 6, :], ysv[:, a0:a0 + 6, tsl], writes=[ys_b, hh_b])
        gv = self.gT.rearrange("(a p) t -> p a t", p=128)
        for a0 in range(0, 24, 6):
            kb.dma("pool", gt[:, a0:a0 + 6, :], gv[:, a0:a0 + 6, tsl], writes=[gt_b, hh_b] + ([macc_b] if moe else []))
        mg, mg_b = GB[0]
        wbr = []
        for n_ in range(3):
            wbr.append(load_w(self.wc["w_branch"][n_], 4, D))
        for dt_ in range(8):
            pss = []
            for n_ in range(3):
                wv, w_b = wbr[n_]
                ps, ps_b = kb.psum()
                for ck in range(4):
                    kb.op("pe", lambda e, ps=ps, wv=wv, ck=ck, n_=n_, dt_=dt_: e.matmul(ps[:], wv[:, ck, dt_ * 128:(dt_ + 1) * 128], ys[:, n_ * 4 + ck, :], start=(ck == 0), stop=(ck == 3)),
                          reads=[w_b, ys_b], writes=[ps_b])
                pss.append((ps, ps_b))
            (a0_, a0_b), (a1_, a1_b), (a2_, a2_b) = tf(), tf(), tf()
            kb.op("dve", lambda e, p=pss[0][0], dt_=dt_, a0_=a0_: e.tensor_tensor(out=a0_[:], in0=p[:], in1=gt[:, dt_, :], op=ALU.mult), reads=[pss[0][1], gt_b], writes=[a0_b])
            kb.op("dve", lambda e, p=pss[1][0], dt_=dt_, a1_=a1_: e.tensor_tensor(out=a1_[:], in0=p[:], in1=gt[:, 8 + dt_, :], op=ALU.mult), reads=[pss[1][1], gt_b], writes=[a1_b])
            kb.op("dve", lambda e, p=pss[2][0], dt_=dt_, a2_=a2_: e.tensor_tensor(out=a2_[:], in0=p[:], in1=gt[:, 16 + dt_, :], op=ALU.mult), reads=[pss[2][1], gt_b], writes=[a2_b])
            kb.op("pool", lambda e, a0_=a0_, a1_=a1_: e.tensor_tensor(out=a0_[:], in0=a0_[:], in1=a1_[:], op=ALU.add), reads=[a0_b, a1_b], writes=[a0_b])
            kb.op("pool", lambda e, a0_=a0_, a2_=a2_, dt_=dt_: e.tensor_tensor(out=mg[:, dt_, :], in0=a0_[:], in1=a2_[:], op=ALU.add), reads=[a0_b, a2_b], writes=[mg_b])
        linear(self.wc["w_mix_out"], 8, D, lambda kt: mg[:, kt, :], [mg_b], resid_consume)
        layer_norm(0)
        xb, xb_b = GB[3]
        kb.op("act", lambda e: e.activation(out=xb[:], in_=xr[:], func=AF.Copy), reads=[xr_b], writes=[xb_b])
        qT, qT_b = GB[0]

        def cons_q(ot, ps, ps_b):
            kb.op("act", lambda e: e.activation(out=qT[:, ot, :], in_=ps[:], func=AF.Copy, scale=1.0 / 16), reads=[ps_b], writes=[qT_b])
        linear(self.wc["w_xq"], 8, D, lambda kt: xb[:, kt, :], [xb_b], cons_q)
        oT, oT_b = GB[1]
        for h in range(4):
            pt, pt_b = PT[h % 2]
            for mt in range(2):
                ps, ps_b = kb.psum()
                for ee in range(2):
                    et = 2 * h + ee
                    kb.op("pe", lambda e, ps=ps, et=et, mt=mt, ee=ee: e.matmul(ps[:], KT[:, et, mt * 128:(mt + 1) * 128], qT[:, et, :], start=(ee == 0), stop=(ee == 1)),
                          reads=[KT_b, qT_b], writes=[ps_b])
                kb.op("act", lambda e, ps=ps, pt=pt, mt=mt: e.activation(out=pt[:, mt, :], in_=ps[:], func=AF.Exp), reads=[ps_b], writes=[pt_b])
            pd, pd_b = kb.psum()
            for mt in range(2):
                kb.op("pe", lambda e, pt=pt, mt=mt, pd=pd: e.matmul(pd[:], ones_1[:], pt[:, mt, :], start=(mt == 0), stop=(mt == 1)), reads=[ones_1_b, pt_b], writes=[pd_b])
            rd, rd_b = tf()
            kb.op("dve", lambda e, rd=rd, pd=pd: e.reciprocal(out=rd[:], in_=pd[:]), reads=[pd_b], writes=[rd_b])
            for ee in range(2):
                et = 2 * h + ee
                ps, ps_b = kb.psum()
                for mt in range(2):
                    kb.op("pe", lambda e, ps=ps, et=et, mt=mt, pt=pt: e.matmul(ps[:], Vm[:, mt, et * 128:(et + 1) * 128], pt[:, mt, :], start=(mt == 0), stop=(mt == 1)),
                          reads=[Vm_b, pt_b], writes=[ps_b])
                kb.op("dve", lambda e, ps=ps, et=et, rd=rd: e.tensor_tensor(out=oT[:, et, :], in0=ps[:], in1=rd[:], op=ALU.mult), reads=[ps_b, rd_b], writes=[oT_b])
        linear(self.wc["w_xo"], 8, D, lambda kt: oT[:, kt, :], [oT_b], resid_consume)
        layer_norm(2)
        kb.op("act", lambda e: e.activation(out=xb[:], in_=xr[:], func=AF.Copy), reads=[xr_b], writes=[xb_b])
        def swiglu(Wg, Wu, Wd, nft_, gate_sb=None, down_consume=None):
            gl = {}

            def cons_g(ot, ps, ps_b):
                sg, sg_b = tbf()
                kb.op("act", lambda e: e.activation(out=sg[:], in_=ps[:], func=AF.Silu), reads=[ps_b], writes=[sg_b])
                gl[ot] = (sg, sg_b)

            def cons_u(ot, ps, ps_b):
                sg, sg_b = gl[ot]
                if gate_sb is None:
                    kb.op("dve", lambda e: e.tensor_tensor(out=hh[:, ot, :], in0=ps[:], in1=sg[:], op=ALU.mult), reads=[ps_b, sg_b], writes=[hh_b])
                else:
                    t_, t_b = tf()
                    kb.op("dve", lambda e: e.tensor_tensor(out=t_[:], in0=ps[:], in1=sg[:], op=ALU.mult), reads=[ps_b, sg_b], writes=[t_b])
                    kb.op("pool", lambda e: e.tensor_tensor(out=hh[:, ot, :], in0=t_[:], in1=gate_sb[0][:], op=ALU.mult), reads=[t_b, gate_sb[1]], writes=[hh_b])
            for f0 in range(0, nft_, 3):
                f1 = min(nft_, f0 + 3)
                n_c = (f1 - f0) * 128
                for (W, cons) in ((Wg, cons_g), (Wu, cons_u)):
                    wv, w_b = load_w(W[:, f0 * 128:f0 * 128 + n_c], 8, n_c)
                    for j in range(f1 - f0):
                        ps, ps_b = kb.psum()
                        for kt in range(8):
                            kb.op("pe", lambda e, ps=ps, wv=wv, kt=kt, j=j: e.matmul(ps[:], wv[:, kt, j * 128:(j + 1) * 128], xb[:, kt, :], start=(kt == 0), stop=(kt == 7)),
                                  reads=[w_b, xb_b], writes=[ps_b])
                        cons(f0 + j, ps, ps_b)
            linear(Wd, nft_, D, lambda kt: hh[:, kt, :], [hh_b], down_consume)

        if not moe:
            swiglu(self.wc["ffn_w_gate"], self.wc["ffn_w_up"], self.wc["ffn_w_down"], 22, None, resid_consume)
        else:
            kb.op("dve", lambda e: e.tensor_tensor(out=vv[:], in0=xr[:], in1=xb[:], op=ALU.subtract), reads=[xr_b, xb_b], writes=[vv_b])
            kb.op("act", lambda e: e.activation(out=xlo[:], in_=vv[:], func=AF.Copy), reads=[vv_b], writes=[xlo_b])
            pl, pl_b = kb.psum()
            for tt in range(4):
                tk = slice(tt * 128, (tt + 1) * 128)
                combos = [(xb, xb_b, 0), (xb, xb_b, 1), (xlo, xlo_b, 0)]
                n_mm = 0
                for (xs, xs_b, wpart) in combos:
                    for kt in range(8):
                        kb.op("pe", lambda e, xs=xs, wpart=wpart, kt=kt, tk=tk, tt=tt, n_mm=n_mm, pl=pl: e.matmul(pl[:, tt * 8:(tt + 1) * 8], xs[:, kt, tk], wrh[:, wpart, kt, :], start=(n_mm == 0), stop=(n_mm == 23)),
                              reads=[xs_b, wrh_b], writes=[pl_b])
                        n_mm += 1
            kb.op("dve", lambda e, pl=pl: e.tensor_tensor(out=lg[:], in0=pl[:, 0:32].rearrange("p (t e) -> p t e", t=4), in1=brB[:].unsqueeze(1).to_broadcast([128, 4, 8]), op=ALU.add),
                  reads=[pl_b, brB_b], writes=[lg_b])
            GQ = Buf("gateq")
            eq1, eq2, lg2, gate, gr = (gsm[n_][0] for n_ in ("eq1", "eq2", "lg2", "gate", "gr"))
            m1, m2_, w1, w2 = (gs1[n_][0] for n_ in ("m1", "m2", "w1", "w2"))

            def gd(fn, eng="dve"):
                kb.op(eng, fn, reads=[GQ, lg_b], writes=[GQ])
            gd(lambda e: e.tensor_reduce(out=m1[:], in_=lg[:], axis=AX.X, op=ALU.max))
            gd(lambda e: e.tensor_tensor(out=eq1[:], in0=lg[:], in1=m1[:].unsqueeze(2).to_broadcast([128, 4, 8]), op=ALU.is_equal))
            gd(lambda e: e.scalar_tensor_tensor(out=lg2[:], in0=eq1[:], scalar=-1.0e30, in1=lg[:], op0=ALU.mult, op1=ALU.add))
            gd(lambda e: e.tensor_reduce(out=m2_[:], in_=lg2[:], axis=AX.X, op=ALU.max))
            gd(lambda e: e.tensor_tensor(out=eq2[:], in0=lg2[:], in1=m2_[:].unsqueeze(2).to_broadcast([128, 4, 8]), op=ALU.is_equal))
            gd(lambda e: e.tensor_tensor(out=w2[:], in0=m1[:], in1=m2_[:], op=ALU.subtract))
            gd(lambda e: e.activation(out=w1[:], in_=w2[:], func=AF.Sigmoid), "act")
            gd(lambda e: e.tensor_scalar(out=w2[:], in0=w1[:], scalar1=-1.0, scalar2=1.0, op0=ALU.mult, op1=ALU.add))
            gd(lambda e: e.tensor_tensor(out=gate[:], in0=eq1[:], in1=w1[:].unsqueeze(2).to_broadcast([128, 4, 8]), op=ALU.mult))
            gd(lambda e: e.tensor_tensor(out=gr[:], in0=eq2[:], in1=w2[:].unsqueeze(2).to_broadcast([128, 4, 8]), op=ALU.mult))
            gd(lambda e: e.tensor_tensor(out=gate[:], in0=gate[:], in1=gr[:], op=ALU.add))
            pg, pg_b = kb.psum()
            for tt in range(4):
                kb.op("pe", lambda e, tt=tt, pg=pg: e.transpose(pg[0:8, tt * 128:(tt + 1) * 128], gate[:, tt, :], idf[:]), reads=[GQ, idf_b], writes=[pg_b])
            kb.op("dve", lambda e, pg=pg: e.tensor_copy(out=gTs[:], in_=pg[0:8, :]), reads=[pg_b], writes=[gTs_b])
            kb.op("dve", lambda e: e.tensor_copy(out=gTh[:, 0, :], in_=gTs[:]), reads=[gTs_b], writes=[gTh_b])
            kb.op("dve", lambda e: e.tensor_tensor(out=gTr[:], in0=gTs[:], in1=gTh[:, 0, :], op=ALU.subtract), reads=[gTs_b, gTh_b], writes=[gTr_b])
            kb.op("dve", lambda e: e.tensor_copy(out=gTh[:, 1, :], in_=gTr[:]), reads=[gTr_b, gTh_b], writes=[gTh_b])
            for ex in range(NEXP):
                pgb, pgb_b = kb.psum()
                for part in range(2):
                    kb.op("pe", lambda e, ex=ex, part=part, pgb=pgb: e.matmul(pgb[:], sel[:, ex, :], gTh[:, part, :], start=(part == 0), stop=(part == 1)),
                          reads=[sel_b, gTh_b], writes=[pgb_b])
                kb.op("act", lambda e, pgb=pgb: e.activation(out=gbs[:], in_=pgb[:], func=AF.Copy), reads=[pgb_b], writes=[gbs_b])

                def dcons(ot, ps, ps_b, ex=ex):
                    if ex == 0:
                        kb.op("dve", lambda e: e.tensor_copy(out=macc[:, ot, :], in_=ps[:]), reads=[ps_b], writes=[macc_b])
                    elif ex < NEXP - 1:
                        kb.op("dve", lambda e: e.tensor_tensor(out=macc[:, ot, :], in0=macc[:, ot, :], in1=ps[:], op=ALU.add), reads=[ps_b, macc_b], writes=[macc_b])
                    else:
                        t_, t_b = tf()
                        kb.op("dve", lambda e: e.tensor_tensor(out=t_[:], in0=macc[:, ot, :], in1=ps[:], op=ALU.add), reads=[ps_b, macc_b], writes=[t_b])
                        kb.op("dve", lambda e: e.scalar_tensor_tensor(out=vv[:, ot, :], in0=xr[:, ot, :], scalar=float(ALPHA), in1=t_[:], op0=ALU.mult, op1=ALU.add),
                              reads=[xr_b, t_b], writes=[vv_b])
                swiglu(self.wc["moe_w_gate"][ex], self.wc["moe_w_up"][ex], self.wc["moe_w_down"][ex], 11, (gbs, gbs_b), dcons)
        layer_norm(4)
        kb.dma("pool", xres_v[:, :, tsl], xr[:], reads=[xr_b])
        kb.op("act", lambda e: e.activation(out=xb[:], in_=xr[:], func=AF.Copy), reads=[xr_b], writes=[xb_b])
        kb.dma("pool", xTd_v[:, :, tsl], xb[:], reads=[xb_b])
        if last:
            for tt in range(4):
                og, og_b = ostg[tt % 2]
                for half in range(2):
                    ps, ps_b = kb.psum()
                    for j in range(4):
                        kt = half * 4 + j
                        kb.op("pe", lambda e, ps=ps, kt=kt, j=j, tt=tt: e.transpose(ps[:, j * 128:(j + 1) * 128], xr[:, kt, tt * 128:(tt + 1) * 128], idf[:]),
                              reads=[xr_b, idf_b], writes=[ps_b])
                    kb.op("act", lambda e, ps=ps, og=og, half=half: e.activation(out=og[:, half * 512:(half + 1) * 512], in_=ps[:], func=AF.Copy), reads=[ps_b], writes=[og_b])
                r0 = tg * 512 + tt * 128
                kb.dma("sp", self.out[r0:r0 + 128, :], og[:], reads=[og_b])
    kb.barrier()


Prog.stageC = _stageC
```
